# Optimizing a Trainium2 kernel written in Bass

```python
import jax, jax.numpy as jnp
from jax import lax
import numpy as np

D_MODEL = 2048
BATCH = 2
SEQ = 4096
DEPTH = 1

CTX_LEN = 256
GRID_W = 64
N_ADALN = 6
EPS = 1e-6
N_HEADS = 8
KV_HEADS = 2
GROUP = N_HEADS // KV_HEADS
HEAD_DIM = 128
AXIS_DIM = HEAD_DIM // 2
ROPE_THETA = 10000.0
Q_BLOCK = 128
ATT_WIDTH = N_HEADS * HEAD_DIM
ATT_SCALE = HEAD_DIM ** -0.5
M_HEADS = 4
M_DK = 128
M_DV = 256
M_CHUNK = 128
M_WIDTH = M_HEADS * M_DV
FORGET_BIAS = 3.0
MIX_WIDTH = ATT_WIDTH + M_WIDTH
IN_WIDTHS = (ATT_WIDTH, KV_HEADS * HEAD_DIM, KV_HEADS * HEAD_DIM,
             M_HEADS * M_DK, M_HEADS * M_DK, M_WIDTH, M_WIDTH, 2 * M_HEADS, 2 * M_HEADS)
IN_COLS = sum(IN_WIDTHS)
N_EXPERTS = 32
TOP_K = 4
D_FF = D_MODEL
SWIGLU_LIMIT = 7.0
SWIGLU_ALPHA = 1.702
MOE_BLOCK = 128

kernel_name = 'hymba_style_gqa_mlstm_moe_dit_layer'


def rmsnorm(x, g):
    xf = x.astype(jnp.float32)
    y = xf * lax.rsqrt(jnp.mean(xf * xf, axis=-1, keepdims=True) + EPS)
    return (y * g).astype(x.dtype)


def modulate(h, shift, scale):
    return h * (1 + scale) + shift


def split_columns(p):
    points = np.cumsum(IN_WIDTHS)[:-1].tolist()
    return jnp.split(p, points, axis=-1)


def axial_rope_tables(rows):
    row = jnp.repeat(jnp.arange(rows, dtype=jnp.float32), GRID_W)
    col = jnp.tile(jnp.arange(GRID_W, dtype=jnp.float32), rows)
    inv = ROPE_THETA ** (-jnp.arange(0, AXIS_DIM, 2, dtype=jnp.float32) / AXIS_DIM)
    ang = jnp.concatenate([row[:, None] * inv, col[:, None] * inv], axis=-1)
    return jnp.cos(ang), jnp.sin(ang)


def apply_rope(x, cos, sin):
    xf = x.astype(jnp.float32).reshape(x.shape[:-1] + (HEAD_DIM // 2, 2))
    x1, x2 = xf[..., 0], xf[..., 1]
    cs, sn = cos[:, None, :], sin[:, None, :]
    out = jnp.stack([x1 * cs - x2 * sn, x1 * sn + x2 * cs], axis=-1)
    return out.reshape(x.shape).astype(x.dtype)


def attention_qkv(aq, ak, av, g_q, g_k):
    B, T = aq.shape[0], aq.shape[1]
    q = rmsnorm(aq.reshape(B, T, N_HEADS, HEAD_DIM), g_q)
    k = rmsnorm(ak.reshape(B, T, KV_HEADS, HEAD_DIM), g_k)
    v = av.reshape(B, T, KV_HEADS, HEAD_DIM)
    return q, k, v


def dense_attention(q, k, v):
    s = jnp.einsum('bqkgd,bskd->bkgqs', q, k).astype(jnp.float32) * ATT_SCALE
    p = jax.nn.softmax(s, axis=-1).astype(v.dtype)
    return jnp.einsum('bkgqs,bskd->bqkgd', p, v)


def latent_attention(q, k_all, v_all):
    B, N = q.shape[0], q.shape[1]
    nb = N // Q_BLOCK
    qb = q.reshape(B, nb, Q_BLOCK, KV_HEADS, GROUP, HEAD_DIM).swapaxes(0, 1)
    o = lax.map(lambda qblk: dense_attention(qblk, k_all, v_all), qb)
    return o.swapaxes(0, 1).reshape(B, N, ATT_WIDTH)


def mlstm_heads(mq, mk, mv, mi, mf):
    B, T = mq.shape[0], mq.shape[1]
    q = mq.astype(jnp.float32).reshape(B, T, M_HEADS, M_DK).transpose(0, 2, 1, 3)
    k = mk.astype(jnp.float32).reshape(B, T, M_HEADS, M_DK).transpose(0, 2, 1, 3) * (M_DK ** -0.5)
    v = mv.astype(jnp.float32).reshape(B, T, M_HEADS, M_DV).transpose(0, 2, 1, 3)
    i_pre = mi.astype(jnp.float32).reshape(B, T, 2, M_HEADS).transpose(2, 0, 3, 1)
    logf = jax.nn.log_sigmoid(mf.astype(jnp.float32).reshape(B, T, 2, M_HEADS).transpose(2, 0, 3, 1))
    return q, k, v, i_pre, logf


def mlstm_chunkwise(q, k, v, i_pre, logf, state):
    B, H, T = q.shape[0], q.shape[1], q.shape[2]
    nc = T // M_CHUNK

    def to_chunks(a):
        return jnp.moveaxis(a.reshape((B, H, nc, M_CHUNK) + a.shape[3:]), 2, 0)

    tril = jnp.tril(jnp.ones((M_CHUNK, M_CHUNK), dtype=bool))

    def step(carry, inp):
        C, n, m = carry
        qc, kc, vc, ic, fc = inp
        b = jnp.cumsum(fc, axis=-1)
        d = jnp.where(tril, b[..., :, None] - b[..., None, :] + ic[..., None, :], -jnp.inf)
        inter = b + m[..., None]
        m_t = jnp.maximum(inter, jnp.max(d, axis=-1))
        w_intra = jnp.exp(d - m_t[..., None])
        w_inter = jnp.exp(inter - m_t)
        s = jnp.einsum('bhtd,bhsd->bhts', qc, kc) * w_intra
        num = jnp.einsum('bhts,bhsv->bhtv', s, vc) + w_inter[..., None] * jnp.einsum('bhvd,bhtd->bhtv', C, qc)
        den = jnp.sum(s, axis=-1) + w_inter * jnp.einsum('bhd,bhtd->bht', n, qc)
        h = num / jnp.maximum(jnp.abs(den), jnp.exp(-m_t))[..., None]
        b_last = b[..., -1]
        g = b_last[..., None] - b + ic
        m_new = jnp.maximum(b_last + m, jnp.max(g, axis=-1))
        w_state = jnp.exp(g - m_new[..., None])
        decay = jnp.exp(b_last + m - m_new)
        C = decay[..., None, None] * C + jnp.einsum('bhs,bhsv,bhsd->bhvd', w_state, vc, kc)
        n = decay[..., None] * n + jnp.einsum('bhs,bhsd->bhd', w_state, kc)
        return (C, n, m_new), h

    state, h = lax.scan(step, state, (to_chunks(q), to_chunks(k), to_chunks(v), to_chunks(i_pre), to_chunks(logf)))
    return state, jnp.moveaxis(h, 0, 2).reshape(B, H, T, M_DV)


def flip_t(a):
    return jnp.flip(a, axis=2)


def mlstm_output(h, o_pre, g_mlstm):
    B, H, T = h.shape[0], h.shape[1], h.shape[2]
    hn = h * lax.rsqrt(jnp.mean(h * h, axis=-1, keepdims=True) + EPS)
    hn = hn.transpose(0, 2, 1, 3).reshape(B, T, M_WIDTH) * g_mlstm
    return (jax.nn.sigmoid(o_pre.astype(jnp.float32)) * hn).astype(o_pre.dtype)


def mlstm_bidirectional(lat, cx, g_mlstm, with_ctx):
    mq_l, mk_l, mv_l, mo_l, mi_l, mf_l = lat
    mq_c, mk_c, mv_c, mo_c, mi_c, mf_c = cx
    ql, kl, vl, il, fl = mlstm_heads(mq_l, mk_l, mv_l, mi_l, mf_l)
    qc, kc, vc, ic, fc = mlstm_heads(mq_c, mk_c, mv_c, mi_c, mf_c)
    B = ql.shape[0]
    zero = (jnp.zeros((B, M_HEADS, M_DV, M_DK), jnp.float32),
            jnp.zeros((B, M_HEADS, M_DK), jnp.float32),
            jnp.zeros((B, M_HEADS), jnp.float32))
    st_f, hc_f = mlstm_chunkwise(qc, kc, vc, ic[0], fc[0], zero)
    st_b, hc_b = mlstm_chunkwise(flip_t(qc), flip_t(kc), flip_t(vc), flip_t(ic[1]), flip_t(fc[1]), zero)
    _, hl_f = mlstm_chunkwise(ql, kl, vl, il[0], fl[0], st_f)
    _, hl_b = mlstm_chunkwise(flip_t(ql), flip_t(kl), flip_t(vl), flip_t(il[1]), flip_t(fl[1]), st_b)
    out_lat = mlstm_output(hl_f + flip_t(hl_b), mo_l, g_mlstm)
    out_ctx = mlstm_output(hc_f + flip_t(hc_b), mo_c, g_mlstm) if with_ctx else None
    return out_lat, out_ctx


def mixer_group(h_lat, h_ctx, w_in, b_in, g_q, g_k, g_mlstm, cos, sin, with_ctx):
    lat = split_columns(h_lat @ w_in + b_in)
    cx = split_columns(h_ctx @ w_in + b_in)
    q_l, k_l, v_l = attention_qkv(lat[0], lat[1], lat[2], g_q, g_k)
    q_l = apply_rope(q_l, cos, sin)
    k_l = apply_rope(k_l, cos, sin)
    q_c, k_c, v_c = attention_qkv(cx[0], cx[1], cx[2], g_q, g_k)
    a_lat = latent_attention(q_l, jnp.concatenate([k_l, k_c], axis=1), jnp.concatenate([v_l, v_c], axis=1))
    m_lat, m_ctx = mlstm_bidirectional(lat[3:], cx[3:], g_mlstm, with_ctx)
    y_lat = jnp.concatenate([a_lat, m_lat], axis=-1)
    y_ctx = None
    if with_ctx:
        B, C = q_c.shape[0], q_c.shape[1]
        a_ctx = dense_attention(q_c.reshape(B, C, KV_HEADS, GROUP, HEAD_DIM), k_c, v_c).reshape(B, C, ATT_WIDTH)
        y_ctx = jnp.concatenate([a_ctx, m_ctx], axis=-1)
    return y_lat, y_ctx


def moe(h, w_router, b_router, w1, b1, w2, b2):
    T, D = h.shape
    logits = (h @ w_router).astype(jnp.float32) + b_router
    top_val, top_idx = lax.top_k(logits, TOP_K)
    weights = jax.nn.softmax(top_val, axis=-1)
    A = T * TOP_K
    e_flat = top_idx.reshape(A)
    tok_flat = jnp.repeat(jnp.arange(T, dtype=jnp.int32), TOP_K)
    w_flat = weights.reshape(A)
    order = jnp.argsort(e_flat)
    e_sorted, tok_sorted, w_sorted = e_flat[order], tok_flat[order], w_flat[order]
    counts = jnp.zeros((N_EXPERTS,), jnp.int32).at[e_flat].add(1)
    padded = (counts + MOE_BLOCK - 1) // MOE_BLOCK * MOE_BLOCK
    start = jnp.cumsum(counts) - counts
    pstart = jnp.cumsum(padded) - padded
    pend = pstart + padded
    dest = pstart[e_sorted] + jnp.arange(A, dtype=jnp.int32) - start[e_sorted]
    nblk = -(-A // MOE_BLOCK) + N_EXPERTS
    P = nblk * MOE_BLOCK
    row_tok = jnp.full((P,), T, jnp.int32).at[dest].set(tok_sorted)
    row_w = jnp.zeros((P,), jnp.float32).at[dest].set(w_sorted)
    blk_start = jnp.arange(nblk, dtype=jnp.int32) * MOE_BLOCK
    blk_expert = jnp.minimum(jnp.sum(pend[None, :] <= blk_start[:, None], axis=1), N_EXPERTS - 1)
    h_pad = jnp.concatenate([h, jnp.zeros((1, D), h.dtype)], axis=0)

    def block(args):
        e, toks, wts = args
        xb = h_pad[toks]
        gu = xb @ w1[e] + b1[e]
        gate, up = gu[:, :D_FF], gu[:, D_FF:]
        gate = jnp.minimum(gate, SWIGLU_LIMIT)
        up = jnp.clip(up, -SWIGLU_LIMIT, SWIGLU_LIMIT)
        glu = gate * jax.nn.sigmoid(SWIGLU_ALPHA * gate)
        y = ((up + 1) * glu) @ w2[e] + b2[e]
        return y * wts[:, None].astype(y.dtype)

    y = lax.map(block, (blk_expert, row_tok.reshape(nblk, MOE_BLOCK), row_w.reshape(nblk, MOE_BLOCK)))
    out = jnp.zeros((T + 1, D), h.dtype).at[row_tok].add(y.reshape(P, D).astype(h.dtype))
    return out[:T]


def setup_inputs(seed: int = 0) -> dict:
    key = jax.random.key(seed)
    ks = jax.random.split(key, 22)
    nrm = jax.random.normal
    f32 = jnp.float32
    b_in = 0.02 * nrm(ks[8], (DEPTH, IN_COLS), f32)
    b_in = b_in.at[:, IN_COLS - 2 * M_HEADS:].add(FORGET_BIAS)
    return {
        'x': nrm(ks[0], (BATCH, SEQ, D_MODEL), f32),
        'c': nrm(ks[1], (BATCH, D_MODEL), f32),
        'ctx': nrm(ks[2], (BATCH, CTX_LEN, D_MODEL), f32),
        'c_ctx': nrm(ks[3], (D_MODEL,), f32),
        'w_mod': nrm(ks[4], (DEPTH, D_MODEL, N_ADALN * D_MODEL), f32) * (0.5 * D_MODEL ** -0.5),
        'b_mod': 0.02 * nrm(ks[5], (DEPTH, N_ADALN * D_MODEL), f32),
        'g_norm1': 1.0 + 0.05 * nrm(ks[6], (DEPTH, D_MODEL), f32),
        'w_in': nrm(ks[7], (DEPTH, D_MODEL, IN_COLS), f32) * (D_MODEL ** -0.5),
        'b_in': b_in,
        'g_q': 1.0 + 0.05 * nrm(ks[9], (DEPTH, HEAD_DIM), f32),
        'g_k': 1.0 + 0.05 * nrm(ks[10], (DEPTH, HEAD_DIM), f32),
        'g_mlstm': 1.0 + 0.05 * nrm(ks[11], (DEPTH, M_WIDTH), f32),
        'w_out': nrm(ks[12], (DEPTH, MIX_WIDTH, D_MODEL), f32) * (MIX_WIDTH ** -0.5),
        'g_norm2': 1.0 + 0.05 * nrm(ks[13], (DEPTH, D_MODEL), f32),
        'w_router': nrm(ks[14], (DEPTH, D_MODEL, N_EXPERTS), f32) * (D_MODEL ** -0.5),
        'b_router': 0.01 * nrm(ks[15], (DEPTH, N_EXPERTS), f32),
        'w1': nrm(ks[16], (DEPTH, N_EXPERTS, D_MODEL, 2 * D_FF), f32) * (D_MODEL ** -0.5),
        'b1': 0.02 * nrm(ks[17], (DEPTH, N_EXPERTS, 2 * D_FF), f32),
        'w2': nrm(ks[18], (DEPTH, N_EXPERTS, D_FF, D_MODEL), f32) * (D_FF ** -0.5),
        'b2': 0.02 * nrm(ks[19], (DEPTH, N_EXPERTS, D_MODEL), f32),
        'g_final': 1.0 + 0.05 * nrm(ks[20], (D_MODEL,), f32),
    }


def reference(x, c, ctx, c_ctx, w_mod, b_mod, g_norm1, w_in, b_in, g_q, g_k, g_mlstm, w_out,
              g_norm2, w_router, b_router, w1, b1, w2, b2, g_final):
    B, N, D = x.shape[0], x.shape[1], x.shape[2]
    rows = N // GRID_W
    cos, sin = axial_rope_tables(rows)
    for l in range(DEPTH):
        with_ctx = l + 1 < DEPTH
        mod = jax.nn.silu(c) @ w_mod[l] + b_mod[l]
        mod_c = jax.nn.silu(c_ctx) @ w_mod[l] + b_mod[l]
        sh1, sc1, gt1, sh2, sc2, gt2 = jnp.split(mod[:, None, :], N_ADALN, axis=-1)
        sh1c, sc1c, gt1c, sh2c, sc2c, gt2c = jnp.split(mod_c, N_ADALN, axis=-1)
        h_lat = modulate(rmsnorm(x, g_norm1[l]), sh1, sc1)
        h_ctx = modulate(rmsnorm(ctx, g_norm1[l]), sh1c, sc1c)
        y_lat, y_ctx = mixer_group(h_lat, h_ctx, w_in[l], b_in[l], g_q[l], g_k[l], g_mlstm[l], cos, sin, with_ctx)
        x = x + gt1 * (y_lat @ w_out[l])
        h2 = modulate(rmsnorm(x, g_norm2[l]), sh2, sc2)
        x = x + gt2 * moe(h2.reshape(B * N, D), w_router[l], b_router[l], w1[l], b1[l], w2[l], b2[l]).reshape(B, N, D)
        if with_ctx:
            C = ctx.shape[1]
            ctx = ctx + gt1c * (y_ctx @ w_out[l])
            h2c = modulate(rmsnorm(ctx, g_norm2[l]), sh2c, sc2c)
            ctx = ctx + gt2c * moe(h2c.reshape(B * C, D), w_router[l], b_router[l], w1[l], b1[l], w2[l], b2[l]).reshape(B, C, D)
    return rmsnorm(x, g_final)
```

```python
import numpy as np
import ml_dtypes
from contextlib import ExitStack
import concourse.bass as bass
import concourse.mybir as mybir
from concourse.bass_utils import run_bass_kernel_spmd

F32 = mybir.dt.float32
BF16 = mybir.dt.bfloat16
ALU = mybir.AluOpType
AF = mybir.ActivationFunctionType
AX = mybir.AxisListType

D = 2048
NCH = 16
NSLOT = 34
NOWN = 8
NOTH = 26
EPS = 1e-6
NEG = -30000.0
CAP = 512
NEXP = 32
Q0, K0, V0, MQ0, MK0, MV0, MO0, MI0, MF0 = 0, 1024, 1280, 1536, 2048, 2560, 3584, 4608, 4616


class Op:
    __slots__ = ("eng", "fn", "deps", "is_dma", "signal", "count", "semkey", "value", "name")

    def __init__(self, eng, fn, is_dma, name=""):
        self.eng = eng
        self.fn = fn
        self.deps = []
        self.is_dma = is_dma
        self.signal = False
        self.count = None
        self.semkey = None
        self.value = None
        self.name = name


class Rec:
    ENG = ["pe", "act", "dve", "pool", "sp"]
    NS = 8

    def __init__(self):
        self.streams = {e: [] for e in self.ENG}
        self.last_w = {}
        self.readers = {}
        self.pending = {e: [] for e in self.ENG}
        self.dma_ops = {e: [] for e in self.ENG}
        self.final_ops = []

    def op(self, eng, fn, reads=(), writes=(), dma=False, name=""):
        o = Op(eng, fn, dma, name)
        deps = []
        for k in reads:
            w = self.last_w.get(k)
            if w is not None:
                if not (w.eng == eng and not w.is_dma and eng == "pe"):
                    deps.append(w)
        for k in writes:
            w = self.last_w.get(k)
            if w is not None and (w.eng != eng or w.is_dma):
                deps.append(w)
            for r in self.readers.get(k, ()):
                if r.eng != eng or r.is_dma or eng != "pe":
                    if r is not o:
                        deps.append(r)
        deps.extend(self.pending[eng])
        self.pending[eng] = []
        if dma:
            lst = self.dma_ops[eng]
            if len(lst) >= self.NS:
                deps.append(lst[len(lst) - self.NS])
            lst.append(o)
        o.deps = deps
        for k in writes:
            self.last_w[k] = o
            self.readers[k] = []
        for k in reads:
            self.readers.setdefault(k, []).append(o)
        self.streams[eng].append(o)
        return o

    def barrier(self):
        lasts = []
        for e in self.ENG:
            if self.streams[e]:
                lasts.append(self.streams[e][-1])
            lasts.extend(self.dma_ops[e][-self.NS:])
        for e in self.ENG:
            self.pending[e] = [o for o in lasts if (o.eng != e or o.is_dma)]
        self.last_w = {}
        self.readers = {}

    def emit(self, nc, block):
        for e in self.ENG:
            for o in self.streams[e]:
                for d in o.deps:
                    d.signal = True
        for o in self.final_ops:
            o.signal = True
        nsem = {}
        for e in self.ENG:
            c = 0
            ndma = 0
            for o in self.streams[e]:
                if o.is_dma:
                    slot = ndma % self.NS
                    o.semkey = ("dma", e, slot)
                    o.value = 16 * (ndma // self.NS + 1)
                    ndma += 1
                    o.signal = True
                elif o.signal:
                    c += 1
                    o.semkey = ("eng", e)
                    o.value = c
        sems = {}

        def sem(key):
            if key not in sems:
                sems[key] = self._es.enter_context(nc.semaphore("s_" + "_".join(str(k) for k in key)))
            return sems[key]

        final_ops = self.final_ops

        def run(ename, eh):
            seen = {}
            for o in self.streams[ename]:
                need = {}
                for d in o.deps:
                    if need.get(d.semkey, 0) < d.value:
                        need[d.semkey] = d.value
                for k, v in need.items():
                    if seen.get(k, 0) < v:
                        eh.wait_ge(sem(k), v)
                        seen[k] = v
                ins = o.fn(eh)
                if o.signal:
                    ins.then_inc(sem(o.semkey), 16 if o.is_dma else 1)
            if ename == "sp":
                for o in final_ops:
                    if seen.get(o.semkey, 0) < o.value:
                        eh.wait_ge(sem(o.semkey), o.value)
                        seen[o.semkey] = o.value

        for e in self.ENG:
            sem(("eng", e))
            for s in range(self.NS):
                if e in ("sp", "pool", "act"):
                    sem(("dma", e, s))

        @block.tensor
        def _(eh):
            run("pe", eh)

        @block.scalar
        def _(eh):
            run("act", eh)

        @block.vector
        def _(eh):
            run("dve", eh)

        @block.gpsimd
        def _(eh):
            run("pool", eh)

        @block.sync
        def _(eh):
            run("sp", eh)


class Arena:
    def __init__(self, t, n):
        self.t = t
        self.n = n
        self.off = 0

    def f(self, cols):
        lo = self.off
        self.off += cols
        assert self.off <= self.n, ("arena overflow", self.off, self.n)
        return self.t[:, lo:lo + cols]

    def b(self, cols):
        c2 = (cols + 1) // 2
        return self.f(c2).bitcast(BF16)[:, 0:cols]


def pbc(ap):
    v = ap.partition_broadcast(128)
    return v[:, 0, :]


def build(debug=None, n_oth=NOTH, n_exp=NEXP):
    nc = bass.Bass("TRN2", target_bir_lowering=False)
    R = Rec()
    es = ExitStack()
    R._es = es
    declared = []
    big = debug is None or debug.startswith("moe")

    def din(name, shape, dt=F32):
        if name in ("w1", "w2") and not big:
            return None
        declared.append(name)
        return nc.dram_tensor(name, list(shape), dt, kind="ExternalInput").ap()

    xs_d = din("xs", [NSLOT * 128, D])
    rope_d = din("rope", [NSLOT * 128, 128])
    gmask_d = din("gmask", [128, NSLOT * 16])
    cfm_d = din("cfm", [128, 32])
    bmod_d = din("bmod", [128, 96])
    g1_d = din("g1fm", [128, 16])
    g2fm_d = din("g2fm", [128, 16])
    wmod_d = din("w_mod", [D, 6 * D])
    win_d = din("w_in", [D, 4624])
    bin_d = din("b_in", [1, 4624])
    bfm_d = din("bfm", [128, 8])
    gq_d = din("g_q", [1, 128])
    gk_d = din("g_k", [1, 128])
    gm_d = din("g_mlstm", [1, 1024])
    wout_d = din("w_out", [D, D])
    g2_d = din("g_norm2", [1, D])
    gf_d = din("g_final", [1, D])
    wr_d = din("w_router", [D, NEXP])
    br_d = din("b_router", [1, NEXP])
    w1_d = din("w1", [NEXP, D, 2 * D])
    b1_d = din("b1fm", [128, NEXP * 32])
    w2_d = din("w2", [NEXP, D, D])
    b2_d = din("b2", [NEXP, D])
    cst_d = din("consts", [128, 6 * 128 + 512 + 1])
    out_d = nc.dram_tensor("out", [NOWN * 128, D], F32, kind="ExternalOutput").ap()
    dbg_d = None
    if debug is not None:
        dbg_d = nc.dram_tensor("dbg", [128, 8192], F32, kind="ExternalOutput").ap()

    NF = 52500
    fa_t = es.enter_context(nc.sbuf_tensor("fa", [128, NF], F32))
    A = Arena(fa_t, NF)
    pT = es.enter_context(nc.psum_tensor("pT", [128, 2048], BF16))
    psum = [None, None] + [es.enter_context(nc.psum_tensor("ps%d" % i, [128, 512], F32)) for i in range(2, 8)]

    def PS(i, lo=0, n=512):
        return psum[i][:, lo:lo + n]

    def pk(i):
        return "ps%d" % i

    def finish():
        with nc.Block() as block:
            R.emit(nc, block)
        return nc, es, declared

    def dump(ap, key, lo, n):
        o = R.op("sp", lambda e: e.dma_start(out=dbg_d[:, lo:lo + n], in_=ap), reads=[key], dma=True)
        R.final_ops.append(o)

    dbgf = None
    if debug is not None:
        dbgf = A.f(512)

    def dump_bf(ap, key, lo, n):
        for p0 in range(0, n, 512):
            m = min(512, n - p0)
            R.op("dve", lambda e, p0=p0, m=m: e.tensor_copy(out=dbgf[:, 0:m], in_=ap[:, p0:p0 + m]), reads=[key], writes=["dbgf"])
            dump(dbgf[:, 0:m], "dbgf", lo + p0, m)

    cst = A.f(6 * 128 + 512 + 1)
    ident = cst[:, 0:128]
    tri_f = cst[:, 128:256]
    tri_b = cst[:, 256:384]
    nm_f = cst[:, 384:512]
    nm_b = cst[:, 512:640]
    ones = cst[:, 640:768]
    iota_c = cst[:, 768:1280]
    iota_p = cst[:, 1280:1281]
    identb = A.b(128)
    onesb = A.b(128)
    modT = A.f(192).rearrange("p (a b) -> p a b", b=2)
    gml = A.f(16)
    gmc = A.f(16)
    g1 = A.f(16)
    cfm = A.f(32)
    bmod = A.f(96)
    bfm = A.f(8)
    csT = A.b(32).rearrange("p (a b) -> p a b", b=2)
    stC = [A.f(257) for _ in range(8)]
    stCb = [A.b(258)[:, 0:257] for _ in range(8)]
    epsb = A.f(2)
    R.op("pool", lambda e: e.memset(epsb[:, 0:1], EPS), writes=["epsb"])
    R.op("pool", lambda e: e.memset(epsb[:, 1:2], 1.0), writes=["epsb"])
    gmask = A.f(NSLOT * 16).rearrange("p (s g) -> p s g", g=16)

    R.op("sp", lambda e: e.dma_start(out=cst, in_=cst_d), writes=["cst"], dma=True)
    R.op("sp", lambda e: e.dma_start(out=cfm, in_=cfm_d), writes=["cfm"], dma=True)
    R.op("sp", lambda e: e.dma_start(out=bmod, in_=bmod_d), writes=["bmod"], dma=True)
    R.op("sp", lambda e: e.dma_start(out=g1, in_=g1_d), writes=["g1"], dma=True)
    R.op("sp", lambda e: e.dma_start(out=bfm, in_=bfm_d), writes=["bfm"], dma=True)
    R.op("sp", lambda e: e.dma_start(out=gmask.rearrange("p s g -> p (s g)"), in_=gmask_d), writes=["gmask"], dma=True)
    R.op("dve", lambda e: e.tensor_copy(out=identb, in_=ident), reads=["cst"], writes=["identb"])
    R.op("dve", lambda e: e.tensor_copy(out=onesb, in_=ones), reads=["cst"], writes=["onesb"])
    for j in range(8):
        R.op("pool", lambda e, j=j: e.memset(stC[j], 0.0), writes=["stC%d" % j])
        R.op("pool", lambda e, j=j: e.memset(stCb[j], 0.0), writes=["stCb%d" % j])

    R.op("act", lambda e: e.activation(out=csT[:, :, 0], in_=cfm[:, 0:16], func=AF.Silu), reads=["cfm"], writes=["csT"])
    R.op("act", lambda e: e.activation(out=csT[:, :, 1], in_=cfm[:, 16:32], func=AF.Silu), reads=["cfm"], writes=["csT"])
    mark0 = A.off
    wm = [A.b(16 * 512).rearrange("p (c n) -> p c n", n=512) for _ in range(2)]
    wmod_v = wmod_d.rearrange("(c p) n -> p c n", p=128)
    PM = psum[7][:, 0:192].rearrange("p (a b) -> p a b", b=2)

    def mod_block(blk):
        buf = wm[blk % 2]
        key = "wm%d" % (blk % 2)
        R.op("pool", lambda e: e.dma_start(out=buf, in_=wmod_v[:, :, blk * 512:(blk + 1) * 512]), writes=[key], dma=True)
        for q in range(4):
            cc = blk * 4 + q
            for c in range(NCH):
                R.op("pe", lambda e, c=c, q=q, cc=cc: e.matmul(PM[:, cc, :], lhsT=buf[:, c, q * 128:(q + 1) * 128], rhs=csT[:, c, :],
                                                             start=(c == 0), stop=(c == NCH - 1)),
                     reads=[key, "csT"], writes=[pk(7)])
        R.op("dve", lambda e: e.tensor_tensor(out=modT[:, blk * 4:blk * 4 + 4, :], in0=PM[:, blk * 4:blk * 4 + 4, :],
                                              in1=bmod[:, blk * 4:blk * 4 + 4].unsqueeze(2).to_broadcast([128, 4, 2]), op=ALU.add),
             reads=[pk(7), "bmod"], writes=["modT"])

    for blk in range(24):
        mod_block(blk)
    R.op("dve", lambda e: e.scalar_tensor_tensor(out=gml, in0=modT[:, 16:32, 0], scalar=1.0, in1=g1, op0=ALU.add, op1=ALU.mult),
         reads=["modT", "g1"], writes=["gml"])
    R.op("dve", lambda e: e.scalar_tensor_tensor(out=gmc, in0=modT[:, 16:32, 1], scalar=1.0, in1=g1, op0=ALU.add, op1=ALU.mult),
         reads=["modT", "g1"], writes=["gmc"])
    if debug == "mod":
        dump(modT.rearrange("p a b -> p (a b)"), "modT", 0, 192)
        dump(gml, "gml", 192, 16)
        return finish()
    R.barrier()
    A.off = mark0

    win_v = win_d.rearrange("(c p) n -> p c n", p=128)
    SC = 128.0 ** -0.5
    WCOLS = 2064
    mark_mix = A.off
    Wt = A.b(16 * WCOLS).rearrange("p (c n) -> p c n", n=WCOLS)
    Wflat = Wt.rearrange("p c n -> p (c n)")
    mark_w_end = A.off
    W_regs = A
    bo = A.f(WCOLS)
    gkb = A.f(128)
    gqb = A.f(128)
    xt0 = A.f(D)
    xt = [xt0, xt0]
    xsb = A.b(D)
    hT = [A.b(D).rearrange("p (c t) -> p c t", t=128) for _ in range(2)]
    kTst = A.b(2 * NSLOT * 128).rearrange("p (g t) -> p g t", g=2)
    Vst = A.b(NSLOT * 256).rearrange("p (s v) -> p s v", v=256)
    sm = A.f(64)
    rp = [A.f(128) for _ in range(2)]
    kf = A.f(256)
    kn = A.f(256)
    rt = [A.f(128) for _ in range(4)]
    krot = A.b(256)
    NB_ = 2
    Kt = [A.b(512) for _ in range(NB_)]
    Vx = [A.b(4 * 258).rearrange("p (h v) -> p h v", v=258) for _ in range(NB_)]
    Gt = [A.f(16) for _ in range(NB_)]
    cq = [A.f(64) for _ in range(NB_)]
    expb = [A.f(8) for _ in range(NB_)]
    Kw = [A.b(128) for _ in range(2)]

    def load_w(segs):
        off = 0
        for (lo, hi) in segs:
            n = hi - lo
            R.op("pool", lambda e, off=off, lo=lo, hi=hi, n=n: e.dma_start(out=Wt[:, :, off:off + n], in_=win_v[:, :, lo:hi]), writes=["W"], dma=True)
            off += n

    load_w([(K0, K0 + 512), (MK0, MK0 + 1536), (MI0, MI0 + 16)])
    R.op("sp", lambda e: e.dma_start(out=bo[:, 0:512], in_=pbc(bin_d[:, K0:K0 + 512])), writes=["bo"], dma=True)
    R.op("sp", lambda e: e.dma_start(out=bo[:, 512:2048], in_=pbc(bin_d[:, MK0:MK0 + 1536])), writes=["bo"], dma=True)
    R.op("sp", lambda e: e.dma_start(out=bo[:, 2048:2064], in_=pbc(bin_d[:, MI0:MI0 + 16])), writes=["bo"], dma=True)
    R.op("sp", lambda e: e.dma_start(out=gkb, in_=pbc(gk_d)), writes=["gkb"], dma=True)
    R.op("sp", lambda e: e.dma_start(out=gqb, in_=pbc(gq_d)), writes=["gqb"], dma=True)
    R.op("dve", lambda e: e.tensor_scalar(out=bo[:, 512:1024], in0=bo[:, 512:1024], scalar1=SC, scalar2=None, op0=ALU.mult), reads=["bo"], writes=["bo"])
    R.op("dve", lambda e: e.tensor_scalar(out=gqb, in0=gqb, scalar1=SC, scalar2=None, op0=ALU.mult), reads=["gqb"], writes=["gqb"])
    R.op("dve", lambda e: e.tensor_scalar(out=bfm[:, 4:8], in0=bfm[:, 4:8], scalar1=SC, scalar2=None, op0=ALU.mult), reads=["bfm"], writes=["bfm"])
    for b_ in range(NB_):
        R.op("pool", lambda e, b_=b_: e.memset(Vx[b_][:, :, 256:257], 1.0), writes=["Vx%d" % b_])

    def rstd_op(dst, src, n_el, keys_r, key_w):
        R.op("act", lambda e: e.activation(out=dst, in_=src, func=AF.Ln, scale=1.0 / n_el, bias=epsb[:, 0:1]), reads=keys_r + ["epsb"], writes=[key_w])
        R.op("act", lambda e: e.activation(out=dst, in_=dst, func=AF.Exp, scale=-0.5), reads=[key_w], writes=[key_w])

    def make_hT(s):
        b2 = s % 2
        is_ctx = s < 2
        xk = "xt"
        R.op("sp", lambda e: e.dma_start(out=xt[b2], in_=xs_d[s * 128:(s + 1) * 128, :]), writes=[xk], dma=True)
        R.op("pool", lambda e: e.memset(sm[:, b2:b2 + 1], 0.0), writes=["ss%d" % b2])
        jk = hT[b2].rearrange("p c t -> p (c t)")
        R.op("act", lambda e: e.activation(out=jk, in_=xt[b2], func=AF.Square, accum_out=sm[:, b2:b2 + 1]), reads=[xk, "ss%d" % b2], writes=["hT%d" % b2, "ss%d" % b2])
        rstd_op(sm[:, 2 + b2:3 + b2], sm[:, b2:b2 + 1], float(D), ["ss%d" % b2], "rs%d" % b2)
        R.op("dve", lambda e: e.tensor_scalar(out=xsb, in0=xt[b2], scalar1=sm[:, 2 + b2:3 + b2], scalar2=None, op0=ALU.mult),
             reads=[xk, "rs%d" % b2], writes=["xsb"])
        for c in range(NCH):
            R.op("pe", lambda e, c=c: e.transpose(out=pT[:, c * 128:(c + 1) * 128], in_=xsb[:, c * 128:(c + 1) * 128], identity=identb),
                 reads=["xsb", "identb"], writes=["pT%d" % (c // 8)])
        gm = gmc if is_ctx else gml
        w = 1 if is_ctx else 0
        hk = "hT%d" % b2
        for c in range(NCH):
            R.op("act", lambda e, c=c: e.activation(out=hT[b2][:, c, :], in_=pT[:, c * 128:(c + 1) * 128], func=AF.Identity,
                                                   scale=gm[:, c:c + 1], bias=modT[:, c, w:w + 1]),
                 reads=["pT%d" % (c // 8), "gml", "gmc", "modT"], writes=[hk])
        return hT[b2], hk

    def proj_tok(h, hk, col_lo, n, bank, wkey="W", w=None):
        w = Wt if w is None else w
        for c in range(NCH):
            R.op("pe", lambda e, c=c: e.matmul(PS(bank, 0, n), lhsT=h[:, c, :], rhs=w[:, c, col_lo:col_lo + n], start=(c == 0), stop=(c == NCH - 1)),
                 reads=[hk, wkey], writes=[pk(bank)])

    def slot_common(s, db):
        h, hk = make_hT(s)
        b2 = s % 2
        R.op("sp", lambda e: e.dma_start(out=rp[b2], in_=rope_d[s * 128:(s + 1) * 128, :]), writes=["rp%d" % b2], dma=True)
        proj_tok(h, hk, 0, 512, 2)
        proj_tok(h, hk, 512, 512, 3)
        proj_tok(h, hk, 1024, 512, 4)
        proj_tok(h, hk, 1536, 512, 5)
        proj_tok(h, hk, 2048, 16, 6)
        R.op("dve", lambda e: e.tensor_tensor(out=kf, in0=PS(2, 0, 256), in1=bo[:, 0:256], op=ALU.add), reads=[pk(2), "bo"], writes=["kf"])
        R.op("dve", lambda e: e.tensor_tensor(out=Vst[:, s, :], in0=PS(2, 256, 256), in1=bo[:, 256:512], op=ALU.add), reads=[pk(2), "bo"], writes=["Vst"])
        R.op("dve", lambda e: e.scalar_tensor_tensor(out=Kt[db], in0=PS(3), scalar=SC, in1=bo[:, 512:1024], op0=ALU.mult, op1=ALU.add),
             reads=[pk(3), "bo"], writes=["Kt%d" % db])
        for half in range(2):
            R.op("dve", lambda e, half=half: e.tensor_tensor(out=Vx[db][:, 2 * half:2 * half + 2, 0:256],
                                                             in0=PS(4 + half).rearrange("p (h v) -> p h v", v=256),
                                                             in1=bo[:, 1024 + 512 * half:1536 + 512 * half].rearrange("p (h v) -> p h v", v=256), op=ALU.add),
                 reads=[pk(4 + half), "bo"], writes=["Vx%d" % db])
        R.op("dve", lambda e: e.tensor_tensor(out=Gt[db], in0=PS(6, 0, 16), in1=bo[:, 2048:2064], op=ALU.add), reads=[pk(6), "bo"], writes=["G%d" % db])
        R.op("pool", lambda e: e.memset(sm[:, 4:6], 0.0), writes=["kss"])
        for g in range(2):
            R.op("act", lambda e, g=g: e.activation(out=kn[:, g * 128:(g + 1) * 128], in_=kf[:, g * 128:(g + 1) * 128], func=AF.Square, accum_out=sm[:, 4 + g:5 + g]),
                 reads=["kf", "kss"], writes=["kn", "kss"])
        rstd_op(sm[:, 8:10], sm[:, 4:6], 128.0, ["kss"], "krs")
        for g in range(2):
            R.op("dve", lambda e, g=g: e.scalar_tensor_tensor(out=kn[:, g * 128:(g + 1) * 128], in0=kf[:, g * 128:(g + 1) * 128], scalar=sm[:, 8 + g:9 + g],
                                                              in1=gkb, op0=ALU.mult, op1=ALU.mult), reads=["kf", "krs", "gkb"], writes=["kn"])
        rope(kn, "kn", krot, "krot", 2, rp[b2], "rp%d" % b2)
        for g in range(2):
            R.op("pe", lambda e, g=g: e.transpose(out=pT[:, g * 128:(g + 1) * 128], in_=krot[:, g * 128:(g + 1) * 128], identity=identb),
                 reads=["krot", "identb"], writes=["pT0"])
        R.op("act", lambda e: e.tensor_copy(out=kTst[:, :, s * 128:(s + 1) * 128], in_=pT[:, 0:256].rearrange("p (g t) -> p g t", g=2))
             if False else e.activation(out=kTst[:, :, s * 128:(s + 1) * 128], in_=pT[:, 0:256].rearrange("p (g t) -> p g t", g=2), func=AF.Copy),
             reads=["pT0"], writes=["kTst"])
        chunk_gates(s, db)

    def rope(src, skey, dst, dkey, nh, rpt, rkey):
        v = src.rearrange("p (h i two) -> p h i two", h=nh, two=2)
        o = dst.rearrange("p (h i two) -> p h i two", h=nh, two=2)
        cosb = rpt[:, 0:64].unsqueeze(1).to_broadcast([128, nh, 64])
        sinb = rpt[:, 64:128].unsqueeze(1).to_broadcast([128, nh, 64])
        n = nh * 64
        t = [rt[i][:, 0:n].rearrange("p (h i) -> p h i", h=nh) if n <= 128 else None for i in range(4)]
        if n > 128:
            t = [rtq[i].rearrange("p (h i) -> p h i", h=nh) for i in range(4)]
        x1, x2 = v[:, :, :, 0], v[:, :, :, 1]
        tk = ["rt0", "rt1", "rt2", "rt3"]
        R.op("pool", lambda e: e.tensor_tensor(out=t[0], in0=x1, in1=cosb, op=ALU.mult), reads=[skey, rkey], writes=[tk[0]])
        R.op("pool", lambda e: e.tensor_tensor(out=t[1], in0=x2, in1=sinb, op=ALU.mult), reads=[skey, rkey], writes=[tk[1]])
        R.op("pool", lambda e: e.tensor_tensor(out=t[2], in0=x1, in1=sinb, op=ALU.mult), reads=[skey, rkey], writes=[tk[2]])
        R.op("pool", lambda e: e.tensor_tensor(out=t[3], in0=x2, in1=cosb, op=ALU.mult), reads=[skey, rkey], writes=[tk[3]])
        R.op("dve", lambda e: e.tensor_tensor(out=o[:, :, :, 0], in0=t[0], in1=t[1], op=ALU.subtract), reads=[tk[0], tk[1]], writes=[dkey])
        R.op("dve", lambda e: e.tensor_tensor(out=o[:, :, :, 1], in0=t[2], in1=t[3], op=ALU.add), reads=[tk[2], tk[3]], writes=[dkey])

    def chunk_gates(s, db):
        q_ = cq[db]
        e1, Lf, lgf, ie, imb, gg, wst, dec = [q_[:, 8 * i:8 * i + 8] for i in range(8)]
        ck = "cq%d" % db
        R.op("act", lambda e: e.activation(out=e1, in_=Gt[db][:, 8:16], func=AF.Exp, scale=-1.0), reads=["G%d" % db], writes=[ck + "a"])
        R.op("act", lambda e: e.activation(out=Lf, in_=e1, func=AF.Ln, bias=epsb[:, 1:2]), reads=[ck + "a", "epsb"], writes=[ck + "b"])
        R.op("dve", lambda e: e.tensor_tensor(out=lgf, in0=Lf, in1=gmask[:, s, 8:16], op=ALU.mult), reads=[ck + "b", "gmask"], writes=[ck + "lgf"])
        R.op("dve", lambda e: e.tensor_tensor(out=ie, in0=Gt[db][:, 0:8], in1=gmask[:, s, 0:8], op=ALU.add), reads=["G%d" % db, "gmask"], writes=[ck + "ie"])
        R.op("pe", lambda e: e.matmul(PS(6, 16, 4), lhsT=tri_f, rhs=lgf[:, 0:4], start=True, stop=True), reads=["cst", ck + "lgf"], writes=[pk(6)])
        R.op("pe", lambda e: e.matmul(PS(6, 20, 4), lhsT=tri_b, rhs=lgf[:, 4:8], start=True, stop=True), reads=["cst", ck + "lgf"], writes=[pk(6)])
        R.op("pe", lambda e: e.matmul(PS(6, 24, 8), lhsT=ones, rhs=lgf, start=True, stop=True), reads=["cst", ck + "lgf"], writes=[pk(6)])
        R.op("dve", lambda e: e.tensor_tensor(out=imb, in0=ie, in1=PS(6, 16, 8), op=ALU.subtract), reads=[ck + "ie", pk(6)], writes=[ck + "imb"])
        R.op("dve", lambda e: e.tensor_tensor(out=gg, in0=imb, in1=PS(6, 24, 8), op=ALU.add), reads=[ck + "imb", pk(6)], writes=[ck + "gg"])
        R.op("act", lambda e: e.activation(out=wst, in_=gg, func=AF.Exp), reads=[ck + "gg"], writes=[ck + "wst"])
        R.op("act", lambda e: e.activation(out=dec, in_=PS(6, 24, 8), func=AF.Exp), reads=[pk(6)], writes=[ck + "dec"])
        R.op("act", lambda e: e.activation(out=expb[db], in_=PS(6, 16, 8), func=AF.Exp), reads=[pk(6)], writes=[ck + "expb"])

    def state_step(db, j, refresh_bf=False):
        h = j % 4
        q_ = cq[db]
        wst, dec = q_[:, 48:56], q_[:, 56:64]
        ck = "cq%d" % db
        kb = j % 2
        R.op("dve", lambda e: e.tensor_scalar(out=Kw[kb], in0=Kt[db][:, h * 128:(h + 1) * 128], scalar1=wst[:, j:j + 1], scalar2=None, op0=ALU.mult),
             reads=["Kt%d" % db, ck + "wst"], writes=["Kw%d" % kb])
        R.op("pe", lambda e: e.matmul(PS(7, 0, 257), lhsT=Kw[kb], rhs=Vx[db][:, h, 0:257], start=True, stop=True),
             reads=["Kw%d" % kb, "Vx%d" % db], writes=[pk(7)])
        R.op("dve", lambda e: e.scalar_tensor_tensor(out=stC[j], in0=stC[j], scalar=dec[:, j:j + 1], in1=PS(7, 0, 257), op0=ALU.mult, op1=ALU.add),
             reads=["stC%d" % j, ck + "dec", pk(7)], writes=["stC%d" % j])
        if refresh_bf:
            R.op("act", lambda e: e.activation(out=stCb[j], in_=stC[j], func=AF.Copy), reads=["stC%d" % j], writes=["stCb%d" % j])

    rtq = None
    for s in range(n_oth):
        db = s % 2
        slot_common(s, db)
        if s == 0:
            for j in range(4):
                state_step(db, j)
        elif s == 1:
            for j in range(4):
                state_step(1, j)
            for j in range(4, 8):
                state_step(1, j)
            for j in range(4, 8):
                state_step(0, j)
        else:
            for j in range(8):
                state_step(db, j)
    if debug == "oth":
        s = n_oth - 1
        dump(hT[s % 2].rearrange("p c t -> p (c t)")[:, 0:0], "x", 0, 0) if False else None
        dump_bf(hT[s % 2].rearrange("p c t -> p (c t)"), "hT%d" % (s % 2), 0, 2048)
        dump_bf(kTst[:, :, s * 128:(s + 1) * 128], "kTst", 2048, 256) if False else None
        dump_bf(Vst[:, s, :], "Vst", 2304, 256)
        dump_bf(Kt[s % 2], "Kt%d" % (s % 2), 2560, 512)
        dump(Gt[s % 2], "G%d" % (s % 2), 3072, 16)
        dump(cq[s % 2], "cq%dwst" % (s % 2), 3088, 64)
        for j in range(8):
            dump(stC[j], "stC%d" % j, 3200 + 257 * j, 257)
        dump_bf(krot, "krot", 5300, 256)
        return finish()


    hacc = A.b(NOWN * 1024).rearrange("p (o h v) -> p o h v", o=NOWN, h=4)
    mark_own = A.off
    Wq = A.b(16 * 512).rearrange("p (c n) -> p c n", n=512)
    R.op("pool", lambda e: e.dma_start(out=Wq, in_=win_v[:, :, MQ0:MQ0 + 512]), writes=["Wq"], dma=True)
    qmT = A.b(512).rearrange("p (h t) -> p h t", h=4)
    kmT = A.b(512).rearrange("p (h t) -> p h t", h=4)
    lb = A.f(128)
    DTt = A.f(128)
    STt = A.b(128)
    tmpn = A.f(257)
    tot = A.f(257)
    ddr = A.f(2)
    for j in range(8):
        R.op("act", lambda e, j=j: e.activation(out=stCb[j], in_=stC[j], func=AF.Copy), reads=["stC%d" % j], writes=["stCb%d" % j])

    def full_step(s, db, j, o, first):
        h = j % 4
        dirn = j // 4
        TRI = tri_f if dirn == 0 else tri_b
        NM = nm_f if dirn == 0 else nm_b
        q_ = cq[db]
        lgf, imb = q_[:, 16:24], q_[:, 32:40]
        ck = "cq%d" % db
        R.op("dve", lambda e: e.tensor_scalar(out=lb, in0=ones, scalar1=lgf[:, j:j + 1], scalar2=None, op0=ALU.mult), reads=["cst", ck + "lgf"], writes=["lb"])
        R.op("pe", lambda e: e.matmul(PS(4, 0, 128), lhsT=lb, rhs=TRI, start=True, stop=False), reads=["lb", "cst"], writes=[pk(4)])
        R.op("pe", lambda e: e.matmul(PS(4, 0, 128), lhsT=ident, rhs=NM, start=False, stop=True), reads=["cst"], writes=[pk(4)])
        R.op("act", lambda e: e.activation(out=DTt, in_=PS(4, 0, 128), func=AF.Exp, bias=imb[:, j:j + 1]), reads=[pk(4), ck + "imb"], writes=["DT"])
        R.op("pe", lambda e: e.matmul(PS(5, 0, 128), lhsT=kmT[:, h, :], rhs=qmT[:, h, :], start=True, stop=True), reads=["kmT", "qmT"], writes=[pk(5)])
        R.op("dve", lambda e: e.tensor_tensor(out=STt, in0=PS(5, 0, 128), in1=DTt, op=ALU.mult), reads=[pk(5), "DT"], writes=["ST"])
        R.op("pe", lambda e: e.matmul(PS(2, 0, 257), lhsT=STt, rhs=Vx[db][:, h, 0:257], start=True, stop=True), reads=["ST", "Vx%d" % db], writes=[pk(2)])
        R.op("pe", lambda e: e.matmul(PS(3, 0, 257), lhsT=qmT[:, h, :], rhs=stCb[j], start=True, stop=True), reads=["qmT", "stCb%d" % j], writes=[pk(3)])
        R.op("act", lambda e: e.activation(out=tmpn, in_=PS(2, 0, 257), func=AF.Copy), reads=[pk(2)], writes=["tmpn"])
        R.op("dve", lambda e: e.scalar_tensor_tensor(out=tot, in0=PS(3, 0, 257), scalar=expb[db][:, j:j + 1], in1=tmpn, op0=ALU.mult, op1=ALU.add),
             reads=[pk(3), ck + "expb", "tmpn"], writes=["tot"])
        R.op("dve", lambda e: e.scalar_tensor_tensor(out=ddr[:, 0:1], in0=tot[:, 256:257], scalar=-1.0, in1=tot[:, 256:257], op0=ALU.mult, op1=ALU.max), reads=["tot"], writes=["dd"])
        R.op("dve", lambda e: e.tensor_scalar(out=ddr[:, 0:1], in0=ddr[:, 0:1], scalar1=1.0, scalar2=None, op0=ALU.max), reads=["dd"], writes=["dd"])
        R.op("dve", lambda e: e.reciprocal(out=ddr[:, 1:2], in_=ddr[:, 0:1]), reads=["dd"], writes=["rr"])
        hk_ = "hacc%d" % o
        if first:
            R.op("dve", lambda e: e.tensor_scalar(out=hacc[:, o, h, :], in0=tot[:, 0:256], scalar1=ddr[:, 1:2], scalar2=None, op0=ALU.mult), reads=["tot", "rr"], writes=[hk_])
        else:
            R.op("dve", lambda e: e.scalar_tensor_tensor(out=hacc[:, o, h, :], in0=tot[:, 0:256], scalar=ddr[:, 1:2], in1=hacc[:, o, h, :], op0=ALU.mult, op1=ALU.add),
                 reads=["tot", "rr", hk_], writes=[hk_])
        state_step(db, j, refresh_bf=True)

    def own_visit(o, dirn):
        s = NOTH + o
        db = s % 2
        slot_common(s, db)
        h_, hk = hT[s % 2], "hT%d" % (s % 2)
        for hh in range(4):
            for c in range(NCH):
                R.op("pe", lambda e, c=c, hh=hh: e.matmul(PS(2, hh * 128, 128), lhsT=Wq[:, c, hh * 128:(hh + 1) * 128], rhs=h_[:, c, :], start=(c == 0), stop=(c == NCH - 1)),
                     reads=["Wq", hk], writes=[pk(2)])
        for hh in range(4):
            R.op("act", lambda e, hh=hh: e.activation(out=qmT[:, hh, :], in_=PS(2, hh * 128, 128), func=AF.Identity, bias=bfm[:, hh:hh + 1]), reads=[pk(2), "bfm"], writes=["qmT"])
        for hh in range(4):
            for c in range(NCH):
                R.op("pe", lambda e, c=c, hh=hh: e.matmul(PS(3, hh * 128, 128), lhsT=Wt[:, c, 512 + hh * 128:512 + (hh + 1) * 128], rhs=h_[:, c, :], start=(c == 0), stop=(c == NCH - 1)),
                     reads=["W", hk], writes=[pk(3)])
        for hh in range(4):
            R.op("act", lambda e, hh=hh: e.activation(out=kmT[:, hh, :], in_=PS(3, hh * 128, 128), func=AF.Identity, scale=SC, bias=bfm[:, 4 + hh:5 + hh]), reads=[pk(3), "bfm"], writes=["kmT"])
        for hh in range(4):
            full_step(s, db, dirn * 4 + hh, o, first=(dirn == 0))

    n_own = NOWN if debug != "own" else 2
    for o in range(n_own):
        own_visit(o, 0)
    for o in range(n_own - 1, -1, -1):
        own_visit(o, 1)
    if debug == "own":
        dump_bf(hacc[:, 0, :, :].rearrange("p h v -> p (h v)"), "hacc0", 0, 1024)
        dump_bf(hacc[:, 1, :, :].rearrange("p h v -> p (h v)"), "hacc1", 1024, 1024)
        return finish()


    R.barrier()
    A.off = mark_own
    Wc = Wflat[:, 0:16 * 1024].rearrange("p (c n) -> p c n", n=1024)
    yT = Wflat[:, 16 * 1024:32 * 1024].rearrange("p (k t) -> p k t", t=1024)
    qf = A.f(1024)
    rtq_all = A.f(2048)
    rtq = [rtq_all[:, i * 512:(i + 1) * 512] for i in range(4)]
    qrot = A.b(1024)
    qTc = A.b(1024).rearrange("p (h t) -> p h t", h=8)
    PTt = [A.b(512) for _ in range(2)]
    dsb = A.f(512)
    qss = A.f(16)
    gmb = A.f(1024)

    def load_wc(lo, wd=win_v):
        R.op("pool", lambda e: e.dma_start(out=Wc, in_=wd[:, :, lo:lo + 1024]), writes=["W"], dma=True)

    load_wc(Q0)
    R.op("sp", lambda e: e.dma_start(out=bo[:, 0:1024], in_=pbc(bin_d[:, Q0:Q0 + 1024])), writes=["bo"], dma=True)
    R.op("sp", lambda e: e.dma_start(out=bo[:, 1024:2048], in_=pbc(bin_d[:, MO0:MO0 + 1024])), writes=["bo"], dma=True)
    R.op("sp", lambda e: e.dma_start(out=gmb, in_=pbc(gm_d)), writes=["gmb"], dma=True)
    qf3 = qf.rearrange("p (h d) -> p h d", h=8)
    for o in range(NOWN):
        s = NOTH + o
        b2 = s % 2
        h_, hk = make_hT(s)
        R.op("sp", lambda e, s=s, b2=b2: e.dma_start(out=rp[b2], in_=rope_d[s * 128:(s + 1) * 128, :]), writes=["rp%d" % b2], dma=True)
        proj_tok(h_, hk, 0, 512, 2, w=Wc)
        proj_tok(h_, hk, 512, 512, 3, w=Wc)
        for hf_ in range(2):
            R.op("dve", lambda e, hf_=hf_: e.tensor_tensor(out=qf[:, hf_ * 512:(hf_ + 1) * 512], in0=PS(2 + hf_), in1=bo[:, hf_ * 512:(hf_ + 1) * 512], op=ALU.add),
                 reads=[pk(2 + hf_), "bo"], writes=["qf"])
        R.op("pool", lambda e: e.memset(qss[:, 0:8], 0.0), writes=["qss"])
        for hh in range(8):
            R.op("act", lambda e, hh=hh: e.activation(out=rtq[0][:, 0:128], in_=qf[:, hh * 128:(hh + 1) * 128], func=AF.Square, accum_out=qss[:, hh:hh + 1]),
                 reads=["qf", "qss"], writes=["rt0", "qss"])
        rstd_op(qss[:, 8:16], qss[:, 0:8], 128.0, ["qss"], "qrs")
        R.op("dve", lambda e: e.tensor_tensor(out=qf3, in0=qf3, in1=qss[:, 8:16].unsqueeze(2).to_broadcast([128, 8, 128]), op=ALU.mult), reads=["qf", "qrs"], writes=["qf"])
        R.op("dve", lambda e: e.tensor_tensor(out=qf3, in0=qf3, in1=gqb.unsqueeze(1).to_broadcast([128, 8, 128]), op=ALU.mult), reads=["qf", "gqb"], writes=["qf"])
        rope(qf, "qf", qrot, "qrot", 8, rp[b2], "rp%d" % b2)
        for hh in range(8):
            R.op("pe", lambda e, hh=hh: e.transpose(out=pT[:, hh * 128:(hh + 1) * 128], in_=qrot[:, hh * 128:(hh + 1) * 128], identity=identb),
                 reads=["qrot", "identb"], writes=["pT0"])
        R.op("act", lambda e: e.activation(out=qTc, in_=pT[:, 0:1024].rearrange("p (h t) -> p h t", h=8), func=AF.Copy), reads=["pT0"], writes=["qTc"])
        for g in range(2):
            for sk in range(NSLOT):
                sb = 2 + (sk % 2)
                pb = sk % 2
                R.op("pe", lambda e, g=g, sk=sk, sb=sb: e.matmul(PS(sb), lhsT=kTst[:, g, sk * 128:(sk + 1) * 128], rhs=qTc[:, 4 * g:4 * g + 4, :], start=True, stop=True),
                     reads=["kTst", "qTc"], writes=[pk(sb)])
                R.op("act", lambda e, sb=sb, pb=pb: e.activation(out=PTt[pb], in_=PS(sb), func=AF.Exp), reads=[pk(sb)], writes=["PT%d" % pb])
                R.op("pe", lambda e, g=g, sk=sk, pb=pb: e.matmul(PS(4 + g), lhsT=Vst[:, sk, g * 128:(g + 1) * 128], rhs=PTt[pb], start=(sk == 0), stop=(sk == NSLOT - 1)),
                     reads=["Vst", "PT%d" % pb], writes=[pk(4 + g)])
                R.op("pe", lambda e, g=g, sk=sk, pb=pb: e.matmul(PS(6 + g), lhsT=onesb, rhs=PTt[pb], start=(sk == 0), stop=(sk == NSLOT - 1)),
                     reads=["onesb", "PT%d" % pb], writes=[pk(6 + g)])
            R.op("act", lambda e, g=g: e.activation(out=dsb, in_=PS(6 + g), func=AF.Copy), reads=[pk(6 + g)], writes=["dsb"])
            R.op("dve", lambda e: e.reciprocal(out=dsb, in_=dsb), reads=["dsb"], writes=["dsb"])
            R.op("dve", lambda e, g=g, o=o: e.tensor_tensor(out=yT[:, 4 * g:4 * g + 4, o * 128:(o + 1) * 128], in0=PS(4 + g).rearrange("p (h t) -> p h t", h=4),
                                                          in1=dsb.rearrange("p (h t) -> p h t", h=4), op=ALU.mult),
                 reads=[pk(4 + g), "dsb"], writes=["yT"])
    if debug == "att":
        for k in range(8):
            dump_bf(yT[:, k, 0:256], "yT", k * 256, 256)
        return finish()

    R.barrier()
    load_wc(MO0)
    hn = rtq_all[:, 0:1024]
    ym = qrot
    hn3 = hn.rearrange("p (h v) -> p h v", h=4)
    for o in range(NOWN):
        s = NOTH + o
        h_, hk = make_hT(s)
        proj_tok(h_, hk, 0, 512, 2, w=Wc)
        proj_tok(h_, hk, 512, 512, 3, w=Wc)
        for hf_ in range(2):
            R.op("dve", lambda e, hf_=hf_: e.tensor_tensor(out=qf[:, hf_ * 512:(hf_ + 1) * 512], in0=PS(2 + hf_), in1=bo[:, 1024 + hf_ * 512:1024 + (hf_ + 1) * 512], op=ALU.add),
                 reads=[pk(2 + hf_), "bo"], writes=["qf"])
        R.op("act", lambda e: e.activation(out=qf, in_=qf, func=AF.Sigmoid), reads=["qf"], writes=["qf"])
        R.op("pool", lambda e: e.memset(qss[:, 0:4], 0.0), writes=["qss"])
        for hh in range(4):
            R.op("act", lambda e, hh=hh, o=o: e.activation(out=hn[:, hh * 256:(hh + 1) * 256], in_=hacc[:, o, hh, :], func=AF.Square, accum_out=qss[:, hh:hh + 1]),
                 reads=["hacc%d" % o, "qss"], writes=["hn", "qss"])
        rstd_op(qss[:, 8:12], qss[:, 0:4], 256.0, ["qss"], "qrs")
        R.op("dve", lambda e, o=o: e.tensor_tensor(out=hn3, in0=hacc[:, o, :, :], in1=qss[:, 8:12].unsqueeze(2).to_broadcast([128, 4, 256]), op=ALU.mult),
             reads=["hacc%d" % o, "qrs"], writes=["hn"])
        R.op("pool", lambda e: e.tensor_tensor(out=hn, in0=hn, in1=gmb, op=ALU.mult), reads=["hn", "gmb"], writes=["hn"])
        R.op("dve", lambda e: e.tensor_tensor(out=ym, in0=hn, in1=qf, op=ALU.mult), reads=["hn", "qf"], writes=["qrot"])
        for k in range(8):
            R.op("pe", lambda e, k=k: e.transpose(out=pT[:, k * 128:(k + 1) * 128], in_=ym[:, k * 128:(k + 1) * 128], identity=identb),
                 reads=["qrot", "identb"], writes=["pT0"])
        R.op("act", lambda e, o=o: e.activation(out=yT[:, 8:16, o * 128:(o + 1) * 128], in_=pT[:, 0:1024].rearrange("p (k t) -> p k t", k=8), func=AF.Copy),
             reads=["pT0"], writes=["yT"])
    if debug == "ym":
        for k in range(8):
            dump_bf(yT[:, 8 + k, 0:256], "yT", k * 256, 256)
        return finish()

    R.barrier()
    A.off = mark_w_end
    x1 = A.f(NOWN * D).rearrange("p (o d) -> p o d", o=NOWN)
    bcA = A.f(1024)
    tmpd = [A.f(512) for _ in range(2)]
    lbm = A.f(128)
    wout_v = wout_d.rearrange("(c p) n -> p c n", p=128)

    def make_bc(dst, key, chunk_lo, nchunks, col, src=None, skey="modT"):
        for c in range(nchunks):
            if src is None:
                vec = modT[:, chunk_lo + c, col:col + 1]
            else:
                vec = src[:, chunk_lo + c:chunk_lo + c + 1]
            R.op("dve", lambda e, vec=vec: e.tensor_scalar(out=lbm, in0=ones, scalar1=vec, scalar2=None, op0=ALU.mult), reads=["cst", skey], writes=["lbm"])
            R.op("pe", lambda e, c=c: e.matmul(PS(2, (c % 4) * 128, 128), lhsT=lbm, rhs=ident, start=True, stop=True), reads=["lbm", "cst"], writes=[pk(2)])
            if c % 4 == 3:
                R.op("act", lambda e, c=c: e.activation(out=dst[:, (c - 3) * 128:(c + 1) * 128], in_=PS(2), func=AF.Copy), reads=[pk(2)], writes=[key])

    for o in range(NOWN):
        s = NOTH + o
        R.op("sp", lambda e, s=s, o=o: e.dma_start(out=x1[:, o, :], in_=xs_d[s * 128:(s + 1) * 128, :]), writes=["x1_%d" % o], dma=True)
    for half in range(2):
        load_wc(half * 1024, wd=wout_v)
        make_bc(bcA, "bcA", 32 + half * 8, 8, 0)
        for o in range(NOWN):
            for dh in range(2):
                for k in range(NCH):
                    R.op("pe", lambda e, o=o, dh=dh, k=k: e.matmul(PS(4 + dh), lhsT=yT[:, k, o * 128:(o + 1) * 128], rhs=Wc[:, k, dh * 512:(dh + 1) * 512],
                                                                  start=(k == 0), stop=(k == NCH - 1)), reads=["yT", "W"], writes=[pk(4 + dh)])
                cols = slice(half * 1024 + dh * 512, half * 1024 + (dh + 1) * 512)
                R.op("dve", lambda e, dh=dh: e.tensor_tensor(out=tmpd[dh], in0=PS(4 + dh), in1=bcA[:, dh * 512:(dh + 1) * 512], op=ALU.mult),
                     reads=[pk(4 + dh), "bcA"], writes=["tmpd%d" % dh])
                R.op("pool", lambda e, o=o, dh=dh, cols=cols: e.tensor_tensor(out=x1[:, o, cols], in0=x1[:, o, cols], in1=tmpd[dh], op=ALU.add),
                     reads=["x1_%d" % o, "tmpd%d" % dh], writes=["x1_%d" % o])
    if debug == "x1":
        for q in range(4):
            dump(x1[:, 0, q * 512:(q + 1) * 512], "x1_0", q * 512, 512)
        for q in range(4):
            dump(x1[:, 7, q * 512:(q + 1) * 512], "x1_7", 2048 + q * 512, 512)
        return finish()

    R.barrier()
    h2T = Wflat[:, 0:16 * 1024].rearrange("p (c t) -> p c t", t=1024)
    wfree = Wflat[:, 16 * 1024:16 * 1024 + 16384].bitcast(F32)
    gm2b = wfree[:, 0:2048]
    sh2b = wfree[:, 2048:4096]
    h2f = wfree[:, 4096:6144]
    h2Tr = wfree[:, 6144:8192].rearrange("p (c t) -> p c t", t=128)
    A.off = mark_w_end + NOWN * D
    sm2 = A.f(8)
    g2fm = A.f(16)
    gm2fm = A.f(16)
    wr = A.f(16 * NEXP).rearrange("p (c e) -> p c e", e=NEXP)
    brb = A.f(NEXP)
    wt = A.f(NOWN * NEXP).rearrange("p (o e) -> p o e", e=NEXP)
    rsm = A.f(64)
    ex = A.f(NEXP)
    msk = A.f(NEXP)
    R.op("sp", lambda e: e.dma_start(out=g2fm, in_=g2fm_d), writes=["g2fm"], dma=True)
    R.op("sp", lambda e: e.dma_start(out=wr, in_=wr_d.rearrange("(c p) e -> p c e", p=128)), writes=["wr"], dma=True)
    R.op("sp", lambda e: e.dma_start(out=brb, in_=pbc(br_d)), writes=["brb"], dma=True)
    R.op("dve", lambda e: e.scalar_tensor_tensor(out=gm2fm, in0=modT[:, 64:80, 0], scalar=1.0, in1=g2fm, op0=ALU.add, op1=ALU.mult), reads=["modT", "g2fm"], writes=["gm2fm"])
    make_bc(gm2b, "gm2b", 0, 16, 0, src=gm2fm, skey="gm2fm")
    make_bc(sh2b, "sh2b", 48, 16, 0)
    lg = rsm[:, 0:32]
    top8 = rsm[:, 32:40]
    for o in range(NOWN):
        xk = "x1_%d" % o
        R.op("pool", lambda e: e.memset(sm2[:, 0:1], 0.0), writes=["ss0"])
        R.op("act", lambda e, o=o: e.activation(out=h2f, in_=x1[:, o, :], func=AF.Square, accum_out=sm2[:, 0:1]), reads=[xk, "ss0"], writes=["h2f", "ss0"])
        rstd_op(sm2[:, 2:3], sm2[:, 0:1], float(D), ["ss0"], "rs0")
        R.op("dve", lambda e, o=o: e.scalar_tensor_tensor(out=h2f, in0=x1[:, o, :], scalar=sm2[:, 2:3], in1=gm2b, op0=ALU.mult, op1=ALU.mult),
             reads=[xk, "rs0", "gm2b"], writes=["h2f"])
        R.op("pool", lambda e: e.tensor_tensor(out=h2f, in0=h2f, in1=sh2b, op=ALU.add), reads=["h2f", "sh2b"], writes=["h2f"])
        for c in range(NCH):
            bk = 4 + (c // 4)
            R.op("pe", lambda e, c=c, bk=bk: e.transpose(out=PS(bk, (c % 4) * 128, 128), in_=h2f[:, c * 128:(c + 1) * 128], identity=ident),
                 reads=["h2f", "cst"], writes=[pk(bk)])
        for q in range(4):
            R.op("act", lambda e, q=q: e.activation(out=h2Tr[:, 4 * q:4 * q + 4, :], in_=PS(4 + q).rearrange("p (c t) -> p c t", t=128), func=AF.Copy),
                 reads=[pk(4 + q)], writes=["h2Tr"])
        R.op("dve", lambda e, o=o: e.tensor_copy(out=h2T[:, :, o * 128:(o + 1) * 128], in_=h2Tr), reads=["h2Tr"], writes=["h2T"])
        for c in range(NCH):
            R.op("pe", lambda e, c=c: e.matmul(PS(3, 0, NEXP), lhsT=h2Tr[:, c, :], rhs=wr[:, c, :], start=(c == 0), stop=(c == NCH - 1)), reads=["h2Tr", "wr"], writes=[pk(3)])
        R.op("dve", lambda e: e.tensor_tensor(out=lg, in0=PS(3, 0, NEXP), in1=brb, op=ALU.add), reads=[pk(3), "brb"], writes=["lg"])
        R.op("dve", lambda e: e.max(out=top8, in_=lg), reads=["lg"], writes=["top8"])
        R.op("dve", lambda e: e.tensor_scalar(out=msk, in0=lg, scalar1=top8[:, 3:4], scalar2=None, op0=ALU.is_ge), reads=["lg", "top8"], writes=["msk"])
        R.op("dve", lambda e: e.tensor_scalar(out=rsm[:, 40:41], in0=top8[:, 0:1], scalar1=-1.0, scalar2=None, op0=ALU.mult), reads=["top8"], writes=["nmx"])
        R.op("act", lambda e: e.activation(out=ex, in_=lg, func=AF.Exp, bias=rsm[:, 40:41]), reads=["lg", "nmx"], writes=["ex"])
        R.op("dve", lambda e: e.tensor_tensor(out=ex, in0=ex, in1=msk, op=ALU.mult), reads=["ex", "msk"], writes=["ex"])
        R.op("dve", lambda e: e.reduce_sum(out=rsm[:, 41:42], in_=ex, axis=AX.X), reads=["ex"], writes=["esum"])
        R.op("dve", lambda e: e.reciprocal(out=rsm[:, 42:43], in_=rsm[:, 41:42]), reads=["esum"], writes=["ersum"])
        R.op("dve", lambda e, o=o: e.tensor_scalar(out=wt[:, o, :], in0=ex, scalar1=rsm[:, 42:43], scalar2=None, op0=ALU.mult), reads=["ex", "ersum"], writes=["wt"])
    if debug == "rt":
        dump(wt.rearrange("p o e -> p (o e)"), "wt", 0, 256)
        dump_bf(h2T[:, 0, 0:256], "h2T", 256, 256)
        return finish()

    R.barrier()
    gt2b = wfree[:, 0:2048]
    ring = [wfree[:, 2048 * (1 + i):2048 * (2 + i)].bitcast(BF16).rearrange("p (c n) -> p c n", n=256) for i in range(3)]
    ring.append(A.b(16 * 256).rearrange("p (c n) -> p c n", n=256))
    actT = A.b(16 * 1024).rearrange("p (f t) -> p f t", t=1024)
    b1e = [A.f(32) for _ in range(2)]
    gtt = [A.f(512) for _ in range(2)]
    sgt = [A.f(512), wr.rearrange("p c e -> p (c e)")]
    utt = [A.f(512), A.f(512)]
    t10 = A.f(256)
    t1 = [t10, t10]
    make_bc(gt2b, "gt2b", 80, 16, 0)
    b2g = actT.rearrange("p f t -> p (f t)")[:, 0:4096].bitcast(F32)
    wtT = actT.rearrange("p f t -> p (f t)")[:, 4096:4096 + 2048].bitcast(F32)
    R.op("sp", lambda e: e.dma_start(out=b2g[0:NEXP, :], in_=b2_d), writes=["b2g"], dma=True)
    for o in range(NOWN):
        R.op("pe", lambda e, o=o: e.transpose(out=PS(2, o * 64, 128)[0:NEXP, :] if False else PS(2 + o // 4, (o % 4) * 128, 128)[0:NEXP, :], in_=wt[:, o, :], identity=ident),
             reads=["wt", "cst"], writes=[pk(2 + o // 4)])
    for q in range(2):
        R.op("act", lambda e, q=q: e.activation(out=wtT[0:NEXP, q * 512:(q + 1) * 512], in_=PS(2 + q)[0:NEXP, :], func=AF.Copy), reads=[pk(2 + q)], writes=["wtT"])
    for o in range(NOWN):
        for dq in range(4):
            bk = 4 + (dq % 2)
            R.op("pe", lambda e, o=o, dq=dq, bk=bk: e.matmul(PS(bk), lhsT=wtT[0:NEXP, o * 128:(o + 1) * 128], rhs=b2g[0:NEXP, dq * 512:(dq + 1) * 512], start=True, stop=True),
                 reads=["wtT", "b2g"], writes=[pk(bk)])
            R.op("dve", lambda e, dq=dq, bk=bk: e.tensor_tensor(out=gtt[dq % 2], in0=PS(bk), in1=gt2b[:, dq * 512:(dq + 1) * 512], op=ALU.mult),
                 reads=[pk(bk), "gt2b"], writes=["gtt%d" % (dq % 2)])
            R.op("pool", lambda e, o=o, dq=dq: e.tensor_tensor(out=x1[:, o, dq * 512:(dq + 1) * 512], in0=x1[:, o, dq * 512:(dq + 1) * 512], in1=gtt[dq % 2], op=ALU.add),
                 reads=["x1_%d" % o, "gtt%d" % (dq % 2)], writes=["x1_%d" % o])
    R.barrier()
    if big:
        w1_v = [w1_d[e_].rearrange("(c p) n -> p c n", p=128) for e_ in range(NEXP)]
        w2_v = [w2_d[e_].rearrange("(c p) n -> p c n", p=128) for e_ in range(NEXP)]
    nld = [0]

    def ring_load(src):
        i = nld[0] % 4
        nld[0] += 1
        R.op("pool", lambda e, i=i: e.dma_start(out=ring[i], in_=src), writes=["ring%d" % i], dma=True)
        return ring[i], "ring%d" % i

    for e_ in range(n_exp if big else 0):
        b1 = b1e[e_ % 2]
        b1k = "b1_%d" % (e_ % 2)
        R.op("sp", lambda e, e_=e_, b1=b1: e.dma_start(out=b1, in_=b1_d[:, e_ * 32:(e_ + 1) * 32]), writes=[b1k], dma=True)
        pend = []
        it = 0
        for u in range(8):
            rg, rgk = ring_load(w1_v[e_][:, :, u * 256:(u + 1) * 256])
            ru, ruk = ring_load(w1_v[e_][:, :, D + u * 256:D + (u + 1) * 256])
            for fq in range(2):
                fc = u * 2 + fq
                for th in range(2):
                    pb = it % 2
                    it += 1
                    for c in range(NCH):
                        R.op("pe", lambda e, c=c, fq=fq, th=th, rg=rg: e.matmul(PS(2 + th), lhsT=rg[:, c, fq * 128:(fq + 1) * 128], rhs=h2T[:, c, th * 512:(th + 1) * 512],
                                                                               start=(c == 0), stop=(c == NCH - 1)), reads=[rgk, "h2T"], writes=[pk(2 + th)])
                    for c in range(NCH):
                        R.op("pe", lambda e, c=c, fq=fq, th=th, ru=ru: e.matmul(PS(4 + th), lhsT=ru[:, c, fq * 128:(fq + 1) * 128], rhs=h2T[:, c, th * 512:(th + 1) * 512],
                                                                               start=(c == 0), stop=(c == NCH - 1)), reads=[ruk, "h2T"], writes=[pk(4 + th)])
                    bg = b1[:, fc:fc + 1]
                    bu = b1[:, 16 + fc:16 + fc + 1]
                    gk_, sk_, uk_ = "gtt%d" % pb, "sgt%d" % pb, "utt%d" % pb
                    R.op("dve", lambda e, th=th, bg=bg, pb=pb: e.tensor_scalar(out=gtt[pb], in0=PS(2 + th), scalar1=bg, scalar2=7.0, op0=ALU.add, op1=ALU.min),
                         reads=[pk(2 + th), b1k], writes=[gk_])
                    R.op("act", lambda e, pb=pb: e.activation(out=sgt[pb], in_=gtt[pb], func=AF.Sigmoid, scale=1.702), reads=[gk_], writes=[sk_])
                    R.op("dve", lambda e, th=th, bu=bu, pb=pb: e.tensor_scalar(out=utt[pb], in0=PS(4 + th), scalar1=bu, scalar2=7.0, op0=ALU.add, op1=ALU.min),
                         reads=[pk(4 + th), b1k], writes=[uk_])
                    R.op("dve", lambda e, pb=pb: e.tensor_scalar(out=utt[pb], in0=utt[pb], scalar1=-7.0, scalar2=1.0, op0=ALU.max, op1=ALU.add), reads=[uk_], writes=[uk_])
                    R.op("pool", lambda e, pb=pb: e.tensor_tensor(out=gtt[pb], in0=gtt[pb], in1=sgt[pb], op=ALU.mult), reads=[gk_, sk_], writes=[gk_])
                    for p_ in pend:
                        p_()
                    pend = [lambda th=th, fc=fc, pb=pb, gk_=gk_, uk_=uk_: R.op(
                        "dve", lambda e: e.tensor_tensor(out=actT[:, fc, th * 512:(th + 1) * 512], in0=utt[pb], in1=gtt[pb], op=ALU.mult),
                        reads=[uk_, gk_], writes=["actT"])]
        for p_ in pend:
            p_()
        for u in range(8):
            r2, r2k = ring_load(w2_v[e_][:, :, u * 256:(u + 1) * 256])
            for o in range(NOWN):
                bk = 6 + (o % 2)
                for fc in range(NCH):
                    R.op("pe", lambda e, o=o, fc=fc, bk=bk, r2=r2: e.matmul(PS(bk, 0, 256), lhsT=actT[:, fc, o * 128:(o + 1) * 128], rhs=r2[:, fc, :],
                                                                           start=(fc == 0), stop=(fc == NCH - 1)), reads=["actT", r2k], writes=[pk(bk)])
                cols = slice(u * 256, (u + 1) * 256)
                R.op("dve", lambda e, o=o, bk=bk, cols=cols: e.tensor_tensor(out=t1[o % 2], in0=PS(bk, 0, 256), in1=gt2b[:, cols], op=ALU.mult),
                     reads=[pk(bk), "gt2b"], writes=["t1"])
                R.op("dve", lambda e, o=o, cols=cols, e_=e_: e.scalar_tensor_tensor(out=x1[:, o, cols], in0=t1[o % 2], scalar=wt[:, o, e_:e_ + 1], in1=x1[:, o, cols],
                                                                                  op0=ALU.mult, op1=ALU.add),
                     reads=["t1", "wt", "x1_%d" % o], writes=["x1_%d" % o])
    R.barrier()
    gfb = actT.rearrange("p f t -> p (f t)")[:, 0:4096].bitcast(F32)
    R.op("sp", lambda e: e.dma_start(out=gfb, in_=pbc(gf_d)), writes=["gfb"], dma=True)
    for o in range(NOWN):
        xk = "x1_%d" % o
        R.op("pool", lambda e: e.memset(sm2[:, 0:1], 0.0), writes=["ss0"])
        R.op("act", lambda e, o=o: e.activation(out=h2f, in_=x1[:, o, :], func=AF.Square, accum_out=sm2[:, 0:1]), reads=[xk, "ss0"], writes=["h2f", "ss0"])
        rstd_op(sm2[:, 2:3], sm2[:, 0:1], float(D), ["ss0"], "rs0")
        R.op("dve", lambda e, o=o: e.scalar_tensor_tensor(out=x1[:, o, :], in0=x1[:, o, :], scalar=sm2[:, 2:3], in1=gfb, op0=ALU.mult, op1=ALU.mult),
             reads=[xk, "rs0", "gfb"], writes=[xk])
        oo = R.op("sp", lambda e, o=o: e.dma_start(out=out_d[o * 128:(o + 1) * 128, :], in_=x1[:, o, :]), reads=[xk], dma=True)
        R.final_ops.append(oo)
    if debug is not None and debug.startswith("moe"):
        for q in range(4):
            dump(x1[:, 0, q * 512:(q + 1) * 512], "x1_0", q * 512, 512)
    return finish()


def _consts():
    r = np.arange(128)
    ident = np.eye(128, dtype=np.float32)
    tri_f = (r[:, None] <= r[None, :]).astype(np.float32)
    tri_b = (r[:, None] >= r[None, :]).astype(np.float32)
    nm_f = np.where(r[:, None] <= r[None, :], 0.0, NEG).astype(np.float32)
    nm_b = np.where(r[:, None] >= r[None, :], 0.0, NEG).astype(np.float32)
    ones = np.ones((128, 128), np.float32)
    iota_c = np.tile(np.arange(512, dtype=np.float32)[None, :], (128, 1))
    iota_p = r.astype(np.float32)[:, None]
    return np.ascontiguousarray(np.concatenate([ident, tri_f, tri_b, nm_f, nm_b, ones, iota_c, iota_p], axis=1))


def _rope_tables():
    rows = 64
    row = np.repeat(np.arange(rows, dtype=np.float32), 64)
    col = np.tile(np.arange(64, dtype=np.float32), rows)
    inv = (np.float32(10000.0) ** (-np.arange(0, 64, 2, dtype=np.float32) / np.float32(64))).astype(np.float32)
    ang = np.concatenate([row[:, None] * inv, col[:, None] * inv], axis=-1).astype(np.float32)
    return np.cos(ang).astype(np.float32), np.sin(ang).astype(np.float32)


def slot_chunks(j):
    pre = list(range(0, 8 * j))
    post = list(range(31, 8 * j + 7, -1))
    own = list(range(8 * j, 8 * j + 8))
    return pre, post, own


def make_in_maps(inp):
    f = lambda a: np.ascontiguousarray(np.asarray(a, dtype=np.float32))
    x, c, ctx, c_ctx = f(inp["x"]), f(inp["c"]), f(inp["ctx"]), f(inp["c_ctx"])
    cos, sin = _rope_tables()
    consts = _consts()
    fm = lambda v, n: np.ascontiguousarray(v.reshape(n, 128).T)
    shared = {
        "bmod": fm(f(inp["b_mod"])[0], 96),
        "g1fm": fm(f(inp["g_norm1"])[0], 16),
        "g2fm": fm(f(inp["g_norm2"])[0], 16),
        "w_mod": f(inp["w_mod"])[0],
        "w_in": f(inp["w_in"])[0],
        "b_in": f(inp["b_in"]),
        "bfm": np.ascontiguousarray(np.concatenate([fm(f(inp["b_in"])[0, MQ0:MQ0 + 512], 4), fm(f(inp["b_in"])[0, MK0:MK0 + 512], 4)], axis=1)),
        "g_q": f(inp["g_q"]), "g_k": f(inp["g_k"]), "g_mlstm": f(inp["g_mlstm"]),
        "w_out": f(inp["w_out"])[0],
        "g_norm2": f(inp["g_norm2"]), "g_final": f(inp["g_final"])[None, :],
        "w_router": f(inp["w_router"])[0], "b_router": f(inp["b_router"]),
        "w1": inp["w1"],
        "b1fm": np.ascontiguousarray(f(inp["b1"])[0].reshape(NEXP, 32, 128).transpose(2, 0, 1).reshape(128, NEXP * 32)),
        "w2": inp["w2"], "b2": f(inp["b2"])[0],
        "consts": consts,
    }
    maps = []
    for core in range(8):
        b, j = core // 4, core % 4
        pre, post, own = slot_chunks(j)
        xs = np.empty((NSLOT * 128, D), np.float32)
        rope = np.empty((NSLOT * 128, 128), np.float32)
        gmask = np.zeros((NSLOT, 16), np.float32)
        xs[0:256] = ctx[b]
        rope[0:256, 0:64] = 1.0
        rope[0:256, 64:128] = 0.0
        gmask[0:2, 0:8] = 0.0
        gmask[0:2, 8:16] = -1.0
        s = 2
        for kind, lst in (("pre", pre), ("post", post), ("own", own)):
            for ch in lst:
                xs[s * 128:(s + 1) * 128] = x[b, ch * 128:(ch + 1) * 128]
                rope[s * 128:(s + 1) * 128, 0:64] = cos[ch * 128:(ch + 1) * 128]
                rope[s * 128:(s + 1) * 128, 64:128] = sin[ch * 128:(ch + 1) * 128]
                fa = kind in ("pre", "own")
                ba = kind in ("post", "own")
                gmask[s, 0:4] = 0.0 if fa else NEG
                gmask[s, 4:8] = 0.0 if ba else NEG
                gmask[s, 8:12] = -1.0 if fa else 0.0
                gmask[s, 12:16] = -1.0 if ba else 0.0
                s += 1
        assert s == NSLOT
        m = dict(shared)
        m["xs"] = xs
        m["rope"] = rope
        m["gmask"] = np.ascontiguousarray(np.tile(gmask.reshape(1, NSLOT * 16), (128, 1)))
        m["cfm"] = np.ascontiguousarray(np.concatenate([fm(c[b], 16), fm(c_ctx, 16)], axis=1))
        maps.append(m)
    return maps


_CACHE = {}


def kernel(**inputs):
    if "nc" not in _CACHE:
        _CACHE["nc"] = build()
    nc, es, declared = _CACHE["nc"]
    maps = make_in_maps(inputs)
    maps = [{k: v for k, v in m.items() if k in declared} for m in maps]
    res = run_bass_kernel_spmd(nc, maps, core_ids=list(range(8)))
    out = np.empty((2, 4096, D), np.float32)
    for core in range(8):
        b, j = core // 4, core % 4
        out[b, j * 1024:(j + 1) * 1024] = res.results[core]["out"]
    return out
```

```python
import numpy as np
import ml_dtypes
from contextlib import ExitStack
import concourse.bass as bass
import concourse.mybir as mybir
from concourse.bass_utils import run_bass_kernel_spmd

F32 = mybir.dt.float32
BF16 = mybir.dt.bfloat16
ALU = mybir.AluOpType
AF = mybir.ActivationFunctionType
AX = mybir.AxisListType

D = 2048
NCH = 16
NSLOT = 34
NOWN = 8
NOTH = 26
EPS = 1e-6
NEG = -30000.0
CAP = 512
NEXP = 32
Q0, K0, V0, MQ0, MK0, MV0, MO0, MI0, MF0 = 0, 1024, 1280, 1536, 2048, 2560, 3584, 4608, 4616


class Op:
    __slots__ = ("eng", "fn", "deps", "is_dma", "signal", "count", "semkey", "value", "name")

    def __init__(self, eng, fn, is_dma, name=""):
        self.eng = eng
        self.fn = fn
        self.deps = []
        self.is_dma = is_dma
        self.signal = False
        self.count = None
        self.semkey = None
        self.value = None
        self.name = name


class Rec:
    ENG = ["pe", "act", "dve", "pool", "sp"]
    NS = 8

    def __init__(self):
        self.streams = {e: [] for e in self.ENG}
        self.last_w = {}
        self.readers = {}
        self.pending = {e: [] for e in self.ENG}
        self.dma_ops = {e: [] for e in self.ENG}
        self.final_ops = []

    def op(self, eng, fn, reads=(), writes=(), dma=False, name=""):
        o = Op(eng, fn, dma, name)
        deps = []
        for k in reads:
            w = self.last_w.get(k)
            if w is not None:
                if not (w.eng == eng and not w.is_dma and eng == "pe"):
                    deps.append(w)
        for k in writes:
            w = self.last_w.get(k)
            if w is not None and (w.eng != eng or w.is_dma):
                deps.append(w)
            for r in self.readers.get(k, ()):
                if r.eng != eng or r.is_dma or eng != "pe":
                    if r is not o:
                        deps.append(r)
        deps.extend(self.pending[eng])
        self.pending[eng] = []
        if dma:
            lst = self.dma_ops[eng]
            if len(lst) >= self.NS:
                deps.append(lst[len(lst) - self.NS])
            lst.append(o)
        o.deps = deps
        for k in writes:
            self.last_w[k] = o
            self.readers[k] = []
        for k in reads:
            self.readers.setdefault(k, []).append(o)
        self.streams[eng].append(o)
        return o

    def barrier(self):
        lasts = []
        for e in self.ENG:
            if self.streams[e]:
                lasts.append(self.streams[e][-1])
            lasts.extend(self.dma_ops[e][-self.NS:])
        for e in self.ENG:
            self.pending[e] = [o for o in lasts if (o.eng != e or o.is_dma)]
        self.last_w = {}
        self.readers = {}

    def emit(self, nc, block):
        for e in self.ENG:
            for o in self.streams[e]:
                for d in o.deps:
                    d.signal = True
        for o in self.final_ops:
            o.signal = True
        nsem = {}
        for e in self.ENG:
            c = 0
            ndma = 0
            for o in self.streams[e]:
                if o.is_dma:
                    slot = ndma % self.NS
                    o.semkey = ("dma", e, slot)
                    o.value = 16 * (ndma // self.NS + 1)
                    ndma += 1
                    o.signal = True
                elif o.signal:
                    c += 1
                    o.semkey = ("eng", e)
                    o.value = c
        sems = {}

        def sem(key):
            if key not in sems:
                sems[key] = self._es.enter_context(nc.semaphore("s_" + "_".join(str(k) for k in key)))
            return sems[key]

        final_ops = self.final_ops

        def run(ename, eh):
            seen = {}
            for o in self.streams[ename]:
                need = {}
                for d in o.deps:
                    if need.get(d.semkey, 0) < d.value:
                        need[d.semkey] = d.value
                for k, v in need.items():
                    if seen.get(k, 0) < v:
                        eh.wait_ge(sem(k), v)
                        seen[k] = v
                ins = o.fn(eh)
                if o.signal:
                    ins.then_inc(sem(o.semkey), 16 if o.is_dma else 1)
            if ename == "sp":
                for o in final_ops:
                    if seen.get(o.semkey, 0) < o.value:
                        eh.wait_ge(sem(o.semkey), o.value)
                        seen[o.semkey] = o.value

        for e in self.ENG:
            sem(("eng", e))
            for s in range(self.NS):
                if e in ("sp", "pool", "act"):
                    sem(("dma", e, s))

        @block.tensor
        def _(eh):
            run("pe", eh)

        @block.scalar
        def _(eh):
            run("act", eh)

        @block.vector
        def _(eh):
            run("dve", eh)

        @block.gpsimd
        def _(eh):
            run("pool", eh)

        @block.sync
        def _(eh):
            run("sp", eh)


class Arena:
    def __init__(self, t, n):
        self.t = t
        self.n = n
        self.off = 0

    def f(self, cols):
        lo = self.off
        self.off += cols
        assert self.off <= self.n, ("arena overflow", self.off, self.n)
        return self.t[:, lo:lo + cols]

    def b(self, cols):
        c2 = (cols + 1) // 2
        return self.f(c2).bitcast(BF16)[:, 0:cols]


def pbc(ap):
    v = ap.partition_broadcast(128)
    return v[:, 0, :]


def build(debug=None, n_oth=NOTH, n_exp=NEXP):
    nc = bass.Bass("TRN2", target_bir_lowering=False)
    R = Rec()
    es = ExitStack()
    R._es = es
    declared = []
    big = debug is None or debug.startswith("moe")

    def din(name, shape, dt=F32):
        if name in ("w1", "w2") and not big:
            return None
        declared.append(name)
        return nc.dram_tensor(name, list(shape), dt, kind="ExternalInput").ap()

    xs_d = din("xs", [NSLOT * 128, D])
    rope_d = din("rope", [NSLOT * 128, 128])
    gmask_d = din("gmask", [128, NSLOT * 16])
    cfm_d = din("cfm", [128, 32])
    bmod_d = din("bmod", [128, 96])
    g1_d = din("g1fm", [128, 16])
    g2fm_d = din("g2fm", [128, 16])
    wmod_d = din("w_mod", [D, 6 * D])
    win_d = din("w_in", [D, 4624])
    bin_d = din("b_in", [1, 4624])
    bfm_d = din("bfm", [128, 8])
    gq_d = din("g_q", [1, 128])
    gk_d = din("g_k", [1, 128])
    gm_d = din("g_mlstm", [1, 1024])
    wout_d = din("w_out", [D, D])
    g2_d = din("g_norm2", [1, D])
    gf_d = din("g_final", [1, D])
    wr_d = din("w_router", [D, NEXP])
    br_d = din("b_router", [1, NEXP])
    w1_d = din("w1", [NEXP, D, 2 * D])
    b1_d = din("b1fm", [128, NEXP * 32])
    w2_d = din("w2", [NEXP, D, D])
    b2_d = din("b2", [NEXP, D])
    cst_d = din("consts", [128, 6 * 128 + 512 + 1])
    out_d = nc.dram_tensor("out", [NOWN * 128, D], F32, kind="ExternalOutput").ap()
    dbg_d = None
    if debug is not None:
        dbg_d = nc.dram_tensor("dbg", [128, 8192], F32, kind="ExternalOutput").ap()

    NF = 52500
    fa_t = es.enter_context(nc.sbuf_tensor("fa", [128, NF], F32))
    A = Arena(fa_t, NF)
    pT = es.enter_context(nc.psum_tensor("pT", [128, 2048], BF16))
    psum = [None, None] + [es.enter_context(nc.psum_tensor("ps%d" % i, [128, 512], F32)) for i in range(2, 8)]

    def PS(i, lo=0, n=512):
        return psum[i][:, lo:lo + n]

    def pk(i):
        return "ps%d" % i

    def finish():
        with nc.Block() as block:
            R.emit(nc, block)
        return nc, es, declared

    def dump(ap, key, lo, n):
        o = R.op("sp", lambda e: e.dma_start(out=dbg_d[:, lo:lo + n], in_=ap), reads=[key], dma=True)
        R.final_ops.append(o)

    dbgf = None
    if debug is not None:
        dbgf = A.f(512)

    def dump_bf(ap, key, lo, n):
        for p0 in range(0, n, 512):
            m = min(512, n - p0)
            R.op("dve", lambda e, p0=p0, m=m: e.tensor_copy(out=dbgf[:, 0:m], in_=ap[:, p0:p0 + m]), reads=[key], writes=["dbgf"])
            dump(dbgf[:, 0:m], "dbgf", lo + p0, m)

    cst = A.f(6 * 128 + 512 + 1)
    ident = cst[:, 0:128]
    tri_f = cst[:, 128:256]
    tri_b = cst[:, 256:384]
    nm_f = cst[:, 384:512]
    nm_b = cst[:, 512:640]
    ones = cst[:, 640:768]
    iota_c = cst[:, 768:1280]
    iota_p = cst[:, 1280:1281]
    identb = A.b(128)
    onesb = A.b(128)
    modT = A.f(192).rearrange("p (a b) -> p a b", b=2)
    gml = A.f(16)
    gmc = A.f(16)
    g1 = A.f(16)
    cfm = A.f(32)
    bmod = A.f(96)
    bfm = A.f(8)
    csT = A.b(32).rearrange("p (a b) -> p a b", b=2)
    stC = [A.f(257) for _ in range(8)]
    stCb = [A.b(258)[:, 0:257] for _ in range(8)]
    epsb = A.f(2)
    R.op("pool", lambda e: e.memset(epsb[:, 0:1], EPS), writes=["epsb"])
    R.op("pool", lambda e: e.memset(epsb[:, 1:2], 1.0), writes=["epsb"])
    gmask = A.f(NSLOT * 16).rearrange("p (s g) -> p s g", g=16)

    R.op("sp", lambda e: e.dma_start(out=cst, in_=cst_d), writes=["cst"], dma=True)
    R.op("sp", lambda e: e.dma_start(out=cfm, in_=cfm_d), writes=["cfm"], dma=True)
    R.op("sp", lambda e: e.dma_start(out=bmod, in_=bmod_d), writes=["bmod"], dma=True)
    R.op("sp", lambda e: e.dma_start(out=g1, in_=g1_d), writes=["g1"], dma=True)
    R.op("sp", lambda e: e.dma_start(out=bfm, in_=bfm_d), writes=["bfm"], dma=True)
    R.op("sp", lambda e: e.dma_start(out=gmask.rearrange("p s g -> p (s g)"), in_=gmask_d), writes=["gmask"], dma=True)
    R.op("dve", lambda e: e.tensor_copy(out=identb, in_=ident), reads=["cst"], writes=["identb"])
    R.op("dve", lambda e: e.tensor_copy(out=onesb, in_=ones), reads=["cst"], writes=["onesb"])
    for j in range(8):
        R.op("pool", lambda e, j=j: e.memset(stC[j], 0.0), writes=["stC%d" % j])
        R.op("pool", lambda e, j=j: e.memset(stCb[j], 0.0), writes=["stCb%d" % j])

    R.op("act", lambda e: e.activation(out=csT[:, :, 0], in_=cfm[:, 0:16], func=AF.Silu), reads=["cfm"], writes=["csT"])
    R.op("act", lambda e: e.activation(out=csT[:, :, 1], in_=cfm[:, 16:32], func=AF.Silu), reads=["cfm"], writes=["csT"])
    mark0 = A.off
    wm = [A.b(16 * 512).rearrange("p (c n) -> p c n", n=512) for _ in range(2)]
    wmod_v = wmod_d.rearrange("(c p) n -> p c n", p=128)
    PM = psum[7][:, 0:192].rearrange("p (a b) -> p a b", b=2)

    def mod_block(blk):
        buf = wm[blk % 2]
        key = "wm%d" % (blk % 2)
        R.op("pool", lambda e: e.dma_start(out=buf, in_=wmod_v[:, :, blk * 512:(blk + 1) * 512]), writes=[key], dma=True)
        for q in range(4):
            cc = blk * 4 + q
            for c in range(NCH):
                R.op("pe", lambda e, c=c, q=q, cc=cc: e.matmul(PM[:, cc, :], lhsT=buf[:, c, q * 128:(q + 1) * 128], rhs=csT[:, c, :],
                                                             start=(c == 0), stop=(c == NCH - 1)),
                     reads=[key, "csT"], writes=[pk(7)])
        R.op("dve", lambda e: e.tensor_tensor(out=modT[:, blk * 4:blk * 4 + 4, :], in0=PM[:, blk * 4:blk * 4 + 4, :],
                                              in1=bmod[:, blk * 4:blk * 4 + 4].unsqueeze(2).to_broadcast([128, 4, 2]), op=ALU.add),
             reads=[pk(7), "bmod"], writes=["modT"])

    for blk in range(24):
        mod_block(blk)
    R.op("dve", lambda e: e.scalar_tensor_tensor(out=gml, in0=modT[:, 16:32, 0], scalar=1.0, in1=g1, op0=ALU.add, op1=ALU.mult),
         reads=["modT", "g1"], writes=["gml"])
    R.op("dve", lambda e: e.scalar_tensor_tensor(out=gmc, in0=modT[:, 16:32, 1], scalar=1.0, in1=g1, op0=ALU.add, op1=ALU.mult),
         reads=["modT", "g1"], writes=["gmc"])
    if debug == "mod":
        dump(modT.rearrange("p a b -> p (a b)"), "modT", 0, 192)
        dump(gml, "gml", 192, 16)
        return finish()
    R.barrier()
    A.off = mark0

    win_v = win_d.rearrange("(c p) n -> p c n", p=128)
    SC = 128.0 ** -0.5
    WCOLS = 2064
    mark_mix = A.off
    Wt = A.b(16 * WCOLS).rearrange("p (c n) -> p c n", n=WCOLS)
    Wflat = Wt.rearrange("p c n -> p (c n)")
    mark_w_end = A.off
    W_regs = A
    bo = A.f(WCOLS)
    gkb = A.f(128)
    gqb = A.f(128)
    xt0 = A.f(D)
    xt = [xt0, xt0]
    xsb = A.b(D)
    hT = [A.b(D).rearrange("p (c t) -> p c t", t=128) for _ in range(2)]
    kTst = A.b(2 * NSLOT * 128).rearrange("p (g t) -> p g t", g=2)
    Vst = A.b(NSLOT * 256).rearrange("p (s v) -> p s v", v=256)
    sm = A.f(64)
    rp = [A.f(128) for _ in range(2)]
    kf = A.f(256)
    kn = A.f(256)
    rt = [A.f(128) for _ in range(4)]
    krot = A.b(256)
    NB_ = 2
    Kt = [A.b(512) for _ in range(NB_)]
    Vx = [A.b(4 * 258).rearrange("p (h v) -> p h v", v=258) for _ in range(NB_)]
    Gt = [A.f(16) for _ in range(NB_)]
    cq = [A.f(64) for _ in range(NB_)]
    expb = [A.f(8) for _ in range(NB_)]
    Kw = [A.b(128) for _ in range(2)]

    def load_w(segs):
        off = 0
        for (lo, hi) in segs:
            n = hi - lo
            R.op("pool", lambda e, off=off, lo=lo, hi=hi, n=n: e.dma_start(out=Wt[:, :, off:off + n], in_=win_v[:, :, lo:hi]), writes=["W"], dma=True)
            off += n

    load_w([(K0, K0 + 512), (MK0, MK0 + 1536), (MI0, MI0 + 16)])
    R.op("sp", lambda e: e.dma_start(out=bo[:, 0:512], in_=pbc(bin_d[:, K0:K0 + 512])), writes=["bo"], dma=True)
    R.op("sp", lambda e: e.dma_start(out=bo[:, 512:2048], in_=pbc(bin_d[:, MK0:MK0 + 1536])), writes=["bo"], dma=True)
    R.op("sp", lambda e: e.dma_start(out=bo[:, 2048:2064], in_=pbc(bin_d[:, MI0:MI0 + 16])), writes=["bo"], dma=True)
    R.op("sp", lambda e: e.dma_start(out=gkb, in_=pbc(gk_d)), writes=["gkb"], dma=True)
    R.op("sp", lambda e: e.dma_start(out=gqb, in_=pbc(gq_d)), writes=["gqb"], dma=True)
    R.op("dve", lambda e: e.tensor_scalar(out=bo[:, 512:1024], in0=bo[:, 512:1024], scalar1=SC, scalar2=None, op0=ALU.mult), reads=["bo"], writes=["bo"])
    R.op("dve", lambda e: e.tensor_scalar(out=gqb, in0=gqb, scalar1=SC, scalar2=None, op0=ALU.mult), reads=["gqb"], writes=["gqb"])
    R.op("dve", lambda e: e.tensor_scalar(out=bfm[:, 4:8], in0=bfm[:, 4:8], scalar1=SC, scalar2=None, op0=ALU.mult), reads=["bfm"], writes=["bfm"])
    for b_ in range(NB_):
        R.op("pool", lambda e, b_=b_: e.memset(Vx[b_][:, :, 256:257], 1.0), writes=["Vx%d" % b_])

    def rstd_op(dst, src, n_el, keys_r, key_w):
        R.op("act", lambda e: e.activation(out=dst, in_=src, func=AF.Ln, scale=1.0 / n_el, bias=epsb[:, 0:1]), reads=keys_r + ["epsb"], writes=[key_w])
        R.op("act", lambda e: e.activation(out=dst, in_=dst, func=AF.Exp, scale=-0.5), reads=[key_w], writes=[key_w])

    def make_hT(s):
        b2 = s % 2
        is_ctx = s < 2
        xk = "xt"
        R.op("sp", lambda e: e.dma_start(out=xt[b2], in_=xs_d[s * 128:(s + 1) * 128, :]), writes=[xk], dma=True)
        R.op("pool", lambda e: e.memset(sm[:, b2:b2 + 1], 0.0), writes=["ss%d" % b2])
        jk = hT[b2].rearrange("p c t -> p (c t)")
        R.op("act", lambda e: e.activation(out=jk, in_=xt[b2], func=AF.Square, accum_out=sm[:, b2:b2 + 1]), reads=[xk, "ss%d" % b2], writes=["hT%d" % b2, "ss%d" % b2])
        rstd_op(sm[:, 2 + b2:3 + b2], sm[:, b2:b2 + 1], float(D), ["ss%d" % b2], "rs%d" % b2)
        R.op("dve", lambda e: e.tensor_scalar(out=xsb, in0=xt[b2], scalar1=sm[:, 2 + b2:3 + b2], scalar2=None, op0=ALU.mult),
             reads=[xk, "rs%d" % b2], writes=["xsb"])
        for c in range(NCH):
            R.op("pe", lambda e, c=c: e.transpose(out=pT[:, c * 128:(c + 1) * 128], in_=xsb[:, c * 128:(c + 1) * 128], identity=identb),
                 reads=["xsb", "identb"], writes=["pT%d" % (c // 8)])
        gm = gmc if is_ctx else gml
        w = 1 if is_ctx else 0
        hk = "hT%d" % b2
        for c in range(NCH):
            R.op("act", lambda e, c=c: e.activation(out=hT[b2][:, c, :], in_=pT[:, c * 128:(c + 1) * 128], func=AF.Identity,
                                                   scale=gm[:, c:c + 1], bias=modT[:, c, w:w + 1]),
                 reads=["pT%d" % (c // 8), "gml", "gmc", "modT"], writes=[hk])
        return hT[b2], hk

    def proj_tok(h, hk, col_lo, n, bank, wkey="W", w=None):
        w = Wt if w is None else w
        for c in range(NCH):
            R.op("pe", lambda e, c=c: e.matmul(PS(bank, 0, n), lhsT=h[:, c, :], rhs=w[:, c, col_lo:col_lo + n], start=(c == 0), stop=(c == NCH - 1)),
                 reads=[hk, wkey], writes=[pk(bank)])

    def slot_common(s, db):
        h, hk = make_hT(s)
        b2 = s % 2
        R.op("sp", lambda e: e.dma_start(out=rp[b2], in_=rope_d[s * 128:(s + 1) * 128, :]), writes=["rp%d" % b2], dma=True)
        proj_tok(h, hk, 0, 512, 2)
        proj_tok(h, hk, 512, 512, 3)
        proj_tok(h, hk, 1024, 512, 4)
        proj_tok(h, hk, 1536, 512, 5)
        proj_tok(h, hk, 2048, 16, 6)
        R.op("dve", lambda e: e.tensor_tensor(out=kf, in0=PS(2, 0, 256), in1=bo[:, 0:256], op=ALU.add), reads=[pk(2), "bo"], writes=["kf"])
        R.op("dve", lambda e: e.tensor_tensor(out=Vst[:, s, :], in0=PS(2, 256, 256), in1=bo[:, 256:512], op=ALU.add), reads=[pk(2), "bo"], writes=["Vst"])
        R.op("dve", lambda e: e.scalar_tensor_tensor(out=Kt[db], in0=PS(3), scalar=SC, in1=bo[:, 512:1024], op0=ALU.mult, op1=ALU.add),
             reads=[pk(3), "bo"], writes=["Kt%d" % db])
        for half in range(2):
            R.op("dve", lambda e, half=half: e.tensor_tensor(out=Vx[db][:, 2 * half:2 * half + 2, 0:256],
                                                             in0=PS(4 + half).rearrange("p (h v) -> p h v", v=256),
                                                             in1=bo[:, 1024 + 512 * half:1536 + 512 * half].rearrange("p (h v) -> p h v", v=256), op=ALU.add),
                 reads=[pk(4 + half), "bo"], writes=["Vx%d" % db])
        R.op("dve", lambda e: e.tensor_tensor(out=Gt[db], in0=PS(6, 0, 16), in1=bo[:, 2048:2064], op=ALU.add), reads=[pk(6), "bo"], writes=["G%d" % db])
        R.op("pool", lambda e: e.memset(sm[:, 4:6], 0.0), writes=["kss"])
        for g in range(2):
            R.op("act", lambda e, g=g: e.activation(out=kn[:, g * 128:(g + 1) * 128], in_=kf[:, g * 128:(g + 1) * 128], func=AF.Square, accum_out=sm[:, 4 + g:5 + g]),
                 reads=["kf", "kss"], writes=["kn", "kss"])
        rstd_op(sm[:, 8:10], sm[:, 4:6], 128.0, ["kss"], "krs")
        for g in range(2):
            R.op("dve", lambda e, g=g: e.scalar_tensor_tensor(out=kn[:, g * 128:(g + 1) * 128], in0=kf[:, g * 128:(g + 1) * 128], scalar=sm[:, 8 + g:9 + g],
                                                              in1=gkb, op0=ALU.mult, op1=ALU.mult), reads=["kf", "krs", "gkb"], writes=["kn"])
        rope(kn, "kn", krot, "krot", 2, rp[b2], "rp%d" % b2)
        for g in range(2):
            R.op("pe", lambda e, g=g: e.transpose(out=pT[:, g * 128:(g + 1) * 128], in_=krot[:, g * 128:(g + 1) * 128], identity=identb),
                 reads=["krot", "identb"], writes=["pT0"])
        R.op("act", lambda e: e.tensor_copy(out=kTst[:, :, s * 128:(s + 1) * 128], in_=pT[:, 0:256].rearrange("p (g t) -> p g t", g=2))
             if False else e.activation(out=kTst[:, :, s * 128:(s + 1) * 128], in_=pT[:, 0:256].rearrange("p (g t) -> p g t", g=2), func=AF.Copy),
             reads=["pT0"], writes=["kTst"])
        chunk_gates(s, db)

    def rope(src, skey, dst, dkey, nh, rpt, rkey):
        v = src.rearrange("p (h i two) -> p h i two", h=nh, two=2)
        o = dst.rearrange("p (h i two) -> p h i two", h=nh, two=2)
        cosb = rpt[:, 0:64].unsqueeze(1).to_broadcast([128, nh, 64])
        sinb = rpt[:, 64:128].unsqueeze(1).to_broadcast([128, nh, 64])
        n = nh * 64
        t = [rt[i][:, 0:n].rearrange("p (h i) -> p h i", h=nh) if n <= 128 else None for i in range(4)]
        if n > 128:
            t = [rtq[i].rearrange("p (h i) -> p h i", h=nh) for i in range(4)]
        x1, x2 = v[:, :, :, 0], v[:, :, :, 1]
        tk = ["rt0", "rt1", "rt2", "rt3"]
        R.op("pool", lambda e: e.tensor_tensor(out=t[0], in0=x1, in1=cosb, op=ALU.mult), reads=[skey, rkey], writes=[tk[0]])
        R.op("pool", lambda e: e.tensor_tensor(out=t[1], in0=x2, in1=sinb, op=ALU.mult), reads=[skey, rkey], writes=[tk[1]])
        R.op("pool", lambda e: e.tensor_tensor(out=t[2], in0=x1, in1=sinb, op=ALU.mult), reads=[skey, rkey], writes=[tk[2]])
        R.op("pool", lambda e: e.tensor_tensor(out=t[3], in0=x2, in1=cosb, op=ALU.mult), reads=[skey, rkey], writes=[tk[3]])
        R.op("dve", lambda e: e.tensor_tensor(out=o[:, :, :, 0], in0=t[0], in1=t[1], op=ALU.subtract), reads=[tk[0], tk[1]], writes=[dkey])
        R.op("dve", lambda e: e.tensor_tensor(out=o[:, :, :, 1], in0=t[2], in1=t[3], op=ALU.add), reads=[tk[2], tk[3]], writes=[dkey])

    def chunk_gates(s, db):
        q_ = cq[db]
        e1, Lf, lgf, ie, imb, gg, wst, dec = [q_[:, 8 * i:8 * i + 8] for i in range(8)]
        ck = "cq%d" % db
        R.op("act", lambda e: e.activation(out=e1, in_=Gt[db][:, 8:16], func=AF.Exp, scale=-1.0), reads=["G%d" % db], writes=[ck + "a"])
        R.op("act", lambda e: e.activation(out=Lf, in_=e1, func=AF.Ln, bias=epsb[:, 1:2]), reads=[ck + "a", "epsb"], writes=[ck + "b"])
        R.op("dve", lambda e: e.tensor_tensor(out=lgf, in0=Lf, in1=gmask[:, s, 8:16], op=ALU.mult), reads=[ck + "b", "gmask"], writes=[ck + "lgf"])
        R.op("dve", lambda e: e.tensor_tensor(out=ie, in0=Gt[db][:, 0:8], in1=gmask[:, s, 0:8], op=ALU.add), reads=["G%d" % db, "gmask"], writes=[ck + "ie"])
        R.op("pe", lambda e: e.matmul(PS(6, 16, 4), lhsT=tri_f, rhs=lgf[:, 0:4], start=True, stop=True), reads=["cst", ck + "lgf"], writes=[pk(6)])
        R.op("pe", lambda e: e.matmul(PS(6, 20, 4), lhsT=tri_b, rhs=lgf[:, 4:8], start=True, stop=True), reads=["cst", ck + "lgf"], writes=[pk(6)])
        R.op("pe", lambda e: e.matmul(PS(6, 24, 8), lhsT=ones, rhs=lgf, start=True, stop=True), reads=["cst", ck + "lgf"], writes=[pk(6)])
        R.op("dve", lambda e: e.tensor_tensor(out=imb, in0=ie, in1=PS(6, 16, 8), op=ALU.subtract), reads=[ck + "ie", pk(6)], writes=[ck + "imb"])
        R.op("dve", lambda e: e.tensor_tensor(out=gg, in0=imb, in1=PS(6, 24, 8), op=ALU.add), reads=[ck + "imb", pk(6)], writes=[ck + "gg"])
        R.op("act", lambda e: e.activation(out=wst, in_=gg, func=AF.Exp), reads=[ck + "gg"], writes=[ck + "wst"])
        R.op("act", lambda e: e.activation(out=dec, in_=PS(6, 24, 8), func=AF.Exp), reads=[pk(6)], writes=[ck + "dec"])
        R.op("act", lambda e: e.activation(out=expb[db], in_=PS(6, 16, 8), func=AF.Exp), reads=[pk(6)], writes=[ck + "expb"])

    def state_step(db, j, refresh_bf=False):
        h = j % 4
        q_ = cq[db]
        wst, dec = q_[:, 48:56], q_[:, 56:64]
        ck = "cq%d" % db
        kb = j % 2
        R.op("dve", lambda e: e.tensor_scalar(out=Kw[kb], in0=Kt[db][:, h * 128:(h + 1) * 128], scalar1=wst[:, j:j + 1], scalar2=None, op0=ALU.mult),
             reads=["Kt%d" % db, ck + "wst"], writes=["Kw%d" % kb])
        R.op("pe", lambda e: e.matmul(PS(7, 0, 257), lhsT=Kw[kb], rhs=Vx[db][:, h, 0:257], start=True, stop=True),
             reads=["Kw%d" % kb, "Vx%d" % db], writes=[pk(7)])
        R.op("dve", lambda e: e.scalar_tensor_tensor(out=stC[j], in0=stC[j], scalar=dec[:, j:j + 1], in1=PS(7, 0, 257), op0=ALU.mult, op1=ALU.add),
             reads=["stC%d" % j, ck + "dec", pk(7)], writes=["stC%d" % j])
        if refresh_bf:
            R.op("act", lambda e: e.activation(out=stCb[j], in_=stC[j], func=AF.Copy), reads=["stC%d" % j], writes=["stCb%d" % j])

    rtq = None
    for s in range(n_oth):
        db = s % 2
        slot_common(s, db)
        if s == 0:
            for j in range(4):
                state_step(db, j)
        elif s == 1:
            for j in range(4):
                state_step(1, j)
            for j in range(4, 8):
                state_step(1, j)
            for j in range(4, 8):
                state_step(0, j)
        else:
            for j in range(8):
                state_step(db, j)
    if debug == "oth":
        s = n_oth - 1
        dump(hT[s % 2].rearrange("p c t -> p (c t)")[:, 0:0], "x", 0, 0) if False else None
        dump_bf(hT[s % 2].rearrange("p c t -> p (c t)"), "hT%d" % (s % 2), 0, 2048)
        dump_bf(kTst[:, :, s * 128:(s + 1) * 128], "kTst", 2048, 256) if False else None
        dump_bf(Vst[:, s, :], "Vst", 2304, 256)
        dump_bf(Kt[s % 2], "Kt%d" % (s % 2), 2560, 512)
        dump(Gt[s % 2], "G%d" % (s % 2), 3072, 16)
        dump(cq[s % 2], "cq%dwst" % (s % 2), 3088, 64)
        for j in range(8):
            dump(stC[j], "stC%d" % j, 3200 + 257 * j, 257)
        dump_bf(krot, "krot", 5300, 256)
        return finish()


    hacc = A.b(NOWN * 1024).rearrange("p (o h v) -> p o h v", o=NOWN, h=4)
    mark_own = A.off
    Wq = A.b(16 * 512).rearrange("p (c n) -> p c n", n=512)
    R.op("pool", lambda e: e.dma_start(out=Wq, in_=win_v[:, :, MQ0:MQ0 + 512]), writes=["Wq"], dma=True)
    qmT = A.b(512).rearrange("p (h t) -> p h t", h=4)
    kmT = A.b(512).rearrange("p (h t) -> p h t", h=4)
    lb = A.f(128)
    DTt = A.f(128)
    STt = A.b(128)
    tmpn = A.f(257)
    tot = A.f(257)
    ddr = A.f(2)
    for j in range(8):
        R.op("act", lambda e, j=j: e.activation(out=stCb[j], in_=stC[j], func=AF.Copy), reads=["stC%d" % j], writes=["stCb%d" % j])

    def full_step(s, db, j, o, first):
        h = j % 4
        dirn = j // 4
        TRI = tri_f if dirn == 0 else tri_b
        NM = nm_f if dirn == 0 else nm_b
        q_ = cq[db]
        lgf, imb = q_[:, 16:24], q_[:, 32:40]
        ck = "cq%d" % db
        R.op("dve", lambda e: e.tensor_scalar(out=lb, in0=ones, scalar1=lgf[:, j:j + 1], scalar2=None, op0=ALU.mult), reads=["cst", ck + "lgf"], writes=["lb"])
        R.op("pe", lambda e: e.matmul(PS(4, 0, 128), lhsT=lb, rhs=TRI, start=True, stop=False), reads=["lb", "cst"], writes=[pk(4)])
        R.op("pe", lambda e: e.matmul(PS(4, 0, 128), lhsT=ident, rhs=NM, start=False, stop=True), reads=["cst"], writes=[pk(4)])
        R.op("act", lambda e: e.activation(out=DTt, in_=PS(4, 0, 128), func=AF.Exp, bias=imb[:, j:j + 1]), reads=[pk(4), ck + "imb"], writes=["DT"])
        R.op("pe", lambda e: e.matmul(PS(5, 0, 128), lhsT=kmT[:, h, :], rhs=qmT[:, h, :], start=True, stop=True), reads=["kmT", "qmT"], writes=[pk(5)])
        R.op("dve", lambda e: e.tensor_tensor(out=STt, in0=PS(5, 0, 128), in1=DTt, op=ALU.mult), reads=[pk(5), "DT"], writes=["ST"])
        R.op("pe", lambda e: e.matmul(PS(2, 0, 257), lhsT=STt, rhs=Vx[db][:, h, 0:257], start=True, stop=True), reads=["ST", "Vx%d" % db], writes=[pk(2)])
        R.op("pe", lambda e: e.matmul(PS(3, 0, 257), lhsT=qmT[:, h, :], rhs=stCb[j], start=True, stop=True), reads=["qmT", "stCb%d" % j], writes=[pk(3)])
        R.op("act", lambda e: e.activation(out=tmpn, in_=PS(2, 0, 257), func=AF.Copy), reads=[pk(2)], writes=["tmpn"])
        R.op("dve", lambda e: e.scalar_tensor_tensor(out=tot, in0=PS(3, 0, 257), scalar=expb[db][:, j:j + 1], in1=tmpn, op0=ALU.mult, op1=ALU.add),
             reads=[pk(3), ck + "expb", "tmpn"], writes=["tot"])
        R.op("dve", lambda e: e.scalar_tensor_tensor(out=ddr[:, 0:1], in0=tot[:, 256:257], scalar=-1.0, in1=tot[:, 256:257], op0=ALU.mult, op1=ALU.max), reads=["tot"], writes=["dd"])
        R.op("dve", lambda e: e.tensor_scalar(out=ddr[:, 0:1], in0=ddr[:, 0:1], scalar1=1.0, scalar2=None, op0=ALU.max), reads=["dd"], writes=["dd"])
        R.op("dve", lambda e: e.reciprocal(out=ddr[:, 1:2], in_=ddr[:, 0:1]), reads=["dd"], writes=["rr"])
        hk_ = "hacc%d" % o
        if first:
            R.op("dve", lambda e: e.tensor_scalar(out=hacc[:, o, h, :], in0=tot[:, 0:256], scalar1=ddr[:, 1:2], scalar2=None, op0=ALU.mult), reads=["tot", "rr"], writes=[hk_])
        else:
            R.op("dve", lambda e: e.scalar_tensor_tensor(out=hacc[:, o, h, :], in0=tot[:, 0:256], scalar=ddr[:, 1:2], in1=hacc[:, o, h, :], op0=ALU.mult, op1=ALU.add),
                 reads=["tot", "rr", hk_], writes=[hk_])
        state_step(db, j, refresh_bf=True)

    def own_visit(o, dirn):
        s = NOTH + o
        db = s % 2
        slot_common(s, db)
        h_, hk = hT[s % 2], "hT%d" % (s % 2)
        for hh in range(4):
            for c in range(NCH):
                R.op("pe", lambda e, c=c, hh=hh: e.matmul(PS(2, hh * 128, 128), lhsT=Wq[:, c, hh * 128:(hh + 1) * 128], rhs=h_[:, c, :], start=(c == 0), stop=(c == NCH - 1)),
                     reads=["Wq", hk], writes=[pk(2)])
        for hh in range(4):
            R.op("act", lambda e, hh=hh: e.activation(out=qmT[:, hh, :], in_=PS(2, hh * 128, 128), func=AF.Identity, bias=bfm[:, hh:hh + 1]), reads=[pk(2), "bfm"], writes=["qmT"])
        for hh in range(4):
            for c in range(NCH):
                R.op("pe", lambda e, c=c, hh=hh: e.matmul(PS(3, hh * 128, 128), lhsT=Wt[:, c, 512 + hh * 128:512 + (hh + 1) * 128], rhs=h_[:, c, :], start=(c == 0), stop=(c == NCH - 1)),
                     reads=["W", hk], writes=[pk(3)])
        for hh in range(4):
            R.op("act", lambda e, hh=hh: e.activation(out=kmT[:, hh, :], in_=PS(3, hh * 128, 128), func=AF.Identity, scale=SC, bias=bfm[:, 4 + hh:5 + hh]), reads=[pk(3), "bfm"], writes=["kmT"])
        for hh in range(4):
            full_step(s, db, dirn * 4 + hh, o, first=(dirn == 0))

    n_own = NOWN if debug != "own" else 2
    for o in range(n_own):
        own_visit(o, 0)
    for o in range(n_own - 1, -1, -1):
        own_visit(o, 1)
    if debug == "own":
        dump_bf(hacc[:, 0, :, :].rearrange("p h v -> p (h v)"), "hacc0", 0, 1024)
        dump_bf(hacc[:, 1, :, :].rearrange("p h v -> p (h v)"), "hacc1", 1024, 1024)
        return finish()


    R.barrier()
    A.off = mark_own
    Wc = Wflat[:, 0:16 * 1024].rearrange("p (c n) -> p c n", n=1024)
    yT = Wflat[:, 16 * 1024:32 * 1024].rearrange("p (k t) -> p k t", t=1024)
    qf = A.f(1024)
    rtq_all = A.f(2048)
    rtq = [rtq_all[:, i * 512:(i + 1) * 512] for i in range(4)]
    qrot = A.b(1024)
    qTc = A.b(1024).rearrange("p (h t) -> p h t", h=8)
    PTt = [A.b(512) for _ in range(2)]
    dsb = A.f(512)
    qss = A.f(16)
    gmb = A.f(1024)

    def load_wc(lo, wd=win_v):
        R.op("pool", lambda e: e.dma_start(out=Wc, in_=wd[:, :, lo:lo + 1024]), writes=["W"], dma=True)

    load_wc(Q0)
    R.op("sp", lambda e: e.dma_start(out=bo[:, 0:1024], in_=pbc(bin_d[:, Q0:Q0 + 1024])), writes=["bo"], dma=True)
    R.op("sp", lambda e: e.dma_start(out=bo[:, 1024:2048], in_=pbc(bin_d[:, MO0:MO0 + 1024])), writes=["bo"], dma=True)
    R.op("sp", lambda e: e.dma_start(out=gmb, in_=pbc(gm_d)), writes=["gmb"], dma=True)
    qf3 = qf.rearrange("p (h d) -> p h d", h=8)
    for o in range(NOWN):
        s = NOTH + o
        b2 = s % 2
        h_, hk = make_hT(s)
        R.op("sp", lambda e, s=s, b2=b2: e.dma_start(out=rp[b2], in_=rope_d[s * 128:(s + 1) * 128, :]), writes=["rp%d" % b2], dma=True)
        proj_tok(h_, hk, 0, 512, 2, w=Wc)
        proj_tok(h_, hk, 512, 512, 3, w=Wc)
        for hf_ in range(2):
            R.op("dve", lambda e, hf_=hf_: e.tensor_tensor(out=qf[:, hf_ * 512:(hf_ + 1) * 512], in0=PS(2 + hf_), in1=bo[:, hf_ * 512:(hf_ + 1) * 512], op=ALU.add),
                 reads=[pk(2 + hf_), "bo"], writes=["qf"])
        R.op("pool", lambda e: e.memset(qss[:, 0:8], 0.0), writes=["qss"])
        for hh in range(8):
            R.op("act", lambda e, hh=hh: e.activation(out=rtq[0][:, 0:128], in_=qf[:, hh * 128:(hh + 1) * 128], func=AF.Square, accum_out=qss[:, hh:hh + 1]),
                 reads=["qf", "qss"], writes=["rt0", "qss"])
        rstd_op(qss[:, 8:16], qss[:, 0:8], 128.0, ["qss"], "qrs")
        R.op("dve", lambda e: e.tensor_tensor(out=qf3, in0=qf3, in1=qss[:, 8:16].unsqueeze(2).to_broadcast([128, 8, 128]), op=ALU.mult), reads=["qf", "qrs"], writes=["qf"])
        R.op("dve", lambda e: e.tensor_tensor(out=qf3, in0=qf3, in1=gqb.unsqueeze(1).to_broadcast([128, 8, 128]), op=ALU.mult), reads=["qf", "gqb"], writes=["qf"])
        rope(qf, "qf", qrot, "qrot", 8, rp[b2], "rp%d" % b2)
        for hh in range(8):
            R.op("pe", lambda e, hh=hh: e.transpose(out=pT[:, hh * 128:(hh + 1) * 128], in_=qrot[:, hh * 128:(hh + 1) * 128], identity=identb),
                 reads=["qrot", "identb"], writes=["pT0"])
        R.op("act", lambda e: e.activation(out=qTc, in_=pT[:, 0:1024].rearrange("p (h t) -> p h t", h=8), func=AF.Copy), reads=["pT0"], writes=["qTc"])
        for g in range(2):
            for sk in range(NSLOT):
                sb = 2 + (sk % 2)
                pb = sk % 2
                R.op("pe", lambda e, g=g, sk=sk, sb=sb: e.matmul(PS(sb), lhsT=kTst[:, g, sk * 128:(sk + 1) * 128], rhs=qTc[:, 4 * g:4 * g + 4, :], start=True, stop=True),
                     reads=["kTst", "qTc"], writes=[pk(sb)])
                R.op("act", lambda e, sb=sb, pb=pb: e.activation(out=PTt[pb], in_=PS(sb), func=AF.Exp), reads=[pk(sb)], writes=["PT%d" % pb])
                R.op("pe", lambda e, g=g, sk=sk, pb=pb: e.matmul(PS(4 + g), lhsT=Vst[:, sk, g * 128:(g + 1) * 128], rhs=PTt[pb], start=(sk == 0), stop=(sk == NSLOT - 1)),
                     reads=["Vst", "PT%d" % pb], writes=[pk(4 + g)])
                R.op("pe", lambda e, g=g, sk=sk, pb=pb: e.matmul(PS(6 + g), lhsT=onesb, rhs=PTt[pb], start=(sk == 0), stop=(sk == NSLOT - 1)),
                     reads=["onesb", "PT%d" % pb], writes=[pk(6 + g)])
            R.op("act", lambda e, g=g: e.activation(out=dsb, in_=PS(6 + g), func=AF.Copy), reads=[pk(6 + g)], writes=["dsb"])
            R.op("dve", lambda e: e.reciprocal(out=dsb, in_=dsb), reads=["dsb"], writes=["dsb"])
            R.op("dve", lambda e, g=g, o=o: e.tensor_tensor(out=yT[:, 4 * g:4 * g + 4, o * 128:(o + 1) * 128], in0=PS(4 + g).rearrange("p (h t) -> p h t", h=4),
                                                          in1=dsb.rearrange("p (h t) -> p h t", h=4), op=ALU.mult),
                 reads=[pk(4 + g), "dsb"], writes=["yT"])
    if debug == "att":
        for k in range(8):
            dump_bf(yT[:, k, 0:256], "yT", k * 256, 256)
        return finish()

    R.barrier()
    load_wc(MO0)
    hn = rtq_all[:, 0:1024]
    ym = qrot
    hn3 = hn.rearrange("p (h v) -> p h v", h=4)
    for o in range(NOWN):
        s = NOTH + o
        h_, hk = make_hT(s)
        proj_tok(h_, hk, 0, 512, 2, w=Wc)
        proj_tok(h_, hk, 512, 512, 3, w=Wc)
        for hf_ in range(2):
            R.op("dve", lambda e, hf_=hf_: e.tensor_tensor(out=qf[:, hf_ * 512:(hf_ + 1) * 512], in0=PS(2 + hf_), in1=bo[:, 1024 + hf_ * 512:1024 + (hf_ + 1) * 512], op=ALU.add),
                 reads=[pk(2 + hf_), "bo"], writes=["qf"])
        R.op("act", lambda e: e.activation(out=qf, in_=qf, func=AF.Sigmoid), reads=["qf"], writes=["qf"])
        R.op("pool", lambda e: e.memset(qss[:, 0:4], 0.0), writes=["qss"])
        for hh in range(4):
            R.op("act", lambda e, hh=hh, o=o: e.activation(out=hn[:, hh * 256:(hh + 1) * 256], in_=hacc[:, o, hh, :], func=AF.Square, accum_out=qss[:, hh:hh + 1]),
                 reads=["hacc%d" % o, "qss"], writes=["hn", "qss"])
        rstd_op(qss[:, 8:12], qss[:, 0:4], 256.0, ["qss"], "qrs")
        R.op("dve", lambda e, o=o: e.tensor_tensor(out=hn3, in0=hacc[:, o, :, :], in1=qss[:, 8:12].unsqueeze(2).to_broadcast([128, 4, 256]), op=ALU.mult),
             reads=["hacc%d" % o, "qrs"], writes=["hn"])
        R.op("pool", lambda e: e.tensor_tensor(out=hn, in0=hn, in1=gmb, op=ALU.mult), reads=["hn", "gmb"], writes=["hn"])
        R.op("dve", lambda e: e.tensor_tensor(out=ym, in0=hn, in1=qf, op=ALU.mult), reads=["hn", "qf"], writes=["qrot"])
        for k in range(8):
            R.op("pe", lambda e, k=k: e.transpose(out=pT[:, k * 128:(k + 1) * 128], in_=ym[:, k * 128:(k + 1) * 128], identity=identb),
                 reads=["qrot", "identb"], writes=["pT0"])
        R.op("act", lambda e, o=o: e.activation(out=yT[:, 8:16, o * 128:(o + 1) * 128], in_=pT[:, 0:1024].rearrange("p (k t) -> p k t", k=8), func=AF.Copy),
             reads=["pT0"], writes=["yT"])
    if debug == "ym":
        for k in range(8):
            dump_bf(yT[:, 8 + k, 0:256], "yT", k * 256, 256)
        return finish()

    R.barrier()
    A.off = mark_w_end
    x1 = A.f(NOWN * D).rearrange("p (o d) -> p o d", o=NOWN)
    bcA = A.f(1024)
    tmpd = [A.f(512) for _ in range(2)]
    lbm = A.f(128)
    wout_v = wout_d.rearrange("(c p) n -> p c n", p=128)

    def make_bc(dst, key, chunk_lo, nchunks, col, src=None, skey="modT"):
        for c in range(nchunks):
            if src is None:
                vec = modT[:, chunk_lo + c, col:col + 1]
            else:
                vec = src[:, chunk_lo + c:chunk_lo + c + 1]
            R.op("dve", lambda e, vec=vec: e.tensor_scalar(out=lbm, in0=ones, scalar1=vec, scalar2=None, op0=ALU.mult), reads=["cst", skey], writes=["lbm"])
            R.op("pe", lambda e, c=c: e.matmul(PS(2, (c % 4) * 128, 128), lhsT=lbm, rhs=ident, start=True, stop=True), reads=["lbm", "cst"], writes=[pk(2)])
            if c % 4 == 3:
                R.op("act", lambda e, c=c: e.activation(out=dst[:, (c - 3) * 128:(c + 1) * 128], in_=PS(2), func=AF.Copy), reads=[pk(2)], writes=[key])

    for o in range(NOWN):
        s = NOTH + o
        R.op("sp", lambda e, s=s, o=o: e.dma_start(out=x1[:, o, :], in_=xs_d[s * 128:(s + 1) * 128, :]), writes=["x1_%d" % o], dma=True)
    for half in range(2):
        load_wc(half * 1024, wd=wout_v)
        make_bc(bcA, "bcA", 32 + half * 8, 8, 0)
        for o in range(NOWN):
            for dh in range(2):
                for k in range(NCH):
                    R.op("pe", lambda e, o=o, dh=dh, k=k: e.matmul(PS(4 + dh), lhsT=yT[:, k, o * 128:(o + 1) * 128], rhs=Wc[:, k, dh * 512:(dh + 1) * 512],
                                                                  start=(k == 0), stop=(k == NCH - 1)), reads=["yT", "W"], writes=[pk(4 + dh)])
                cols = slice(half * 1024 + dh * 512, half * 1024 + (dh + 1) * 512)
                R.op("dve", lambda e, dh=dh: e.tensor_tensor(out=tmpd[dh], in0=PS(4 + dh), in1=bcA[:, dh * 512:(dh + 1) * 512], op=ALU.mult),
                     reads=[pk(4 + dh), "bcA"], writes=["tmpd%d" % dh])
                R.op("pool", lambda e, o=o, dh=dh, cols=cols: e.tensor_tensor(out=x1[:, o, cols], in0=x1[:, o, cols], in1=tmpd[dh], op=ALU.add),
                     reads=["x1_%d" % o, "tmpd%d" % dh], writes=["x1_%d" % o])
    if debug == "x1":
        for q in range(4):
            dump(x1[:, 0, q * 512:(q + 1) * 512], "x1_0", q * 512, 512)
        for q in range(4):
            dump(x1[:, 7, q * 512:(q + 1) * 512], "x1_7", 2048 + q * 512, 512)
        return finish()

    R.barrier()
    h2T = Wflat[:, 0:16 * 1024].rearrange("p (c t) -> p c t", t=1024)
    wfree = Wflat[:, 16 * 1024:16 * 1024 + 16384].bitcast(F32)
    gm2b = wfree[:, 0:2048]
    sh2b = wfree[:, 2048:4096]
    h2f = wfree[:, 4096:6144]
    h2Tr = wfree[:, 6144:8192].rearrange("p (c t) -> p c t", t=128)
    A.off = mark_w_end + NOWN * D
    sm2 = A.f(8)
    g2fm = A.f(16)
    gm2fm = A.f(16)
    wr = A.f(16 * NEXP).rearrange("p (c e) -> p c e", e=NEXP)
    brb = A.f(NEXP)
    wt = A.f(NOWN * NEXP).rearrange("p (o e) -> p o e", e=NEXP)
    rsm = A.f(64)
    ex = A.f(NEXP)
    msk = A.f(NEXP)
    R.op("sp", lambda e: e.dma_start(out=g2fm, in_=g2fm_d), writes=["g2fm"], dma=True)
    R.op("sp", lambda e: e.dma_start(out=wr, in_=wr_d.rearrange("(c p) e -> p c e", p=128)), writes=["wr"], dma=True)
    R.op("sp", lambda e: e.dma_start(out=brb, in_=pbc(br_d)), writes=["brb"], dma=True)
    R.op("dve", lambda e: e.scalar_tensor_tensor(out=gm2fm, in0=modT[:, 64:80, 0], scalar=1.0, in1=g2fm, op0=ALU.add, op1=ALU.mult), reads=["modT", "g2fm"], writes=["gm2fm"])
    make_bc(gm2b, "gm2b", 0, 16, 0, src=gm2fm, skey="gm2fm")
    make_bc(sh2b, "sh2b", 48, 16, 0)
    lg = rsm[:, 0:32]
    top8 = rsm[:, 32:40]
    for o in range(NOWN):
        xk = "x1_%d" % o
        R.op("pool", lambda e: e.memset(sm2[:, 0:1], 0.0), writes=["ss0"])
        R.op("act", lambda e, o=o: e.activation(out=h2f, in_=x1[:, o, :], func=AF.Square, accum_out=sm2[:, 0:1]), reads=[xk, "ss0"], writes=["h2f", "ss0"])
        rstd_op(sm2[:, 2:3], sm2[:, 0:1], float(D), ["ss0"], "rs0")
        R.op("dve", lambda e, o=o: e.scalar_tensor_tensor(out=h2f, in0=x1[:, o, :], scalar=sm2[:, 2:3], in1=gm2b, op0=ALU.mult, op1=ALU.mult),
             reads=[xk, "rs0", "gm2b"], writes=["h2f"])
        R.op("pool", lambda e: e.tensor_tensor(out=h2f, in0=h2f, in1=sh2b, op=ALU.add), reads=["h2f", "sh2b"], writes=["h2f"])
        for c in range(NCH):
            bk = 4 + (c // 4)
            R.op("pe", lambda e, c=c, bk=bk: e.transpose(out=PS(bk, (c % 4) * 128, 128), in_=h2f[:, c * 128:(c + 1) * 128], identity=ident),
                 reads=["h2f", "cst"], writes=[pk(bk)])
        for q in range(4):
            R.op("act", lambda e, q=q: e.activation(out=h2Tr[:, 4 * q:4 * q + 4, :], in_=PS(4 + q).rearrange("p (c t) -> p c t", t=128), func=AF.Copy),
                 reads=[pk(4 + q)], writes=["h2Tr"])
        R.op("dve", lambda e, o=o: e.tensor_copy(out=h2T[:, :, o * 128:(o + 1) * 128], in_=h2Tr), reads=["h2Tr"], writes=["h2T"])
        for c in range(NCH):
            R.op("pe", lambda e, c=c: e.matmul(PS(3, 0, NEXP), lhsT=h2Tr[:, c, :], rhs=wr[:, c, :], start=(c == 0), stop=(c == NCH - 1)), reads=["h2Tr", "wr"], writes=[pk(3)])
        R.op("dve", lambda e: e.tensor_tensor(out=lg, in0=PS(3, 0, NEXP), in1=brb, op=ALU.add), reads=[pk(3), "brb"], writes=["lg"])
        R.op("dve", lambda e: e.max(out=top8, in_=lg), reads=["lg"], writes=["top8"])
        R.op("dve", lambda e: e.tensor_scalar(out=msk, in0=lg, scalar1=top8[:, 3:4], scalar2=None, op0=ALU.is_ge), reads=["lg", "top8"], writes=["msk"])
        R.op("dve", lambda e: e.tensor_scalar(out=rsm[:, 40:41], in0=top8[:, 0:1], scalar1=-1.0, scalar2=None, op0=ALU.mult), reads=["top8"], writes=["nmx"])
        R.op("act", lambda e: e.activation(out=ex, in_=lg, func=AF.Exp, bias=rsm[:, 40:41]), reads=["lg", "nmx"], writes=["ex"])
        R.op("dve", lambda e: e.tensor_tensor(out=ex, in0=ex, in1=msk, op=ALU.mult), reads=["ex", "msk"], writes=["ex"])
        R.op("dve", lambda e: e.reduce_sum(out=rsm[:, 41:42], in_=ex, axis=AX.X), reads=["ex"], writes=["esum"])
        R.op("dve", lambda e: e.reciprocal(out=rsm[:, 42:43], in_=rsm[:, 41:42]), reads=["esum"], writes=["ersum"])
        R.op("dve", lambda e, o=o: e.tensor_scalar(out=wt[:, o, :], in0=ex, scalar1=rsm[:, 42:43], scalar2=None, op0=ALU.mult), reads=["ex", "ersum"], writes=["wt"])
    if debug == "rt":
        dump(wt.rearrange("p o e -> p (o e)"), "wt", 0, 256)
        dump_bf(h2T[:, 0, 0:256], "h2T", 256, 256)
        return finish()

    R.barrier()
    gt2b = wfree[:, 0:2048]
    ring = [wfree[:, 2048 * (1 + i):2048 * (2 + i)].bitcast(BF16).rearrange("p (c n) -> p c n", n=256) for i in range(3)]
    ring.append(A.b(16 * 256).rearrange("p (c n) -> p c n", n=256))
    actT = A.b(16 * 1024).rearrange("p (f t) -> p f t", t=1024)
    b1e = [A.f(32) for _ in range(2)]
    gtt = [A.f(512) for _ in range(2)]
    sgt = [A.f(512), wr.rearrange("p c e -> p (c e)")]
    utt = [A.f(512), A.f(512)]
    t10 = A.f(256)
    t1 = [t10, t10]
    make_bc(gt2b, "gt2b", 80, 16, 0)
    b2g = actT.rearrange("p f t -> p (f t)")[:, 0:4096].bitcast(F32)
    wtT = actT.rearrange("p f t -> p (f t)")[:, 4096:4096 + 2048].bitcast(F32)
    R.op("sp", lambda e: e.dma_start(out=b2g[0:NEXP, :], in_=b2_d), writes=["b2g"], dma=True)
    for o in range(NOWN):
        R.op("pe", lambda e, o=o: e.transpose(out=PS(2, o * 64, 128)[0:NEXP, :] if False else PS(2 + o // 4, (o % 4) * 128, 128)[0:NEXP, :], in_=wt[:, o, :], identity=ident),
             reads=["wt", "cst"], writes=[pk(2 + o // 4)])
    for q in range(2):
        R.op("act", lambda e, q=q: e.activation(out=wtT[0:NEXP, q * 512:(q + 1) * 512], in_=PS(2 + q)[0:NEXP, :], func=AF.Copy), reads=[pk(2 + q)], writes=["wtT"])
    for o in range(NOWN):
        for dq in range(4):
            bk = 4 + (dq % 2)
            R.op("pe", lambda e, o=o, dq=dq, bk=bk: e.matmul(PS(bk), lhsT=wtT[0:NEXP, o * 128:(o + 1) * 128], rhs=b2g[0:NEXP, dq * 512:(dq + 1) * 512], start=True, stop=True),
                 reads=["wtT", "b2g"], writes=[pk(bk)])
            R.op("dve", lambda e, dq=dq, bk=bk: e.tensor_tensor(out=gtt[dq % 2], in0=PS(bk), in1=gt2b[:, dq * 512:(dq + 1) * 512], op=ALU.mult),
                 reads=[pk(bk), "gt2b"], writes=["gtt%d" % (dq % 2)])
            R.op("pool", lambda e, o=o, dq=dq: e.tensor_tensor(out=x1[:, o, dq * 512:(dq + 1) * 512], in0=x1[:, o, dq * 512:(dq + 1) * 512], in1=gtt[dq % 2], op=ALU.add),
                 reads=["x1_%d" % o, "gtt%d" % (dq % 2)], writes=["x1_%d" % o])
    R.barrier()
    if big:
        w1_v = [w1_d[e_].rearrange("(c p) n -> p c n", p=128) for e_ in range(NEXP)]
        w2_v = [w2_d[e_].rearrange("(c p) n -> p c n", p=128) for e_ in range(NEXP)]
    nld = [0]

    def ring_load(src):
        i = nld[0] % 4
        nld[0] += 1
        R.op("pool", lambda e, i=i: e.dma_start(out=ring[i], in_=src), writes=["ring%d" % i], dma=True)
        return ring[i], "ring%d" % i

    for e_ in range(n_exp if big else 0):
        b1 = b1e[e_ % 2]
        b1k = "b1_%d" % (e_ % 2)
        R.op("sp", lambda e, e_=e_, b1=b1: e.dma_start(out=b1, in_=b1_d[:, e_ * 32:(e_ + 1) * 32]), writes=[b1k], dma=True)
        pend = []
        it = 0
        for u in range(8):
            rg, rgk = ring_load(w1_v[e_][:, :, u * 256:(u + 1) * 256])
            ru, ruk = ring_load(w1_v[e_][:, :, D + u * 256:D + (u + 1) * 256])
            for fq in range(2):
                fc = u * 2 + fq
                for th in range(2):
                    pb = it % 2
                    it += 1
                    for c in range(NCH):
                        R.op("pe", lambda e, c=c, fq=fq, th=th, rg=rg: e.matmul(PS(2 + th), lhsT=rg[:, c, fq * 128:(fq + 1) * 128], rhs=h2T[:, c, th * 512:(th + 1) * 512],
                                                                               start=(c == 0), stop=(c == NCH - 1)), reads=[rgk, "h2T"], writes=[pk(2 + th)])
                    for c in range(NCH):
                        R.op("pe", lambda e, c=c, fq=fq, th=th, ru=ru: e.matmul(PS(4 + th), lhsT=ru[:, c, fq * 128:(fq + 1) * 128], rhs=h2T[:, c, th * 512:(th + 1) * 512],
                                                                               start=(c == 0), stop=(c == NCH - 1)), reads=[ruk, "h2T"], writes=[pk(4 + th)])
                    bg = b1[:, fc:fc + 1]
                    bu = b1[:, 16 + fc:16 + fc + 1]
                    gk_, sk_, uk_ = "gtt%d" % pb, "sgt%d" % pb, "utt%d" % pb
                    R.op("dve", lambda e, th=th, bg=bg, pb=pb: e.tensor_scalar(out=gtt[pb], in0=PS(2 + th), scalar1=bg, scalar2=7.0, op0=ALU.add, op1=ALU.min),
                         reads=[pk(2 + th), b1k], writes=[gk_])
                    R.op("act", lambda e, pb=pb: e.activation(out=sgt[pb], in_=gtt[pb], func=AF.Sigmoid, scale=1.702), reads=[gk_], writes=[sk_])
                    R.op("dve", lambda e, th=th, bu=bu, pb=pb: e.tensor_scalar(out=utt[pb], in0=PS(4 + th), scalar1=bu, scalar2=7.0, op0=ALU.add, op1=ALU.min),
                         reads=[pk(4 + th), b1k], writes=[uk_])
                    R.op("dve", lambda e, pb=pb: e.tensor_scalar(out=utt[pb], in0=utt[pb], scalar1=-7.0, scalar2=1.0, op0=ALU.max, op1=ALU.add), reads=[uk_], writes=[uk_])
                    R.op("dve", lambda e, pb=pb: e.tensor_tensor(out=gtt[pb], in0=gtt[pb], in1=sgt[pb], op=ALU.mult), reads=[gk_, sk_], writes=[gk_])
                    for p_ in pend:
                        p_()
                    pend = [lambda th=th, fc=fc, pb=pb, gk_=gk_, uk_=uk_: R.op(
                        "dve", lambda e: e.tensor_tensor(out=actT[:, fc, th * 512:(th + 1) * 512], in0=utt[pb], in1=gtt[pb], op=ALU.mult),
                        reads=[uk_, gk_], writes=["actT"])]
        for p_ in pend:
            p_()
        for u in range(8):
            r2, r2k = ring_load(w2_v[e_][:, :, u * 256:(u + 1) * 256])
            for o in range(NOWN):
                bk = 6 + (o % 2)
                for fc in range(NCH):
                    R.op("pe", lambda e, o=o, fc=fc, bk=bk, r2=r2: e.matmul(PS(bk, 0, 256), lhsT=actT[:, fc, o * 128:(o + 1) * 128], rhs=r2[:, fc, :],
                                                                           start=(fc == 0), stop=(fc == NCH - 1)), reads=["actT", r2k], writes=[pk(bk)])
                cols = slice(u * 256, (u + 1) * 256)
                R.op("dve", lambda e, o=o, bk=bk, cols=cols: e.tensor_tensor(out=t1[o % 2], in0=PS(bk, 0, 256), in1=gt2b[:, cols], op=ALU.mult),
                     reads=[pk(bk), "gt2b"], writes=["t1"])
                R.op("dve", lambda e, o=o, cols=cols, e_=e_: e.scalar_tensor_tensor(out=x1[:, o, cols], in0=t1[o % 2], scalar=wt[:, o, e_:e_ + 1], in1=x1[:, o, cols],
                                                                                  op0=ALU.mult, op1=ALU.add),
                     reads=["t1", "wt", "x1_%d" % o], writes=["x1_%d" % o])
    R.barrier()
    gfb = actT.rearrange("p f t -> p (f t)")[:, 0:4096].bitcast(F32)
    R.op("sp", lambda e: e.dma_start(out=gfb, in_=pbc(gf_d)), writes=["gfb"], dma=True)
    for o in range(NOWN):
        xk = "x1_%d" % o
        R.op("pool", lambda e: e.memset(sm2[:, 0:1], 0.0), writes=["ss0"])
        R.op("act", lambda e, o=o: e.activation(out=h2f, in_=x1[:, o, :], func=AF.Square, accum_out=sm2[:, 0:1]), reads=[xk, "ss0"], writes=["h2f", "ss0"])
        rstd_op(sm2[:, 2:3], sm2[:, 0:1], float(D), ["ss0"], "rs0")
        R.op("dve", lambda e, o=o: e.scalar_tensor_tensor(out=x1[:, o, :], in0=x1[:, o, :], scalar=sm2[:, 2:3], in1=gfb, op0=ALU.mult, op1=ALU.mult),
             reads=[xk, "rs0", "gfb"], writes=[xk])
        oo = R.op("sp", lambda e, o=o: e.dma_start(out=out_d[o * 128:(o + 1) * 128, :], in_=x1[:, o, :]), reads=[xk], dma=True)
        R.final_ops.append(oo)
    if debug is not None and debug.startswith("moe"):
        for q in range(4):
            dump(x1[:, 0, q * 512:(q + 1) * 512], "x1_0", q * 512, 512)
    return finish()


def _consts():
    r = np.arange(128)
    ident = np.eye(128, dtype=np.float32)
    tri_f = (r[:, None] <= r[None, :]).astype(np.float32)
    tri_b = (r[:, None] >= r[None, :]).astype(np.float32)
    nm_f = np.where(r[:, None] <= r[None, :], 0.0, NEG).astype(np.float32)
    nm_b = np.where(r[:, None] >= r[None, :], 0.0, NEG).astype(np.float32)
    ones = np.ones((128, 128), np.float32)
    iota_c = np.tile(np.arange(512, dtype=np.float32)[None, :], (128, 1))
    iota_p = r.astype(np.float32)[:, None]
    return np.ascontiguousarray(np.concatenate([ident, tri_f, tri_b, nm_f, nm_b, ones, iota_c, iota_p], axis=1))


def _rope_tables():
    rows = 64
    row = np.repeat(np.arange(rows, dtype=np.float32), 64)
    col = np.tile(np.arange(64, dtype=np.float32), rows)
    inv = (np.float32(10000.0) ** (-np.arange(0, 64, 2, dtype=np.float32) / np.float32(64))).astype(np.float32)
    ang = np.concatenate([row[:, None] * inv, col[:, None] * inv], axis=-1).astype(np.float32)
    return np.cos(ang).astype(np.float32), np.sin(ang).astype(np.float32)


def slot_chunks(j):
    pre = list(range(0, 8 * j))
    post = list(range(31, 8 * j + 7, -1))
    own = list(range(8 * j, 8 * j + 8))
    return pre, post, own


def make_in_maps(inp):
    f = lambda a: np.ascontiguousarray(np.asarray(a, dtype=np.float32))
    x, c, ctx, c_ctx = f(inp["x"]), f(inp["c"]), f(inp["ctx"]), f(inp["c_ctx"])
    cos, sin = _rope_tables()
    consts = _consts()
    fm = lambda v, n: np.ascontiguousarray(v.reshape(n, 128).T)
    shared = {
        "bmod": fm(f(inp["b_mod"])[0], 96),
        "g1fm": fm(f(inp["g_norm1"])[0], 16),
        "g2fm": fm(f(inp["g_norm2"])[0], 16),
        "w_mod": f(inp["w_mod"])[0],
        "w_in": f(inp["w_in"])[0],
        "b_in": f(inp["b_in"]),
        "bfm": np.ascontiguousarray(np.concatenate([fm(f(inp["b_in"])[0, MQ0:MQ0 + 512], 4), fm(f(inp["b_in"])[0, MK0:MK0 + 512], 4)], axis=1)),
        "g_q": f(inp["g_q"]), "g_k": f(inp["g_k"]), "g_mlstm": f(inp["g_mlstm"]),
        "w_out": f(inp["w_out"])[0],
        "g_norm2": f(inp["g_norm2"]), "g_final": f(inp["g_final"])[None, :],
        "w_router": f(inp["w_router"])[0], "b_router": f(inp["b_router"]),
        "w1": inp["w1"],
        "b1fm": np.ascontiguousarray(f(inp["b1"])[0].reshape(NEXP, 32, 128).transpose(2, 0, 1).reshape(128, NEXP * 32)),
        "w2": inp["w2"], "b2": f(inp["b2"])[0],
        "consts": consts,
    }
    maps = []
    for core in range(8):
        b, j = core // 4, core % 4
        pre, post, own = slot_chunks(j)
        xs = np.empty((NSLOT * 128, D), np.float32)
        rope = np.empty((NSLOT * 128, 128), np.float32)
        gmask = np.zeros((NSLOT, 16), np.float32)
        xs[0:256] = ctx[b]
        rope[0:256, 0:64] = 1.0
        rope[0:256, 64:128] = 0.0
        gmask[0:2, 0:8] = 0.0
        gmask[0:2, 8:16] = -1.0
        s = 2
        for kind, lst in (("pre", pre), ("post", post), ("own", own)):
            for ch in lst:
                xs[s * 128:(s + 1) * 128] = x[b, ch * 128:(ch + 1) * 128]
                rope[s * 128:(s + 1) * 128, 0:64] = cos[ch * 128:(ch + 1) * 128]
                rope[s * 128:(s + 1) * 128, 64:128] = sin[ch * 128:(ch + 1) * 128]
                fa = kind in ("pre", "own")
                ba = kind in ("post", "own")
                gmask[s, 0:4] = 0.0 if fa else NEG
                gmask[s, 4:8] = 0.0 if ba else NEG
                gmask[s, 8:12] = -1.0 if fa else 0.0
                gmask[s, 12:16] = -1.0 if ba else 0.0
                s += 1
        assert s == NSLOT
        m = dict(shared)
        m["xs"] = xs
        m["rope"] = rope
        m["gmask"] = np.ascontiguousarray(np.tile(gmask.reshape(1, NSLOT * 16), (128, 1)))
        m["cfm"] = np.ascontiguousarray(np.concatenate([fm(c[b], 16), fm(c_ctx, 16)], axis=1))
        maps.append(m)
    return maps


_CACHE = {}


def kernel(**inputs):
    if "nc" not in _CACHE:
        _CACHE["nc"] = build()
    nc, es, declared = _CACHE["nc"]
    maps = make_in_maps(inputs)
    maps = [{k: v for k, v in m.items() if k in declared} for m in maps]
    res = run_bass_kernel_spmd(nc, maps, core_ids=list(range(8)))
    out = np.empty((2, 4096, D), np.float32)
    for core in range(8):
        b, j = core // 4, core % 4
        out[b, j * 1024:(j + 1) * 1024] = res.results[core]["out"]
    return out
```

```python
import numpy as np
import ml_dtypes
from contextlib import ExitStack
import concourse.bass as bass
import concourse.mybir as mybir
from concourse.bass_utils import run_bass_kernel_spmd

F32 = mybir.dt.float32
BF16 = mybir.dt.bfloat16
ALU = mybir.AluOpType
AF = mybir.ActivationFunctionType
AX = mybir.AxisListType

D = 2048
NCH = 16
NSLOT = 34
NOWN = 8
NOTH = 26
EPS = 1e-6
NEG = -30000.0
CAP = 512
NEXP = 32
Q0, K0, V0, MQ0, MK0, MV0, MO0, MI0, MF0 = 0, 1024, 1280, 1536, 2048, 2560, 3584, 4608, 4616


class Op:
    __slots__ = ("eng", "fn", "deps", "is_dma", "signal", "count", "semkey", "value", "name")

    def __init__(self, eng, fn, is_dma, name=""):
        self.eng = eng
        self.fn = fn
        self.deps = []
        self.is_dma = is_dma
        self.signal = False
        self.count = None
        self.semkey = None
        self.value = None
        self.name = name


class Rec:
    ENG = ["pe", "act", "dve", "pool", "sp"]
    NS = 8

    def __init__(self):
        self.streams = {e: [] for e in self.ENG}
        self.last_w = {}
        self.readers = {}
        self.pending = {e: [] for e in self.ENG}
        self.dma_ops = {e: [] for e in self.ENG}
        self.final_ops = []

    def op(self, eng, fn, reads=(), writes=(), dma=False, name=""):
        o = Op(eng, fn, dma, name)
        deps = []
        for k in reads:
            w = self.last_w.get(k)
            if w is not None:
                if not (w.eng == eng and not w.is_dma and eng == "pe"):
                    deps.append(w)
        for k in writes:
            w = self.last_w.get(k)
            if w is not None and (w.eng != eng or w.is_dma):
                deps.append(w)
            for r in self.readers.get(k, ()):
                if r.eng != eng or r.is_dma or eng != "pe":
                    if r is not o:
                        deps.append(r)
        deps.extend(self.pending[eng])
        self.pending[eng] = []
        if dma:
            lst = self.dma_ops[eng]
            if len(lst) >= self.NS:
                deps.append(lst[len(lst) - self.NS])
            lst.append(o)
        o.deps = deps
        for k in writes:
            self.last_w[k] = o
            self.readers[k] = []
        for k in reads:
            self.readers.setdefault(k, []).append(o)
        self.streams[eng].append(o)
        return o

    def barrier(self):
        lasts = []
        for e in self.ENG:
            if self.streams[e]:
                lasts.append(self.streams[e][-1])
            lasts.extend(self.dma_ops[e][-self.NS:])
        for e in self.ENG:
            self.pending[e] = [o for o in lasts if (o.eng != e or o.is_dma)]
        self.last_w = {}
        self.readers = {}

    def emit(self, nc, block):
        for e in self.ENG:
            for o in self.streams[e]:
                for d in o.deps:
                    d.signal = True
        for o in self.final_ops:
            o.signal = True
        nsem = {}
        for e in self.ENG:
            c = 0
            ndma = 0
            for o in self.streams[e]:
                if o.is_dma:
                    slot = ndma % self.NS
                    o.semkey = ("dma", e, slot)
                    o.value = 16 * (ndma // self.NS + 1)
                    ndma += 1
                    o.signal = True
                elif o.signal:
                    c += 1
                    o.semkey = ("eng", e)
                    o.value = c
        sems = {}

        def sem(key):
            if key not in sems:
                sems[key] = self._es.enter_context(nc.semaphore("s_" + "_".join(str(k) for k in key)))
            return sems[key]

        final_ops = self.final_ops

        def run(ename, eh):
            seen = {}
            for o in self.streams[ename]:
                need = {}
                for d in o.deps:
                    if need.get(d.semkey, 0) < d.value:
                        need[d.semkey] = d.value
                for k, v in need.items():
                    if seen.get(k, 0) < v:
                        eh.wait_ge(sem(k), v)
                        seen[k] = v
                ins = o.fn(eh)
                if o.signal:
                    ins.then_inc(sem(o.semkey), 16 if o.is_dma else 1)
            if ename == "sp":
                for o in final_ops:
                    if seen.get(o.semkey, 0) < o.value:
                        eh.wait_ge(sem(o.semkey), o.value)
                        seen[o.semkey] = o.value

        for e in self.ENG:
            sem(("eng", e))
            for s in range(self.NS):
                if e in ("sp", "pool", "act"):
                    sem(("dma", e, s))

        @block.tensor
        def _(eh):
            run("pe", eh)

        @block.scalar
        def _(eh):
            run("act", eh)

        @block.vector
        def _(eh):
            run("dve", eh)

        @block.gpsimd
        def _(eh):
            run("pool", eh)

        @block.sync
        def _(eh):
            run("sp", eh)


class Arena:
    def __init__(self, t, n):
        self.t = t
        self.n = n
        self.off = 0

    def f(self, cols):
        lo = self.off
        self.off += cols
        assert self.off <= self.n, ("arena overflow", self.off, self.n)
        return self.t[:, lo:lo + cols]

    def b(self, cols):
        c2 = (cols + 1) // 2
        return self.f(c2).bitcast(BF16)[:, 0:cols]


def pbc(ap):
    v = ap.partition_broadcast(128)
    return v[:, 0, :]


def build(debug=None, n_oth=NOTH, n_exp=NEXP):
    nc = bass.Bass("TRN2", target_bir_lowering=False)
    R = Rec()
    es = ExitStack()
    R._es = es
    declared = []
    big = debug is None or debug.startswith("moe")

    def din(name, shape, dt=F32):
        if name in ("w1", "w2") and not big:
            return None
        declared.append(name)
        return nc.dram_tensor(name, list(shape), dt, kind="ExternalInput").ap()

    xs_d = din("xs", [NSLOT * 128, D])
    rope_d = din("rope", [NSLOT * 128, 128])
    gmask_d = din("gmask", [128, NSLOT * 16])
    cfm_d = din("cfm", [128, 32])
    bmod_d = din("bmod", [128, 96])
    g1_d = din("g1fm", [128, 16])
    g2fm_d = din("g2fm", [128, 16])
    wmod_d = din("w_mod", [D, 6 * D])
    win_d = din("w_in", [D, 4624])
    bin_d = din("b_in", [1, 4624])
    bfm_d = din("bfm", [128, 8])
    gq_d = din("g_q", [1, 128])
    gk_d = din("g_k", [1, 128])
    gm_d = din("g_mlstm", [1, 1024])
    wout_d = din("w_out", [D, D])
    g2_d = din("g_norm2", [1, D])
    gf_d = din("g_final", [1, D])
    wr_d = din("w_router", [D, NEXP])
    br_d = din("b_router", [1, NEXP])
    w1_d = din("w1", [NEXP, D, 2 * D])
    b1_d = din("b1fm", [128, NEXP * 32])
    w2_d = din("w2", [NEXP, D, D])
    b2_d = din("b2", [NEXP, D])
    cst_d = din("consts", [128, 6 * 128 + 512 + 1])
    out_d = nc.dram_tensor("out", [NOWN * 128, D], F32, kind="ExternalOutput").ap()
    dbg_d = None
    if debug is not None:
        dbg_d = nc.dram_tensor("dbg", [128, 8192], F32, kind="ExternalOutput").ap()

    NF = 52500
    fa_t = es.enter_context(nc.sbuf_tensor("fa", [128, NF], F32))
    A = Arena(fa_t, NF)
    pT = es.enter_context(nc.psum_tensor("pT", [128, 2048], BF16))
    psum = [None, None] + [es.enter_context(nc.psum_tensor("ps%d" % i, [128, 512], F32)) for i in range(2, 8)]

    def PS(i, lo=0, n=512):
        return psum[i][:, lo:lo + n]

    def pk(i):
        return "ps%d" % i

    def finish():
        with nc.Block() as block:
            R.emit(nc, block)
        return nc, es, declared

    def dump(ap, key, lo, n):
        o = R.op("sp", lambda e: e.dma_start(out=dbg_d[:, lo:lo + n], in_=ap), reads=[key], dma=True)
        R.final_ops.append(o)

    dbgf = None
    if debug is not None:
        dbgf = A.f(512)

    def dump_bf(ap, key, lo, n):
        for p0 in range(0, n, 512):
            m = min(512, n - p0)
            R.op("dve", lambda e, p0=p0, m=m: e.tensor_copy(out=dbgf[:, 0:m], in_=ap[:, p0:p0 + m]), reads=[key], writes=["dbgf"])
            dump(dbgf[:, 0:m], "dbgf", lo + p0, m)

    cst = A.f(6 * 128 + 512 + 1)
    ident = cst[:, 0:128]
    tri_f = cst[:, 128:256]
    tri_b = cst[:, 256:384]
    nm_f = cst[:, 384:512]
    nm_b = cst[:, 512:640]
    ones = cst[:, 640:768]
    iota_c = cst[:, 768:1280]
    iota_p = cst[:, 1280:1281]
    identb = A.b(128)
    onesb = A.b(128)
    modT = A.f(192).rearrange("p (a b) -> p a b", b=2)
    gml = A.f(16)
    gmc = A.f(16)
    g1 = A.f(16)
    cfm = A.f(32)
    bmod = A.f(96)
    bfm = A.f(8)
    csT = A.b(32).rearrange("p (a b) -> p a b", b=2)
    stC = [A.f(257) for _ in range(8)]
    stCb = [A.b(258)[:, 0:257] for _ in range(8)]
    epsb = A.f(2)
    R.op("pool", lambda e: e.memset(epsb[:, 0:1], EPS), writes=["epsb"])
    R.op("pool", lambda e: e.memset(epsb[:, 1:2], 1.0), writes=["epsb"])
    gmask = A.f(NSLOT * 16).rearrange("p (s g) -> p s g", g=16)

    R.op("sp", lambda e: e.dma_start(out=cst, in_=cst_d), writes=["cst"], dma=True)
    R.op("sp", lambda e: e.dma_start(out=cfm, in_=cfm_d), writes=["cfm"], dma=True)
    R.op("sp", lambda e: e.dma_start(out=bmod, in_=bmod_d), writes=["bmod"], dma=True)
    R.op("sp", lambda e: e.dma_start(out=g1, in_=g1_d), writes=["g1"], dma=True)
    R.op("sp", lambda e: e.dma_start(out=bfm, in_=bfm_d), writes=["bfm"], dma=True)
    R.op("sp", lambda e: e.dma_start(out=gmask.rearrange("p s g -> p (s g)"), in_=gmask_d), writes=["gmask"], dma=True)
    R.op("dve", lambda e: e.tensor_copy(out=identb, in_=ident), reads=["cst"], writes=["identb"])
    R.op("dve", lambda e: e.tensor_copy(out=onesb, in_=ones), reads=["cst"], writes=["onesb"])
    for j in range(8):
        R.op("pool", lambda e, j=j: e.memset(stC[j], 0.0), writes=["stC%d" % j])
        R.op("pool", lambda e, j=j: e.memset(stCb[j], 0.0), writes=["stCb%d" % j])

    R.op("act", lambda e: e.activation(out=csT[:, :, 0], in_=cfm[:, 0:16], func=AF.Silu), reads=["cfm"], writes=["csT"])
    R.op("act", lambda e: e.activation(out=csT[:, :, 1], in_=cfm[:, 16:32], func=AF.Silu), reads=["cfm"], writes=["csT"])
    mark0 = A.off
    wm = [A.b(16 * 512).rearrange("p (c n) -> p c n", n=512) for _ in range(2)]
    wmod_v = wmod_d.rearrange("(c p) n -> p c n", p=128)
    PM = psum[7][:, 0:192].rearrange("p (a b) -> p a b", b=2)

    def mod_block(blk):
        buf = wm[blk % 2]
        key = "wm%d" % (blk % 2)
        R.op("pool", lambda e: e.dma_start(out=buf, in_=wmod_v[:, :, blk * 512:(blk + 1) * 512]), writes=[key], dma=True)
        for q in range(4):
            cc = blk * 4 + q
            for c in range(NCH):
                R.op("pe", lambda e, c=c, q=q, cc=cc: e.matmul(PM[:, cc, :], lhsT=buf[:, c, q * 128:(q + 1) * 128], rhs=csT[:, c, :],
                                                             start=(c == 0), stop=(c == NCH - 1)),
                     reads=[key, "csT"], writes=[pk(7)])
        R.op("dve", lambda e: e.tensor_tensor(out=modT[:, blk * 4:blk * 4 + 4, :], in0=PM[:, blk * 4:blk * 4 + 4, :],
                                              in1=bmod[:, blk * 4:blk * 4 + 4].unsqueeze(2).to_broadcast([128, 4, 2]), op=ALU.add),
             reads=[pk(7), "bmod"], writes=["modT"])

    for blk in range(24):
        mod_block(blk)
    R.op("dve", lambda e: e.scalar_tensor_tensor(out=gml, in0=modT[:, 16:32, 0], scalar=1.0, in1=g1, op0=ALU.add, op1=ALU.mult),
         reads=["modT", "g1"], writes=["gml"])
    R.op("dve", lambda e: e.scalar_tensor_tensor(out=gmc, in0=modT[:, 16:32, 1], scalar=1.0, in1=g1, op0=ALU.add, op1=ALU.mult),
         reads=["modT", "g1"], writes=["gmc"])
    if debug == "mod":
        dump(modT.rearrange("p a b -> p (a b)"), "modT", 0, 192)
        dump(gml, "gml", 192, 16)
        return finish()
    R.barrier()
    A.off = mark0

    win_v = win_d.rearrange("(c p) n -> p c n", p=128)
    SC = 128.0 ** -0.5
    WCOLS = 2064
    mark_mix = A.off
    Wt = A.b(16 * WCOLS).rearrange("p (c n) -> p c n", n=WCOLS)
    Wflat = Wt.rearrange("p c n -> p (c n)")
    mark_w_end = A.off
    W_regs = A
    bo = A.f(WCOLS)
    gkb = A.f(128)
    gqb = A.f(128)
    xt0 = A.f(D)
    xt = [xt0, xt0]
    xsb = A.b(D)
    hT = [A.b(D).rearrange("p (c t) -> p c t", t=128) for _ in range(2)]
    kTst = A.b(2 * NSLOT * 128).rearrange("p (g t) -> p g t", g=2)
    Vst = A.b(NSLOT * 256).rearrange("p (s v) -> p s v", v=256)
    sm = A.f(64)
    rp = [A.f(128) for _ in range(2)]
    kf = A.f(256)
    kn = A.f(256)
    rt = [A.f(128) for _ in range(4)]
    krot = A.b(256)
    NB_ = 2
    Kt = [A.b(512) for _ in range(NB_)]
    Vx = [A.b(4 * 258).rearrange("p (h v) -> p h v", v=258) for _ in range(NB_)]
    Gt = [A.f(16) for _ in range(NB_)]
    cq = [A.f(64) for _ in range(NB_)]
    expb = [A.f(8) for _ in range(NB_)]
    Kw = [A.b(128) for _ in range(2)]

    def load_w(segs):
        off = 0
        for (lo, hi) in segs:
            n = hi - lo
            R.op("pool", lambda e, off=off, lo=lo, hi=hi, n=n: e.dma_start(out=Wt[:, :, off:off + n], in_=win_v[:, :, lo:hi]), writes=["W"], dma=True)
            off += n

    load_w([(K0, K0 + 512), (MK0, MK0 + 1536), (MI0, MI0 + 16)])
    R.op("sp", lambda e: e.dma_start(out=bo[:, 0:512], in_=pbc(bin_d[:, K0:K0 + 512])), writes=["bo"], dma=True)
    R.op("sp", lambda e: e.dma_start(out=bo[:, 512:2048], in_=pbc(bin_d[:, MK0:MK0 + 1536])), writes=["bo"], dma=True)
    R.op("sp", lambda e: e.dma_start(out=bo[:, 2048:2064], in_=pbc(bin_d[:, MI0:MI0 + 16])), writes=["bo"], dma=True)
    R.op("sp", lambda e: e.dma_start(out=gkb, in_=pbc(gk_d)), writes=["gkb"], dma=True)
    R.op("sp", lambda e: e.dma_start(out=gqb, in_=pbc(gq_d)), writes=["gqb"], dma=True)
    R.op("dve", lambda e: e.tensor_scalar(out=bo[:, 512:1024], in0=bo[:, 512:1024], scalar1=SC, scalar2=None, op0=ALU.mult), reads=["bo"], writes=["bo"])
    R.op("dve", lambda e: e.tensor_scalar(out=gqb, in0=gqb, scalar1=SC, scalar2=None, op0=ALU.mult), reads=["gqb"], writes=["gqb"])
    R.op("dve", lambda e: e.tensor_scalar(out=bfm[:, 4:8], in0=bfm[:, 4:8], scalar1=SC, scalar2=None, op0=ALU.mult), reads=["bfm"], writes=["bfm"])
    for b_ in range(NB_):
        R.op("pool", lambda e, b_=b_: e.memset(Vx[b_][:, :, 256:257], 1.0), writes=["Vx%d" % b_])

    def rstd_op(dst, src, n_el, keys_r, key_w):
        R.op("act", lambda e: e.activation(out=dst, in_=src, func=AF.Ln, scale=1.0 / n_el, bias=epsb[:, 0:1]), reads=keys_r + ["epsb"], writes=[key_w])
        R.op("act", lambda e: e.activation(out=dst, in_=dst, func=AF.Exp, scale=-0.5), reads=[key_w], writes=[key_w])

    def make_hT(s):
        b2 = s % 2
        is_ctx = s < 2
        xk = "xt"
        R.op("sp", lambda e: e.dma_start(out=xt[b2], in_=xs_d[s * 128:(s + 1) * 128, :]), writes=[xk], dma=True)
        R.op("pool", lambda e: e.memset(sm[:, b2:b2 + 1], 0.0), writes=["ss%d" % b2])
        jk = hT[b2].rearrange("p c t -> p (c t)")
        R.op("act", lambda e: e.activation(out=jk, in_=xt[b2], func=AF.Square, accum_out=sm[:, b2:b2 + 1]), reads=[xk, "ss%d" % b2], writes=["hT%d" % b2, "ss%d" % b2])
        rstd_op(sm[:, 2 + b2:3 + b2], sm[:, b2:b2 + 1], float(D), ["ss%d" % b2], "rs%d" % b2)
        R.op("dve", lambda e: e.tensor_scalar(out=xsb, in0=xt[b2], scalar1=sm[:, 2 + b2:3 + b2], scalar2=None, op0=ALU.mult),
             reads=[xk, "rs%d" % b2], writes=["xsb"])
        for c in range(NCH):
            R.op("pe", lambda e, c=c: e.transpose(out=pT[:, c * 128:(c + 1) * 128], in_=xsb[:, c * 128:(c + 1) * 128], identity=identb),
                 reads=["xsb", "identb"], writes=["pT%d" % (c // 8)])
        gm = gmc if is_ctx else gml
        w = 1 if is_ctx else 0
        hk = "hT%d" % b2
        for c in range(NCH):
            R.op("act", lambda e, c=c: e.activation(out=hT[b2][:, c, :], in_=pT[:, c * 128:(c + 1) * 128], func=AF.Identity,
                                                   scale=gm[:, c:c + 1], bias=modT[:, c, w:w + 1]),
                 reads=["pT%d" % (c // 8), "gml", "gmc", "modT"], writes=[hk])
        return hT[b2], hk

    def proj_tok(h, hk, col_lo, n, bank, wkey="W", w=None):
        w = Wt if w is None else w
        for c in range(NCH):
            R.op("pe", lambda e, c=c: e.matmul(PS(bank, 0, n), lhsT=h[:, c, :], rhs=w[:, c, col_lo:col_lo + n], start=(c == 0), stop=(c == NCH - 1)),
                 reads=[hk, wkey], writes=[pk(bank)])

    def slot_common(s, db):
        h, hk = make_hT(s)
        b2 = s % 2
        R.op("sp", lambda e: e.dma_start(out=rp[b2], in_=rope_d[s * 128:(s + 1) * 128, :]), writes=["rp%d" % b2], dma=True)
        proj_tok(h, hk, 0, 512, 2)
        proj_tok(h, hk, 512, 512, 3)
        proj_tok(h, hk, 1024, 512, 4)
        proj_tok(h, hk, 1536, 512, 5)
        proj_tok(h, hk, 2048, 16, 6)
        R.op("dve", lambda e: e.tensor_tensor(out=kf, in0=PS(2, 0, 256), in1=bo[:, 0:256], op=ALU.add), reads=[pk(2), "bo"], writes=["kf"])
        R.op("dve", lambda e: e.tensor_tensor(out=Vst[:, s, :], in0=PS(2, 256, 256), in1=bo[:, 256:512], op=ALU.add), reads=[pk(2), "bo"], writes=["Vst"])
        R.op("dve", lambda e: e.scalar_tensor_tensor(out=Kt[db], in0=PS(3), scalar=SC, in1=bo[:, 512:1024], op0=ALU.mult, op1=ALU.add),
             reads=[pk(3), "bo"], writes=["Kt%d" % db])
        for half in range(2):
            R.op("dve", lambda e, half=half: e.tensor_tensor(out=Vx[db][:, 2 * half:2 * half + 2, 0:256],
                                                             in0=PS(4 + half).rearrange("p (h v) -> p h v", v=256),
                                                             in1=bo[:, 1024 + 512 * half:1536 + 512 * half].rearrange("p (h v) -> p h v", v=256), op=ALU.add),
                 reads=[pk(4 + half), "bo"], writes=["Vx%d" % db])
        R.op("dve", lambda e: e.tensor_tensor(out=Gt[db], in0=PS(6, 0, 16), in1=bo[:, 2048:2064], op=ALU.add), reads=[pk(6), "bo"], writes=["G%d" % db])
        R.op("pool", lambda e: e.memset(sm[:, 4:6], 0.0), writes=["kss"])
        for g in range(2):
            R.op("act", lambda e, g=g: e.activation(out=kn[:, g * 128:(g + 1) * 128], in_=kf[:, g * 128:(g + 1) * 128], func=AF.Square, accum_out=sm[:, 4 + g:5 + g]),
                 reads=["kf", "kss"], writes=["kn", "kss"])
        rstd_op(sm[:, 8:10], sm[:, 4:6], 128.0, ["kss"], "krs")
        for g in range(2):
            R.op("dve", lambda e, g=g: e.scalar_tensor_tensor(out=kn[:, g * 128:(g + 1) * 128], in0=kf[:, g * 128:(g + 1) * 128], scalar=sm[:, 8 + g:9 + g],
                                                              in1=gkb, op0=ALU.mult, op1=ALU.mult), reads=["kf", "krs", "gkb"], writes=["kn"])
        rope(kn, "kn", krot, "krot", 2, rp[b2], "rp%d" % b2)
        for g in range(2):
            R.op("pe", lambda e, g=g: e.transpose(out=pT[:, g * 128:(g + 1) * 128], in_=krot[:, g * 128:(g + 1) * 128], identity=identb),
                 reads=["krot", "identb"], writes=["pT0"])
        R.op("act", lambda e: e.tensor_copy(out=kTst[:, :, s * 128:(s + 1) * 128], in_=pT[:, 0:256].rearrange("p (g t) -> p g t", g=2))
             if False else e.activation(out=kTst[:, :, s * 128:(s + 1) * 128], in_=pT[:, 0:256].rearrange("p (g t) -> p g t", g=2), func=AF.Copy),
             reads=["pT0"], writes=["kTst"])
        chunk_gates(s, db)

    def rope(src, skey, dst, dkey, nh, rpt, rkey):
        v = src.rearrange("p (h i two) -> p h i two", h=nh, two=2)
        o = dst.rearrange("p (h i two) -> p h i two", h=nh, two=2)
        cosb = rpt[:, 0:64].unsqueeze(1).to_broadcast([128, nh, 64])
        sinb = rpt[:, 64:128].unsqueeze(1).to_broadcast([128, nh, 64])
        n = nh * 64
        t = [rt[i][:, 0:n].rearrange("p (h i) -> p h i", h=nh) if n <= 128 else None for i in range(4)]
        if n > 128:
            t = [rtq[i].rearrange("p (h i) -> p h i", h=nh) for i in range(4)]
        x1, x2 = v[:, :, :, 0], v[:, :, :, 1]
        tk = ["rt0", "rt1", "rt2", "rt3"]
        R.op("pool", lambda e: e.tensor_tensor(out=t[0], in0=x1, in1=cosb, op=ALU.mult), reads=[skey, rkey], writes=[tk[0]])
        R.op("pool", lambda e: e.tensor_tensor(out=t[1], in0=x2, in1=sinb, op=ALU.mult), reads=[skey, rkey], writes=[tk[1]])
        R.op("pool", lambda e: e.tensor_tensor(out=t[2], in0=x1, in1=sinb, op=ALU.mult), reads=[skey, rkey], writes=[tk[2]])
        R.op("pool", lambda e: e.tensor_tensor(out=t[3], in0=x2, in1=cosb, op=ALU.mult), reads=[skey, rkey], writes=[tk[3]])
        R.op("dve", lambda e: e.tensor_tensor(out=o[:, :, :, 0], in0=t[0], in1=t[1], op=ALU.subtract), reads=[tk[0], tk[1]], writes=[dkey])
        R.op("dve", lambda e: e.tensor_tensor(out=o[:, :, :, 1], in0=t[2], in1=t[3], op=ALU.add), reads=[tk[2], tk[3]], writes=[dkey])

    def chunk_gates(s, db):
        q_ = cq[db]
        e1, Lf, lgf, ie, imb, gg, wst, dec = [q_[:, 8 * i:8 * i + 8] for i in range(8)]
        ck = "cq%d" % db
        R.op("act", lambda e: e.activation(out=e1, in_=Gt[db][:, 8:16], func=AF.Exp, scale=-1.0), reads=["G%d" % db], writes=[ck + "a"])
        R.op("act", lambda e: e.activation(out=Lf, in_=e1, func=AF.Ln, bias=epsb[:, 1:2]), reads=[ck + "a", "epsb"], writes=[ck + "b"])
        R.op("dve", lambda e: e.tensor_tensor(out=lgf, in0=Lf, in1=gmask[:, s, 8:16], op=ALU.mult), reads=[ck + "b", "gmask"], writes=[ck + "lgf"])
        R.op("dve", lambda e: e.tensor_tensor(out=ie, in0=Gt[db][:, 0:8], in1=gmask[:, s, 0:8], op=ALU.add), reads=["G%d" % db, "gmask"], writes=[ck + "ie"])
        R.op("pe", lambda e: e.matmul(PS(6, 16, 4), lhsT=tri_f, rhs=lgf[:, 0:4], start=True, stop=True), reads=["cst", ck + "lgf"], writes=[pk(6)])
        R.op("pe", lambda e: e.matmul(PS(6, 20, 4), lhsT=tri_b, rhs=lgf[:, 4:8], start=True, stop=True), reads=["cst", ck + "lgf"], writes=[pk(6)])
        R.op("pe", lambda e: e.matmul(PS(6, 24, 8), lhsT=ones, rhs=lgf, start=True, stop=True), reads=["cst", ck + "lgf"], writes=[pk(6)])
        R.op("dve", lambda e: e.tensor_tensor(out=imb, in0=ie, in1=PS(6, 16, 8), op=ALU.subtract), reads=[ck + "ie", pk(6)], writes=[ck + "imb"])
        R.op("dve", lambda e: e.tensor_tensor(out=gg, in0=imb, in1=PS(6, 24, 8), op=ALU.add), reads=[ck + "imb", pk(6)], writes=[ck + "gg"])
        R.op("act", lambda e: e.activation(out=wst, in_=gg, func=AF.Exp), reads=[ck + "gg"], writes=[ck + "wst"])
        R.op("act", lambda e: e.activation(out=dec, in_=PS(6, 24, 8), func=AF.Exp), reads=[pk(6)], writes=[ck + "dec"])
        R.op("act", lambda e: e.activation(out=expb[db], in_=PS(6, 16, 8), func=AF.Exp), reads=[pk(6)], writes=[ck + "expb"])

    def state_step(db, j, refresh_bf=False):
        h = j % 4
        q_ = cq[db]
        wst, dec = q_[:, 48:56], q_[:, 56:64]
        ck = "cq%d" % db
        kb = j % 2
        R.op("dve", lambda e: e.tensor_scalar(out=Kw[kb], in0=Kt[db][:, h * 128:(h + 1) * 128], scalar1=wst[:, j:j + 1], scalar2=None, op0=ALU.mult),
             reads=["Kt%d" % db, ck + "wst"], writes=["Kw%d" % kb])
        R.op("pe", lambda e: e.matmul(PS(7, 0, 257), lhsT=Kw[kb], rhs=Vx[db][:, h, 0:257], start=True, stop=True),
             reads=["Kw%d" % kb, "Vx%d" % db], writes=[pk(7)])
        R.op("dve", lambda e: e.scalar_tensor_tensor(out=stC[j], in0=stC[j], scalar=dec[:, j:j + 1], in1=PS(7, 0, 257), op0=ALU.mult, op1=ALU.add),
             reads=["stC%d" % j, ck + "dec", pk(7)], writes=["stC%d" % j])
        if refresh_bf:
            R.op("act", lambda e: e.activation(out=stCb[j], in_=stC[j], func=AF.Copy), reads=["stC%d" % j], writes=["stCb%d" % j])

    rtq = None
    for s in range(n_oth):
        db = s % 2
        slot_common(s, db)
        if s == 0:
            for j in range(4):
                state_step(db, j)
        elif s == 1:
            for j in range(4):
                state_step(1, j)
            for j in range(4, 8):
                state_step(1, j)
            for j in range(4, 8):
                state_step(0, j)
        else:
            for j in range(8):
                state_step(db, j)
    if debug == "oth":
        s = n_oth - 1
        dump(hT[s % 2].rearrange("p c t -> p (c t)")[:, 0:0], "x", 0, 0) if False else None
        dump_bf(hT[s % 2].rearrange("p c t -> p (c t)"), "hT%d" % (s % 2), 0, 2048)
        dump_bf(kTst[:, :, s * 128:(s + 1) * 128], "kTst", 2048, 256) if False else None
        dump_bf(Vst[:, s, :], "Vst", 2304, 256)
        dump_bf(Kt[s % 2], "Kt%d" % (s % 2), 2560, 512)
        dump(Gt[s % 2], "G%d" % (s % 2), 3072, 16)
        dump(cq[s % 2], "cq%dwst" % (s % 2), 3088, 64)
        for j in range(8):
            dump(stC[j], "stC%d" % j, 3200 + 257 * j, 257)
        dump_bf(krot, "krot", 5300, 256)
        return finish()


    hacc = A.b(NOWN * 1024).rearrange("p (o h v) -> p o h v", o=NOWN, h=4)
    mark_own = A.off
    Wq = A.b(16 * 512).rearrange("p (c n) -> p c n", n=512)
    R.op("pool", lambda e: e.dma_start(out=Wq, in_=win_v[:, :, MQ0:MQ0 + 512]), writes=["Wq"], dma=True)
    qmT = A.b(512).rearrange("p (h t) -> p h t", h=4)
    kmT = A.b(512).rearrange("p (h t) -> p h t", h=4)
    lb = A.f(128)
    DTt = A.f(128)
    STt = A.b(128)
    tmpn = A.f(257)
    tot = A.f(257)
    ddr = A.f(2)
    for j in range(8):
        R.op("act", lambda e, j=j: e.activation(out=stCb[j], in_=stC[j], func=AF.Copy), reads=["stC%d" % j], writes=["stCb%d" % j])

    def full_step(s, db, j, o, first):
        h = j % 4
        dirn = j // 4
        TRI = tri_f if dirn == 0 else tri_b
        NM = nm_f if dirn == 0 else nm_b
        q_ = cq[db]
        lgf, imb = q_[:, 16:24], q_[:, 32:40]
        ck = "cq%d" % db
        R.op("dve", lambda e: e.tensor_scalar(out=lb, in0=ones, scalar1=lgf[:, j:j + 1], scalar2=None, op0=ALU.mult), reads=["cst", ck + "lgf"], writes=["lb"])
        R.op("pe", lambda e: e.matmul(PS(4, 0, 128), lhsT=lb, rhs=TRI, start=True, stop=False), reads=["lb", "cst"], writes=[pk(4)])
        R.op("pe", lambda e: e.matmul(PS(4, 0, 128), lhsT=ident, rhs=NM, start=False, stop=True), reads=["cst"], writes=[pk(4)])
        R.op("act", lambda e: e.activation(out=DTt, in_=PS(4, 0, 128), func=AF.Exp, bias=imb[:, j:j + 1]), reads=[pk(4), ck + "imb"], writes=["DT"])
        R.op("pe", lambda e: e.matmul(PS(5, 0, 128), lhsT=kmT[:, h, :], rhs=qmT[:, h, :], start=True, stop=True), reads=["kmT", "qmT"], writes=[pk(5)])
        R.op("dve", lambda e: e.tensor_tensor(out=STt, in0=PS(5, 0, 128), in1=DTt, op=ALU.mult), reads=[pk(5), "DT"], writes=["ST"])
        R.op("pe", lambda e: e.matmul(PS(2, 0, 257), lhsT=STt, rhs=Vx[db][:, h, 0:257], start=True, stop=True), reads=["ST", "Vx%d" % db], writes=[pk(2)])
        R.op("pe", lambda e: e.matmul(PS(3, 0, 257), lhsT=qmT[:, h, :], rhs=stCb[j], start=True, stop=True), reads=["qmT", "stCb%d" % j], writes=[pk(3)])
        R.op("act", lambda e: e.activation(out=tmpn, in_=PS(2, 0, 257), func=AF.Copy), reads=[pk(2)], writes=["tmpn"])
        R.op("dve", lambda e: e.scalar_tensor_tensor(out=tot, in0=PS(3, 0, 257), scalar=expb[db][:, j:j + 1], in1=tmpn, op0=ALU.mult, op1=ALU.add),
             reads=[pk(3), ck + "expb", "tmpn"], writes=["tot"])
        R.op("dve", lambda e: e.scalar_tensor_tensor(out=ddr[:, 0:1], in0=tot[:, 256:257], scalar=-1.0, in1=tot[:, 256:257], op0=ALU.mult, op1=ALU.max), reads=["tot"], writes=["dd"])
        R.op("dve", lambda e: e.tensor_scalar(out=ddr[:, 0:1], in0=ddr[:, 0:1], scalar1=1.0, scalar2=None, op0=ALU.max), reads=["dd"], writes=["dd"])
        R.op("dve", lambda e: e.reciprocal(out=ddr[:, 1:2], in_=ddr[:, 0:1]), reads=["dd"], writes=["rr"])
        hk_ = "hacc%d" % o
        if first:
            R.op("dve", lambda e: e.tensor_scalar(out=hacc[:, o, h, :], in0=tot[:, 0:256], scalar1=ddr[:, 1:2], scalar2=None, op0=ALU.mult), reads=["tot", "rr"], writes=[hk_])
        else:
            R.op("dve", lambda e: e.scalar_tensor_tensor(out=hacc[:, o, h, :], in0=tot[:, 0:256], scalar=ddr[:, 1:2], in1=hacc[:, o, h, :], op0=ALU.mult, op1=ALU.add),
                 reads=["tot", "rr", hk_], writes=[hk_])
        state_step(db, j, refresh_bf=True)

    def own_visit(o, dirn):
        s = NOTH + o
        db = s % 2
        slot_common(s, db)
        h_, hk = hT[s % 2], "hT%d" % (s % 2)
        for hh in range(4):
            for c in range(NCH):
                R.op("pe", lambda e, c=c, hh=hh: e.matmul(PS(2, hh * 128, 128), lhsT=Wq[:, c, hh * 128:(hh + 1) * 128], rhs=h_[:, c, :], start=(c == 0), stop=(c == NCH - 1)),
                     reads=["Wq", hk], writes=[pk(2)])
        for hh in range(4):
            R.op("act", lambda e, hh=hh: e.activation(out=qmT[:, hh, :], in_=PS(2, hh * 128, 128), func=AF.Identity, bias=bfm[:, hh:hh + 1]), reads=[pk(2), "bfm"], writes=["qmT"])
        for hh in range(4):
            for c in range(NCH):
                R.op("pe", lambda e, c=c, hh=hh: e.matmul(PS(3, hh * 128, 128), lhsT=Wt[:, c, 512 + hh * 128:512 + (hh + 1) * 128], rhs=h_[:, c, :], start=(c == 0), stop=(c == NCH - 1)),
                     reads=["W", hk], writes=[pk(3)])
        for hh in range(4):
            R.op("act", lambda e, hh=hh: e.activation(out=kmT[:, hh, :], in_=PS(3, hh * 128, 128), func=AF.Identity, scale=SC, bias=bfm[:, 4 + hh:5 + hh]), reads=[pk(3), "bfm"], writes=["kmT"])
        for hh in range(4):
            full_step(s, db, dirn * 4 + hh, o, first=(dirn == 0))

    n_own = NOWN if debug != "own" else 2
    for o in range(n_own):
        own_visit(o, 0)
    for o in range(n_own - 1, -1, -1):
        own_visit(o, 1)
    if debug == "own":
        dump_bf(hacc[:, 0, :, :].rearrange("p h v -> p (h v)"), "hacc0", 0, 1024)
        dump_bf(hacc[:, 1, :, :].rearrange("p h v -> p (h v)"), "hacc1", 1024, 1024)
        return finish()


    R.barrier()
    A.off = mark_own
    Wc = Wflat[:, 0:16 * 1024].rearrange("p (c n) -> p c n", n=1024)
    yT = Wflat[:, 16 * 1024:32 * 1024].rearrange("p (k t) -> p k t", t=1024)
    qf = A.f(1024)
    rtq_all = A.f(2048)
    rtq = [rtq_all[:, i * 512:(i + 1) * 512] for i in range(4)]
    qrot = A.b(1024)
    qTc = A.b(1024).rearrange("p (h t) -> p h t", h=8)
    PTt = [A.b(512) for _ in range(2)]
    dsb = A.f(512)
    qss = A.f(16)
    gmb = A.f(1024)

    def load_wc(lo, wd=win_v):
        R.op("pool", lambda e: e.dma_start(out=Wc, in_=wd[:, :, lo:lo + 1024]), writes=["W"], dma=True)

    load_wc(Q0)
    R.op("sp", lambda e: e.dma_start(out=bo[:, 0:1024], in_=pbc(bin_d[:, Q0:Q0 + 1024])), writes=["bo"], dma=True)
    R.op("sp", lambda e: e.dma_start(out=bo[:, 1024:2048], in_=pbc(bin_d[:, MO0:MO0 + 1024])), writes=["bo"], dma=True)
    R.op("sp", lambda e: e.dma_start(out=gmb, in_=pbc(gm_d)), writes=["gmb"], dma=True)
    qf3 = qf.rearrange("p (h d) -> p h d", h=8)
    for o in range(NOWN):
        s = NOTH + o
        b2 = s % 2
        h_, hk = make_hT(s)
        R.op("sp", lambda e, s=s, b2=b2: e.dma_start(out=rp[b2], in_=rope_d[s * 128:(s + 1) * 128, :]), writes=["rp%d" % b2], dma=True)
        proj_tok(h_, hk, 0, 512, 2, w=Wc)
        proj_tok(h_, hk, 512, 512, 3, w=Wc)
        for hf_ in range(2):
            R.op("dve", lambda e, hf_=hf_: e.tensor_tensor(out=qf[:, hf_ * 512:(hf_ + 1) * 512], in0=PS(2 + hf_), in1=bo[:, hf_ * 512:(hf_ + 1) * 512], op=ALU.add),
                 reads=[pk(2 + hf_), "bo"], writes=["qf"])
        R.op("pool", lambda e: e.memset(qss[:, 0:8], 0.0), writes=["qss"])
        for hh in range(8):
            R.op("act", lambda e, hh=hh: e.activation(out=rtq[0][:, 0:128], in_=qf[:, hh * 128:(hh + 1) * 128], func=AF.Square, accum_out=qss[:, hh:hh + 1]),
                 reads=["qf", "qss"], writes=["rt0", "qss"])
        rstd_op(qss[:, 8:16], qss[:, 0:8], 128.0, ["qss"], "qrs")
        R.op("dve", lambda e: e.tensor_tensor(out=qf3, in0=qf3, in1=qss[:, 8:16].unsqueeze(2).to_broadcast([128, 8, 128]), op=ALU.mult), reads=["qf", "qrs"], writes=["qf"])
        R.op("dve", lambda e: e.tensor_tensor(out=qf3, in0=qf3, in1=gqb.unsqueeze(1).to_broadcast([128, 8, 128]), op=ALU.mult), reads=["qf", "gqb"], writes=["qf"])
        rope(qf, "qf", qrot, "qrot", 8, rp[b2], "rp%d" % b2)
        for hh in range(8):
            R.op("pe", lambda e, hh=hh: e.transpose(out=pT[:, hh * 128:(hh + 1) * 128], in_=qrot[:, hh * 128:(hh + 1) * 128], identity=identb),
                 reads=["qrot", "identb"], writes=["pT0"])
        R.op("act", lambda e: e.activation(out=qTc, in_=pT[:, 0:1024].rearrange("p (h t) -> p h t", h=8), func=AF.Copy), reads=["pT0"], writes=["qTc"])
        for g in range(2):
            def s_mm(sk, g=g):
                sb = 2 + (sk % 2)
                R.op("pe", lambda e: e.matmul(PS(sb), lhsT=kTst[:, g, sk * 128:(sk + 1) * 128], rhs=qTc[:, 4 * g:4 * g + 4, :], start=True, stop=True),
                     reads=["kTst", "qTc"], writes=[pk(sb)])
            s_mm(0)
            for sk in range(NSLOT):
                sb = 2 + (sk % 2)
                pb = sk % 2
                if sk + 1 < NSLOT:
                    s_mm(sk + 1)
                R.op("act", lambda e, sb=sb, pb=pb: e.activation(out=PTt[pb], in_=PS(sb), func=AF.Exp), reads=[pk(sb)], writes=["PT%d" % pb])
                R.op("pe", lambda e, g=g, sk=sk, pb=pb: e.matmul(PS(4 + g), lhsT=Vst[:, sk, g * 128:(g + 1) * 128], rhs=PTt[pb], start=(sk == 0), stop=(sk == NSLOT - 1)),
                     reads=["Vst", "PT%d" % pb], writes=[pk(4 + g)])
                R.op("pe", lambda e, g=g, sk=sk, pb=pb: e.matmul(PS(6 + g), lhsT=onesb, rhs=PTt[pb], start=(sk == 0), stop=(sk == NSLOT - 1)),
                     reads=["onesb", "PT%d" % pb], writes=[pk(6 + g)])
            R.op("act", lambda e, g=g: e.activation(out=dsb, in_=PS(6 + g), func=AF.Copy), reads=[pk(6 + g)], writes=["dsb"])
            R.op("dve", lambda e: e.reciprocal(out=dsb, in_=dsb), reads=["dsb"], writes=["dsb"])
            R.op("dve", lambda e, g=g, o=o: e.tensor_tensor(out=yT[:, 4 * g:4 * g + 4, o * 128:(o + 1) * 128], in0=PS(4 + g).rearrange("p (h t) -> p h t", h=4),
                                                          in1=dsb.rearrange("p (h t) -> p h t", h=4), op=ALU.mult),
                 reads=[pk(4 + g), "dsb"], writes=["yT"])
    if debug == "att":
        for k in range(8):
            dump_bf(yT[:, k, 0:256], "yT", k * 256, 256)
        return finish()

    R.barrier()
    load_wc(MO0)
    hn = rtq_all[:, 0:1024]
    ym = qrot
    hn3 = hn.rearrange("p (h v) -> p h v", h=4)
    for o in range(NOWN):
        s = NOTH + o
        h_, hk = make_hT(s)
        proj_tok(h_, hk, 0, 512, 2, w=Wc)
        proj_tok(h_, hk, 512, 512, 3, w=Wc)
        for hf_ in range(2):
            R.op("dve", lambda e, hf_=hf_: e.tensor_tensor(out=qf[:, hf_ * 512:(hf_ + 1) * 512], in0=PS(2 + hf_), in1=bo[:, 1024 + hf_ * 512:1024 + (hf_ + 1) * 512], op=ALU.add),
                 reads=[pk(2 + hf_), "bo"], writes=["qf"])
        R.op("act", lambda e: e.activation(out=qf, in_=qf, func=AF.Sigmoid), reads=["qf"], writes=["qf"])
        R.op("pool", lambda e: e.memset(qss[:, 0:4], 0.0), writes=["qss"])
        for hh in range(4):
            R.op("act", lambda e, hh=hh, o=o: e.activation(out=hn[:, hh * 256:(hh + 1) * 256], in_=hacc[:, o, hh, :], func=AF.Square, accum_out=qss[:, hh:hh + 1]),
                 reads=["hacc%d" % o, "qss"], writes=["hn", "qss"])
        rstd_op(qss[:, 8:12], qss[:, 0:4], 256.0, ["qss"], "qrs")
        R.op("dve", lambda e, o=o: e.tensor_tensor(out=hn3, in0=hacc[:, o, :, :], in1=qss[:, 8:12].unsqueeze(2).to_broadcast([128, 4, 256]), op=ALU.mult),
             reads=["hacc%d" % o, "qrs"], writes=["hn"])
        R.op("pool", lambda e: e.tensor_tensor(out=hn, in0=hn, in1=gmb, op=ALU.mult), reads=["hn", "gmb"], writes=["hn"])
        R.op("dve", lambda e: e.tensor_tensor(out=ym, in0=hn, in1=qf, op=ALU.mult), reads=["hn", "qf"], writes=["qrot"])
        for k in range(8):
            R.op("pe", lambda e, k=k: e.transpose(out=pT[:, k * 128:(k + 1) * 128], in_=ym[:, k * 128:(k + 1) * 128], identity=identb),
                 reads=["qrot", "identb"], writes=["pT0"])
        R.op("act", lambda e, o=o: e.activation(out=yT[:, 8:16, o * 128:(o + 1) * 128], in_=pT[:, 0:1024].rearrange("p (k t) -> p k t", k=8), func=AF.Copy),
             reads=["pT0"], writes=["yT"])
    if debug == "ym":
        for k in range(8):
            dump_bf(yT[:, 8 + k, 0:256], "yT", k * 256, 256)
        return finish()

    R.barrier()
    A.off = mark_w_end
    x1 = A.f(NOWN * D).rearrange("p (o d) -> p o d", o=NOWN)
    bcA = A.f(1024)
    tmpd = [A.f(512) for _ in range(2)]
    lbm = A.f(128)
    wout_v = wout_d.rearrange("(c p) n -> p c n", p=128)

    def make_bc(dst, key, chunk_lo, nchunks, col, src=None, skey="modT"):
        for c in range(nchunks):
            if src is None:
                vec = modT[:, chunk_lo + c, col:col + 1]
            else:
                vec = src[:, chunk_lo + c:chunk_lo + c + 1]
            R.op("dve", lambda e, vec=vec: e.tensor_scalar(out=lbm, in0=ones, scalar1=vec, scalar2=None, op0=ALU.mult), reads=["cst", skey], writes=["lbm"])
            R.op("pe", lambda e, c=c: e.matmul(PS(2, (c % 4) * 128, 128), lhsT=lbm, rhs=ident, start=True, stop=True), reads=["lbm", "cst"], writes=[pk(2)])
            if c % 4 == 3:
                R.op("act", lambda e, c=c: e.activation(out=dst[:, (c - 3) * 128:(c + 1) * 128], in_=PS(2), func=AF.Copy), reads=[pk(2)], writes=[key])

    for o in range(NOWN):
        s = NOTH + o
        R.op("sp", lambda e, s=s, o=o: e.dma_start(out=x1[:, o, :], in_=xs_d[s * 128:(s + 1) * 128, :]), writes=["x1_%d" % o], dma=True)
    for half in range(2):
        load_wc(half * 1024, wd=wout_v)
        make_bc(bcA, "bcA", 32 + half * 8, 8, 0)
        for o in range(NOWN):
            for dh in range(2):
                for k in range(NCH):
                    R.op("pe", lambda e, o=o, dh=dh, k=k: e.matmul(PS(4 + dh), lhsT=yT[:, k, o * 128:(o + 1) * 128], rhs=Wc[:, k, dh * 512:(dh + 1) * 512],
                                                                  start=(k == 0), stop=(k == NCH - 1)), reads=["yT", "W"], writes=[pk(4 + dh)])
                cols = slice(half * 1024 + dh * 512, half * 1024 + (dh + 1) * 512)
                R.op("dve", lambda e, dh=dh: e.tensor_tensor(out=tmpd[dh], in0=PS(4 + dh), in1=bcA[:, dh * 512:(dh + 1) * 512], op=ALU.mult),
                     reads=[pk(4 + dh), "bcA"], writes=["tmpd%d" % dh])
                R.op("pool", lambda e, o=o, dh=dh, cols=cols: e.tensor_tensor(out=x1[:, o, cols], in0=x1[:, o, cols], in1=tmpd[dh], op=ALU.add),
                     reads=["x1_%d" % o, "tmpd%d" % dh], writes=["x1_%d" % o])
    if debug == "x1":
        for q in range(4):
            dump(x1[:, 0, q * 512:(q + 1) * 512], "x1_0", q * 512, 512)
        for q in range(4):
            dump(x1[:, 7, q * 512:(q + 1) * 512], "x1_7", 2048 + q * 512, 512)
        return finish()

    R.barrier()
    h2T = Wflat[:, 0:16 * 1024].rearrange("p (c t) -> p c t", t=1024)
    wfree = Wflat[:, 16 * 1024:16 * 1024 + 16384].bitcast(F32)
    gm2b = wfree[:, 0:2048]
    sh2b = wfree[:, 2048:4096]
    h2f = wfree[:, 4096:6144]
    h2Tr = wfree[:, 6144:8192].rearrange("p (c t) -> p c t", t=128)
    A.off = mark_w_end + NOWN * D
    sm2 = A.f(8)
    g2fm = A.f(16)
    gm2fm = A.f(16)
    wr = A.f(16 * NEXP).rearrange("p (c e) -> p c e", e=NEXP)
    brb = A.f(NEXP)
    wt = A.f(NOWN * NEXP).rearrange("p (o e) -> p o e", e=NEXP)
    rsm = A.f(64)
    ex = A.f(NEXP)
    msk = A.f(NEXP)
    R.op("sp", lambda e: e.dma_start(out=g2fm, in_=g2fm_d), writes=["g2fm"], dma=True)
    R.op("sp", lambda e: e.dma_start(out=wr, in_=wr_d.rearrange("(c p) e -> p c e", p=128)), writes=["wr"], dma=True)
    R.op("sp", lambda e: e.dma_start(out=brb, in_=pbc(br_d)), writes=["brb"], dma=True)
    R.op("dve", lambda e: e.scalar_tensor_tensor(out=gm2fm, in0=modT[:, 64:80, 0], scalar=1.0, in1=g2fm, op0=ALU.add, op1=ALU.mult), reads=["modT", "g2fm"], writes=["gm2fm"])
    make_bc(gm2b, "gm2b", 0, 16, 0, src=gm2fm, skey="gm2fm")
    make_bc(sh2b, "sh2b", 48, 16, 0)
    lg = rsm[:, 0:32]
    top8 = rsm[:, 32:40]
    for o in range(NOWN):
        xk = "x1_%d" % o
        R.op("pool", lambda e: e.memset(sm2[:, 0:1], 0.0), writes=["ss0"])
        R.op("act", lambda e, o=o: e.activation(out=h2f, in_=x1[:, o, :], func=AF.Square, accum_out=sm2[:, 0:1]), reads=[xk, "ss0"], writes=["h2f", "ss0"])
        rstd_op(sm2[:, 2:3], sm2[:, 0:1], float(D), ["ss0"], "rs0")
        R.op("dve", lambda e, o=o: e.scalar_tensor_tensor(out=h2f, in0=x1[:, o, :], scalar=sm2[:, 2:3], in1=gm2b, op0=ALU.mult, op1=ALU.mult),
             reads=[xk, "rs0", "gm2b"], writes=["h2f"])
        R.op("pool", lambda e: e.tensor_tensor(out=h2f, in0=h2f, in1=sh2b, op=ALU.add), reads=["h2f", "sh2b"], writes=["h2f"])
        for c in range(NCH):
            bk = 4 + (c // 4)
            R.op("pe", lambda e, c=c, bk=bk: e.transpose(out=PS(bk, (c % 4) * 128, 128), in_=h2f[:, c * 128:(c + 1) * 128], identity=ident),
                 reads=["h2f", "cst"], writes=[pk(bk)])
        for q in range(4):
            R.op("act", lambda e, q=q: e.activation(out=h2Tr[:, 4 * q:4 * q + 4, :], in_=PS(4 + q).rearrange("p (c t) -> p c t", t=128), func=AF.Copy),
                 reads=[pk(4 + q)], writes=["h2Tr"])
        R.op("dve", lambda e, o=o: e.tensor_copy(out=h2T[:, :, o * 128:(o + 1) * 128], in_=h2Tr), reads=["h2Tr"], writes=["h2T"])
        for c in range(NCH):
            R.op("pe", lambda e, c=c: e.matmul(PS(3, 0, NEXP), lhsT=h2Tr[:, c, :], rhs=wr[:, c, :], start=(c == 0), stop=(c == NCH - 1)), reads=["h2Tr", "wr"], writes=[pk(3)])
        R.op("dve", lambda e: e.tensor_tensor(out=lg, in0=PS(3, 0, NEXP), in1=brb, op=ALU.add), reads=[pk(3), "brb"], writes=["lg"])
        R.op("dve", lambda e: e.max(out=top8, in_=lg), reads=["lg"], writes=["top8"])
        R.op("dve", lambda e: e.tensor_scalar(out=msk, in0=lg, scalar1=top8[:, 3:4], scalar2=None, op0=ALU.is_ge), reads=["lg", "top8"], writes=["msk"])
        R.op("dve", lambda e: e.tensor_scalar(out=rsm[:, 40:41], in0=top8[:, 0:1], scalar1=-1.0, scalar2=None, op0=ALU.mult), reads=["top8"], writes=["nmx"])
        R.op("act", lambda e: e.activation(out=ex, in_=lg, func=AF.Exp, bias=rsm[:, 40:41]), reads=["lg", "nmx"], writes=["ex"])
        R.op("dve", lambda e: e.tensor_tensor(out=ex, in0=ex, in1=msk, op=ALU.mult), reads=["ex", "msk"], writes=["ex"])
        R.op("dve", lambda e: e.reduce_sum(out=rsm[:, 41:42], in_=ex, axis=AX.X), reads=["ex"], writes=["esum"])
        R.op("dve", lambda e: e.reciprocal(out=rsm[:, 42:43], in_=rsm[:, 41:42]), reads=["esum"], writes=["ersum"])
        R.op("dve", lambda e, o=o: e.tensor_scalar(out=wt[:, o, :], in0=ex, scalar1=rsm[:, 42:43], scalar2=None, op0=ALU.mult), reads=["ex", "ersum"], writes=["wt"])
    if debug == "rt":
        dump(wt.rearrange("p o e -> p (o e)"), "wt", 0, 256)
        dump_bf(h2T[:, 0, 0:256], "h2T", 256, 256)
        return finish()

    R.barrier()
    gt2b = wfree[:, 0:2048]
    ring = [wfree[:, 2048 * (1 + i):2048 * (2 + i)].bitcast(BF16).rearrange("p (c n) -> p c n", n=256) for i in range(3)]
    ring.append(A.b(16 * 256).rearrange("p (c n) -> p c n", n=256))
    actT = A.b(16 * 1024).rearrange("p (f t) -> p f t", t=1024)
    b1e = [A.f(32) for _ in range(2)]
    gtt = [A.f(512) for _ in range(2)]
    sgt = [A.f(512), wr.rearrange("p c e -> p (c e)")]
    utt = [A.f(512), A.f(512)]
    t10 = A.f(256)
    t1 = [t10, t10]
    make_bc(gt2b, "gt2b", 80, 16, 0)
    b2g = actT.rearrange("p f t -> p (f t)")[:, 0:4096].bitcast(F32)
    wtT = actT.rearrange("p f t -> p (f t)")[:, 4096:4096 + 2048].bitcast(F32)
    R.op("sp", lambda e: e.dma_start(out=b2g[0:NEXP, :], in_=b2_d), writes=["b2g"], dma=True)
    for o in range(NOWN):
        R.op("pe", lambda e, o=o: e.transpose(out=PS(2, o * 64, 128)[0:NEXP, :] if False else PS(2 + o // 4, (o % 4) * 128, 128)[0:NEXP, :], in_=wt[:, o, :], identity=ident),
             reads=["wt", "cst"], writes=[pk(2 + o // 4)])
    for q in range(2):
        R.op("act", lambda e, q=q: e.activation(out=wtT[0:NEXP, q * 512:(q + 1) * 512], in_=PS(2 + q)[0:NEXP, :], func=AF.Copy), reads=[pk(2 + q)], writes=["wtT"])
    for o in range(NOWN):
        for dq in range(4):
            bk = 4 + (dq % 2)
            R.op("pe", lambda e, o=o, dq=dq, bk=bk: e.matmul(PS(bk), lhsT=wtT[0:NEXP, o * 128:(o + 1) * 128], rhs=b2g[0:NEXP, dq * 512:(dq + 1) * 512], start=True, stop=True),
                 reads=["wtT", "b2g"], writes=[pk(bk)])
            R.op("dve", lambda e, dq=dq, bk=bk: e.tensor_tensor(out=gtt[dq % 2], in0=PS(bk), in1=gt2b[:, dq * 512:(dq + 1) * 512], op=ALU.mult),
                 reads=[pk(bk), "gt2b"], writes=["gtt%d" % (dq % 2)])
            R.op("pool", lambda e, o=o, dq=dq: e.tensor_tensor(out=x1[:, o, dq * 512:(dq + 1) * 512], in0=x1[:, o, dq * 512:(dq + 1) * 512], in1=gtt[dq % 2], op=ALU.add),
                 reads=["x1_%d" % o, "gtt%d" % (dq % 2)], writes=["x1_%d" % o])
    R.barrier()
    if big:
        w1_v = [w1_d[e_].rearrange("(c p) n -> p c n", p=128) for e_ in range(NEXP)]
        w2_v = [w2_d[e_].rearrange("(c p) n -> p c n", p=128) for e_ in range(NEXP)]
    nld = [0]

    def ring_load(src):
        i = nld[0] % 4
        nld[0] += 1
        R.op("pool", lambda e, i=i: e.dma_start(out=ring[i], in_=src), writes=["ring%d" % i], dma=True)
        return ring[i], "ring%d" % i

    for e_ in range(n_exp if big else 0):
        b1 = b1e[e_ % 2]
        b1k = "b1_%d" % (e_ % 2)
        R.op("sp", lambda e, e_=e_, b1=b1: e.dma_start(out=b1, in_=b1_d[:, e_ * 32:(e_ + 1) * 32]), writes=[b1k], dma=True)
        pend = []
        it = 0
        for u in range(8):
            rg, rgk = ring_load(w1_v[e_][:, :, u * 256:(u + 1) * 256])
            ru, ruk = ring_load(w1_v[e_][:, :, D + u * 256:D + (u + 1) * 256])
            for fq in range(2):
                fc = u * 2 + fq
                for th in range(2):
                    pb = it % 2
                    it += 1
                    for c in range(NCH):
                        R.op("pe", lambda e, c=c, fq=fq, th=th, rg=rg: e.matmul(PS(2 + th), lhsT=rg[:, c, fq * 128:(fq + 1) * 128], rhs=h2T[:, c, th * 512:(th + 1) * 512],
                                                                               start=(c == 0), stop=(c == NCH - 1)), reads=[rgk, "h2T"], writes=[pk(2 + th)])
                    for c in range(NCH):
                        R.op("pe", lambda e, c=c, fq=fq, th=th, ru=ru: e.matmul(PS(4 + th), lhsT=ru[:, c, fq * 128:(fq + 1) * 128], rhs=h2T[:, c, th * 512:(th + 1) * 512],
                                                                               start=(c == 0), stop=(c == NCH - 1)), reads=[ruk, "h2T"], writes=[pk(4 + th)])
                    bg = b1[:, fc:fc + 1]
                    bu = b1[:, 16 + fc:16 + fc + 1]
                    gk_, sk_, uk_ = "gtt%d" % pb, "sgt%d" % pb, "utt%d" % pb
                    R.op("dve", lambda e, th=th, bg=bg, pb=pb: e.tensor_scalar(out=gtt[pb], in0=PS(2 + th), scalar1=bg, scalar2=7.0, op0=ALU.add, op1=ALU.min),
                         reads=[pk(2 + th), b1k], writes=[gk_])
                    R.op("act", lambda e, pb=pb: e.activation(out=sgt[pb], in_=gtt[pb], func=AF.Sigmoid, scale=1.702), reads=[gk_], writes=[sk_])
                    R.op("dve", lambda e, th=th, bu=bu, pb=pb: e.tensor_scalar(out=utt[pb], in0=PS(4 + th), scalar1=bu, scalar2=7.0, op0=ALU.add, op1=ALU.min),
                         reads=[pk(4 + th), b1k], writes=[uk_])
                    R.op("dve", lambda e, pb=pb: e.tensor_scalar(out=utt[pb], in0=utt[pb], scalar1=-7.0, scalar2=1.0, op0=ALU.max, op1=ALU.add), reads=[uk_], writes=[uk_])
                    R.op("dve", lambda e, pb=pb: e.tensor_tensor(out=gtt[pb], in0=gtt[pb], in1=sgt[pb], op=ALU.mult), reads=[gk_, sk_], writes=[gk_])
                    for p_ in pend:
                        p_()
                    pend = [lambda th=th, fc=fc, pb=pb, gk_=gk_, uk_=uk_: R.op(
                        "dve", lambda e: e.tensor_tensor(out=actT[:, fc, th * 512:(th + 1) * 512], in0=utt[pb], in1=gtt[pb], op=ALU.mult),
                        reads=[uk_, gk_], writes=["actT"])]
        for p_ in pend:
            p_()
        for u in range(8):
            r2, r2k = ring_load(w2_v[e_][:, :, u * 256:(u + 1) * 256])
            for o in range(NOWN):
                bk = 6 + (o % 2)
                for fc in range(NCH):
                    R.op("pe", lambda e, o=o, fc=fc, bk=bk, r2=r2: e.matmul(PS(bk, 0, 256), lhsT=actT[:, fc, o * 128:(o + 1) * 128], rhs=r2[:, fc, :],
                                                                           start=(fc == 0), stop=(fc == NCH - 1)), reads=["actT", r2k], writes=[pk(bk)])
                cols = slice(u * 256, (u + 1) * 256)
                R.op("dve", lambda e, o=o, bk=bk, cols=cols: e.tensor_tensor(out=t1[o % 2], in0=PS(bk, 0, 256), in1=gt2b[:, cols], op=ALU.mult),
                     reads=[pk(bk), "gt2b"], writes=["t1"])
                R.op("dve", lambda e, o=o, cols=cols, e_=e_: e.scalar_tensor_tensor(out=x1[:, o, cols], in0=t1[o % 2], scalar=wt[:, o, e_:e_ + 1], in1=x1[:, o, cols],
                                                                                  op0=ALU.mult, op1=ALU.add),
                     reads=["t1", "wt", "x1_%d" % o], writes=["x1_%d" % o])
    R.barrier()
    gfb = actT.rearrange("p f t -> p (f t)")[:, 0:4096].bitcast(F32)
    R.op("sp", lambda e: e.dma_start(out=gfb, in_=pbc(gf_d)), writes=["gfb"], dma=True)
    for o in range(NOWN):
        xk = "x1_%d" % o
        R.op("pool", lambda e: e.memset(sm2[:, 0:1], 0.0), writes=["ss0"])
        R.op("act", lambda e, o=o: e.activation(out=h2f, in_=x1[:, o, :], func=AF.Square, accum_out=sm2[:, 0:1]), reads=[xk, "ss0"], writes=["h2f", "ss0"])
        rstd_op(sm2[:, 2:3], sm2[:, 0:1], float(D), ["ss0"], "rs0")
        R.op("dve", lambda e, o=o: e.scalar_tensor_tensor(out=x1[:, o, :], in0=x1[:, o, :], scalar=sm2[:, 2:3], in1=gfb, op0=ALU.mult, op1=ALU.mult),
             reads=[xk, "rs0", "gfb"], writes=[xk])
        oo = R.op("sp", lambda e, o=o: e.dma_start(out=out_d[o * 128:(o + 1) * 128, :], in_=x1[:, o, :]), reads=[xk], dma=True)
        R.final_ops.append(oo)
    if debug is not None and debug.startswith("moe"):
        for q in range(4):
            dump(x1[:, 0, q * 512:(q + 1) * 512], "x1_0", q * 512, 512)
    return finish()


def _consts():
    r = np.arange(128)
    ident = np.eye(128, dtype=np.float32)
    tri_f = (r[:, None] <= r[None, :]).astype(np.float32)
    tri_b = (r[:, None] >= r[None, :]).astype(np.float32)
    nm_f = np.where(r[:, None] <= r[None, :], 0.0, NEG).astype(np.float32)
    nm_b = np.where(r[:, None] >= r[None, :], 0.0, NEG).astype(np.float32)
    ones = np.ones((128, 128), np.float32)
    iota_c = np.tile(np.arange(512, dtype=np.float32)[None, :], (128, 1))
    iota_p = r.astype(np.float32)[:, None]
    return np.ascontiguousarray(np.concatenate([ident, tri_f, tri_b, nm_f, nm_b, ones, iota_c, iota_p], axis=1))


def _rope_tables():
    rows = 64
    row = np.repeat(np.arange(rows, dtype=np.float32), 64)
    col = np.tile(np.arange(64, dtype=np.float32), rows)
    inv = (np.float32(10000.0) ** (-np.arange(0, 64, 2, dtype=np.float32) / np.float32(64))).astype(np.float32)
    ang = np.concatenate([row[:, None] * inv, col[:, None] * inv], axis=-1).astype(np.float32)
    return np.cos(ang).astype(np.float32), np.sin(ang).astype(np.float32)


def slot_chunks(j):
    pre = list(range(0, 8 * j))
    post = list(range(31, 8 * j + 7, -1))
    own = list(range(8 * j, 8 * j + 8))
    return pre, post, own


def make_in_maps(inp):
    f = lambda a: np.ascontiguousarray(np.asarray(a, dtype=np.float32))
    x, c, ctx, c_ctx = f(inp["x"]), f(inp["c"]), f(inp["ctx"]), f(inp["c_ctx"])
    cos, sin = _rope_tables()
    consts = _consts()
    fm = lambda v, n: np.ascontiguousarray(v.reshape(n, 128).T)
    shared = {
        "bmod": fm(f(inp["b_mod"])[0], 96),
        "g1fm": fm(f(inp["g_norm1"])[0], 16),
        "g2fm": fm(f(inp["g_norm2"])[0], 16),
        "w_mod": f(inp["w_mod"])[0],
        "w_in": f(inp["w_in"])[0],
        "b_in": f(inp["b_in"]),
        "bfm": np.ascontiguousarray(np.concatenate([fm(f(inp["b_in"])[0, MQ0:MQ0 + 512], 4), fm(f(inp["b_in"])[0, MK0:MK0 + 512], 4)], axis=1)),
        "g_q": f(inp["g_q"]), "g_k": f(inp["g_k"]), "g_mlstm": f(inp["g_mlstm"]),
        "w_out": f(inp["w_out"])[0],
        "g_norm2": f(inp["g_norm2"]), "g_final": f(inp["g_final"])[None, :],
        "w_router": f(inp["w_router"])[0], "b_router": f(inp["b_router"]),
        "w1": inp["w1"],
        "b1fm": np.ascontiguousarray(f(inp["b1"])[0].reshape(NEXP, 32, 128).transpose(2, 0, 1).reshape(128, NEXP * 32)),
        "w2": inp["w2"], "b2": f(inp["b2"])[0],
        "consts": consts,
    }
    maps = []
    for core in range(8):
        b, j = core // 4, core % 4
        pre, post, own = slot_chunks(j)
        xs = np.empty((NSLOT * 128, D), np.float32)
        rope = np.empty((NSLOT * 128, 128), np.float32)
        gmask = np.zeros((NSLOT, 16), np.float32)
        xs[0:256] = ctx[b]
        rope[0:256, 0:64] = 1.0
        rope[0:256, 64:128] = 0.0
        gmask[0:2, 0:8] = 0.0
        gmask[0:2, 8:16] = -1.0
        s = 2
        for kind, lst in (("pre", pre), ("post", post), ("own", own)):
            for ch in lst:
                xs[s * 128:(s + 1) * 128] = x[b, ch * 128:(ch + 1) * 128]
                rope[s * 128:(s + 1) * 128, 0:64] = cos[ch * 128:(ch + 1) * 128]
                rope[s * 128:(s + 1) * 128, 64:128] = sin[ch * 128:(ch + 1) * 128]
                fa = kind in ("pre", "own")
                ba = kind in ("post", "own")
                gmask[s, 0:4] = 0.0 if fa else NEG
                gmask[s, 4:8] = 0.0 if ba else NEG
                gmask[s, 8:12] = -1.0 if fa else 0.0
                gmask[s, 12:16] = -1.0 if ba else 0.0
                s += 1
        assert s == NSLOT
        m = dict(shared)
        m["xs"] = xs
        m["rope"] = rope
        m["gmask"] = np.ascontiguousarray(np.tile(gmask.reshape(1, NSLOT * 16), (128, 1)))
        m["cfm"] = np.ascontiguousarray(np.concatenate([fm(c[b], 16), fm(c_ctx, 16)], axis=1))
        maps.append(m)
    return maps


_CACHE = {}


def kernel(**inputs):
    if "nc" not in _CACHE:
        _CACHE["nc"] = build()
    nc, es, declared = _CACHE["nc"]
    maps = make_in_maps(inputs)
    maps = [{k: v for k, v in m.items() if k in declared} for m in maps]
    res = run_bass_kernel_spmd(nc, maps, core_ids=list(range(8)))
    out = np.empty((2, 4096, D), np.float32)
    for core in range(8):
        b, j = core // 4, core % 4
        out[b, j * 1024:(j + 1) * 1024] = res.results[core]["out"]
    return out
```

```python
import numpy as np
import ml_dtypes
from contextlib import ExitStack
import concourse.bass as bass
import concourse.mybir as mybir
from concourse.bass_utils import run_bass_kernel_spmd

F32 = mybir.dt.float32
BF16 = mybir.dt.bfloat16
ALU = mybir.AluOpType
AF = mybir.ActivationFunctionType
AX = mybir.AxisListType

D = 2048
NCH = 16
NSLOT = 34
NOWN = 8
NOTH = 26
EPS = 1e-6
NEG = -30000.0
CAP = 512
NEXP = 32
Q0, K0, V0, MQ0, MK0, MV0, MO0, MI0, MF0 = 0, 1024, 1280, 1536, 2048, 2560, 3584, 4608, 4616


class Op:
    __slots__ = ("eng", "fn", "deps", "is_dma", "signal", "count", "semkey", "value", "name")

    def __init__(self, eng, fn, is_dma, name=""):
        self.eng = eng
        self.fn = fn
        self.deps = []
        self.is_dma = is_dma
        self.signal = False
        self.count = None
        self.semkey = None
        self.value = None
        self.name = name


class Rec:
    ENG = ["pe", "act", "dve", "pool", "sp"]
    NS = 8

    def __init__(self):
        self.streams = {e: [] for e in self.ENG}
        self.last_w = {}
        self.readers = {}
        self.pending = {e: [] for e in self.ENG}
        self.dma_ops = {e: [] for e in self.ENG}
        self.final_ops = []

    def op(self, eng, fn, reads=(), writes=(), dma=False, name=""):
        o = Op(eng, fn, dma, name)
        deps = []
        for k in reads:
            w = self.last_w.get(k)
            if w is not None:
                if not (w.eng == eng and not w.is_dma and eng == "pe"):
                    deps.append(w)
        for k in writes:
            w = self.last_w.get(k)
            if w is not None and (w.eng != eng or w.is_dma):
                deps.append(w)
            for r in self.readers.get(k, ()):
                if r.eng != eng or r.is_dma or eng != "pe":
                    if r is not o:
                        deps.append(r)
        deps.extend(self.pending[eng])
        self.pending[eng] = []
        if dma:
            lst = self.dma_ops[eng]
            if len(lst) >= self.NS:
                deps.append(lst[len(lst) - self.NS])
            lst.append(o)
        o.deps = deps
        for k in writes:
            self.last_w[k] = o
            self.readers[k] = []
        for k in reads:
            self.readers.setdefault(k, []).append(o)
        self.streams[eng].append(o)
        return o

    def barrier(self):
        lasts = []
        for e in self.ENG:
            if self.streams[e]:
                lasts.append(self.streams[e][-1])
            lasts.extend(self.dma_ops[e][-self.NS:])
        for e in self.ENG:
            self.pending[e] = [o for o in lasts if (o.eng != e or o.is_dma)]
        self.last_w = {}
        self.readers = {}

    def emit(self, nc, block):
        for e in self.ENG:
            for o in self.streams[e]:
                for d in o.deps:
                    d.signal = True
        for o in self.final_ops:
            o.signal = True
        nsem = {}
        for e in self.ENG:
            c = 0
            ndma = 0
            for o in self.streams[e]:
                if o.is_dma:
                    slot = ndma % self.NS
                    o.semkey = ("dma", e, slot)
                    o.value = 16 * (ndma // self.NS + 1)
                    ndma += 1
                    o.signal = True
                elif o.signal:
                    c += 1
                    o.semkey = ("eng", e)
                    o.value = c
        sems = {}

        def sem(key):
            if key not in sems:
                sems[key] = self._es.enter_context(nc.semaphore("s_" + "_".join(str(k) for k in key)))
            return sems[key]

        final_ops = self.final_ops

        def run(ename, eh):
            seen = {}
            for o in self.streams[ename]:
                need = {}
                for d in o.deps:
                    if need.get(d.semkey, 0) < d.value:
                        need[d.semkey] = d.value
                for k, v in need.items():
                    if seen.get(k, 0) < v:
                        eh.wait_ge(sem(k), v)
                        seen[k] = v
                ins = o.fn(eh)
                if o.signal:
                    ins.then_inc(sem(o.semkey), 16 if o.is_dma else 1)
            if ename == "sp":
                for o in final_ops:
                    if seen.get(o.semkey, 0) < o.value:
                        eh.wait_ge(sem(o.semkey), o.value)
                        seen[o.semkey] = o.value

        for e in self.ENG:
            sem(("eng", e))
            for s in range(self.NS):
                if e in ("sp", "pool", "act"):
                    sem(("dma", e, s))

        @block.tensor
        def _(eh):
            run("pe", eh)

        @block.scalar
        def _(eh):
            run("act", eh)

        @block.vector
        def _(eh):
            run("dve", eh)

        @block.gpsimd
        def _(eh):
            run("pool", eh)

        @block.sync
        def _(eh):
            run("sp", eh)


class Arena:
    def __init__(self, t, n):
        self.t = t
        self.n = n
        self.off = 0

    def f(self, cols):
        lo = self.off
        self.off += cols
        assert self.off <= self.n, ("arena overflow", self.off, self.n)
        return self.t[:, lo:lo + cols]

    def b(self, cols):
        c2 = (cols + 1) // 2
        return self.f(c2).bitcast(BF16)[:, 0:cols]


def pbc(ap):
    v = ap.partition_broadcast(128)
    return v[:, 0, :]


def build(debug=None, n_oth=NOTH, n_exp=NEXP):
    nc = bass.Bass("TRN2", target_bir_lowering=False)
    R = Rec()
    es = ExitStack()
    R._es = es
    declared = []
    big = debug is None or debug.startswith("moe")

    def din(name, shape, dt=F32):
        if name in ("w1", "w2") and not big:
            return None
        declared.append(name)
        return nc.dram_tensor(name, list(shape), dt, kind="ExternalInput").ap()

    xs_d = din("xs", [NSLOT * 128, D])
    rope_d = din("rope", [NSLOT * 128, 128])
    gmask_d = din("gmask", [128, NSLOT * 16])
    cfm_d = din("cfm", [128, 32])
    bmod_d = din("bmod", [128, 96])
    g1_d = din("g1fm", [128, 16])
    g2fm_d = din("g2fm", [128, 16])
    wmod_d = din("w_mod", [D, 6 * D])
    win_d = din("w_in", [D, 4624])
    bin_d = din("b_in", [1, 4624])
    bfm_d = din("bfm", [128, 8])
    gq_d = din("g_q", [1, 128])
    gk_d = din("g_k", [1, 128])
    gm_d = din("g_mlstm", [1, 1024])
    wout_d = din("w_out", [D, D])
    g2_d = din("g_norm2", [1, D])
    gf_d = din("g_final", [1, D])
    wr_d = din("w_router", [D, NEXP])
    br_d = din("b_router", [1, NEXP])
    w1_d = din("w1", [NEXP, D, 2 * D])
    b1_d = din("b1fm", [128, NEXP * 32])
    w2_d = din("w2", [NEXP, D, D])
    b2_d = din("b2", [NEXP, D])
    cst_d = din("consts", [128, 6 * 128 + 512 + 1])
    out_d = nc.dram_tensor("out", [NOWN * 128, D], F32, kind="ExternalOutput").ap()
    dbg_d = None
    if debug is not None:
        dbg_d = nc.dram_tensor("dbg", [128, 8192], F32, kind="ExternalOutput").ap()

    NF = 52500
    fa_t = es.enter_context(nc.sbuf_tensor("fa", [128, NF], F32))
    A = Arena(fa_t, NF)
    pT = es.enter_context(nc.psum_tensor("pT", [128, 2048], BF16))
    psum = [None, None] + [es.enter_context(nc.psum_tensor("ps%d" % i, [128, 512], F32)) for i in range(2, 8)]

    def PS(i, lo=0, n=512):
        return psum[i][:, lo:lo + n]

    def pk(i):
        return "ps%d" % i

    def finish():
        with nc.Block() as block:
            R.emit(nc, block)
        return nc, es, declared

    def dump(ap, key, lo, n):
        o = R.op("sp", lambda e: e.dma_start(out=dbg_d[:, lo:lo + n], in_=ap), reads=[key], dma=True)
        R.final_ops.append(o)

    dbgf = None
    if debug is not None:
        dbgf = A.f(512)

    def dump_bf(ap, key, lo, n):
        for p0 in range(0, n, 512):
            m = min(512, n - p0)
            R.op("dve", lambda e, p0=p0, m=m: e.tensor_copy(out=dbgf[:, 0:m], in_=ap[:, p0:p0 + m]), reads=[key], writes=["dbgf"])
            dump(dbgf[:, 0:m], "dbgf", lo + p0, m)

    cst = A.f(6 * 128 + 512 + 1)
    ident = cst[:, 0:128]
    tri_f = cst[:, 128:256]
    tri_b = cst[:, 256:384]
    nm_f = cst[:, 384:512]
    nm_b = cst[:, 512:640]
    ones = cst[:, 640:768]
    iota_c = cst[:, 768:1280]
    iota_p = cst[:, 1280:1281]
    identb = A.b(128)
    onesb = A.b(128)
    modT = A.f(192).rearrange("p (a b) -> p a b", b=2)
    gml = A.f(16)
    gmc = A.f(16)
    g1 = A.f(16)
    cfm = A.f(32)
    bmod = A.f(96)
    bfm = A.f(8)
    csT = A.b(32).rearrange("p (a b) -> p a b", b=2)
    stC = [A.f(257) for _ in range(8)]
    stCb = [A.b(258)[:, 0:257] for _ in range(8)]
    epsb = A.f(2)
    R.op("pool", lambda e: e.memset(epsb[:, 0:1], EPS), writes=["epsb"])
    R.op("pool", lambda e: e.memset(epsb[:, 1:2], 1.0), writes=["epsb"])
    gmask = A.f(NSLOT * 16).rearrange("p (s g) -> p s g", g=16)

    R.op("sp", lambda e: e.dma_start(out=cst, in_=cst_d), writes=["cst"], dma=True)
    R.op("sp", lambda e: e.dma_start(out=cfm, in_=cfm_d), writes=["cfm"], dma=True)
    R.op("sp", lambda e: e.dma_start(out=bmod, in_=bmod_d), writes=["bmod"], dma=True)
    R.op("sp", lambda e: e.dma_start(out=g1, in_=g1_d), writes=["g1"], dma=True)
    R.op("sp", lambda e: e.dma_start(out=bfm, in_=bfm_d), writes=["bfm"], dma=True)
    R.op("sp", lambda e: e.dma_start(out=gmask.rearrange("p s g -> p (s g)"), in_=gmask_d), writes=["gmask"], dma=True)
    R.op("dve", lambda e: e.tensor_copy(out=identb, in_=ident), reads=["cst"], writes=["identb"])
    R.op("dve", lambda e: e.tensor_copy(out=onesb, in_=ones), reads=["cst"], writes=["onesb"])
    for j in range(8):
        R.op("pool", lambda e, j=j: e.memset(stC[j], 0.0), writes=["stC%d" % j])
        R.op("pool", lambda e, j=j: e.memset(stCb[j], 0.0), writes=["stCb%d" % j])

    R.op("act", lambda e: e.activation(out=csT[:, :, 0], in_=cfm[:, 0:16], func=AF.Silu), reads=["cfm"], writes=["csT"])
    R.op("act", lambda e: e.activation(out=csT[:, :, 1], in_=cfm[:, 16:32], func=AF.Silu), reads=["cfm"], writes=["csT"])
    mark0 = A.off
    wm = [A.b(16 * 512).rearrange("p (c n) -> p c n", n=512) for _ in range(2)]
    wmod_v = wmod_d.rearrange("(c p) n -> p c n", p=128)
    PM = psum[7][:, 0:192].rearrange("p (a b) -> p a b", b=2)

    def mod_block(blk):
        buf = wm[blk % 2]
        key = "wm%d" % (blk % 2)
        R.op("pool", lambda e: e.dma_start(out=buf, in_=wmod_v[:, :, blk * 512:(blk + 1) * 512]), writes=[key], dma=True)
        for q in range(4):
            cc = blk * 4 + q
            for c in range(NCH):
                R.op("pe", lambda e, c=c, q=q, cc=cc: e.matmul(PM[:, cc, :], lhsT=buf[:, c, q * 128:(q + 1) * 128], rhs=csT[:, c, :],
                                                             start=(c == 0), stop=(c == NCH - 1)),
                     reads=[key, "csT"], writes=[pk(7)])
        R.op("dve", lambda e: e.tensor_tensor(out=modT[:, blk * 4:blk * 4 + 4, :], in0=PM[:, blk * 4:blk * 4 + 4, :],
                                              in1=bmod[:, blk * 4:blk * 4 + 4].unsqueeze(2).to_broadcast([128, 4, 2]), op=ALU.add),
             reads=[pk(7), "bmod"], writes=["modT"])

    for blk in range(24):
        mod_block(blk)
    R.op("dve", lambda e: e.scalar_tensor_tensor(out=gml, in0=modT[:, 16:32, 0], scalar=1.0, in1=g1, op0=ALU.add, op1=ALU.mult),
         reads=["modT", "g1"], writes=["gml"])
    R.op("dve", lambda e: e.scalar_tensor_tensor(out=gmc, in0=modT[:, 16:32, 1], scalar=1.0, in1=g1, op0=ALU.add, op1=ALU.mult),
         reads=["modT", "g1"], writes=["gmc"])
    if debug == "mod":
        dump(modT.rearrange("p a b -> p (a b)"), "modT", 0, 192)
        dump(gml, "gml", 192, 16)
        return finish()
    R.barrier()
    A.off = mark0

    win_v = win_d.rearrange("(c p) n -> p c n", p=128)
    SC = 128.0 ** -0.5
    WCOLS = 2064
    mark_mix = A.off
    Wt = A.b(16 * WCOLS).rearrange("p (c n) -> p c n", n=WCOLS)
    Wflat = Wt.rearrange("p c n -> p (c n)")
    mark_w_end = A.off
    W_regs = A
    bo = A.f(WCOLS)
    gkb = A.f(128)
    gqb = A.f(128)
    xt0 = A.f(D)
    xt = [xt0, xt0]
    xsb = A.b(D)
    hT = [A.b(D).rearrange("p (c t) -> p c t", t=128) for _ in range(2)]
    kTst = A.b(2 * NSLOT * 128).rearrange("p (g t) -> p g t", g=2)
    Vst = A.b(NSLOT * 256).rearrange("p (s v) -> p s v", v=256)
    sm = A.f(64)
    rp = [A.f(128) for _ in range(2)]
    kf = [A.f(256) for _ in range(2)]
    kn = A.f(256)
    rt = [A.f(128) for _ in range(4)]
    krot = A.b(256)
    NB_ = 2
    Kt = [A.b(512) for _ in range(NB_)]
    Vx = [A.b(4 * 258).rearrange("p (h v) -> p h v", v=258) for _ in range(NB_)]
    Gt = [A.f(16) for _ in range(NB_)]
    cq = [A.f(64) for _ in range(NB_)]
    expb = [A.f(8) for _ in range(NB_)]
    Kw = [A.b(128) for _ in range(2)]

    def load_w(segs):
        off = 0
        for (lo, hi) in segs:
            n = hi - lo
            R.op("pool", lambda e, off=off, lo=lo, hi=hi, n=n: e.dma_start(out=Wt[:, :, off:off + n], in_=win_v[:, :, lo:hi]), writes=["W"], dma=True)
            off += n

    load_w([(K0, K0 + 512), (MK0, MK0 + 1536), (MI0, MI0 + 16)])
    R.op("sp", lambda e: e.dma_start(out=bo[:, 0:512], in_=pbc(bin_d[:, K0:K0 + 512])), writes=["bo"], dma=True)
    R.op("sp", lambda e: e.dma_start(out=bo[:, 512:2048], in_=pbc(bin_d[:, MK0:MK0 + 1536])), writes=["bo"], dma=True)
    R.op("sp", lambda e: e.dma_start(out=bo[:, 2048:2064], in_=pbc(bin_d[:, MI0:MI0 + 16])), writes=["bo"], dma=True)
    R.op("sp", lambda e: e.dma_start(out=gkb, in_=pbc(gk_d)), writes=["gkb"], dma=True)
    R.op("sp", lambda e: e.dma_start(out=gqb, in_=pbc(gq_d)), writes=["gqb"], dma=True)
    R.op("dve", lambda e: e.tensor_scalar(out=bo[:, 512:1024], in0=bo[:, 512:1024], scalar1=SC, scalar2=None, op0=ALU.mult), reads=["bo"], writes=["bo"])
    R.op("dve", lambda e: e.tensor_scalar(out=gqb, in0=gqb, scalar1=SC, scalar2=None, op0=ALU.mult), reads=["gqb"], writes=["gqb"])
    R.op("dve", lambda e: e.tensor_scalar(out=bfm[:, 4:8], in0=bfm[:, 4:8], scalar1=SC, scalar2=None, op0=ALU.mult), reads=["bfm"], writes=["bfm"])
    for b_ in range(NB_):
        R.op("pool", lambda e, b_=b_: e.memset(Vx[b_][:, :, 256:257], 1.0), writes=["Vx%d" % b_])

    def rstd_op(dst, src, n_el, keys_r, key_w):
        R.op("act", lambda e: e.activation(out=dst, in_=src, func=AF.Ln, scale=1.0 / n_el, bias=epsb[:, 0:1]), reads=keys_r + ["epsb"], writes=[key_w])
        R.op("act", lambda e: e.activation(out=dst, in_=dst, func=AF.Exp, scale=-0.5), reads=[key_w], writes=[key_w])

    def make_hT(s, b2=None):
        b2 = s % 2 if b2 is None else b2
        is_ctx = s < 2
        xk = "xt"
        R.op("sp", lambda e: e.dma_start(out=xt[b2], in_=xs_d[s * 128:(s + 1) * 128, :]), writes=[xk], dma=True)
        R.op("pool", lambda e: e.memset(sm[:, b2:b2 + 1], 0.0), writes=["ss%d" % b2])
        jk = hT[b2].rearrange("p c t -> p (c t)")
        R.op("act", lambda e: e.activation(out=jk, in_=xt[b2], func=AF.Square, accum_out=sm[:, b2:b2 + 1]), reads=[xk, "ss%d" % b2], writes=["hT%d" % b2, "ss%d" % b2])
        rstd_op(sm[:, 2 + b2:3 + b2], sm[:, b2:b2 + 1], float(D), ["ss%d" % b2], "rs%d" % b2)
        R.op("dve", lambda e: e.tensor_scalar(out=xsb, in0=xt[b2], scalar1=sm[:, 2 + b2:3 + b2], scalar2=None, op0=ALU.mult),
             reads=[xk, "rs%d" % b2], writes=["xsb"])
        for c in range(NCH):
            R.op("pe", lambda e, c=c: e.transpose(out=pT[:, c * 128:(c + 1) * 128], in_=xsb[:, c * 128:(c + 1) * 128], identity=identb),
                 reads=["xsb", "identb"], writes=["pT%d" % (c // 8)])
        gm = gmc if is_ctx else gml
        w = 1 if is_ctx else 0
        hk = "hT%d" % b2
        for c in range(NCH):
            R.op("act", lambda e, c=c: e.activation(out=hT[b2][:, c, :], in_=pT[:, c * 128:(c + 1) * 128], func=AF.Identity,
                                                   scale=gm[:, c:c + 1], bias=modT[:, c, w:w + 1]),
                 reads=["pT%d" % (c // 8), "gml", "gmc", "modT"], writes=[hk])
        return hT[b2], hk

    def proj_tok(h, hk, col_lo, n, bank, wkey="W", w=None):
        w = Wt if w is None else w
        for c in range(NCH):
            R.op("pe", lambda e, c=c: e.matmul(PS(bank, 0, n), lhsT=h[:, c, :], rhs=w[:, c, col_lo:col_lo + n], start=(c == 0), stop=(c == NCH - 1)),
                 reads=[hk, wkey], writes=[pk(bank)])

    def slot_front(s, db, kv=True):
        h, hk = make_hT(s, db)
        if kv:
            R.op("sp", lambda e: e.dma_start(out=rp[db], in_=rope_d[s * 128:(s + 1) * 128, :]), writes=["rp%d" % db], dma=True)
            proj_tok(h, hk, 0, 512, 2)
        proj_tok(h, hk, 512, 512, 3)
        proj_tok(h, hk, 1024, 512, 4)
        proj_tok(h, hk, 1536, 512, 5)
        proj_tok(h, hk, 2048, 16, 6)
        if kv:
            R.op("dve", lambda e: e.tensor_tensor(out=kf[db], in0=PS(2, 0, 256), in1=bo[:, 0:256], op=ALU.add), reads=[pk(2), "bo"], writes=["kf%d" % db])
            R.op("dve", lambda e: e.tensor_tensor(out=Vst[:, s, :], in0=PS(2, 256, 256), in1=bo[:, 256:512], op=ALU.add), reads=[pk(2), "bo"], writes=["Vst"])
        R.op("dve", lambda e: e.scalar_tensor_tensor(out=Kt[db], in0=PS(3), scalar=SC, in1=bo[:, 512:1024], op0=ALU.mult, op1=ALU.add),
             reads=[pk(3), "bo"], writes=["Kt%d" % db])
        for half in range(2):
            R.op("dve", lambda e, half=half: e.tensor_tensor(out=Vx[db][:, 2 * half:2 * half + 2, 0:256],
                                                             in0=PS(4 + half).rearrange("p (h v) -> p h v", v=256),
                                                             in1=bo[:, 1024 + 512 * half:1536 + 512 * half].rearrange("p (h v) -> p h v", v=256), op=ALU.add),
                 reads=[pk(4 + half), "bo"], writes=["Vx%d" % db])
        R.op("dve", lambda e: e.tensor_tensor(out=Gt[db], in0=PS(6, 0, 16), in1=bo[:, 2048:2064], op=ALU.add), reads=[pk(6), "bo"], writes=["G%d" % db])
        return h, hk

    def slot_back(s, db, kv=True):
        if kv:
            kfb, kfk = kf[db], "kf%d" % db
            R.op("pool", lambda e: e.memset(sm[:, 4:6], 0.0), writes=["kss"])
            for g in range(2):
                R.op("act", lambda e, g=g: e.activation(out=kn[:, g * 128:(g + 1) * 128], in_=kfb[:, g * 128:(g + 1) * 128], func=AF.Square, accum_out=sm[:, 4 + g:5 + g]),
                     reads=[kfk, "kss"], writes=["kn", "kss"])
            rstd_op(sm[:, 8:10], sm[:, 4:6], 128.0, ["kss"], "krs")
            for g in range(2):
                R.op("dve", lambda e, g=g: e.scalar_tensor_tensor(out=kn[:, g * 128:(g + 1) * 128], in0=kfb[:, g * 128:(g + 1) * 128], scalar=sm[:, 8 + g:9 + g],
                                                                  in1=gkb, op0=ALU.mult, op1=ALU.mult), reads=[kfk, "krs", "gkb"], writes=["kn"])
            rope(kn, "kn", krot, "krot", 2, rp[db], "rp%d" % db)
            for g in range(2):
                R.op("pe", lambda e, g=g: e.transpose(out=pT[:, g * 128:(g + 1) * 128], in_=krot[:, g * 128:(g + 1) * 128], identity=identb),
                     reads=["krot", "identb"], writes=["pT0"])
            R.op("act", lambda e: e.activation(out=kTst[:, :, s * 128:(s + 1) * 128], in_=pT[:, 0:256].rearrange("p (g t) -> p g t", g=2), func=AF.Copy),
                 reads=["pT0"], writes=["kTst"])
        chunk_gates(s, db)

    def rope(src, skey, dst, dkey, nh, rpt, rkey):
        v = src.rearrange("p (h i two) -> p h i two", h=nh, two=2)
        o = dst.rearrange("p (h i two) -> p h i two", h=nh, two=2)
        cosb = rpt[:, 0:64].unsqueeze(1).to_broadcast([128, nh, 64])
        sinb = rpt[:, 64:128].unsqueeze(1).to_broadcast([128, nh, 64])
        n = nh * 64
        t = [rt[i][:, 0:n].rearrange("p (h i) -> p h i", h=nh) if n <= 128 else None for i in range(4)]
        if n > 128:
            t = [rtq[i].rearrange("p (h i) -> p h i", h=nh) for i in range(4)]
        x1, x2 = v[:, :, :, 0], v[:, :, :, 1]
        tk = ["rt0", "rt1", "rt2", "rt3"]
        R.op("pool", lambda e: e.tensor_tensor(out=t[0], in0=x1, in1=cosb, op=ALU.mult), reads=[skey, rkey], writes=[tk[0]])
        R.op("pool", lambda e: e.tensor_tensor(out=t[1], in0=x2, in1=sinb, op=ALU.mult), reads=[skey, rkey], writes=[tk[1]])
        R.op("pool", lambda e: e.tensor_tensor(out=t[2], in0=x1, in1=sinb, op=ALU.mult), reads=[skey, rkey], writes=[tk[2]])
        R.op("pool", lambda e: e.tensor_tensor(out=t[3], in0=x2, in1=cosb, op=ALU.mult), reads=[skey, rkey], writes=[tk[3]])
        R.op("dve", lambda e: e.tensor_tensor(out=o[:, :, :, 0], in0=t[0], in1=t[1], op=ALU.subtract), reads=[tk[0], tk[1]], writes=[dkey])
        R.op("dve", lambda e: e.tensor_tensor(out=o[:, :, :, 1], in0=t[2], in1=t[3], op=ALU.add), reads=[tk[2], tk[3]], writes=[dkey])

    def chunk_gates(s, db):
        q_ = cq[db]
        e1, Lf, lgf, ie, imb, gg, wst, dec = [q_[:, 8 * i:8 * i + 8] for i in range(8)]
        ck = "cq%d" % db
        R.op("act", lambda e: e.activation(out=e1, in_=Gt[db][:, 8:16], func=AF.Exp, scale=-1.0), reads=["G%d" % db], writes=[ck + "a"])
        R.op("act", lambda e: e.activation(out=Lf, in_=e1, func=AF.Ln, bias=epsb[:, 1:2]), reads=[ck + "a", "epsb"], writes=[ck + "b"])
        R.op("dve", lambda e: e.tensor_tensor(out=lgf, in0=Lf, in1=gmask[:, s, 8:16], op=ALU.mult), reads=[ck + "b", "gmask"], writes=[ck + "lgf"])
        R.op("dve", lambda e: e.tensor_tensor(out=ie, in0=Gt[db][:, 0:8], in1=gmask[:, s, 0:8], op=ALU.add), reads=["G%d" % db, "gmask"], writes=[ck + "ie"])
        R.op("pe", lambda e: e.matmul(PS(6, 16, 4), lhsT=tri_f, rhs=lgf[:, 0:4], start=True, stop=True), reads=["cst", ck + "lgf"], writes=[pk(6)])
        R.op("pe", lambda e: e.matmul(PS(6, 20, 4), lhsT=tri_b, rhs=lgf[:, 4:8], start=True, stop=True), reads=["cst", ck + "lgf"], writes=[pk(6)])
        R.op("pe", lambda e: e.matmul(PS(6, 24, 8), lhsT=ones, rhs=lgf, start=True, stop=True), reads=["cst", ck + "lgf"], writes=[pk(6)])
        R.op("dve", lambda e: e.tensor_tensor(out=imb, in0=ie, in1=PS(6, 16, 8), op=ALU.subtract), reads=[ck + "ie", pk(6)], writes=[ck + "imb"])
        R.op("dve", lambda e: e.tensor_tensor(out=gg, in0=imb, in1=PS(6, 24, 8), op=ALU.add), reads=[ck + "imb", pk(6)], writes=[ck + "gg"])
        R.op("act", lambda e: e.activation(out=wst, in_=gg, func=AF.Exp), reads=[ck + "gg"], writes=[ck + "wst"])
        R.op("act", lambda e: e.activation(out=dec, in_=PS(6, 24, 8), func=AF.Exp), reads=[pk(6)], writes=[ck + "dec"])
        R.op("act", lambda e: e.activation(out=expb[db], in_=PS(6, 16, 8), func=AF.Exp), reads=[pk(6)], writes=[ck + "expb"])

    def state_step(db, j, refresh_bf=False):
        h = j % 4
        q_ = cq[db]
        wst, dec = q_[:, 48:56], q_[:, 56:64]
        ck = "cq%d" % db
        kb = j % 2
        R.op("dve", lambda e: e.tensor_scalar(out=Kw[kb], in0=Kt[db][:, h * 128:(h + 1) * 128], scalar1=wst[:, j:j + 1], scalar2=None, op0=ALU.mult),
             reads=["Kt%d" % db, ck + "wst"], writes=["Kw%d" % kb])
        R.op("pe", lambda e: e.matmul(PS(7, 0, 257), lhsT=Kw[kb], rhs=Vx[db][:, h, 0:257], start=True, stop=True),
             reads=["Kw%d" % kb, "Vx%d" % db], writes=[pk(7)])
        R.op("dve", lambda e: e.scalar_tensor_tensor(out=stC[j], in0=stC[j], scalar=dec[:, j:j + 1], in1=PS(7, 0, 257), op0=ALU.mult, op1=ALU.add),
             reads=["stC%d" % j, ck + "dec", pk(7)], writes=["stC%d" % j])
        if refresh_bf:
            R.op("act", lambda e: e.activation(out=stCb[j], in_=stC[j], func=AF.Copy), reads=["stC%d" % j], writes=["stCb%d" % j])

    rtq = None
    hacc = A.b(NOWN * 1024).rearrange("p (o h v) -> p o h v", o=NOWN, h=4)
    mark_own = A.off
    Wq = A.b(16 * 512).rearrange("p (c n) -> p c n", n=512)
    R.op("pool", lambda e: e.dma_start(out=Wq, in_=win_v[:, :, MQ0:MQ0 + 512]), writes=["Wq"], dma=True)
    qmT = [A.b(512).rearrange("p (h t) -> p h t", h=4) for _ in range(2)]
    kmT = [A.b(512).rearrange("p (h t) -> p h t", h=4) for _ in range(2)]
    lb = A.f(128)
    DTt = A.f(128)
    STt = A.b(128)
    tmpn = A.f(257)
    tot = A.f(257)
    ddr = A.f(2)

    def full_step(s, db, j, o, first):
        h = j % 4
        dirn = j // 4
        TRI = tri_f if dirn == 0 else tri_b
        NM = nm_f if dirn == 0 else nm_b
        q_ = cq[db]
        lgf, imb = q_[:, 16:24], q_[:, 32:40]
        ck = "cq%d" % db
        R.op("dve", lambda e: e.tensor_scalar(out=lb, in0=ones, scalar1=lgf[:, j:j + 1], scalar2=None, op0=ALU.mult), reads=["cst", ck + "lgf"], writes=["lb"])
        R.op("pe", lambda e: e.matmul(PS(4, 0, 128), lhsT=lb, rhs=TRI, start=True, stop=False), reads=["lb", "cst"], writes=[pk(4)])
        R.op("pe", lambda e: e.matmul(PS(4, 0, 128), lhsT=ident, rhs=NM, start=False, stop=True), reads=["cst"], writes=[pk(4)])
        R.op("act", lambda e: e.activation(out=DTt, in_=PS(4, 0, 128), func=AF.Exp, bias=imb[:, j:j + 1]), reads=[pk(4), ck + "imb"], writes=["DT"])
        R.op("pe", lambda e: e.matmul(PS(5, 0, 128), lhsT=kmT[db][:, h, :], rhs=qmT[db][:, h, :], start=True, stop=True), reads=["kmT%d" % db, "qmT%d" % db], writes=[pk(5)])
        R.op("dve", lambda e: e.tensor_tensor(out=STt, in0=PS(5, 0, 128), in1=DTt, op=ALU.mult), reads=[pk(5), "DT"], writes=["ST"])
        R.op("pe", lambda e: e.matmul(PS(2, 0, 257), lhsT=STt, rhs=Vx[db][:, h, 0:257], start=True, stop=True), reads=["ST", "Vx%d" % db], writes=[pk(2)])
        R.op("pe", lambda e: e.matmul(PS(3, 0, 257), lhsT=qmT[db][:, h, :], rhs=stCb[j], start=True, stop=True), reads=["qmT%d" % db, "stCb%d" % j], writes=[pk(3)])
        R.op("act", lambda e: e.activation(out=tmpn, in_=PS(2, 0, 257), func=AF.Copy), reads=[pk(2)], writes=["tmpn"])
        R.op("dve", lambda e: e.scalar_tensor_tensor(out=tot, in0=PS(3, 0, 257), scalar=expb[db][:, j:j + 1], in1=tmpn, op0=ALU.mult, op1=ALU.add),
             reads=[pk(3), ck + "expb", "tmpn"], writes=["tot"])
        R.op("dve", lambda e: e.scalar_tensor_tensor(out=ddr[:, 0:1], in0=tot[:, 256:257], scalar=-1.0, in1=tot[:, 256:257], op0=ALU.mult, op1=ALU.max), reads=["tot"], writes=["dd"])
        R.op("dve", lambda e: e.tensor_scalar(out=ddr[:, 0:1], in0=ddr[:, 0:1], scalar1=1.0, scalar2=None, op0=ALU.max), reads=["dd"], writes=["dd"])
        R.op("dve", lambda e: e.reciprocal(out=ddr[:, 1:2], in_=ddr[:, 0:1]), reads=["dd"], writes=["rr"])
        hk_ = "hacc%d" % o
        if first:
            R.op("dve", lambda e: e.tensor_scalar(out=hacc[:, o, h, :], in0=tot[:, 0:256], scalar1=ddr[:, 1:2], scalar2=None, op0=ALU.mult), reads=["tot", "rr"], writes=[hk_])
        else:
            R.op("dve", lambda e: e.scalar_tensor_tensor(out=hacc[:, o, h, :], in0=tot[:, 0:256], scalar=ddr[:, 1:2], in1=hacc[:, o, h, :], op0=ALU.mult, op1=ALU.add),
                 reads=["tot", "rr", hk_], writes=[hk_])
        state_step(db, j, refresh_bf=True)


    n_own = NOWN if debug != "own" else 2
    visits = [("oth", s_, None) for s_ in range(n_oth)]
    visits += [("own", NOTH + o_, 0) for o_ in range(n_own)] + [("own", NOTH + o_, 1) for o_ in range(n_own - 1, -1, -1)]

    def v_front(vi):
        kind, s_, dirn = visits[vi]
        db = vi % 2
        if kind == "oth":
            slot_front(s_, db, kv=True)
            return
        h_, hk = slot_front(s_, db, kv=(dirn == 0))
        for hh in range(4):
            for c in range(NCH):
                R.op("pe", lambda e, c=c, hh=hh: e.matmul(PS(2, hh * 128, 128), lhsT=Wq[:, c, hh * 128:(hh + 1) * 128], rhs=h_[:, c, :], start=(c == 0), stop=(c == NCH - 1)),
                     reads=["Wq", hk], writes=[pk(2)])
        for hh in range(4):
            R.op("act", lambda e, hh=hh: e.activation(out=qmT[db][:, hh, :], in_=PS(2, hh * 128, 128), func=AF.Identity, bias=bfm[:, hh:hh + 1]), reads=[pk(2), "bfm"], writes=["qmT%d" % db])
        for hh in range(4):
            for c in range(NCH):
                R.op("pe", lambda e, c=c, hh=hh: e.matmul(PS(3, hh * 128, 128), lhsT=Wt[:, c, 512 + hh * 128:512 + (hh + 1) * 128], rhs=h_[:, c, :], start=(c == 0), stop=(c == NCH - 1)),
                     reads=["W", hk], writes=[pk(3)])
        for hh in range(4):
            R.op("act", lambda e, hh=hh: e.activation(out=kmT[db][:, hh, :], in_=PS(3, hh * 128, 128), func=AF.Identity, scale=SC, bias=bfm[:, 4 + hh:5 + hh]), reads=[pk(3), "bfm"], writes=["kmT%d" % db])

    def v_back(vi):
        kind, s_, dirn = visits[vi]
        db = vi % 2
        if kind == "oth":
            slot_back(s_, db, kv=True)
            if s_ == 0:
                for j in range(4):
                    state_step(0, j)
            elif s_ == 1:
                for j in range(8):
                    state_step(1, j)
                for j in range(4, 8):
                    state_step(0, j)
            else:
                for j in range(8):
                    state_step(db, j)
            return
        if vi == n_oth:
            for j in range(8):
                R.op("act", lambda e, j=j: e.activation(out=stCb[j], in_=stC[j], func=AF.Copy), reads=["stC%d" % j], writes=["stCb%d" % j])
        slot_back(s_, db, kv=(dirn == 0))
        for hh in range(4):
            full_step(s_, db, dirn * 4 + hh, s_ - NOTH, first=(dirn == 0))

    nv = len(visits)
    v_front(0)
    for vi in range(nv):
        if vi + 1 < nv and vi != 1:
            v_front(vi + 1)
        v_back(vi)
        if vi == 1 and vi + 1 < nv:
            v_front(vi + 1)
    if debug == "oth":
        s = n_oth - 1
        dump(hT[s % 2].rearrange("p c t -> p (c t)")[:, 0:0], "x", 0, 0) if False else None
        dump_bf(hT[s % 2].rearrange("p c t -> p (c t)"), "hT%d" % (s % 2), 0, 2048)
        dump_bf(kTst[:, :, s * 128:(s + 1) * 128], "kTst", 2048, 256) if False else None
        dump_bf(Vst[:, s, :], "Vst", 2304, 256)
        dump_bf(Kt[s % 2], "Kt%d" % (s % 2), 2560, 512)
        dump(Gt[s % 2], "G%d" % (s % 2), 3072, 16)
        dump(cq[s % 2], "cq%dwst" % (s % 2), 3088, 64)
        for j in range(8):
            dump(stC[j], "stC%d" % j, 3200 + 257 * j, 257)
        dump_bf(krot, "krot", 5300, 256)
        return finish()


    if debug == "own":
        dump_bf(hacc[:, 0, :, :].rearrange("p h v -> p (h v)"), "hacc0", 0, 1024)
        dump_bf(hacc[:, 1, :, :].rearrange("p h v -> p (h v)"), "hacc1", 1024, 1024)
        return finish()


    R.barrier()
    A.off = mark_own
    Wc = Wflat[:, 0:16 * 1024].rearrange("p (c n) -> p c n", n=1024)
    yT = Wflat[:, 16 * 1024:32 * 1024].rearrange("p (k t) -> p k t", t=1024)
    qf = A.f(1024)
    rtq_all = A.f(2048)
    rtq = [rtq_all[:, i * 512:(i + 1) * 512] for i in range(4)]
    qrot = A.b(1024)
    qTc = A.b(1024).rearrange("p (h t) -> p h t", h=8)
    PTt = [A.b(512) for _ in range(2)]
    dsb = A.f(512)
    qss = A.f(16)
    gmb = A.f(1024)

    def load_wc(lo, wd=win_v):
        R.op("pool", lambda e: e.dma_start(out=Wc, in_=wd[:, :, lo:lo + 1024]), writes=["W"], dma=True)

    load_wc(Q0)
    R.op("sp", lambda e: e.dma_start(out=bo[:, 0:1024], in_=pbc(bin_d[:, Q0:Q0 + 1024])), writes=["bo"], dma=True)
    R.op("sp", lambda e: e.dma_start(out=bo[:, 1024:2048], in_=pbc(bin_d[:, MO0:MO0 + 1024])), writes=["bo"], dma=True)
    R.op("sp", lambda e: e.dma_start(out=gmb, in_=pbc(gm_d)), writes=["gmb"], dma=True)
    qf3 = qf.rearrange("p (h d) -> p h d", h=8)
    for o in range(NOWN):
        s = NOTH + o
        b2 = s % 2
        h_, hk = make_hT(s)
        R.op("sp", lambda e, s=s, b2=b2: e.dma_start(out=rp[b2], in_=rope_d[s * 128:(s + 1) * 128, :]), writes=["rp%d" % b2], dma=True)
        proj_tok(h_, hk, 0, 512, 2, w=Wc)
        proj_tok(h_, hk, 512, 512, 3, w=Wc)
        for hf_ in range(2):
            R.op("dve", lambda e, hf_=hf_: e.tensor_tensor(out=qf[:, hf_ * 512:(hf_ + 1) * 512], in0=PS(2 + hf_), in1=bo[:, hf_ * 512:(hf_ + 1) * 512], op=ALU.add),
                 reads=[pk(2 + hf_), "bo"], writes=["qf"])
        R.op("pool", lambda e: e.memset(qss[:, 0:8], 0.0), writes=["qss"])
        for hh in range(8):
            R.op("act", lambda e, hh=hh: e.activation(out=rtq[0][:, 0:128], in_=qf[:, hh * 128:(hh + 1) * 128], func=AF.Square, accum_out=qss[:, hh:hh + 1]),
                 reads=["qf", "qss"], writes=["rt0", "qss"])
        rstd_op(qss[:, 8:16], qss[:, 0:8], 128.0, ["qss"], "qrs")
        R.op("dve", lambda e: e.tensor_tensor(out=qf3, in0=qf3, in1=qss[:, 8:16].unsqueeze(2).to_broadcast([128, 8, 128]), op=ALU.mult), reads=["qf", "qrs"], writes=["qf"])
        R.op("dve", lambda e: e.tensor_tensor(out=qf3, in0=qf3, in1=gqb.unsqueeze(1).to_broadcast([128, 8, 128]), op=ALU.mult), reads=["qf", "gqb"], writes=["qf"])
        rope(qf, "qf", qrot, "qrot", 8, rp[b2], "rp%d" % b2)
        for hh in range(8):
            R.op("pe", lambda e, hh=hh: e.transpose(out=pT[:, hh * 128:(hh + 1) * 128], in_=qrot[:, hh * 128:(hh + 1) * 128], identity=identb),
                 reads=["qrot", "identb"], writes=["pT0"])
        R.op("act", lambda e: e.activation(out=qTc, in_=pT[:, 0:1024].rearrange("p (h t) -> p h t", h=8), func=AF.Copy), reads=["pT0"], writes=["qTc"])
        for g in range(2):
            def s_mm(sk, g=g):
                sb = 2 + (sk % 2)
                R.op("pe", lambda e: e.matmul(PS(sb), lhsT=kTst[:, g, sk * 128:(sk + 1) * 128], rhs=qTc[:, 4 * g:4 * g + 4, :], start=True, stop=True),
                     reads=["kTst", "qTc"], writes=[pk(sb)])
            s_mm(0)
            for sk in range(NSLOT):
                sb = 2 + (sk % 2)
                pb = sk % 2
                if sk + 1 < NSLOT:
                    s_mm(sk + 1)
                R.op("act", lambda e, sb=sb, pb=pb: e.activation(out=PTt[pb], in_=PS(sb), func=AF.Exp), reads=[pk(sb)], writes=["PT%d" % pb])
                R.op("pe", lambda e, g=g, sk=sk, pb=pb: e.matmul(PS(4 + g), lhsT=Vst[:, sk, g * 128:(g + 1) * 128], rhs=PTt[pb], start=(sk == 0), stop=(sk == NSLOT - 1)),
                     reads=["Vst", "PT%d" % pb], writes=[pk(4 + g)])
                R.op("pe", lambda e, g=g, sk=sk, pb=pb: e.matmul(PS(6 + g), lhsT=onesb, rhs=PTt[pb], start=(sk == 0), stop=(sk == NSLOT - 1)),
                     reads=["onesb", "PT%d" % pb], writes=[pk(6 + g)])
            R.op("act", lambda e, g=g: e.activation(out=dsb, in_=PS(6 + g), func=AF.Copy), reads=[pk(6 + g)], writes=["dsb"])
            R.op("dve", lambda e: e.reciprocal(out=dsb, in_=dsb), reads=["dsb"], writes=["dsb"])
            R.op("dve", lambda e, g=g, o=o: e.tensor_tensor(out=yT[:, 4 * g:4 * g + 4, o * 128:(o + 1) * 128], in0=PS(4 + g).rearrange("p (h t) -> p h t", h=4),
                                                          in1=dsb.rearrange("p (h t) -> p h t", h=4), op=ALU.mult),
                 reads=[pk(4 + g), "dsb"], writes=["yT"])
    if debug == "att":
        for k in range(8):
            dump_bf(yT[:, k, 0:256], "yT", k * 256, 256)
        return finish()

    R.barrier()
    load_wc(MO0)
    hn = rtq_all[:, 0:1024]
    ym = qrot
    hn3 = hn.rearrange("p (h v) -> p h v", h=4)
    for o in range(NOWN):
        s = NOTH + o
        h_, hk = make_hT(s)
        proj_tok(h_, hk, 0, 512, 2, w=Wc)
        proj_tok(h_, hk, 512, 512, 3, w=Wc)
        for hf_ in range(2):
            R.op("dve", lambda e, hf_=hf_: e.tensor_tensor(out=qf[:, hf_ * 512:(hf_ + 1) * 512], in0=PS(2 + hf_), in1=bo[:, 1024 + hf_ * 512:1024 + (hf_ + 1) * 512], op=ALU.add),
                 reads=[pk(2 + hf_), "bo"], writes=["qf"])
        R.op("act", lambda e: e.activation(out=qf, in_=qf, func=AF.Sigmoid), reads=["qf"], writes=["qf"])
        R.op("pool", lambda e: e.memset(qss[:, 0:4], 0.0), writes=["qss"])
        for hh in range(4):
            R.op("act", lambda e, hh=hh, o=o: e.activation(out=hn[:, hh * 256:(hh + 1) * 256], in_=hacc[:, o, hh, :], func=AF.Square, accum_out=qss[:, hh:hh + 1]),
                 reads=["hacc%d" % o, "qss"], writes=["hn", "qss"])
        rstd_op(qss[:, 8:12], qss[:, 0:4], 256.0, ["qss"], "qrs")
        R.op("dve", lambda e, o=o: e.tensor_tensor(out=hn3, in0=hacc[:, o, :, :], in1=qss[:, 8:12].unsqueeze(2).to_broadcast([128, 4, 256]), op=ALU.mult),
             reads=["hacc%d" % o, "qrs"], writes=["hn"])
        R.op("pool", lambda e: e.tensor_tensor(out=hn, in0=hn, in1=gmb, op=ALU.mult), reads=["hn", "gmb"], writes=["hn"])
        R.op("dve", lambda e: e.tensor_tensor(out=ym, in0=hn, in1=qf, op=ALU.mult), reads=["hn", "qf"], writes=["qrot"])
        for k in range(8):
            R.op("pe", lambda e, k=k: e.transpose(out=pT[:, k * 128:(k + 1) * 128], in_=ym[:, k * 128:(k + 1) * 128], identity=identb),
                 reads=["qrot", "identb"], writes=["pT0"])
        R.op("act", lambda e, o=o: e.activation(out=yT[:, 8:16, o * 128:(o + 1) * 128], in_=pT[:, 0:1024].rearrange("p (k t) -> p k t", k=8), func=AF.Copy),
             reads=["pT0"], writes=["yT"])
    if debug == "ym":
        for k in range(8):
            dump_bf(yT[:, 8 + k, 0:256], "yT", k * 256, 256)
        return finish()

    R.barrier()
    A.off = mark_w_end
    x1 = A.f(NOWN * D).rearrange("p (o d) -> p o d", o=NOWN)
    bcA = A.f(1024)
    tmpd = [A.f(512) for _ in range(2)]
    lbm = A.f(128)
    wout_v = wout_d.rearrange("(c p) n -> p c n", p=128)

    def make_bc(dst, key, chunk_lo, nchunks, col, src=None, skey="modT"):
        for c in range(nchunks):
            if src is None:
                vec = modT[:, chunk_lo + c, col:col + 1]
            else:
                vec = src[:, chunk_lo + c:chunk_lo + c + 1]
            R.op("dve", lambda e, vec=vec: e.tensor_scalar(out=lbm, in0=ones, scalar1=vec, scalar2=None, op0=ALU.mult), reads=["cst", skey], writes=["lbm"])
            R.op("pe", lambda e, c=c: e.matmul(PS(2, (c % 4) * 128, 128), lhsT=lbm, rhs=ident, start=True, stop=True), reads=["lbm", "cst"], writes=[pk(2)])
            if c % 4 == 3:
                R.op("act", lambda e, c=c: e.activation(out=dst[:, (c - 3) * 128:(c + 1) * 128], in_=PS(2), func=AF.Copy), reads=[pk(2)], writes=[key])

    for o in range(NOWN):
        s = NOTH + o
        R.op("sp", lambda e, s=s, o=o: e.dma_start(out=x1[:, o, :], in_=xs_d[s * 128:(s + 1) * 128, :]), writes=["x1_%d" % o], dma=True)
    for half in range(2):
        load_wc(half * 1024, wd=wout_v)
        make_bc(bcA, "bcA", 32 + half * 8, 8, 0)
        for o in range(NOWN):
            for dh in range(2):
                for k in range(NCH):
                    R.op("pe", lambda e, o=o, dh=dh, k=k: e.matmul(PS(4 + dh), lhsT=yT[:, k, o * 128:(o + 1) * 128], rhs=Wc[:, k, dh * 512:(dh + 1) * 512],
                                                                  start=(k == 0), stop=(k == NCH - 1)), reads=["yT", "W"], writes=[pk(4 + dh)])
                cols = slice(half * 1024 + dh * 512, half * 1024 + (dh + 1) * 512)
                R.op("dve", lambda e, dh=dh: e.tensor_tensor(out=tmpd[dh], in0=PS(4 + dh), in1=bcA[:, dh * 512:(dh + 1) * 512], op=ALU.mult),
                     reads=[pk(4 + dh), "bcA"], writes=["tmpd%d" % dh])
                R.op("pool", lambda e, o=o, dh=dh, cols=cols: e.tensor_tensor(out=x1[:, o, cols], in0=x1[:, o, cols], in1=tmpd[dh], op=ALU.add),
                     reads=["x1_%d" % o, "tmpd%d" % dh], writes=["x1_%d" % o])
    if debug == "x1":
        for q in range(4):
            dump(x1[:, 0, q * 512:(q + 1) * 512], "x1_0", q * 512, 512)
        for q in range(4):
            dump(x1[:, 7, q * 512:(q + 1) * 512], "x1_7", 2048 + q * 512, 512)
        return finish()

    R.barrier()
    h2T = Wflat[:, 0:16 * 1024].rearrange("p (c t) -> p c t", t=1024)
    wfree = Wflat[:, 16 * 1024:16 * 1024 + 16384].bitcast(F32)
    gm2b = wfree[:, 0:2048]
    sh2b = wfree[:, 2048:4096]
    h2f = wfree[:, 4096:6144]
    h2Tr = wfree[:, 6144:8192].rearrange("p (c t) -> p c t", t=128)
    A.off = mark_w_end + NOWN * D
    sm2 = A.f(8)
    g2fm = A.f(16)
    gm2fm = A.f(16)
    wr = A.f(16 * NEXP).rearrange("p (c e) -> p c e", e=NEXP)
    brb = A.f(NEXP)
    wt = A.f(NOWN * NEXP).rearrange("p (o e) -> p o e", e=NEXP)
    rsm = A.f(64)
    ex = A.f(NEXP)
    msk = A.f(NEXP)
    R.op("sp", lambda e: e.dma_start(out=g2fm, in_=g2fm_d), writes=["g2fm"], dma=True)
    R.op("sp", lambda e: e.dma_start(out=wr, in_=wr_d.rearrange("(c p) e -> p c e", p=128)), writes=["wr"], dma=True)
    R.op("sp", lambda e: e.dma_start(out=brb, in_=pbc(br_d)), writes=["brb"], dma=True)
    R.op("dve", lambda e: e.scalar_tensor_tensor(out=gm2fm, in0=modT[:, 64:80, 0], scalar=1.0, in1=g2fm, op0=ALU.add, op1=ALU.mult), reads=["modT", "g2fm"], writes=["gm2fm"])
    make_bc(gm2b, "gm2b", 0, 16, 0, src=gm2fm, skey="gm2fm")
    make_bc(sh2b, "sh2b", 48, 16, 0)
    lg = rsm[:, 0:32]
    top8 = rsm[:, 32:40]
    for o in range(NOWN):
        xk = "x1_%d" % o
        R.op("pool", lambda e: e.memset(sm2[:, 0:1], 0.0), writes=["ss0"])
        R.op("act", lambda e, o=o: e.activation(out=h2f, in_=x1[:, o, :], func=AF.Square, accum_out=sm2[:, 0:1]), reads=[xk, "ss0"], writes=["h2f", "ss0"])
        rstd_op(sm2[:, 2:3], sm2[:, 0:1], float(D), ["ss0"], "rs0")
        R.op("dve", lambda e, o=o: e.scalar_tensor_tensor(out=h2f, in0=x1[:, o, :], scalar=sm2[:, 2:3], in1=gm2b, op0=ALU.mult, op1=ALU.mult),
             reads=[xk, "rs0", "gm2b"], writes=["h2f"])
        R.op("pool", lambda e: e.tensor_tensor(out=h2f, in0=h2f, in1=sh2b, op=ALU.add), reads=["h2f", "sh2b"], writes=["h2f"])
        for c in range(NCH):
            bk = 4 + (c // 4)
            R.op("pe", lambda e, c=c, bk=bk: e.transpose(out=PS(bk, (c % 4) * 128, 128), in_=h2f[:, c * 128:(c + 1) * 128], identity=ident),
                 reads=["h2f", "cst"], writes=[pk(bk)])
        for q in range(4):
            R.op("act", lambda e, q=q: e.activation(out=h2Tr[:, 4 * q:4 * q + 4, :], in_=PS(4 + q).rearrange("p (c t) -> p c t", t=128), func=AF.Copy),
                 reads=[pk(4 + q)], writes=["h2Tr"])
        R.op("dve", lambda e, o=o: e.tensor_copy(out=h2T[:, :, o * 128:(o + 1) * 128], in_=h2Tr), reads=["h2Tr"], writes=["h2T"])
        for c in range(NCH):
            R.op("pe", lambda e, c=c: e.matmul(PS(3, 0, NEXP), lhsT=h2Tr[:, c, :], rhs=wr[:, c, :], start=(c == 0), stop=(c == NCH - 1)), reads=["h2Tr", "wr"], writes=[pk(3)])
        R.op("dve", lambda e: e.tensor_tensor(out=lg, in0=PS(3, 0, NEXP), in1=brb, op=ALU.add), reads=[pk(3), "brb"], writes=["lg"])
        R.op("dve", lambda e: e.max(out=top8, in_=lg), reads=["lg"], writes=["top8"])
        R.op("dve", lambda e: e.tensor_scalar(out=msk, in0=lg, scalar1=top8[:, 3:4], scalar2=None, op0=ALU.is_ge), reads=["lg", "top8"], writes=["msk"])
        R.op("dve", lambda e: e.tensor_scalar(out=rsm[:, 40:41], in0=top8[:, 0:1], scalar1=-1.0, scalar2=None, op0=ALU.mult), reads=["top8"], writes=["nmx"])
        R.op("act", lambda e: e.activation(out=ex, in_=lg, func=AF.Exp, bias=rsm[:, 40:41]), reads=["lg", "nmx"], writes=["ex"])
        R.op("dve", lambda e: e.tensor_tensor(out=ex, in0=ex, in1=msk, op=ALU.mult), reads=["ex", "msk"], writes=["ex"])
        R.op("dve", lambda e: e.reduce_sum(out=rsm[:, 41:42], in_=ex, axis=AX.X), reads=["ex"], writes=["esum"])
        R.op("dve", lambda e: e.reciprocal(out=rsm[:, 42:43], in_=rsm[:, 41:42]), reads=["esum"], writes=["ersum"])
        R.op("dve", lambda e, o=o: e.tensor_scalar(out=wt[:, o, :], in0=ex, scalar1=rsm[:, 42:43], scalar2=None, op0=ALU.mult), reads=["ex", "ersum"], writes=["wt"])
    if debug == "rt":
        dump(wt.rearrange("p o e -> p (o e)"), "wt", 0, 256)
        dump_bf(h2T[:, 0, 0:256], "h2T", 256, 256)
        return finish()

    R.barrier()
    gt2b = wfree[:, 0:2048]
    ring = [wfree[:, 2048 * (1 + i):2048 * (2 + i)].bitcast(BF16).rearrange("p (c n) -> p c n", n=256) for i in range(3)]
    ring.append(A.b(16 * 256).rearrange("p (c n) -> p c n", n=256))
    actT = A.b(16 * 1024).rearrange("p (f t) -> p f t", t=1024)
    b1e = [A.f(32) for _ in range(2)]
    gtt = [A.f(512) for _ in range(2)]
    sgt = [A.f(512), wr.rearrange("p c e -> p (c e)")]
    utt = [A.f(512), A.f(512)]
    t10 = A.f(256)
    t1 = [t10, t10]
    make_bc(gt2b, "gt2b", 80, 16, 0)
    b2g = actT.rearrange("p f t -> p (f t)")[:, 0:4096].bitcast(F32)
    wtT = actT.rearrange("p f t -> p (f t)")[:, 4096:4096 + 2048].bitcast(F32)
    R.op("sp", lambda e: e.dma_start(out=b2g[0:NEXP, :], in_=b2_d), writes=["b2g"], dma=True)
    for o in range(NOWN):
        R.op("pe", lambda e, o=o: e.transpose(out=PS(2, o * 64, 128)[0:NEXP, :] if False else PS(2 + o // 4, (o % 4) * 128, 128)[0:NEXP, :], in_=wt[:, o, :], identity=ident),
             reads=["wt", "cst"], writes=[pk(2 + o // 4)])
    for q in range(2):
        R.op("act", lambda e, q=q: e.activation(out=wtT[0:NEXP, q * 512:(q + 1) * 512], in_=PS(2 + q)[0:NEXP, :], func=AF.Copy), reads=[pk(2 + q)], writes=["wtT"])
    for o in range(NOWN):
        for dq in range(4):
            bk = 4 + (dq % 2)
            R.op("pe", lambda e, o=o, dq=dq, bk=bk: e.matmul(PS(bk), lhsT=wtT[0:NEXP, o * 128:(o + 1) * 128], rhs=b2g[0:NEXP, dq * 512:(dq + 1) * 512], start=True, stop=True),
                 reads=["wtT", "b2g"], writes=[pk(bk)])
            R.op("dve", lambda e, dq=dq, bk=bk: e.tensor_tensor(out=gtt[dq % 2], in0=PS(bk), in1=gt2b[:, dq * 512:(dq + 1) * 512], op=ALU.mult),
                 reads=[pk(bk), "gt2b"], writes=["gtt%d" % (dq % 2)])
            R.op("pool", lambda e, o=o, dq=dq: e.tensor_tensor(out=x1[:, o, dq * 512:(dq + 1) * 512], in0=x1[:, o, dq * 512:(dq + 1) * 512], in1=gtt[dq % 2], op=ALU.add),
                 reads=["x1_%d" % o, "gtt%d" % (dq % 2)], writes=["x1_%d" % o])
    R.barrier()
    if big:
        w1_v = [w1_d[e_].rearrange("(c p) n -> p c n", p=128) for e_ in range(NEXP)]
        w2_v = [w2_d[e_].rearrange("(c p) n -> p c n", p=128) for e_ in range(NEXP)]
    nld = [0]

    def ring_load(src):
        i = nld[0] % 4
        nld[0] += 1
        R.op("pool", lambda e, i=i: e.dma_start(out=ring[i], in_=src), writes=["ring%d" % i], dma=True)
        return ring[i], "ring%d" % i

    for e_ in range(n_exp if big else 0):
        b1 = b1e[e_ % 2]
        b1k = "b1_%d" % (e_ % 2)
        R.op("sp", lambda e, e_=e_, b1=b1: e.dma_start(out=b1, in_=b1_d[:, e_ * 32:(e_ + 1) * 32]), writes=[b1k], dma=True)
        pend = []
        it = 0
        for u in range(8):
            rg, rgk = ring_load(w1_v[e_][:, :, u * 256:(u + 1) * 256])
            ru, ruk = ring_load(w1_v[e_][:, :, D + u * 256:D + (u + 1) * 256])
            for fq in range(2):
                fc = u * 2 + fq
                for th in range(2):
                    pb = it % 2
                    it += 1
                    for c in range(NCH):
                        R.op("pe", lambda e, c=c, fq=fq, th=th, rg=rg: e.matmul(PS(2 + th), lhsT=rg[:, c, fq * 128:(fq + 1) * 128], rhs=h2T[:, c, th * 512:(th + 1) * 512],
                                                                               start=(c == 0), stop=(c == NCH - 1)), reads=[rgk, "h2T"], writes=[pk(2 + th)])
                    for c in range(NCH):
                        R.op("pe", lambda e, c=c, fq=fq, th=th, ru=ru: e.matmul(PS(4 + th), lhsT=ru[:, c, fq * 128:(fq + 1) * 128], rhs=h2T[:, c, th * 512:(th + 1) * 512],
                                                                               start=(c == 0), stop=(c == NCH - 1)), reads=[ruk, "h2T"], writes=[pk(4 + th)])
                    bg = b1[:, fc:fc + 1]
                    bu = b1[:, 16 + fc:16 + fc + 1]
                    gk_, sk_, uk_ = "gtt%d" % pb, "sgt%d" % pb, "utt%d" % pb
                    R.op("dve", lambda e, th=th, bg=bg, pb=pb: e.tensor_scalar(out=gtt[pb], in0=PS(2 + th), scalar1=bg, scalar2=7.0, op0=ALU.add, op1=ALU.min),
                         reads=[pk(2 + th), b1k], writes=[gk_])
                    R.op("act", lambda e, pb=pb: e.activation(out=sgt[pb], in_=gtt[pb], func=AF.Sigmoid, scale=1.702), reads=[gk_], writes=[sk_])
                    R.op("dve", lambda e, th=th, bu=bu, pb=pb: e.tensor_scalar(out=utt[pb], in0=PS(4 + th), scalar1=bu, scalar2=7.0, op0=ALU.add, op1=ALU.min),
                         reads=[pk(4 + th), b1k], writes=[uk_])
                    R.op("dve", lambda e, pb=pb: e.tensor_scalar(out=utt[pb], in0=utt[pb], scalar1=-7.0, scalar2=1.0, op0=ALU.max, op1=ALU.add), reads=[uk_], writes=[uk_])
                    R.op("dve", lambda e, pb=pb: e.tensor_tensor(out=gtt[pb], in0=gtt[pb], in1=sgt[pb], op=ALU.mult), reads=[gk_, sk_], writes=[gk_])
                    for p_ in pend:
                        p_()
                    pend = [lambda th=th, fc=fc, pb=pb, gk_=gk_, uk_=uk_: R.op(
                        "dve", lambda e: e.tensor_tensor(out=actT[:, fc, th * 512:(th + 1) * 512], in0=utt[pb], in1=gtt[pb], op=ALU.mult),
                        reads=[uk_, gk_], writes=["actT"])]
        for p_ in pend:
            p_()
        for u in range(8):
            r2, r2k = ring_load(w2_v[e_][:, :, u * 256:(u + 1) * 256])
            for o in range(NOWN):
                bk = 6 + (o % 2)
                for fc in range(NCH):
                    R.op("pe", lambda e, o=o, fc=fc, bk=bk, r2=r2: e.matmul(PS(bk, 0, 256), lhsT=actT[:, fc, o * 128:(o + 1) * 128], rhs=r2[:, fc, :],
                                                                           start=(fc == 0), stop=(fc == NCH - 1)), reads=["actT", r2k], writes=[pk(bk)])
                cols = slice(u * 256, (u + 1) * 256)
                R.op("dve", lambda e, o=o, bk=bk, cols=cols: e.tensor_tensor(out=t1[o % 2], in0=PS(bk, 0, 256), in1=gt2b[:, cols], op=ALU.mult),
                     reads=[pk(bk), "gt2b"], writes=["t1"])
                R.op("dve", lambda e, o=o, cols=cols, e_=e_: e.scalar_tensor_tensor(out=x1[:, o, cols], in0=t1[o % 2], scalar=wt[:, o, e_:e_ + 1], in1=x1[:, o, cols],
                                                                                  op0=ALU.mult, op1=ALU.add),
                     reads=["t1", "wt", "x1_%d" % o], writes=["x1_%d" % o])
    R.barrier()
    gfb = actT.rearrange("p f t -> p (f t)")[:, 0:4096].bitcast(F32)
    R.op("sp", lambda e: e.dma_start(out=gfb, in_=pbc(gf_d)), writes=["gfb"], dma=True)
    for o in range(NOWN):
        xk = "x1_%d" % o
        R.op("pool", lambda e: e.memset(sm2[:, 0:1], 0.0), writes=["ss0"])
        R.op("act", lambda e, o=o: e.activation(out=h2f, in_=x1[:, o, :], func=AF.Square, accum_out=sm2[:, 0:1]), reads=[xk, "ss0"], writes=["h2f", "ss0"])
        rstd_op(sm2[:, 2:3], sm2[:, 0:1], float(D), ["ss0"], "rs0")
        R.op("dve", lambda e, o=o: e.scalar_tensor_tensor(out=x1[:, o, :], in0=x1[:, o, :], scalar=sm2[:, 2:3], in1=gfb, op0=ALU.mult, op1=ALU.mult),
             reads=[xk, "rs0", "gfb"], writes=[xk])
        oo = R.op("sp", lambda e, o=o: e.dma_start(out=out_d[o * 128:(o + 1) * 128, :], in_=x1[:, o, :]), reads=[xk], dma=True)
        R.final_ops.append(oo)
    if debug is not None and debug.startswith("moe"):
        for q in range(4):
            dump(x1[:, 0, q * 512:(q + 1) * 512], "x1_0", q * 512, 512)
    return finish()


def _consts():
    r = np.arange(128)
    ident = np.eye(128, dtype=np.float32)
    tri_f = (r[:, None] <= r[None, :]).astype(np.float32)
    tri_b = (r[:, None] >= r[None, :]).astype(np.float32)
    nm_f = np.where(r[:, None] <= r[None, :], 0.0, NEG).astype(np.float32)
    nm_b = np.where(r[:, None] >= r[None, :], 0.0, NEG).astype(np.float32)
    ones = np.ones((128, 128), np.float32)
    iota_c = np.tile(np.arange(512, dtype=np.float32)[None, :], (128, 1))
    iota_p = r.astype(np.float32)[:, None]
    return np.ascontiguousarray(np.concatenate([ident, tri_f, tri_b, nm_f, nm_b, ones, iota_c, iota_p], axis=1))


def _rope_tables():
    rows = 64
    row = np.repeat(np.arange(rows, dtype=np.float32), 64)
    col = np.tile(np.arange(64, dtype=np.float32), rows)
    inv = (np.float32(10000.0) ** (-np.arange(0, 64, 2, dtype=np.float32) / np.float32(64))).astype(np.float32)
    ang = np.concatenate([row[:, None] * inv, col[:, None] * inv], axis=-1).astype(np.float32)
    return np.cos(ang).astype(np.float32), np.sin(ang).astype(np.float32)


def slot_chunks(j):
    pre = list(range(0, 8 * j))
    post = list(range(31, 8 * j + 7, -1))
    own = list(range(8 * j, 8 * j + 8))
    return pre, post, own


def make_in_maps(inp):
    f = lambda a: np.ascontiguousarray(np.asarray(a, dtype=np.float32))
    x, c, ctx, c_ctx = f(inp["x"]), f(inp["c"]), f(inp["ctx"]), f(inp["c_ctx"])
    cos, sin = _rope_tables()
    consts = _consts()
    fm = lambda v, n: np.ascontiguousarray(v.reshape(n, 128).T)
    shared = {
        "bmod": fm(f(inp["b_mod"])[0], 96),
        "g1fm": fm(f(inp["g_norm1"])[0], 16),
        "g2fm": fm(f(inp["g_norm2"])[0], 16),
        "w_mod": f(inp["w_mod"])[0],
        "w_in": f(inp["w_in"])[0],
        "b_in": f(inp["b_in"]),
        "bfm": np.ascontiguousarray(np.concatenate([fm(f(inp["b_in"])[0, MQ0:MQ0 + 512], 4), fm(f(inp["b_in"])[0, MK0:MK0 + 512], 4)], axis=1)),
        "g_q": f(inp["g_q"]), "g_k": f(inp["g_k"]), "g_mlstm": f(inp["g_mlstm"]),
        "w_out": f(inp["w_out"])[0],
        "g_norm2": f(inp["g_norm2"]), "g_final": f(inp["g_final"])[None, :],
        "w_router": f(inp["w_router"])[0], "b_router": f(inp["b_router"]),
        "w1": inp["w1"],
        "b1fm": np.ascontiguousarray(f(inp["b1"])[0].reshape(NEXP, 32, 128).transpose(2, 0, 1).reshape(128, NEXP * 32)),
        "w2": inp["w2"], "b2": f(inp["b2"])[0],
        "consts": consts,
    }
    maps = []
    for core in range(8):
        b, j = core // 4, core % 4
        pre, post, own = slot_chunks(j)
        xs = np.empty((NSLOT * 128, D), np.float32)
        rope = np.empty((NSLOT * 128, 128), np.float32)
        gmask = np.zeros((NSLOT, 16), np.float32)
        xs[0:256] = ctx[b]
        rope[0:256, 0:64] = 1.0
        rope[0:256, 64:128] = 0.0
        gmask[0:2, 0:8] = 0.0
        gmask[0:2, 8:16] = -1.0
        s = 2
        for kind, lst in (("pre", pre), ("post", post), ("own", own)):
            for ch in lst:
                xs[s * 128:(s + 1) * 128] = x[b, ch * 128:(ch + 1) * 128]
                rope[s * 128:(s + 1) * 128, 0:64] = cos[ch * 128:(ch + 1) * 128]
                rope[s * 128:(s + 1) * 128, 64:128] = sin[ch * 128:(ch + 1) * 128]
                fa = kind in ("pre", "own")
                ba = kind in ("post", "own")
                gmask[s, 0:4] = 0.0 if fa else NEG
                gmask[s, 4:8] = 0.0 if ba else NEG
                gmask[s, 8:12] = -1.0 if fa else 0.0
                gmask[s, 12:16] = -1.0 if ba else 0.0
                s += 1
        assert s == NSLOT
        m = dict(shared)
        m["xs"] = xs
        m["rope"] = rope
        m["gmask"] = np.ascontiguousarray(np.tile(gmask.reshape(1, NSLOT * 16), (128, 1)))
        m["cfm"] = np.ascontiguousarray(np.concatenate([fm(c[b], 16), fm(c_ctx, 16)], axis=1))
        maps.append(m)
    return maps


_CACHE = {}


def kernel(**inputs):
    if "nc" not in _CACHE:
        _CACHE["nc"] = build()
    nc, es, declared = _CACHE["nc"]
    maps = make_in_maps(inputs)
    maps = [{k: v for k, v in m.items() if k in declared} for m in maps]
    res = run_bass_kernel_spmd(nc, maps, core_ids=list(range(8)))
    out = np.empty((2, 4096, D), np.float32)
    for core in range(8):
        b, j = core // 4, core % 4
        out[b, j * 1024:(j + 1) * 1024] = res.results[core]["out"]
    return out
```

```python
import numpy as np
import ml_dtypes
from contextlib import ExitStack
import concourse.bass as bass
import concourse.mybir as mybir
from concourse.bass_utils import run_bass_kernel_spmd

F32 = mybir.dt.float32
BF16 = mybir.dt.bfloat16
ALU = mybir.AluOpType
AF = mybir.ActivationFunctionType
AX = mybir.AxisListType

D = 2048
NCH = 16
NSLOT = 34
NOWN = 8
NOTH = 26
EPS = 1e-6
NEG = -30000.0
CAP = 512
NEXP = 32
Q0, K0, V0, MQ0, MK0, MV0, MO0, MI0, MF0 = 0, 1024, 1280, 1536, 2048, 2560, 3584, 4608, 4616


class Op:
    __slots__ = ("eng", "fn", "deps", "is_dma", "signal", "count", "semkey", "value", "name")

    def __init__(self, eng, fn, is_dma, name=""):
        self.eng = eng
        self.fn = fn
        self.deps = []
        self.is_dma = is_dma
        self.signal = False
        self.count = None
        self.semkey = None
        self.value = None
        self.name = name


class Rec:
    ENG = ["pe", "act", "dve", "pool", "sp"]
    NS = 8

    def __init__(self):
        self.streams = {e: [] for e in self.ENG}
        self.last_w = {}
        self.readers = {}
        self.pending = {e: [] for e in self.ENG}
        self.dma_ops = {e: [] for e in self.ENG}
        self.final_ops = []

    def op(self, eng, fn, reads=(), writes=(), dma=False, name=""):
        o = Op(eng, fn, dma, name)
        deps = []
        for k in reads:
            w = self.last_w.get(k)
            if w is not None:
                if not (w.eng == eng and not w.is_dma and eng == "pe"):
                    deps.append(w)
        for k in writes:
            w = self.last_w.get(k)
            if w is not None and (w.eng != eng or w.is_dma):
                deps.append(w)
            for r in self.readers.get(k, ()):
                if r.eng != eng or r.is_dma or eng != "pe":
                    if r is not o:
                        deps.append(r)
        deps.extend(self.pending[eng])
        self.pending[eng] = []
        if dma:
            lst = self.dma_ops[eng]
            if len(lst) >= self.NS:
                deps.append(lst[len(lst) - self.NS])
            lst.append(o)
        o.deps = deps
        for k in writes:
            self.last_w[k] = o
            self.readers[k] = []
        for k in reads:
            self.readers.setdefault(k, []).append(o)
        self.streams[eng].append(o)
        return o

    def barrier(self):
        lasts = []
        for e in self.ENG:
            if self.streams[e]:
                lasts.append(self.streams[e][-1])
            lasts.extend(self.dma_ops[e][-self.NS:])
        for e in self.ENG:
            self.pending[e] = [o for o in lasts if (o.eng != e or o.is_dma)]
        self.last_w = {}
        self.readers = {}

    def emit(self, nc, block):
        for e in self.ENG:
            for o in self.streams[e]:
                for d in o.deps:
                    d.signal = True
        for o in self.final_ops:
            o.signal = True
        nsem = {}
        for e in self.ENG:
            c = 0
            ndma = 0
            for o in self.streams[e]:
                if o.is_dma:
                    slot = ndma % self.NS
                    o.semkey = ("dma", e, slot)
                    o.value = 16 * (ndma // self.NS + 1)
                    ndma += 1
                    o.signal = True
                elif o.signal:
                    c += 1
                    o.semkey = ("eng", e)
                    o.value = c
        sems = {}

        def sem(key):
            if key not in sems:
                sems[key] = self._es.enter_context(nc.semaphore("s_" + "_".join(str(k) for k in key)))
            return sems[key]

        final_ops = self.final_ops

        def run(ename, eh):
            seen = {}
            for o in self.streams[ename]:
                need = {}
                for d in o.deps:
                    if need.get(d.semkey, 0) < d.value:
                        need[d.semkey] = d.value
                for k, v in need.items():
                    if seen.get(k, 0) < v:
                        eh.wait_ge(sem(k), v)
                        seen[k] = v
                ins = o.fn(eh)
                if o.signal:
                    ins.then_inc(sem(o.semkey), 16 if o.is_dma else 1)
            if ename == "sp":
                for o in final_ops:
                    if seen.get(o.semkey, 0) < o.value:
                        eh.wait_ge(sem(o.semkey), o.value)
                        seen[o.semkey] = o.value

        for e in self.ENG:
            sem(("eng", e))
            for s in range(self.NS):
                if e in ("sp", "pool", "act"):
                    sem(("dma", e, s))

        @block.tensor
        def _(eh):
            run("pe", eh)

        @block.scalar
        def _(eh):
            run("act", eh)

        @block.vector
        def _(eh):
            run("dve", eh)

        @block.gpsimd
        def _(eh):
            run("pool", eh)

        @block.sync
        def _(eh):
            run("sp", eh)


class Arena:
    def __init__(self, t, n):
        self.t = t
        self.n = n
        self.off = 0

    def f(self, cols):
        lo = self.off
        self.off += cols
        assert self.off <= self.n, ("arena overflow", self.off, self.n)
        return self.t[:, lo:lo + cols]

    def b(self, cols):
        c2 = (cols + 1) // 2
        return self.f(c2).bitcast(BF16)[:, 0:cols]


def pbc(ap):
    v = ap.partition_broadcast(128)
    return v[:, 0, :]


def build(debug=None, n_oth=NOTH, n_exp=NEXP):
    nc = bass.Bass("TRN2", target_bir_lowering=False)
    R = Rec()
    es = ExitStack()
    R._es = es
    declared = []
    big = debug is None or debug.startswith("moe")

    def din(name, shape, dt=F32):
        if name in ("w1", "w2") and not big:
            return None
        declared.append(name)
        return nc.dram_tensor(name, list(shape), dt, kind="ExternalInput").ap()

    xs_d = din("xs", [NSLOT * 128, D])
    rope_d = din("rope", [NSLOT * 128, 128])
    gmask_d = din("gmask", [128, NSLOT * 16])
    cfm_d = din("cfm", [128, 32])
    bmod_d = din("bmod", [128, 96])
    g1_d = din("g1fm", [128, 16])
    g2fm_d = din("g2fm", [128, 16])
    wmod_d = din("w_mod", [D, 6 * D])
    win_d = din("w_in", [D, 4624])
    bin_d = din("b_in", [1, 4624])
    bfm_d = din("bfm", [128, 8])
    gq_d = din("g_q", [1, 128])
    gk_d = din("g_k", [1, 128])
    gm_d = din("g_mlstm", [1, 1024])
    wout_d = din("w_out", [D, D])
    g2_d = din("g_norm2", [1, D])
    gf_d = din("g_final", [1, D])
    wr_d = din("w_router", [D, NEXP])
    br_d = din("b_router", [1, NEXP])
    w1_d = din("w1", [NEXP, D, 2 * D])
    b1_d = din("b1fm", [128, NEXP * 32])
    w2_d = din("w2", [NEXP, D, D])
    b2_d = din("b2", [NEXP, D])
    cst_d = din("consts", [128, 6 * 128 + 512 + 1])
    out_d = nc.dram_tensor("out", [NOWN * 128, D], F32, kind="ExternalOutput").ap()
    dbg_d = None
    if debug is not None:
        dbg_d = nc.dram_tensor("dbg", [128, 8192], F32, kind="ExternalOutput").ap()

    NF = 52500
    fa_t = es.enter_context(nc.sbuf_tensor("fa", [128, NF], F32))
    A = Arena(fa_t, NF)
    pT = es.enter_context(nc.psum_tensor("pT", [128, 2048], BF16))
    psum = [None, None] + [es.enter_context(nc.psum_tensor("ps%d" % i, [128, 512], F32)) for i in range(2, 8)]

    def PS(i, lo=0, n=512):
        return psum[i][:, lo:lo + n]

    def pk(i):
        return "ps%d" % i

    def finish():
        with nc.Block() as block:
            R.emit(nc, block)
        return nc, es, declared

    def dump(ap, key, lo, n):
        o = R.op("sp", lambda e: e.dma_start(out=dbg_d[:, lo:lo + n], in_=ap), reads=[key], dma=True)
        R.final_ops.append(o)

    dbgf = None
    if debug is not None:
        dbgf = A.f(512)

    def dump_bf(ap, key, lo, n):
        for p0 in range(0, n, 512):
            m = min(512, n - p0)
            R.op("dve", lambda e, p0=p0, m=m: e.tensor_copy(out=dbgf[:, 0:m], in_=ap[:, p0:p0 + m]), reads=[key], writes=["dbgf"])
            dump(dbgf[:, 0:m], "dbgf", lo + p0, m)

    cst = A.f(6 * 128 + 512 + 1)
    ident = cst[:, 0:128]
    tri_f = cst[:, 128:256]
    tri_b = cst[:, 256:384]
    nm_f = cst[:, 384:512]
    nm_b = cst[:, 512:640]
    ones = cst[:, 640:768]
    iota_c = cst[:, 768:1280]
    iota_p = cst[:, 1280:1281]
    identb = A.b(128)
    onesb = A.b(128)
    modT = A.f(192).rearrange("p (a b) -> p a b", b=2)
    gml = A.f(16)
    gmc = A.f(16)
    g1 = A.f(16)
    cfm = A.f(32)
    bmod = A.f(96)
    bfm = A.f(8)
    csT = A.b(32).rearrange("p (a b) -> p a b", b=2)
    stC = [A.f(257) for _ in range(8)]
    stCb = [A.b(258)[:, 0:257] for _ in range(8)]
    epsb = A.f(2)
    R.op("pool", lambda e: e.memset(epsb[:, 0:1], EPS), writes=["epsb"])
    R.op("pool", lambda e: e.memset(epsb[:, 1:2], 1.0), writes=["epsb"])
    gmask = A.f(NSLOT * 16).rearrange("p (s g) -> p s g", g=16)

    R.op("sp", lambda e: e.dma_start(out=cst, in_=cst_d), writes=["cst"], dma=True)
    R.op("sp", lambda e: e.dma_start(out=cfm, in_=cfm_d), writes=["cfm"], dma=True)
    R.op("sp", lambda e: e.dma_start(out=bmod, in_=bmod_d), writes=["bmod"], dma=True)
    R.op("sp", lambda e: e.dma_start(out=g1, in_=g1_d), writes=["g1"], dma=True)
    R.op("sp", lambda e: e.dma_start(out=bfm, in_=bfm_d), writes=["bfm"], dma=True)
    R.op("sp", lambda e: e.dma_start(out=gmask.rearrange("p s g -> p (s g)"), in_=gmask_d), writes=["gmask"], dma=True)
    R.op("dve", lambda e: e.tensor_copy(out=identb, in_=ident), reads=["cst"], writes=["identb"])
    R.op("dve", lambda e: e.tensor_copy(out=onesb, in_=ones), reads=["cst"], writes=["onesb"])
    for j in range(8):
        R.op("pool", lambda e, j=j: e.memset(stC[j], 0.0), writes=["stC%d" % j])
        R.op("pool", lambda e, j=j: e.memset(stCb[j], 0.0), writes=["stCb%d" % j])

    R.op("act", lambda e: e.activation(out=csT[:, :, 0], in_=cfm[:, 0:16], func=AF.Silu), reads=["cfm"], writes=["csT"])
    R.op("act", lambda e: e.activation(out=csT[:, :, 1], in_=cfm[:, 16:32], func=AF.Silu), reads=["cfm"], writes=["csT"])
    mark0 = A.off
    wm = [A.b(16 * 512).rearrange("p (c n) -> p c n", n=512) for _ in range(2)]
    wmod_v = wmod_d.rearrange("(c p) n -> p c n", p=128)
    PM = psum[7][:, 0:192].rearrange("p (a b) -> p a b", b=2)

    def mod_block(blk):
        buf = wm[blk % 2]
        key = "wm%d" % (blk % 2)
        R.op("pool", lambda e: e.dma_start(out=buf, in_=wmod_v[:, :, blk * 512:(blk + 1) * 512]), writes=[key], dma=True)
        for q in range(4):
            cc = blk * 4 + q
            for c in range(NCH):
                R.op("pe", lambda e, c=c, q=q, cc=cc: e.matmul(PM[:, cc, :], lhsT=buf[:, c, q * 128:(q + 1) * 128], rhs=csT[:, c, :],
                                                             start=(c == 0), stop=(c == NCH - 1)),
                     reads=[key, "csT"], writes=[pk(7)])
        R.op("dve", lambda e: e.tensor_tensor(out=modT[:, blk * 4:blk * 4 + 4, :], in0=PM[:, blk * 4:blk * 4 + 4, :],
                                              in1=bmod[:, blk * 4:blk * 4 + 4].unsqueeze(2).to_broadcast([128, 4, 2]), op=ALU.add),
             reads=[pk(7), "bmod"], writes=["modT"])

    for blk in range(24):
        mod_block(blk)
    R.op("dve", lambda e: e.scalar_tensor_tensor(out=gml, in0=modT[:, 16:32, 0], scalar=1.0, in1=g1, op0=ALU.add, op1=ALU.mult),
         reads=["modT", "g1"], writes=["gml"])
    R.op("dve", lambda e: e.scalar_tensor_tensor(out=gmc, in0=modT[:, 16:32, 1], scalar=1.0, in1=g1, op0=ALU.add, op1=ALU.mult),
         reads=["modT", "g1"], writes=["gmc"])
    if debug == "mod":
        dump(modT.rearrange("p a b -> p (a b)"), "modT", 0, 192)
        dump(gml, "gml", 192, 16)
        return finish()
    R.barrier()
    A.off = mark0

    win_v = win_d.rearrange("(c p) n -> p c n", p=128)
    SC = 128.0 ** -0.5
    WCOLS = 2064
    mark_mix = A.off
    Wt = A.b(16 * WCOLS).rearrange("p (c n) -> p c n", n=WCOLS)
    Wflat = Wt.rearrange("p c n -> p (c n)")
    mark_w_end = A.off
    W_regs = A
    bo = A.f(WCOLS)
    gkb = A.f(128)
    gqb = A.f(128)
    xt0 = A.f(D)
    xt = [xt0, xt0]
    xsb = A.b(D)
    hT = [A.b(D).rearrange("p (c t) -> p c t", t=128) for _ in range(2)]
    kTst = A.b(2 * NSLOT * 128).rearrange("p (g t) -> p g t", g=2)
    Vst = A.b(NSLOT * 256).rearrange("p (s v) -> p s v", v=256)
    sm = A.f(64)
    rp = [A.f(128) for _ in range(2)]
    kf = [A.f(256) for _ in range(2)]
    kn = A.f(256)
    rt = [A.f(128) for _ in range(4)]
    krot = A.b(256)
    NB_ = 2
    Kt = [A.b(512) for _ in range(NB_)]
    Vx = [A.b(4 * 258).rearrange("p (h v) -> p h v", v=258) for _ in range(NB_)]
    Gt = [A.f(16) for _ in range(NB_)]
    cq = [A.f(64) for _ in range(NB_)]
    expb = [A.f(8) for _ in range(NB_)]
    Kw = [A.b(128) for _ in range(2)]

    def load_w(segs):
        off = 0
        for (lo, hi) in segs:
            n = hi - lo
            R.op("pool", lambda e, off=off, lo=lo, hi=hi, n=n: e.dma_start(out=Wt[:, :, off:off + n], in_=win_v[:, :, lo:hi]), writes=["W"], dma=True)
            off += n

    load_w([(K0, K0 + 512), (MK0, MK0 + 1536), (MI0, MI0 + 16)])
    R.op("sp", lambda e: e.dma_start(out=bo[:, 0:512], in_=pbc(bin_d[:, K0:K0 + 512])), writes=["bo"], dma=True)
    R.op("sp", lambda e: e.dma_start(out=bo[:, 512:2048], in_=pbc(bin_d[:, MK0:MK0 + 1536])), writes=["bo"], dma=True)
    R.op("sp", lambda e: e.dma_start(out=bo[:, 2048:2064], in_=pbc(bin_d[:, MI0:MI0 + 16])), writes=["bo"], dma=True)
    R.op("sp", lambda e: e.dma_start(out=gkb, in_=pbc(gk_d)), writes=["gkb"], dma=True)
    R.op("sp", lambda e: e.dma_start(out=gqb, in_=pbc(gq_d)), writes=["gqb"], dma=True)
    R.op("dve", lambda e: e.tensor_scalar(out=bo[:, 512:1024], in0=bo[:, 512:1024], scalar1=SC, scalar2=None, op0=ALU.mult), reads=["bo"], writes=["bo"])
    R.op("dve", lambda e: e.tensor_scalar(out=gqb, in0=gqb, scalar1=SC, scalar2=None, op0=ALU.mult), reads=["gqb"], writes=["gqb"])
    R.op("dve", lambda e: e.tensor_scalar(out=bfm[:, 4:8], in0=bfm[:, 4:8], scalar1=SC, scalar2=None, op0=ALU.mult), reads=["bfm"], writes=["bfm"])
    for b_ in range(NB_):
        R.op("pool", lambda e, b_=b_: e.memset(Vx[b_][:, :, 256:257], 1.0), writes=["Vx%d" % b_])

    def rstd_op(dst, src, n_el, keys_r, key_w):
        R.op("act", lambda e: e.activation(out=dst, in_=src, func=AF.Ln, scale=1.0 / n_el, bias=epsb[:, 0:1]), reads=keys_r + ["epsb"], writes=[key_w])
        R.op("act", lambda e: e.activation(out=dst, in_=dst, func=AF.Exp, scale=-0.5), reads=[key_w], writes=[key_w])

    def make_hT(s, b2=None):
        b2 = s % 2 if b2 is None else b2
        is_ctx = s < 2
        xk = "xt"
        R.op("sp", lambda e: e.dma_start(out=xt[b2], in_=xs_d[s * 128:(s + 1) * 128, :]), writes=[xk], dma=True)
        R.op("pool", lambda e: e.memset(sm[:, b2:b2 + 1], 0.0), writes=["ss%d" % b2])
        jk = hT[b2].rearrange("p c t -> p (c t)")
        R.op("act", lambda e: e.activation(out=jk, in_=xt[b2], func=AF.Square, accum_out=sm[:, b2:b2 + 1]), reads=[xk, "ss%d" % b2], writes=["hT%d" % b2, "ss%d" % b2])
        rstd_op(sm[:, 2 + b2:3 + b2], sm[:, b2:b2 + 1], float(D), ["ss%d" % b2], "rs%d" % b2)
        R.op("dve", lambda e: e.tensor_scalar(out=xsb, in0=xt[b2], scalar1=sm[:, 2 + b2:3 + b2], scalar2=None, op0=ALU.mult),
             reads=[xk, "rs%d" % b2], writes=["xsb"])
        for c in range(NCH):
            R.op("pe", lambda e, c=c: e.transpose(out=pT[:, c * 128:(c + 1) * 128], in_=xsb[:, c * 128:(c + 1) * 128], identity=identb),
                 reads=["xsb", "identb"], writes=["pT%d" % (c // 8)])
        gm = gmc if is_ctx else gml
        w = 1 if is_ctx else 0
        hk = "hT%d" % b2
        for c in range(NCH):
            R.op("act", lambda e, c=c: e.activation(out=hT[b2][:, c, :], in_=pT[:, c * 128:(c + 1) * 128], func=AF.Identity,
                                                   scale=gm[:, c:c + 1], bias=modT[:, c, w:w + 1]),
                 reads=["pT%d" % (c // 8), "gml", "gmc", "modT"], writes=[hk])
        return hT[b2], hk

    def proj_tok(h, hk, col_lo, n, bank, wkey="W", w=None):
        w = Wt if w is None else w
        for c in range(NCH):
            R.op("pe", lambda e, c=c: e.matmul(PS(bank, 0, n), lhsT=h[:, c, :], rhs=w[:, c, col_lo:col_lo + n], start=(c == 0), stop=(c == NCH - 1)),
                 reads=[hk, wkey], writes=[pk(bank)])

    def slot_front(s, db, kv=True):
        h, hk = make_hT(s, db)
        if kv:
            R.op("sp", lambda e: e.dma_start(out=rp[db], in_=rope_d[s * 128:(s + 1) * 128, :]), writes=["rp%d" % db], dma=True)
            proj_tok(h, hk, 0, 512, 2)
        proj_tok(h, hk, 512, 512, 3)
        proj_tok(h, hk, 1024, 512, 4)
        proj_tok(h, hk, 1536, 512, 5)
        proj_tok(h, hk, 2048, 16, 6)
        if kv:
            R.op("dve", lambda e: e.tensor_tensor(out=kf[db], in0=PS(2, 0, 256), in1=bo[:, 0:256], op=ALU.add), reads=[pk(2), "bo"], writes=["kf%d" % db])
            R.op("dve", lambda e: e.tensor_tensor(out=Vst[:, s, :], in0=PS(2, 256, 256), in1=bo[:, 256:512], op=ALU.add), reads=[pk(2), "bo"], writes=["Vst"])
        R.op("dve", lambda e: e.scalar_tensor_tensor(out=Kt[db], in0=PS(3), scalar=SC, in1=bo[:, 512:1024], op0=ALU.mult, op1=ALU.add),
             reads=[pk(3), "bo"], writes=["Kt%d" % db])
        for half in range(2):
            R.op("dve", lambda e, half=half: e.tensor_tensor(out=Vx[db][:, 2 * half:2 * half + 2, 0:256],
                                                             in0=PS(4 + half).rearrange("p (h v) -> p h v", v=256),
                                                             in1=bo[:, 1024 + 512 * half:1536 + 512 * half].rearrange("p (h v) -> p h v", v=256), op=ALU.add),
                 reads=[pk(4 + half), "bo"], writes=["Vx%d" % db])
        R.op("dve", lambda e: e.tensor_tensor(out=Gt[db], in0=PS(6, 0, 16), in1=bo[:, 2048:2064], op=ALU.add), reads=[pk(6), "bo"], writes=["G%d" % db])
        return h, hk

    def slot_back(s, db, kv=True):
        if kv:
            kfb, kfk = kf[db], "kf%d" % db
            R.op("pool", lambda e: e.memset(sm[:, 4:6], 0.0), writes=["kss"])
            for g in range(2):
                R.op("act", lambda e, g=g: e.activation(out=kn[:, g * 128:(g + 1) * 128], in_=kfb[:, g * 128:(g + 1) * 128], func=AF.Square, accum_out=sm[:, 4 + g:5 + g]),
                     reads=[kfk, "kss"], writes=["kn", "kss"])
            rstd_op(sm[:, 8:10], sm[:, 4:6], 128.0, ["kss"], "krs")
            for g in range(2):
                R.op("dve", lambda e, g=g: e.scalar_tensor_tensor(out=kn[:, g * 128:(g + 1) * 128], in0=kfb[:, g * 128:(g + 1) * 128], scalar=sm[:, 8 + g:9 + g],
                                                                  in1=gkb, op0=ALU.mult, op1=ALU.mult), reads=[kfk, "krs", "gkb"], writes=["kn"])
            rope(kn, "kn", krot, "krot", 2, rp[db], "rp%d" % db)
            for g in range(2):
                R.op("pe", lambda e, g=g: e.transpose(out=pT[:, g * 128:(g + 1) * 128], in_=krot[:, g * 128:(g + 1) * 128], identity=identb),
                     reads=["krot", "identb"], writes=["pT0"])
            R.op("act", lambda e: e.activation(out=kTst[:, :, s * 128:(s + 1) * 128], in_=pT[:, 0:256].rearrange("p (g t) -> p g t", g=2), func=AF.Copy),
                 reads=["pT0"], writes=["kTst"])
        chunk_gates(s, db)

    def rope(src, skey, dst, dkey, nh, rpt, rkey):
        v = src.rearrange("p (h i two) -> p h i two", h=nh, two=2)
        o = dst.rearrange("p (h i two) -> p h i two", h=nh, two=2)
        cosb = rpt[:, 0:64].unsqueeze(1).to_broadcast([128, nh, 64])
        sinb = rpt[:, 64:128].unsqueeze(1).to_broadcast([128, nh, 64])
        n = nh * 64
        t = [rt[i][:, 0:n].rearrange("p (h i) -> p h i", h=nh) if n <= 128 else None for i in range(4)]
        if n > 128:
            t = [rtq[i].rearrange("p (h i) -> p h i", h=nh) for i in range(4)]
        x1, x2 = v[:, :, :, 0], v[:, :, :, 1]
        tk = ["rt0", "rt1", "rt2", "rt3"]
        R.op("dve", lambda e: e.tensor_tensor(out=t[0], in0=x1, in1=cosb, op=ALU.mult), reads=[skey, rkey], writes=[tk[0]])
        R.op("dve", lambda e: e.tensor_tensor(out=t[1], in0=x2, in1=sinb, op=ALU.mult), reads=[skey, rkey], writes=[tk[1]])
        R.op("dve", lambda e: e.tensor_tensor(out=t[2], in0=x1, in1=sinb, op=ALU.mult), reads=[skey, rkey], writes=[tk[2]])
        R.op("dve", lambda e: e.tensor_tensor(out=t[3], in0=x2, in1=cosb, op=ALU.mult), reads=[skey, rkey], writes=[tk[3]])
        R.op("dve", lambda e: e.tensor_tensor(out=o[:, :, :, 0], in0=t[0], in1=t[1], op=ALU.subtract), reads=[tk[0], tk[1]], writes=[dkey])
        R.op("dve", lambda e: e.tensor_tensor(out=o[:, :, :, 1], in0=t[2], in1=t[3], op=ALU.add), reads=[tk[2], tk[3]], writes=[dkey])

    def chunk_gates(s, db):
        q_ = cq[db]
        e1, Lf, lgf, ie, imb, gg, wst, dec = [q_[:, 8 * i:8 * i + 8] for i in range(8)]
        ck = "cq%d" % db
        R.op("act", lambda e: e.activation(out=e1, in_=Gt[db][:, 8:16], func=AF.Exp, scale=-1.0), reads=["G%d" % db], writes=[ck + "a"])
        R.op("act", lambda e: e.activation(out=Lf, in_=e1, func=AF.Ln, bias=epsb[:, 1:2]), reads=[ck + "a", "epsb"], writes=[ck + "b"])
        R.op("dve", lambda e: e.tensor_tensor(out=lgf, in0=Lf, in1=gmask[:, s, 8:16], op=ALU.mult), reads=[ck + "b", "gmask"], writes=[ck + "lgf"])
        R.op("dve", lambda e: e.tensor_tensor(out=ie, in0=Gt[db][:, 0:8], in1=gmask[:, s, 0:8], op=ALU.add), reads=["G%d" % db, "gmask"], writes=[ck + "ie"])
        R.op("pe", lambda e: e.matmul(PS(6, 16, 4), lhsT=tri_f, rhs=lgf[:, 0:4], start=True, stop=True), reads=["cst", ck + "lgf"], writes=[pk(6)])
        R.op("pe", lambda e: e.matmul(PS(6, 20, 4), lhsT=tri_b, rhs=lgf[:, 4:8], start=True, stop=True), reads=["cst", ck + "lgf"], writes=[pk(6)])
        R.op("pe", lambda e: e.matmul(PS(6, 24, 8), lhsT=ones, rhs=lgf, start=True, stop=True), reads=["cst", ck + "lgf"], writes=[pk(6)])
        R.op("dve", lambda e: e.tensor_tensor(out=imb, in0=ie, in1=PS(6, 16, 8), op=ALU.subtract), reads=[ck + "ie", pk(6)], writes=[ck + "imb"])
        R.op("dve", lambda e: e.tensor_tensor(out=gg, in0=imb, in1=PS(6, 24, 8), op=ALU.add), reads=[ck + "imb", pk(6)], writes=[ck + "gg"])
        R.op("act", lambda e: e.activation(out=wst, in_=gg, func=AF.Exp), reads=[ck + "gg"], writes=[ck + "wst"])
        R.op("act", lambda e: e.activation(out=dec, in_=PS(6, 24, 8), func=AF.Exp), reads=[pk(6)], writes=[ck + "dec"])
        R.op("act", lambda e: e.activation(out=expb[db], in_=PS(6, 16, 8), func=AF.Exp), reads=[pk(6)], writes=[ck + "expb"])

    def state_step(db, j, refresh_bf=False):
        h = j % 4
        q_ = cq[db]
        wst, dec = q_[:, 48:56], q_[:, 56:64]
        ck = "cq%d" % db
        kb = j % 2
        R.op("dve", lambda e: e.tensor_scalar(out=Kw[kb], in0=Kt[db][:, h * 128:(h + 1) * 128], scalar1=wst[:, j:j + 1], scalar2=None, op0=ALU.mult),
             reads=["Kt%d" % db, ck + "wst"], writes=["Kw%d" % kb])
        R.op("pe", lambda e: e.matmul(PS(7, 0, 257), lhsT=Kw[kb], rhs=Vx[db][:, h, 0:257], start=True, stop=True),
             reads=["Kw%d" % kb, "Vx%d" % db], writes=[pk(7)])
        R.op("dve", lambda e: e.scalar_tensor_tensor(out=stC[j], in0=stC[j], scalar=dec[:, j:j + 1], in1=PS(7, 0, 257), op0=ALU.mult, op1=ALU.add),
             reads=["stC%d" % j, ck + "dec", pk(7)], writes=["stC%d" % j])
        if refresh_bf:
            R.op("act", lambda e: e.activation(out=stCb[j], in_=stC[j], func=AF.Copy), reads=["stC%d" % j], writes=["stCb%d" % j])

    rtq = None
    hacc = A.b(NOWN * 1024).rearrange("p (o h v) -> p o h v", o=NOWN, h=4)
    mark_own = A.off
    Wq = A.b(16 * 512).rearrange("p (c n) -> p c n", n=512)
    R.op("pool", lambda e: e.dma_start(out=Wq, in_=win_v[:, :, MQ0:MQ0 + 512]), writes=["Wq"], dma=True)
    qmT = [A.b(512).rearrange("p (h t) -> p h t", h=4) for _ in range(2)]
    kmT = [A.b(512).rearrange("p (h t) -> p h t", h=4) for _ in range(2)]
    lb = A.f(128)
    DTt = A.f(128)
    STt = A.b(128)
    tmpn = A.f(257)
    tot = A.f(257)
    ddr = A.f(2)

    def full_step(s, db, j, o, first):
        h = j % 4
        dirn = j // 4
        TRI = tri_f if dirn == 0 else tri_b
        NM = nm_f if dirn == 0 else nm_b
        q_ = cq[db]
        lgf, imb = q_[:, 16:24], q_[:, 32:40]
        ck = "cq%d" % db
        R.op("dve", lambda e: e.tensor_scalar(out=lb, in0=ones, scalar1=lgf[:, j:j + 1], scalar2=None, op0=ALU.mult), reads=["cst", ck + "lgf"], writes=["lb"])
        R.op("pe", lambda e: e.matmul(PS(4, 0, 128), lhsT=lb, rhs=TRI, start=True, stop=False), reads=["lb", "cst"], writes=[pk(4)])
        R.op("pe", lambda e: e.matmul(PS(4, 0, 128), lhsT=ident, rhs=NM, start=False, stop=True), reads=["cst"], writes=[pk(4)])
        R.op("act", lambda e: e.activation(out=DTt, in_=PS(4, 0, 128), func=AF.Exp, bias=imb[:, j:j + 1]), reads=[pk(4), ck + "imb"], writes=["DT"])
        R.op("pe", lambda e: e.matmul(PS(5, 0, 128), lhsT=kmT[db][:, h, :], rhs=qmT[db][:, h, :], start=True, stop=True), reads=["kmT%d" % db, "qmT%d" % db], writes=[pk(5)])
        R.op("dve", lambda e: e.tensor_tensor(out=STt, in0=PS(5, 0, 128), in1=DTt, op=ALU.mult), reads=[pk(5), "DT"], writes=["ST"])
        R.op("pe", lambda e: e.matmul(PS(2, 0, 257), lhsT=STt, rhs=Vx[db][:, h, 0:257], start=True, stop=True), reads=["ST", "Vx%d" % db], writes=[pk(2)])
        R.op("pe", lambda e: e.matmul(PS(3, 0, 257), lhsT=qmT[db][:, h, :], rhs=stCb[j], start=True, stop=True), reads=["qmT%d" % db, "stCb%d" % j], writes=[pk(3)])
        R.op("act", lambda e: e.activation(out=tmpn, in_=PS(2, 0, 257), func=AF.Copy), reads=[pk(2)], writes=["tmpn"])
        R.op("dve", lambda e: e.scalar_tensor_tensor(out=tot, in0=PS(3, 0, 257), scalar=expb[db][:, j:j + 1], in1=tmpn, op0=ALU.mult, op1=ALU.add),
             reads=[pk(3), ck + "expb", "tmpn"], writes=["tot"])
        R.op("dve", lambda e: e.scalar_tensor_tensor(out=ddr[:, 0:1], in0=tot[:, 256:257], scalar=-1.0, in1=tot[:, 256:257], op0=ALU.mult, op1=ALU.max), reads=["tot"], writes=["dd"])
        R.op("dve", lambda e: e.tensor_scalar(out=ddr[:, 0:1], in0=ddr[:, 0:1], scalar1=1.0, scalar2=None, op0=ALU.max), reads=["dd"], writes=["dd"])
        R.op("dve", lambda e: e.reciprocal(out=ddr[:, 1:2], in_=ddr[:, 0:1]), reads=["dd"], writes=["rr"])
        hk_ = "hacc%d" % o
        if first:
            R.op("dve", lambda e: e.tensor_scalar(out=hacc[:, o, h, :], in0=tot[:, 0:256], scalar1=ddr[:, 1:2], scalar2=None, op0=ALU.mult), reads=["tot", "rr"], writes=[hk_])
        else:
            R.op("dve", lambda e: e.scalar_tensor_tensor(out=hacc[:, o, h, :], in0=tot[:, 0:256], scalar=ddr[:, 1:2], in1=hacc[:, o, h, :], op0=ALU.mult, op1=ALU.add),
                 reads=["tot", "rr", hk_], writes=[hk_])
        state_step(db, j, refresh_bf=True)


    n_own = NOWN if debug != "own" else 2
    visits = [("oth", s_, None) for s_ in range(n_oth)]
    visits += [("own", NOTH + o_, 0) for o_ in range(n_own)] + [("own", NOTH + o_, 1) for o_ in range(n_own - 1, -1, -1)]

    def v_front(vi):
        kind, s_, dirn = visits[vi]
        db = vi % 2
        if kind == "oth":
            slot_front(s_, db, kv=True)
            return
        h_, hk = slot_front(s_, db, kv=(dirn == 0))
        for hh in range(4):
            for c in range(NCH):
                R.op("pe", lambda e, c=c, hh=hh: e.matmul(PS(2, hh * 128, 128), lhsT=Wq[:, c, hh * 128:(hh + 1) * 128], rhs=h_[:, c, :], start=(c == 0), stop=(c == NCH - 1)),
                     reads=["Wq", hk], writes=[pk(2)])
        for hh in range(4):
            R.op("act", lambda e, hh=hh: e.activation(out=qmT[db][:, hh, :], in_=PS(2, hh * 128, 128), func=AF.Identity, bias=bfm[:, hh:hh + 1]), reads=[pk(2), "bfm"], writes=["qmT%d" % db])
        for hh in range(4):
            for c in range(NCH):
                R.op("pe", lambda e, c=c, hh=hh: e.matmul(PS(3, hh * 128, 128), lhsT=Wt[:, c, 512 + hh * 128:512 + (hh + 1) * 128], rhs=h_[:, c, :], start=(c == 0), stop=(c == NCH - 1)),
                     reads=["W", hk], writes=[pk(3)])
        for hh in range(4):
            R.op("act", lambda e, hh=hh: e.activation(out=kmT[db][:, hh, :], in_=PS(3, hh * 128, 128), func=AF.Identity, scale=SC, bias=bfm[:, 4 + hh:5 + hh]), reads=[pk(3), "bfm"], writes=["kmT%d" % db])

    def v_back(vi):
        kind, s_, dirn = visits[vi]
        db = vi % 2
        if kind == "oth":
            slot_back(s_, db, kv=True)
            if s_ == 0:
                for j in range(4):
                    state_step(0, j)
            elif s_ == 1:
                for j in range(8):
                    state_step(1, j)
                for j in range(4, 8):
                    state_step(0, j)
            else:
                for j in range(8):
                    state_step(db, j)
            return
        if vi == n_oth:
            for j in range(8):
                R.op("act", lambda e, j=j: e.activation(out=stCb[j], in_=stC[j], func=AF.Copy), reads=["stC%d" % j], writes=["stCb%d" % j])
        slot_back(s_, db, kv=(dirn == 0))
        for hh in range(4):
            full_step(s_, db, dirn * 4 + hh, s_ - NOTH, first=(dirn == 0))

    nv = len(visits)
    v_front(0)
    for vi in range(nv):
        if vi + 1 < nv and vi != 1:
            v_front(vi + 1)
        v_back(vi)
        if vi == 1 and vi + 1 < nv:
            v_front(vi + 1)
    if debug == "oth":
        s = n_oth - 1
        dump(hT[s % 2].rearrange("p c t -> p (c t)")[:, 0:0], "x", 0, 0) if False else None
        dump_bf(hT[s % 2].rearrange("p c t -> p (c t)"), "hT%d" % (s % 2), 0, 2048)
        dump_bf(kTst[:, :, s * 128:(s + 1) * 128], "kTst", 2048, 256) if False else None
        dump_bf(Vst[:, s, :], "Vst", 2304, 256)
        dump_bf(Kt[s % 2], "Kt%d" % (s % 2), 2560, 512)
        dump(Gt[s % 2], "G%d" % (s % 2), 3072, 16)
        dump(cq[s % 2], "cq%dwst" % (s % 2), 3088, 64)
        for j in range(8):
            dump(stC[j], "stC%d" % j, 3200 + 257 * j, 257)
        dump_bf(krot, "krot", 5300, 256)
        return finish()


    if debug == "own":
        dump_bf(hacc[:, 0, :, :].rearrange("p h v -> p (h v)"), "hacc0", 0, 1024)
        dump_bf(hacc[:, 1, :, :].rearrange("p h v -> p (h v)"), "hacc1", 1024, 1024)
        return finish()


    R.barrier()
    A.off = mark_own
    Wc = Wflat[:, 0:16 * 1024].rearrange("p (c n) -> p c n", n=1024)
    yT = Wflat[:, 16 * 1024:32 * 1024].rearrange("p (k t) -> p k t", t=1024)
    qf = A.f(1024)
    rtq_all = A.f(2048)
    rtq = [rtq_all[:, i * 512:(i + 1) * 512] for i in range(4)]
    qrot = A.b(1024)
    qTc = A.b(1024).rearrange("p (h t) -> p h t", h=8)
    PTt = [A.b(512) for _ in range(2)]
    dsb = A.f(512)
    qss = A.f(16)
    gmb = A.f(1024)

    def load_wc(lo, wd=win_v):
        R.op("pool", lambda e: e.dma_start(out=Wc, in_=wd[:, :, lo:lo + 1024]), writes=["W"], dma=True)

    load_wc(Q0)
    R.op("sp", lambda e: e.dma_start(out=bo[:, 0:1024], in_=pbc(bin_d[:, Q0:Q0 + 1024])), writes=["bo"], dma=True)
    R.op("sp", lambda e: e.dma_start(out=bo[:, 1024:2048], in_=pbc(bin_d[:, MO0:MO0 + 1024])), writes=["bo"], dma=True)
    R.op("sp", lambda e: e.dma_start(out=gmb, in_=pbc(gm_d)), writes=["gmb"], dma=True)
    qf3 = qf.rearrange("p (h d) -> p h d", h=8)
    for o in range(NOWN):
        s = NOTH + o
        b2 = s % 2
        h_, hk = make_hT(s)
        R.op("sp", lambda e, s=s, b2=b2: e.dma_start(out=rp[b2], in_=rope_d[s * 128:(s + 1) * 128, :]), writes=["rp%d" % b2], dma=True)
        proj_tok(h_, hk, 0, 512, 2, w=Wc)
        proj_tok(h_, hk, 512, 512, 3, w=Wc)
        for hf_ in range(2):
            R.op("dve", lambda e, hf_=hf_: e.tensor_tensor(out=qf[:, hf_ * 512:(hf_ + 1) * 512], in0=PS(2 + hf_), in1=bo[:, hf_ * 512:(hf_ + 1) * 512], op=ALU.add),
                 reads=[pk(2 + hf_), "bo"], writes=["qf"])
        R.op("pool", lambda e: e.memset(qss[:, 0:8], 0.0), writes=["qss"])
        for hh in range(8):
            R.op("act", lambda e, hh=hh: e.activation(out=rtq[0][:, 0:128], in_=qf[:, hh * 128:(hh + 1) * 128], func=AF.Square, accum_out=qss[:, hh:hh + 1]),
                 reads=["qf", "qss"], writes=["rt0", "qss"])
        rstd_op(qss[:, 8:16], qss[:, 0:8], 128.0, ["qss"], "qrs")
        R.op("dve", lambda e: e.tensor_tensor(out=qf3, in0=qf3, in1=qss[:, 8:16].unsqueeze(2).to_broadcast([128, 8, 128]), op=ALU.mult), reads=["qf", "qrs"], writes=["qf"])
        R.op("dve", lambda e: e.tensor_tensor(out=qf3, in0=qf3, in1=gqb.unsqueeze(1).to_broadcast([128, 8, 128]), op=ALU.mult), reads=["qf", "gqb"], writes=["qf"])
        rope(qf, "qf", qrot, "qrot", 8, rp[b2], "rp%d" % b2)
        for hh in range(8):
            R.op("pe", lambda e, hh=hh: e.transpose(out=pT[:, hh * 128:(hh + 1) * 128], in_=qrot[:, hh * 128:(hh + 1) * 128], identity=identb),
                 reads=["qrot", "identb"], writes=["pT0"])
        R.op("act", lambda e: e.activation(out=qTc, in_=pT[:, 0:1024].rearrange("p (h t) -> p h t", h=8), func=AF.Copy), reads=["pT0"], writes=["qTc"])
        for g in range(2):
            def s_mm(sk, g=g):
                sb = 2 + (sk % 2)
                R.op("pe", lambda e: e.matmul(PS(sb), lhsT=kTst[:, g, sk * 128:(sk + 1) * 128], rhs=qTc[:, 4 * g:4 * g + 4, :], start=True, stop=True),
                     reads=["kTst", "qTc"], writes=[pk(sb)])
            s_mm(0)
            for sk in range(NSLOT):
                sb = 2 + (sk % 2)
                pb = sk % 2
                if sk + 1 < NSLOT:
                    s_mm(sk + 1)
                R.op("act", lambda e, sb=sb, pb=pb: e.activation(out=PTt[pb], in_=PS(sb), func=AF.Exp), reads=[pk(sb)], writes=["PT%d" % pb])
                R.op("pe", lambda e, g=g, sk=sk, pb=pb: e.matmul(PS(4 + g), lhsT=Vst[:, sk, g * 128:(g + 1) * 128], rhs=PTt[pb], start=(sk == 0), stop=(sk == NSLOT - 1)),
                     reads=["Vst", "PT%d" % pb], writes=[pk(4 + g)])
                R.op("pe", lambda e, g=g, sk=sk, pb=pb: e.matmul(PS(6 + g), lhsT=onesb, rhs=PTt[pb], start=(sk == 0), stop=(sk == NSLOT - 1)),
                     reads=["onesb", "PT%d" % pb], writes=[pk(6 + g)])
            R.op("act", lambda e, g=g: e.activation(out=dsb, in_=PS(6 + g), func=AF.Copy), reads=[pk(6 + g)], writes=["dsb"])
            R.op("dve", lambda e: e.reciprocal(out=dsb, in_=dsb), reads=["dsb"], writes=["dsb"])
            R.op("dve", lambda e, g=g, o=o: e.tensor_tensor(out=yT[:, 4 * g:4 * g + 4, o * 128:(o + 1) * 128], in0=PS(4 + g).rearrange("p (h t) -> p h t", h=4),
                                                          in1=dsb.rearrange("p (h t) -> p h t", h=4), op=ALU.mult),
                 reads=[pk(4 + g), "dsb"], writes=["yT"])
    if debug == "att":
        for k in range(8):
            dump_bf(yT[:, k, 0:256], "yT", k * 256, 256)
        return finish()

    R.barrier()
    load_wc(MO0)
    hn = rtq_all[:, 0:1024]
    ym = qrot
    hn3 = hn.rearrange("p (h v) -> p h v", h=4)
    for o in range(NOWN):
        s = NOTH + o
        h_, hk = make_hT(s)
        proj_tok(h_, hk, 0, 512, 2, w=Wc)
        proj_tok(h_, hk, 512, 512, 3, w=Wc)
        for hf_ in range(2):
            R.op("dve", lambda e, hf_=hf_: e.tensor_tensor(out=qf[:, hf_ * 512:(hf_ + 1) * 512], in0=PS(2 + hf_), in1=bo[:, 1024 + hf_ * 512:1024 + (hf_ + 1) * 512], op=ALU.add),
                 reads=[pk(2 + hf_), "bo"], writes=["qf"])
        R.op("act", lambda e: e.activation(out=qf, in_=qf, func=AF.Sigmoid), reads=["qf"], writes=["qf"])
        R.op("pool", lambda e: e.memset(qss[:, 0:4], 0.0), writes=["qss"])
        for hh in range(4):
            R.op("act", lambda e, hh=hh, o=o: e.activation(out=hn[:, hh * 256:(hh + 1) * 256], in_=hacc[:, o, hh, :], func=AF.Square, accum_out=qss[:, hh:hh + 1]),
                 reads=["hacc%d" % o, "qss"], writes=["hn", "qss"])
        rstd_op(qss[:, 8:12], qss[:, 0:4], 256.0, ["qss"], "qrs")
        R.op("dve", lambda e, o=o: e.tensor_tensor(out=hn3, in0=hacc[:, o, :, :], in1=qss[:, 8:12].unsqueeze(2).to_broadcast([128, 4, 256]), op=ALU.mult),
             reads=["hacc%d" % o, "qrs"], writes=["hn"])
        R.op("pool", lambda e: e.tensor_tensor(out=hn, in0=hn, in1=gmb, op=ALU.mult), reads=["hn", "gmb"], writes=["hn"])
        R.op("dve", lambda e: e.tensor_tensor(out=ym, in0=hn, in1=qf, op=ALU.mult), reads=["hn", "qf"], writes=["qrot"])
        for k in range(8):
            R.op("pe", lambda e, k=k: e.transpose(out=pT[:, k * 128:(k + 1) * 128], in_=ym[:, k * 128:(k + 1) * 128], identity=identb),
                 reads=["qrot", "identb"], writes=["pT0"])
        R.op("act", lambda e, o=o: e.activation(out=yT[:, 8:16, o * 128:(o + 1) * 128], in_=pT[:, 0:1024].rearrange("p (k t) -> p k t", k=8), func=AF.Copy),
             reads=["pT0"], writes=["yT"])
    if debug == "ym":
        for k in range(8):
            dump_bf(yT[:, 8 + k, 0:256], "yT", k * 256, 256)
        return finish()

    R.barrier()
    A.off = mark_w_end
    x1 = A.f(NOWN * D).rearrange("p (o d) -> p o d", o=NOWN)
    bcA = A.f(1024)
    tmpd = [A.f(512) for _ in range(2)]
    lbm = A.f(128)
    wout_v = wout_d.rearrange("(c p) n -> p c n", p=128)

    def make_bc(dst, key, chunk_lo, nchunks, col, src=None, skey="modT"):
        for c in range(nchunks):
            if src is None:
                vec = modT[:, chunk_lo + c, col:col + 1]
            else:
                vec = src[:, chunk_lo + c:chunk_lo + c + 1]
            R.op("dve", lambda e, vec=vec: e.tensor_scalar(out=lbm, in0=ones, scalar1=vec, scalar2=None, op0=ALU.mult), reads=["cst", skey], writes=["lbm"])
            R.op("pe", lambda e, c=c: e.matmul(PS(2, (c % 4) * 128, 128), lhsT=lbm, rhs=ident, start=True, stop=True), reads=["lbm", "cst"], writes=[pk(2)])
            if c % 4 == 3:
                R.op("act", lambda e, c=c: e.activation(out=dst[:, (c - 3) * 128:(c + 1) * 128], in_=PS(2), func=AF.Copy), reads=[pk(2)], writes=[key])

    for o in range(NOWN):
        s = NOTH + o
        R.op("sp", lambda e, s=s, o=o: e.dma_start(out=x1[:, o, :], in_=xs_d[s * 128:(s + 1) * 128, :]), writes=["x1_%d" % o], dma=True)
    for half in range(2):
        load_wc(half * 1024, wd=wout_v)
        make_bc(bcA, "bcA", 32 + half * 8, 8, 0)
        for o in range(NOWN):
            for dh in range(2):
                for k in range(NCH):
                    R.op("pe", lambda e, o=o, dh=dh, k=k: e.matmul(PS(4 + dh), lhsT=yT[:, k, o * 128:(o + 1) * 128], rhs=Wc[:, k, dh * 512:(dh + 1) * 512],
                                                                  start=(k == 0), stop=(k == NCH - 1)), reads=["yT", "W"], writes=[pk(4 + dh)])
                cols = slice(half * 1024 + dh * 512, half * 1024 + (dh + 1) * 512)
                R.op("dve", lambda e, dh=dh: e.tensor_tensor(out=tmpd[dh], in0=PS(4 + dh), in1=bcA[:, dh * 512:(dh + 1) * 512], op=ALU.mult),
                     reads=[pk(4 + dh), "bcA"], writes=["tmpd%d" % dh])
                R.op("pool", lambda e, o=o, dh=dh, cols=cols: e.tensor_tensor(out=x1[:, o, cols], in0=x1[:, o, cols], in1=tmpd[dh], op=ALU.add),
                     reads=["x1_%d" % o, "tmpd%d" % dh], writes=["x1_%d" % o])
    if debug == "x1":
        for q in range(4):
            dump(x1[:, 0, q * 512:(q + 1) * 512], "x1_0", q * 512, 512)
        for q in range(4):
            dump(x1[:, 7, q * 512:(q + 1) * 512], "x1_7", 2048 + q * 512, 512)
        return finish()

    R.barrier()
    h2T = Wflat[:, 0:16 * 1024].rearrange("p (c t) -> p c t", t=1024)
    wfree = Wflat[:, 16 * 1024:16 * 1024 + 16384].bitcast(F32)
    gm2b = wfree[:, 0:2048]
    sh2b = wfree[:, 2048:4096]
    h2f = wfree[:, 4096:6144]
    h2Tr = wfree[:, 6144:8192].rearrange("p (c t) -> p c t", t=128)
    A.off = mark_w_end + NOWN * D
    sm2 = A.f(8)
    g2fm = A.f(16)
    gm2fm = A.f(16)
    wr = A.f(16 * NEXP).rearrange("p (c e) -> p c e", e=NEXP)
    brb = A.f(NEXP)
    wt = A.f(NOWN * NEXP).rearrange("p (o e) -> p o e", e=NEXP)
    rsm = A.f(64)
    ex = A.f(NEXP)
    msk = A.f(NEXP)
    R.op("sp", lambda e: e.dma_start(out=g2fm, in_=g2fm_d), writes=["g2fm"], dma=True)
    R.op("sp", lambda e: e.dma_start(out=wr, in_=wr_d.rearrange("(c p) e -> p c e", p=128)), writes=["wr"], dma=True)
    R.op("sp", lambda e: e.dma_start(out=brb, in_=pbc(br_d)), writes=["brb"], dma=True)
    R.op("dve", lambda e: e.scalar_tensor_tensor(out=gm2fm, in0=modT[:, 64:80, 0], scalar=1.0, in1=g2fm, op0=ALU.add, op1=ALU.mult), reads=["modT", "g2fm"], writes=["gm2fm"])
    make_bc(gm2b, "gm2b", 0, 16, 0, src=gm2fm, skey="gm2fm")
    make_bc(sh2b, "sh2b", 48, 16, 0)
    lg = rsm[:, 0:32]
    top8 = rsm[:, 32:40]
    for o in range(NOWN):
        xk = "x1_%d" % o
        R.op("pool", lambda e: e.memset(sm2[:, 0:1], 0.0), writes=["ss0"])
        R.op("act", lambda e, o=o: e.activation(out=h2f, in_=x1[:, o, :], func=AF.Square, accum_out=sm2[:, 0:1]), reads=[xk, "ss0"], writes=["h2f", "ss0"])
        rstd_op(sm2[:, 2:3], sm2[:, 0:1], float(D), ["ss0"], "rs0")
        R.op("dve", lambda e, o=o: e.scalar_tensor_tensor(out=h2f, in0=x1[:, o, :], scalar=sm2[:, 2:3], in1=gm2b, op0=ALU.mult, op1=ALU.mult),
             reads=[xk, "rs0", "gm2b"], writes=["h2f"])
        R.op("pool", lambda e: e.tensor_tensor(out=h2f, in0=h2f, in1=sh2b, op=ALU.add), reads=["h2f", "sh2b"], writes=["h2f"])
        for c in range(NCH):
            bk = 4 + (c // 4)
            R.op("pe", lambda e, c=c, bk=bk: e.transpose(out=PS(bk, (c % 4) * 128, 128), in_=h2f[:, c * 128:(c + 1) * 128], identity=ident),
                 reads=["h2f", "cst"], writes=[pk(bk)])
        for q in range(4):
            R.op("act", lambda e, q=q: e.activation(out=h2Tr[:, 4 * q:4 * q + 4, :], in_=PS(4 + q).rearrange("p (c t) -> p c t", t=128), func=AF.Copy),
                 reads=[pk(4 + q)], writes=["h2Tr"])
        R.op("dve", lambda e, o=o: e.tensor_copy(out=h2T[:, :, o * 128:(o + 1) * 128], in_=h2Tr), reads=["h2Tr"], writes=["h2T"])
        for c in range(NCH):
            R.op("pe", lambda e, c=c: e.matmul(PS(3, 0, NEXP), lhsT=h2Tr[:, c, :], rhs=wr[:, c, :], start=(c == 0), stop=(c == NCH - 1)), reads=["h2Tr", "wr"], writes=[pk(3)])
        R.op("dve", lambda e: e.tensor_tensor(out=lg, in0=PS(3, 0, NEXP), in1=brb, op=ALU.add), reads=[pk(3), "brb"], writes=["lg"])
        R.op("dve", lambda e: e.max(out=top8, in_=lg), reads=["lg"], writes=["top8"])
        R.op("dve", lambda e: e.tensor_scalar(out=msk, in0=lg, scalar1=top8[:, 3:4], scalar2=None, op0=ALU.is_ge), reads=["lg", "top8"], writes=["msk"])
        R.op("dve", lambda e: e.tensor_scalar(out=rsm[:, 40:41], in0=top8[:, 0:1], scalar1=-1.0, scalar2=None, op0=ALU.mult), reads=["top8"], writes=["nmx"])
        R.op("act", lambda e: e.activation(out=ex, in_=lg, func=AF.Exp, bias=rsm[:, 40:41]), reads=["lg", "nmx"], writes=["ex"])
        R.op("dve", lambda e: e.tensor_tensor(out=ex, in0=ex, in1=msk, op=ALU.mult), reads=["ex", "msk"], writes=["ex"])
        R.op("dve", lambda e: e.reduce_sum(out=rsm[:, 41:42], in_=ex, axis=AX.X), reads=["ex"], writes=["esum"])
        R.op("dve", lambda e: e.reciprocal(out=rsm[:, 42:43], in_=rsm[:, 41:42]), reads=["esum"], writes=["ersum"])
        R.op("dve", lambda e, o=o: e.tensor_scalar(out=wt[:, o, :], in0=ex, scalar1=rsm[:, 42:43], scalar2=None, op0=ALU.mult), reads=["ex", "ersum"], writes=["wt"])
    if debug == "rt":
        dump(wt.rearrange("p o e -> p (o e)"), "wt", 0, 256)
        dump_bf(h2T[:, 0, 0:256], "h2T", 256, 256)
        return finish()

    R.barrier()
    gt2b = wfree[:, 0:2048]
    ring = [wfree[:, 2048 * (1 + i):2048 * (2 + i)].bitcast(BF16).rearrange("p (c n) -> p c n", n=256) for i in range(3)]
    ring.append(A.b(16 * 256).rearrange("p (c n) -> p c n", n=256))
    actT = A.b(16 * 1024).rearrange("p (f t) -> p f t", t=1024)
    b1e = [A.f(32) for _ in range(2)]
    gtt = [A.f(512) for _ in range(2)]
    sgt = [A.f(512), wr.rearrange("p c e -> p (c e)")]
    utt = [A.f(512), A.f(512)]
    t10 = A.f(256)
    t1 = [t10, t10]
    make_bc(gt2b, "gt2b", 80, 16, 0)
    b2g = actT.rearrange("p f t -> p (f t)")[:, 0:4096].bitcast(F32)
    wtT = actT.rearrange("p f t -> p (f t)")[:, 4096:4096 + 2048].bitcast(F32)
    R.op("sp", lambda e: e.dma_start(out=b2g[0:NEXP, :], in_=b2_d), writes=["b2g"], dma=True)
    for o in range(NOWN):
        R.op("pe", lambda e, o=o: e.transpose(out=PS(2, o * 64, 128)[0:NEXP, :] if False else PS(2 + o // 4, (o % 4) * 128, 128)[0:NEXP, :], in_=wt[:, o, :], identity=ident),
             reads=["wt", "cst"], writes=[pk(2 + o // 4)])
    for q in range(2):
        R.op("act", lambda e, q=q: e.activation(out=wtT[0:NEXP, q * 512:(q + 1) * 512], in_=PS(2 + q)[0:NEXP, :], func=AF.Copy), reads=[pk(2 + q)], writes=["wtT"])
    for o in range(NOWN):
        for dq in range(4):
            bk = 4 + (dq % 2)
            R.op("pe", lambda e, o=o, dq=dq, bk=bk: e.matmul(PS(bk), lhsT=wtT[0:NEXP, o * 128:(o + 1) * 128], rhs=b2g[0:NEXP, dq * 512:(dq + 1) * 512], start=True, stop=True),
                 reads=["wtT", "b2g"], writes=[pk(bk)])
            R.op("dve", lambda e, dq=dq, bk=bk: e.tensor_tensor(out=gtt[dq % 2], in0=PS(bk), in1=gt2b[:, dq * 512:(dq + 1) * 512], op=ALU.mult),
                 reads=[pk(bk), "gt2b"], writes=["gtt%d" % (dq % 2)])
            R.op("pool", lambda e, o=o, dq=dq: e.tensor_tensor(out=x1[:, o, dq * 512:(dq + 1) * 512], in0=x1[:, o, dq * 512:(dq + 1) * 512], in1=gtt[dq % 2], op=ALU.add),
                 reads=["x1_%d" % o, "gtt%d" % (dq % 2)], writes=["x1_%d" % o])
    R.barrier()
    if big:
        w1_v = [w1_d[e_].rearrange("(c p) n -> p c n", p=128) for e_ in range(NEXP)]
        w2_v = [w2_d[e_].rearrange("(c p) n -> p c n", p=128) for e_ in range(NEXP)]
    nld = [0]

    def ring_load(src):
        i = nld[0] % 4
        nld[0] += 1
        R.op("pool", lambda e, i=i: e.dma_start(out=ring[i], in_=src), writes=["ring%d" % i], dma=True)
        return ring[i], "ring%d" % i

    for e_ in range(n_exp if big else 0):
        b1 = b1e[e_ % 2]
        b1k = "b1_%d" % (e_ % 2)
        R.op("sp", lambda e, e_=e_, b1=b1: e.dma_start(out=b1, in_=b1_d[:, e_ * 32:(e_ + 1) * 32]), writes=[b1k], dma=True)
        pend = []
        it = 0
        for u in range(8):
            rg, rgk = ring_load(w1_v[e_][:, :, u * 256:(u + 1) * 256])
            ru, ruk = ring_load(w1_v[e_][:, :, D + u * 256:D + (u + 1) * 256])
            for fq in range(2):
                fc = u * 2 + fq
                for th in range(2):
                    pb = it % 2
                    it += 1
                    for c in range(NCH):
                        R.op("pe", lambda e, c=c, fq=fq, th=th, rg=rg: e.matmul(PS(2 + th), lhsT=rg[:, c, fq * 128:(fq + 1) * 128], rhs=h2T[:, c, th * 512:(th + 1) * 512],
                                                                               start=(c == 0), stop=(c == NCH - 1)), reads=[rgk, "h2T"], writes=[pk(2 + th)])
                    for c in range(NCH):
                        R.op("pe", lambda e, c=c, fq=fq, th=th, ru=ru: e.matmul(PS(4 + th), lhsT=ru[:, c, fq * 128:(fq + 1) * 128], rhs=h2T[:, c, th * 512:(th + 1) * 512],
                                                                               start=(c == 0), stop=(c == NCH - 1)), reads=[ruk, "h2T"], writes=[pk(4 + th)])
                    bg = b1[:, fc:fc + 1]
                    bu = b1[:, 16 + fc:16 + fc + 1]
                    gk_, sk_, uk_ = "gtt%d" % pb, "sgt%d" % pb, "utt%d" % pb
                    R.op("dve", lambda e, th=th, bg=bg, pb=pb: e.tensor_scalar(out=gtt[pb], in0=PS(2 + th), scalar1=bg, scalar2=7.0, op0=ALU.add, op1=ALU.min),
                         reads=[pk(2 + th), b1k], writes=[gk_])
                    R.op("act", lambda e, pb=pb: e.activation(out=sgt[pb], in_=gtt[pb], func=AF.Sigmoid, scale=1.702), reads=[gk_], writes=[sk_])
                    R.op("dve", lambda e, th=th, bu=bu, pb=pb: e.tensor_scalar(out=utt[pb], in0=PS(4 + th), scalar1=bu, scalar2=7.0, op0=ALU.add, op1=ALU.min),
                         reads=[pk(4 + th), b1k], writes=[uk_])
                    R.op("dve", lambda e, pb=pb: e.tensor_scalar(out=utt[pb], in0=utt[pb], scalar1=-7.0, scalar2=1.0, op0=ALU.max, op1=ALU.add), reads=[uk_], writes=[uk_])
                    R.op("dve", lambda e, pb=pb: e.tensor_tensor(out=gtt[pb], in0=gtt[pb], in1=sgt[pb], op=ALU.mult), reads=[gk_, sk_], writes=[gk_])
                    for p_ in pend:
                        p_()
                    pend = [lambda th=th, fc=fc, pb=pb, gk_=gk_, uk_=uk_: R.op(
                        "dve", lambda e: e.tensor_tensor(out=actT[:, fc, th * 512:(th + 1) * 512], in0=utt[pb], in1=gtt[pb], op=ALU.mult),
                        reads=[uk_, gk_], writes=["actT"])]
        for p_ in pend:
            p_()
        for u in range(8):
            r2, r2k = ring_load(w2_v[e_][:, :, u * 256:(u + 1) * 256])
            for o in range(NOWN):
                bk = 6 + (o % 2)
                for fc in range(NCH):
                    R.op("pe", lambda e, o=o, fc=fc, bk=bk, r2=r2: e.matmul(PS(bk, 0, 256), lhsT=actT[:, fc, o * 128:(o + 1) * 128], rhs=r2[:, fc, :],
                                                                           start=(fc == 0), stop=(fc == NCH - 1)), reads=["actT", r2k], writes=[pk(bk)])
                cols = slice(u * 256, (u + 1) * 256)
                R.op("dve", lambda e, o=o, bk=bk, cols=cols: e.tensor_tensor(out=t1[o % 2], in0=PS(bk, 0, 256), in1=gt2b[:, cols], op=ALU.mult),
                     reads=[pk(bk), "gt2b"], writes=["t1"])
                R.op("dve", lambda e, o=o, cols=cols, e_=e_: e.scalar_tensor_tensor(out=x1[:, o, cols], in0=t1[o % 2], scalar=wt[:, o, e_:e_ + 1], in1=x1[:, o, cols],
                                                                                  op0=ALU.mult, op1=ALU.add),
                     reads=["t1", "wt", "x1_%d" % o], writes=["x1_%d" % o])
    R.barrier()
    gfb = actT.rearrange("p f t -> p (f t)")[:, 0:4096].bitcast(F32)
    R.op("sp", lambda e: e.dma_start(out=gfb, in_=pbc(gf_d)), writes=["gfb"], dma=True)
    for o in range(NOWN):
        xk = "x1_%d" % o
        R.op("pool", lambda e: e.memset(sm2[:, 0:1], 0.0), writes=["ss0"])
        R.op("act", lambda e, o=o: e.activation(out=h2f, in_=x1[:, o, :], func=AF.Square, accum_out=sm2[:, 0:1]), reads=[xk, "ss0"], writes=["h2f", "ss0"])
        rstd_op(sm2[:, 2:3], sm2[:, 0:1], float(D), ["ss0"], "rs0")
        R.op("dve", lambda e, o=o: e.scalar_tensor_tensor(out=x1[:, o, :], in0=x1[:, o, :], scalar=sm2[:, 2:3], in1=gfb, op0=ALU.mult, op1=ALU.mult),
             reads=[xk, "rs0", "gfb"], writes=[xk])
        oo = R.op("sp", lambda e, o=o: e.dma_start(out=out_d[o * 128:(o + 1) * 128, :], in_=x1[:, o, :]), reads=[xk], dma=True)
        R.final_ops.append(oo)
    if debug is not None and debug.startswith("moe"):
        for q in range(4):
            dump(x1[:, 0, q * 512:(q + 1) * 512], "x1_0", q * 512, 512)
    return finish()


def _consts():
    r = np.arange(128)
    ident = np.eye(128, dtype=np.float32)
    tri_f = (r[:, None] <= r[None, :]).astype(np.float32)
    tri_b = (r[:, None] >= r[None, :]).astype(np.float32)
    nm_f = np.where(r[:, None] <= r[None, :], 0.0, NEG).astype(np.float32)
    nm_b = np.where(r[:, None] >= r[None, :], 0.0, NEG).astype(np.float32)
    ones = np.ones((128, 128), np.float32)
    iota_c = np.tile(np.arange(512, dtype=np.float32)[None, :], (128, 1))
    iota_p = r.astype(np.float32)[:, None]
    return np.ascontiguousarray(np.concatenate([ident, tri_f, tri_b, nm_f, nm_b, ones, iota_c, iota_p], axis=1))


def _rope_tables():
    rows = 64
    row = np.repeat(np.arange(rows, dtype=np.float32), 64)
    col = np.tile(np.arange(64, dtype=np.float32), rows)
    inv = (np.float32(10000.0) ** (-np.arange(0, 64, 2, dtype=np.float32) / np.float32(64))).astype(np.float32)
    ang = np.concatenate([row[:, None] * inv, col[:, None] * inv], axis=-1).astype(np.float32)
    return np.cos(ang).astype(np.float32), np.sin(ang).astype(np.float32)


def slot_chunks(j):
    pre = list(range(0, 8 * j))
    post = list(range(31, 8 * j + 7, -1))
    own = list(range(8 * j, 8 * j + 8))
    return pre, post, own


def make_in_maps(inp):
    f = lambda a: np.ascontiguousarray(np.asarray(a, dtype=np.float32))
    x, c, ctx, c_ctx = f(inp["x"]), f(inp["c"]), f(inp["ctx"]), f(inp["c_ctx"])
    cos, sin = _rope_tables()
    consts = _consts()
    fm = lambda v, n: np.ascontiguousarray(v.reshape(n, 128).T)
    shared = {
        "bmod": fm(f(inp["b_mod"])[0], 96),
        "g1fm": fm(f(inp["g_norm1"])[0], 16),
        "g2fm": fm(f(inp["g_norm2"])[0], 16),
        "w_mod": f(inp["w_mod"])[0],
        "w_in": f(inp["w_in"])[0],
        "b_in": f(inp["b_in"]),
        "bfm": np.ascontiguousarray(np.concatenate([fm(f(inp["b_in"])[0, MQ0:MQ0 + 512], 4), fm(f(inp["b_in"])[0, MK0:MK0 + 512], 4)], axis=1)),
        "g_q": f(inp["g_q"]), "g_k": f(inp["g_k"]), "g_mlstm": f(inp["g_mlstm"]),
        "w_out": f(inp["w_out"])[0],
        "g_norm2": f(inp["g_norm2"]), "g_final": f(inp["g_final"])[None, :],
        "w_router": f(inp["w_router"])[0], "b_router": f(inp["b_router"]),
        "w1": inp["w1"],
        "b1fm": np.ascontiguousarray(f(inp["b1"])[0].reshape(NEXP, 32, 128).transpose(2, 0, 1).reshape(128, NEXP * 32)),
        "w2": inp["w2"], "b2": f(inp["b2"])[0],
        "consts": consts,
    }
    maps = []
    for core in range(8):
        b, j = core // 4, core % 4
        pre, post, own = slot_chunks(j)
        xs = np.empty((NSLOT * 128, D), np.float32)
        rope = np.empty((NSLOT * 128, 128), np.float32)
        gmask = np.zeros((NSLOT, 16), np.float32)
        xs[0:256] = ctx[b]
        rope[0:256, 0:64] = 1.0
        rope[0:256, 64:128] = 0.0
        gmask[0:2, 0:8] = 0.0
        gmask[0:2, 8:16] = -1.0
        s = 2
        for kind, lst in (("pre", pre), ("post", post), ("own", own)):
            for ch in lst:
                xs[s * 128:(s + 1) * 128] = x[b, ch * 128:(ch + 1) * 128]
                rope[s * 128:(s + 1) * 128, 0:64] = cos[ch * 128:(ch + 1) * 128]
                rope[s * 128:(s + 1) * 128, 64:128] = sin[ch * 128:(ch + 1) * 128]
                fa = kind in ("pre", "own")
                ba = kind in ("post", "own")
                gmask[s, 0:4] = 0.0 if fa else NEG
                gmask[s, 4:8] = 0.0 if ba else NEG
                gmask[s, 8:12] = -1.0 if fa else 0.0
                gmask[s, 12:16] = -1.0 if ba else 0.0
                s += 1
        assert s == NSLOT
        m = dict(shared)
        m["xs"] = xs
        m["rope"] = rope
        m["gmask"] = np.ascontiguousarray(np.tile(gmask.reshape(1, NSLOT * 16), (128, 1)))
        m["cfm"] = np.ascontiguousarray(np.concatenate([fm(c[b], 16), fm(c_ctx, 16)], axis=1))
        maps.append(m)
    return maps


_CACHE = {}


def kernel(**inputs):
    if "nc" not in _CACHE:
        _CACHE["nc"] = build()
    nc, es, declared = _CACHE["nc"]
    maps = make_in_maps(inputs)
    maps = [{k: v for k, v in m.items() if k in declared} for m in maps]
    res = run_bass_kernel_spmd(nc, maps, core_ids=list(range(8)))
    out = np.empty((2, 4096, D), np.float32)
    for core in range(8):
        b, j = core // 4, core % 4
        out[b, j * 1024:(j + 1) * 1024] = res.results[core]["out"]
    return out
```

```python
import numpy as np
import ml_dtypes
from contextlib import ExitStack
import concourse.bass as bass
import concourse.mybir as mybir
from concourse.bass_utils import run_bass_kernel_spmd

F32 = mybir.dt.float32
BF16 = mybir.dt.bfloat16
ALU = mybir.AluOpType
AF = mybir.ActivationFunctionType
AX = mybir.AxisListType

D = 2048
NCH = 16
NSLOT = 34
NOWN = 8
NOTH = 26
EPS = 1e-6
NEG = -30000.0
CAP = 512
NEXP = 32
Q0, K0, V0, MQ0, MK0, MV0, MO0, MI0, MF0 = 0, 1024, 1280, 1536, 2048, 2560, 3584, 4608, 4616


class Op:
    __slots__ = ("eng", "fn", "deps", "is_dma", "signal", "count", "semkey", "value", "name")

    def __init__(self, eng, fn, is_dma, name=""):
        self.eng = eng
        self.fn = fn
        self.deps = []
        self.is_dma = is_dma
        self.signal = False
        self.count = None
        self.semkey = None
        self.value = None
        self.name = name


class Rec:
    ENG = ["pe", "act", "dve", "pool", "sp"]
    NS = 8

    def __init__(self):
        self.streams = {e: [] for e in self.ENG}
        self.last_w = {}
        self.readers = {}
        self.pending = {e: [] for e in self.ENG}
        self.dma_ops = {e: [] for e in self.ENG}
        self.final_ops = []

    def op(self, eng, fn, reads=(), writes=(), dma=False, name=""):
        o = Op(eng, fn, dma, name)
        deps = []
        for k in reads:
            w = self.last_w.get(k)
            if w is not None:
                if not (w.eng == eng and not w.is_dma and eng == "pe"):
                    deps.append(w)
        for k in writes:
            w = self.last_w.get(k)
            if w is not None and (w.eng != eng or w.is_dma):
                deps.append(w)
            for r in self.readers.get(k, ()):
                if r.eng != eng or r.is_dma or eng != "pe":
                    if r is not o:
                        deps.append(r)
        deps.extend(self.pending[eng])
        self.pending[eng] = []
        if dma:
            lst = self.dma_ops[eng]
            if len(lst) >= self.NS:
                deps.append(lst[len(lst) - self.NS])
            lst.append(o)
        o.deps = deps
        for k in writes:
            self.last_w[k] = o
            self.readers[k] = []
        for k in reads:
            self.readers.setdefault(k, []).append(o)
        self.streams[eng].append(o)
        return o

    def barrier(self):
        lasts = []
        for e in self.ENG:
            if self.streams[e]:
                lasts.append(self.streams[e][-1])
            lasts.extend(self.dma_ops[e][-self.NS:])
        for e in self.ENG:
            self.pending[e] = [o for o in lasts if (o.eng != e or o.is_dma)]
        self.last_w = {}
        self.readers = {}

    def emit(self, nc, block):
        for e in self.ENG:
            for o in self.streams[e]:
                for d in o.deps:
                    d.signal = True
        for o in self.final_ops:
            o.signal = True
        nsem = {}
        for e in self.ENG:
            c = 0
            ndma = 0
            for o in self.streams[e]:
                if o.is_dma:
                    slot = ndma % self.NS
                    o.semkey = ("dma", e, slot)
                    o.value = 16 * (ndma // self.NS + 1)
                    ndma += 1
                    o.signal = True
                elif o.signal:
                    c += 1
                    o.semkey = ("eng", e)
                    o.value = c
        sems = {}

        def sem(key):
            if key not in sems:
                sems[key] = self._es.enter_context(nc.semaphore("s_" + "_".join(str(k) for k in key)))
            return sems[key]

        final_ops = self.final_ops

        def run(ename, eh):
            seen = {}
            for o in self.streams[ename]:
                need = {}
                for d in o.deps:
                    if need.get(d.semkey, 0) < d.value:
                        need[d.semkey] = d.value
                for k, v in need.items():
                    if seen.get(k, 0) < v:
                        eh.wait_ge(sem(k), v)
                        seen[k] = v
                ins = o.fn(eh)
                if o.signal:
                    ins.then_inc(sem(o.semkey), 16 if o.is_dma else 1)
            if ename == "sp":
                for o in final_ops:
                    if seen.get(o.semkey, 0) < o.value:
                        eh.wait_ge(sem(o.semkey), o.value)
                        seen[o.semkey] = o.value

        for e in self.ENG:
            sem(("eng", e))
            for s in range(self.NS):
                if e in ("sp", "pool", "act"):
                    sem(("dma", e, s))

        @block.tensor
        def _(eh):
            run("pe", eh)

        @block.scalar
        def _(eh):
            run("act", eh)

        @block.vector
        def _(eh):
            run("dve", eh)

        @block.gpsimd
        def _(eh):
            run("pool", eh)

        @block.sync
        def _(eh):
            run("sp", eh)


class Arena:
    def __init__(self, t, n):
        self.t = t
        self.n = n
        self.off = 0

    def f(self, cols):
        lo = self.off
        self.off += cols
        assert self.off <= self.n, ("arena overflow", self.off, self.n)
        return self.t[:, lo:lo + cols]

    def b(self, cols):
        c2 = (cols + 1) // 2
        return self.f(c2).bitcast(BF16)[:, 0:cols]


def pbc(ap):
    v = ap.partition_broadcast(128)
    return v[:, 0, :]


def build(debug=None, n_oth=NOTH, n_exp=NEXP):
    nc = bass.Bass("TRN2", target_bir_lowering=False)
    R = Rec()
    es = ExitStack()
    R._es = es
    declared = []
    big = debug is None or debug.startswith("moe")

    def din(name, shape, dt=F32):
        if name in ("w1", "w2") and not big:
            return None
        declared.append(name)
        return nc.dram_tensor(name, list(shape), dt, kind="ExternalInput").ap()

    xs_d = din("xs", [NSLOT * 128, D])
    rope_d = din("rope", [NSLOT * 128, 128])
    gmask_d = din("gmask", [128, NSLOT * 16])
    cfm_d = din("cfm", [128, 32])
    bmod_d = din("bmod", [128, 96])
    g1_d = din("g1fm", [128, 16])
    g2fm_d = din("g2fm", [128, 16])
    wmod_d = din("w_mod", [D, 6 * D])
    win_d = din("w_in", [D, 4624])
    bin_d = din("b_in", [1, 4624])
    bfm_d = din("bfm", [128, 8])
    gq_d = din("g_q", [1, 128])
    gk_d = din("g_k", [1, 128])
    gm_d = din("g_mlstm", [1, 1024])
    wout_d = din("w_out", [D, D])
    g2_d = din("g_norm2", [1, D])
    gf_d = din("g_final", [1, D])
    wr_d = din("w_router", [D, NEXP])
    br_d = din("b_router", [1, NEXP])
    w1_d = din("w1", [NEXP, D, 2 * D])
    b1_d = din("b1fm", [128, NEXP * 32])
    w2_d = din("w2", [NEXP, D, D])
    b2_d = din("b2", [NEXP, D])
    cst_d = din("consts", [128, 6 * 128 + 512 + 1])
    out_d = nc.dram_tensor("out", [NOWN * 128, D], F32, kind="ExternalOutput").ap()
    dbg_d = None
    if debug is not None:
        dbg_d = nc.dram_tensor("dbg", [128, 8192], F32, kind="ExternalOutput").ap()

    NF = 52500
    fa_t = es.enter_context(nc.sbuf_tensor("fa", [128, NF], F32))
    A = Arena(fa_t, NF)
    pT = es.enter_context(nc.psum_tensor("pT", [128, 2048], BF16))
    psum = [None, None] + [es.enter_context(nc.psum_tensor("ps%d" % i, [128, 512], F32)) for i in range(2, 8)]

    def PS(i, lo=0, n=512):
        return psum[i][:, lo:lo + n]

    def pk(i):
        return "ps%d" % i

    def finish():
        with nc.Block() as block:
            R.emit(nc, block)
        return nc, es, declared

    def dump(ap, key, lo, n):
        o = R.op("sp", lambda e: e.dma_start(out=dbg_d[:, lo:lo + n], in_=ap), reads=[key], dma=True)
        R.final_ops.append(o)

    dbgf = None
    if debug is not None:
        dbgf = A.f(512)

    def dump_bf(ap, key, lo, n):
        for p0 in range(0, n, 512):
            m = min(512, n - p0)
            R.op("dve", lambda e, p0=p0, m=m: e.tensor_copy(out=dbgf[:, 0:m], in_=ap[:, p0:p0 + m]), reads=[key], writes=["dbgf"])
            dump(dbgf[:, 0:m], "dbgf", lo + p0, m)

    cst = A.f(6 * 128 + 512 + 1)
    ident = cst[:, 0:128]
    tri_f = cst[:, 128:256]
    tri_b = cst[:, 256:384]
    nm_f = cst[:, 384:512]
    nm_b = cst[:, 512:640]
    ones = cst[:, 640:768]
    iota_c = cst[:, 768:1280]
    iota_p = cst[:, 1280:1281]
    identb = A.b(128)
    onesb = A.b(128)
    modT = A.f(192).rearrange("p (a b) -> p a b", b=2)
    gml = A.f(16)
    gmc = A.f(16)
    g1 = A.f(16)
    cfm = A.f(32)
    bmod = A.f(96)
    bfm = A.f(8)
    csT = A.b(32).rearrange("p (a b) -> p a b", b=2)
    stC = [A.f(257) for _ in range(8)]
    stCb = [A.b(258)[:, 0:257] for _ in range(8)]
    epsb = A.f(2)
    R.op("pool", lambda e: e.memset(epsb[:, 0:1], EPS), writes=["epsb"])
    R.op("pool", lambda e: e.memset(epsb[:, 1:2], 1.0), writes=["epsb"])
    gmask = A.f(NSLOT * 16).rearrange("p (s g) -> p s g", g=16)

    R.op("sp", lambda e: e.dma_start(out=cst, in_=cst_d), writes=["cst"], dma=True)
    R.op("sp", lambda e: e.dma_start(out=cfm, in_=cfm_d), writes=["cfm"], dma=True)
    R.op("sp", lambda e: e.dma_start(out=bmod, in_=bmod_d), writes=["bmod"], dma=True)
    R.op("sp", lambda e: e.dma_start(out=g1, in_=g1_d), writes=["g1"], dma=True)
    R.op("sp", lambda e: e.dma_start(out=bfm, in_=bfm_d), writes=["bfm"], dma=True)
    R.op("sp", lambda e: e.dma_start(out=gmask.rearrange("p s g -> p (s g)"), in_=gmask_d), writes=["gmask"], dma=True)
    R.op("dve", lambda e: e.tensor_copy(out=identb, in_=ident), reads=["cst"], writes=["identb"])
    R.op("dve", lambda e: e.tensor_copy(out=onesb, in_=ones), reads=["cst"], writes=["onesb"])
    for j in range(8):
        R.op("pool", lambda e, j=j: e.memset(stC[j], 0.0), writes=["stC%d" % j])
        R.op("pool", lambda e, j=j: e.memset(stCb[j], 0.0), writes=["stCb%d" % j])

    R.op("act", lambda e: e.activation(out=csT[:, :, 0], in_=cfm[:, 0:16], func=AF.Silu), reads=["cfm"], writes=["csT"])
    R.op("act", lambda e: e.activation(out=csT[:, :, 1], in_=cfm[:, 16:32], func=AF.Silu), reads=["cfm"], writes=["csT"])
    mark0 = A.off
    wm = [A.b(16 * 512).rearrange("p (c n) -> p c n", n=512) for _ in range(2)]
    wmod_v = wmod_d.rearrange("(c p) n -> p c n", p=128)
    PM = psum[7][:, 0:192].rearrange("p (a b) -> p a b", b=2)

    def mod_block(blk):
        buf = wm[blk % 2]
        key = "wm%d" % (blk % 2)
        R.op("pool", lambda e: e.dma_start(out=buf, in_=wmod_v[:, :, blk * 512:(blk + 1) * 512]), writes=[key], dma=True)
        for q in range(4):
            cc = blk * 4 + q
            for c in range(NCH):
                R.op("pe", lambda e, c=c, q=q, cc=cc: e.matmul(PM[:, cc, :], lhsT=buf[:, c, q * 128:(q + 1) * 128], rhs=csT[:, c, :],
                                                             start=(c == 0), stop=(c == NCH - 1)),
                     reads=[key, "csT"], writes=[pk(7)])
        R.op("dve", lambda e: e.tensor_tensor(out=modT[:, blk * 4:blk * 4 + 4, :], in0=PM[:, blk * 4:blk * 4 + 4, :],
                                              in1=bmod[:, blk * 4:blk * 4 + 4].unsqueeze(2).to_broadcast([128, 4, 2]), op=ALU.add),
             reads=[pk(7), "bmod"], writes=["modT"])

    for blk in range(24):
        mod_block(blk)
    R.op("dve", lambda e: e.scalar_tensor_tensor(out=gml, in0=modT[:, 16:32, 0], scalar=1.0, in1=g1, op0=ALU.add, op1=ALU.mult),
         reads=["modT", "g1"], writes=["gml"])
    R.op("dve", lambda e: e.scalar_tensor_tensor(out=gmc, in0=modT[:, 16:32, 1], scalar=1.0, in1=g1, op0=ALU.add, op1=ALU.mult),
         reads=["modT", "g1"], writes=["gmc"])
    if debug == "mod":
        dump(modT.rearrange("p a b -> p (a b)"), "modT", 0, 192)
        dump(gml, "gml", 192, 16)
        return finish()
    R.barrier()
    A.off = mark0

    win_v = win_d.rearrange("(c p) n -> p c n", p=128)
    SC = 128.0 ** -0.5
    WCOLS = 2064
    mark_mix = A.off
    Wt = A.b(16 * WCOLS).rearrange("p (c n) -> p c n", n=WCOLS)
    Wflat = Wt.rearrange("p c n -> p (c n)")
    mark_w_end = A.off
    W_regs = A
    bo = A.f(WCOLS)
    gkb = A.f(128)
    gqb = A.f(128)
    xt0 = A.f(D)
    xt = [xt0, xt0]
    xsb = A.b(D)
    hT = [A.b(D).rearrange("p (c t) -> p c t", t=128) for _ in range(2)]
    kTst = A.b(2 * NSLOT * 128).rearrange("p (g t) -> p g t", g=2)
    Vst = A.b(NSLOT * 256).rearrange("p (s v) -> p s v", v=256)
    sm = A.f(64)
    rp = [A.f(128) for _ in range(2)]
    kf = [A.f(256) for _ in range(2)]
    kn = A.f(256)
    rt = [A.f(128) for _ in range(4)]
    krot = A.b(256)
    NB_ = 2
    Kt = [A.b(512) for _ in range(NB_)]
    Vx = [A.b(4 * 258).rearrange("p (h v) -> p h v", v=258) for _ in range(NB_)]
    Gt = [A.f(16) for _ in range(NB_)]
    cq = [A.f(64) for _ in range(NB_)]
    expb = [A.f(8) for _ in range(NB_)]
    Kw = [A.b(128) for _ in range(2)]

    def load_w(segs):
        off = 0
        for (lo, hi) in segs:
            n = hi - lo
            R.op("pool", lambda e, off=off, lo=lo, hi=hi, n=n: e.dma_start(out=Wt[:, :, off:off + n], in_=win_v[:, :, lo:hi]), writes=["W"], dma=True)
            off += n

    load_w([(K0, K0 + 512), (MK0, MK0 + 1536), (MI0, MI0 + 16)])
    R.op("sp", lambda e: e.dma_start(out=bo[:, 0:512], in_=pbc(bin_d[:, K0:K0 + 512])), writes=["bo"], dma=True)
    R.op("sp", lambda e: e.dma_start(out=bo[:, 512:2048], in_=pbc(bin_d[:, MK0:MK0 + 1536])), writes=["bo"], dma=True)
    R.op("sp", lambda e: e.dma_start(out=bo[:, 2048:2064], in_=pbc(bin_d[:, MI0:MI0 + 16])), writes=["bo"], dma=True)
    R.op("sp", lambda e: e.dma_start(out=gkb, in_=pbc(gk_d)), writes=["gkb"], dma=True)
    R.op("sp", lambda e: e.dma_start(out=gqb, in_=pbc(gq_d)), writes=["gqb"], dma=True)
    R.op("dve", lambda e: e.tensor_scalar(out=bo[:, 512:1024], in0=bo[:, 512:1024], scalar1=SC, scalar2=None, op0=ALU.mult), reads=["bo"], writes=["bo"])
    R.op("dve", lambda e: e.tensor_scalar(out=gqb, in0=gqb, scalar1=SC, scalar2=None, op0=ALU.mult), reads=["gqb"], writes=["gqb"])
    R.op("dve", lambda e: e.tensor_scalar(out=bfm[:, 4:8], in0=bfm[:, 4:8], scalar1=SC, scalar2=None, op0=ALU.mult), reads=["bfm"], writes=["bfm"])
    for b_ in range(NB_):
        R.op("pool", lambda e, b_=b_: e.memset(Vx[b_][:, :, 256:257], 1.0), writes=["Vx%d" % b_])

    def rstd_op(dst, src, n_el, keys_r, key_w):
        R.op("act", lambda e: e.activation(out=dst, in_=src, func=AF.Ln, scale=1.0 / n_el, bias=epsb[:, 0:1]), reads=keys_r + ["epsb"], writes=[key_w])
        R.op("act", lambda e: e.activation(out=dst, in_=dst, func=AF.Exp, scale=-0.5), reads=[key_w], writes=[key_w])

    def make_hT(s, b2=None):
        b2 = s % 2 if b2 is None else b2
        is_ctx = s < 2
        xk = "xt"
        R.op("sp", lambda e: e.dma_start(out=xt[b2], in_=xs_d[s * 128:(s + 1) * 128, :]), writes=[xk], dma=True)
        R.op("pool", lambda e: e.memset(sm[:, b2:b2 + 1], 0.0), writes=["ss%d" % b2])
        jk = hT[b2].rearrange("p c t -> p (c t)")
        R.op("act", lambda e: e.activation(out=jk, in_=xt[b2], func=AF.Square, accum_out=sm[:, b2:b2 + 1]), reads=[xk, "ss%d" % b2], writes=["hT%d" % b2, "ss%d" % b2])
        rstd_op(sm[:, 2 + b2:3 + b2], sm[:, b2:b2 + 1], float(D), ["ss%d" % b2], "rs%d" % b2)
        R.op("dve", lambda e: e.tensor_scalar(out=xsb, in0=xt[b2], scalar1=sm[:, 2 + b2:3 + b2], scalar2=None, op0=ALU.mult),
             reads=[xk, "rs%d" % b2], writes=["xsb"])
        for c in range(NCH):
            R.op("pe", lambda e, c=c: e.transpose(out=pT[:, c * 128:(c + 1) * 128], in_=xsb[:, c * 128:(c + 1) * 128], identity=identb),
                 reads=["xsb", "identb"], writes=["pT%d" % (c // 8)])
        gm = gmc if is_ctx else gml
        w = 1 if is_ctx else 0
        hk = "hT%d" % b2
        for c in range(NCH):
            R.op("act", lambda e, c=c: e.activation(out=hT[b2][:, c, :], in_=pT[:, c * 128:(c + 1) * 128], func=AF.Identity,
                                                   scale=gm[:, c:c + 1], bias=modT[:, c, w:w + 1]),
                 reads=["pT%d" % (c // 8), "gml", "gmc", "modT"], writes=[hk])
        return hT[b2], hk

    def proj_tok(h, hk, col_lo, n, bank, wkey="W", w=None):
        w = Wt if w is None else w
        for c in range(NCH):
            R.op("pe", lambda e, c=c: e.matmul(PS(bank, 0, n), lhsT=h[:, c, :], rhs=w[:, c, col_lo:col_lo + n], start=(c == 0), stop=(c == NCH - 1)),
                 reads=[hk, wkey], writes=[pk(bank)])

    def slot_front(s, db, kv=True):
        h, hk = make_hT(s, db)
        if kv:
            R.op("sp", lambda e: e.dma_start(out=rp[db], in_=rope_d[s * 128:(s + 1) * 128, :]), writes=["rp%d" % db], dma=True)
            proj_tok(h, hk, 0, 512, 2)
        proj_tok(h, hk, 512, 512, 3)
        proj_tok(h, hk, 1024, 512, 4)
        proj_tok(h, hk, 1536, 512, 5)
        proj_tok(h, hk, 2048, 16, 6)
        if kv:
            R.op("dve", lambda e: e.tensor_tensor(out=kf[db], in0=PS(2, 0, 256), in1=bo[:, 0:256], op=ALU.add), reads=[pk(2), "bo"], writes=["kf%d" % db])
            R.op("dve", lambda e: e.tensor_tensor(out=Vst[:, s, :], in0=PS(2, 256, 256), in1=bo[:, 256:512], op=ALU.add), reads=[pk(2), "bo"], writes=["Vst"])
        R.op("dve", lambda e: e.scalar_tensor_tensor(out=Kt[db], in0=PS(3), scalar=SC, in1=bo[:, 512:1024], op0=ALU.mult, op1=ALU.add),
             reads=[pk(3), "bo"], writes=["Kt%d" % db])
        for half in range(2):
            R.op("dve", lambda e, half=half: e.tensor_tensor(out=Vx[db][:, 2 * half:2 * half + 2, 0:256],
                                                             in0=PS(4 + half).rearrange("p (h v) -> p h v", v=256),
                                                             in1=bo[:, 1024 + 512 * half:1536 + 512 * half].rearrange("p (h v) -> p h v", v=256), op=ALU.add),
                 reads=[pk(4 + half), "bo"], writes=["Vx%d" % db])
        R.op("dve", lambda e: e.tensor_tensor(out=Gt[db], in0=PS(6, 0, 16), in1=bo[:, 2048:2064], op=ALU.add), reads=[pk(6), "bo"], writes=["G%d" % db])
        return h, hk

    def slot_back(s, db, kv=True):
        if kv:
            kfb, kfk = kf[db], "kf%d" % db
            R.op("pool", lambda e: e.memset(sm[:, 4:6], 0.0), writes=["kss"])
            for g in range(2):
                R.op("act", lambda e, g=g: e.activation(out=kn[:, g * 128:(g + 1) * 128], in_=kfb[:, g * 128:(g + 1) * 128], func=AF.Square, accum_out=sm[:, 4 + g:5 + g]),
                     reads=[kfk, "kss"], writes=["kn", "kss"])
            rstd_op(sm[:, 8:10], sm[:, 4:6], 128.0, ["kss"], "krs")
            for g in range(2):
                R.op("dve", lambda e, g=g: e.scalar_tensor_tensor(out=kn[:, g * 128:(g + 1) * 128], in0=kfb[:, g * 128:(g + 1) * 128], scalar=sm[:, 8 + g:9 + g],
                                                                  in1=gkb, op0=ALU.mult, op1=ALU.mult), reads=[kfk, "krs", "gkb"], writes=["kn"])
            rope(kn, "kn", krot, "krot", 2, rp[db], "rp%d" % db)
            for g in range(2):
                R.op("pe", lambda e, g=g: e.transpose(out=pT[:, g * 128:(g + 1) * 128], in_=krot[:, g * 128:(g + 1) * 128], identity=identb),
                     reads=["krot", "identb"], writes=["pT0"])
            R.op("act", lambda e: e.activation(out=kTst[:, :, s * 128:(s + 1) * 128], in_=pT[:, 0:256].rearrange("p (g t) -> p g t", g=2), func=AF.Copy),
                 reads=["pT0"], writes=["kTst"])
        chunk_gates(s, db)

    def rope(src, skey, dst, dkey, nh, rpt, rkey):
        v = src.rearrange("p (h i two) -> p h i two", h=nh, two=2)
        o = dst.rearrange("p (h i two) -> p h i two", h=nh, two=2)
        cosb = rpt[:, 0:64].unsqueeze(1).to_broadcast([128, nh, 64])
        sinb = rpt[:, 64:128].unsqueeze(1).to_broadcast([128, nh, 64])
        n = nh * 64
        t = [rt[i][:, 0:n].rearrange("p (h i) -> p h i", h=nh) if n <= 128 else None for i in range(4)]
        if n > 128:
            t = [rtq[i].rearrange("p (h i) -> p h i", h=nh) for i in range(4)]
        x1, x2 = v[:, :, :, 0], v[:, :, :, 1]
        tk = ["rt0", "rt1", "rt2", "rt3"]
        R.op("dve", lambda e: e.tensor_tensor(out=t[0], in0=x1, in1=cosb, op=ALU.mult), reads=[skey, rkey], writes=[tk[0]])
        R.op("dve", lambda e: e.tensor_tensor(out=t[1], in0=x2, in1=sinb, op=ALU.mult), reads=[skey, rkey], writes=[tk[1]])
        R.op("dve", lambda e: e.tensor_tensor(out=t[2], in0=x1, in1=sinb, op=ALU.mult), reads=[skey, rkey], writes=[tk[2]])
        R.op("dve", lambda e: e.tensor_tensor(out=t[3], in0=x2, in1=cosb, op=ALU.mult), reads=[skey, rkey], writes=[tk[3]])
        R.op("dve", lambda e: e.tensor_tensor(out=o[:, :, :, 0], in0=t[0], in1=t[1], op=ALU.subtract), reads=[tk[0], tk[1]], writes=[dkey])
        R.op("dve", lambda e: e.tensor_tensor(out=o[:, :, :, 1], in0=t[2], in1=t[3], op=ALU.add), reads=[tk[2], tk[3]], writes=[dkey])

    def chunk_gates(s, db):
        q_ = cq[db]
        e1, Lf, lgf, ie, imb, gg, wst, dec = [q_[:, 8 * i:8 * i + 8] for i in range(8)]
        ck = "cq%d" % db
        R.op("act", lambda e: e.activation(out=e1, in_=Gt[db][:, 8:16], func=AF.Exp, scale=-1.0), reads=["G%d" % db], writes=[ck + "a"])
        R.op("act", lambda e: e.activation(out=Lf, in_=e1, func=AF.Ln, bias=epsb[:, 1:2]), reads=[ck + "a", "epsb"], writes=[ck + "b"])
        R.op("dve", lambda e: e.tensor_tensor(out=lgf, in0=Lf, in1=gmask[:, s, 8:16], op=ALU.mult), reads=[ck + "b", "gmask"], writes=[ck + "lgf"])
        R.op("dve", lambda e: e.tensor_tensor(out=ie, in0=Gt[db][:, 0:8], in1=gmask[:, s, 0:8], op=ALU.add), reads=["G%d" % db, "gmask"], writes=[ck + "ie"])
        R.op("pe", lambda e: e.matmul(PS(6, 16, 4), lhsT=tri_f, rhs=lgf[:, 0:4], start=True, stop=True), reads=["cst", ck + "lgf"], writes=[pk(6)])
        R.op("pe", lambda e: e.matmul(PS(6, 20, 4), lhsT=tri_b, rhs=lgf[:, 4:8], start=True, stop=True), reads=["cst", ck + "lgf"], writes=[pk(6)])
        R.op("pe", lambda e: e.matmul(PS(6, 24, 8), lhsT=ones, rhs=lgf, start=True, stop=True), reads=["cst", ck + "lgf"], writes=[pk(6)])
        R.op("dve", lambda e: e.tensor_tensor(out=imb, in0=ie, in1=PS(6, 16, 8), op=ALU.subtract), reads=[ck + "ie", pk(6)], writes=[ck + "imb"])
        R.op("dve", lambda e: e.tensor_tensor(out=gg, in0=imb, in1=PS(6, 24, 8), op=ALU.add), reads=[ck + "imb", pk(6)], writes=[ck + "gg"])
        R.op("act", lambda e: e.activation(out=wst, in_=gg, func=AF.Exp), reads=[ck + "gg"], writes=[ck + "wst"])
        R.op("act", lambda e: e.activation(out=dec, in_=PS(6, 24, 8), func=AF.Exp), reads=[pk(6)], writes=[ck + "dec"])
        R.op("act", lambda e: e.activation(out=expb[db], in_=PS(6, 16, 8), func=AF.Exp), reads=[pk(6)], writes=[ck + "expb"])

    def state_step(db, j, refresh_bf=False):
        h = j % 4
        q_ = cq[db]
        wst, dec = q_[:, 48:56], q_[:, 56:64]
        ck = "cq%d" % db
        kb = j % 2
        R.op("dve", lambda e: e.tensor_scalar(out=Kw[kb], in0=Kt[db][:, h * 128:(h + 1) * 128], scalar1=wst[:, j:j + 1], scalar2=None, op0=ALU.mult),
             reads=["Kt%d" % db, ck + "wst"], writes=["Kw%d" % kb])
        ub, uo = (7, 0) if j % 2 == 0 else (6, 32)
        R.op("pe", lambda e: e.matmul(PS(ub, uo, 257), lhsT=Kw[kb], rhs=Vx[db][:, h, 0:257], start=True, stop=True),
             reads=["Kw%d" % kb, "Vx%d" % db], writes=[pk(ub)])
        R.op("dve", lambda e: e.scalar_tensor_tensor(out=stC[j], in0=stC[j], scalar=dec[:, j:j + 1], in1=PS(ub, uo, 257), op0=ALU.mult, op1=ALU.add),
             reads=["stC%d" % j, ck + "dec", pk(ub)], writes=["stC%d" % j])
        if refresh_bf:
            R.op("act", lambda e: e.activation(out=stCb[j], in_=stC[j], func=AF.Copy), reads=["stC%d" % j], writes=["stCb%d" % j])

    rtq = None
    hacc = A.b(NOWN * 1024).rearrange("p (o h v) -> p o h v", o=NOWN, h=4)
    mark_own = A.off
    Wq = A.b(16 * 512).rearrange("p (c n) -> p c n", n=512)
    R.op("pool", lambda e: e.dma_start(out=Wq, in_=win_v[:, :, MQ0:MQ0 + 512]), writes=["Wq"], dma=True)
    qmT = [A.b(512).rearrange("p (h t) -> p h t", h=4) for _ in range(2)]
    kmT = [A.b(512).rearrange("p (h t) -> p h t", h=4) for _ in range(2)]
    lb = A.f(128)
    DTt = A.f(128)
    STt = A.b(128)
    tmpn = A.f(257)
    tot = A.f(257)
    ddr = A.f(2)

    def full_step(s, db, j, o, first):
        h = j % 4
        dirn = j // 4
        TRI = tri_f if dirn == 0 else tri_b
        NM = nm_f if dirn == 0 else nm_b
        q_ = cq[db]
        lgf, imb = q_[:, 16:24], q_[:, 32:40]
        ck = "cq%d" % db
        R.op("dve", lambda e: e.tensor_scalar(out=lb, in0=ones, scalar1=lgf[:, j:j + 1], scalar2=None, op0=ALU.mult), reads=["cst", ck + "lgf"], writes=["lb"])
        R.op("pe", lambda e: e.matmul(PS(4, 0, 128), lhsT=lb, rhs=TRI, start=True, stop=False), reads=["lb", "cst"], writes=[pk(4)])
        R.op("pe", lambda e: e.matmul(PS(4, 0, 128), lhsT=ident, rhs=NM, start=False, stop=True), reads=["cst"], writes=[pk(4)])
        R.op("act", lambda e: e.activation(out=DTt, in_=PS(4, 0, 128), func=AF.Exp, bias=imb[:, j:j + 1]), reads=[pk(4), ck + "imb"], writes=["DT"])
        R.op("pe", lambda e: e.matmul(PS(5, 0, 128), lhsT=kmT[db][:, h, :], rhs=qmT[db][:, h, :], start=True, stop=True), reads=["kmT%d" % db, "qmT%d" % db], writes=[pk(5)])
        R.op("dve", lambda e: e.tensor_tensor(out=STt, in0=PS(5, 0, 128), in1=DTt, op=ALU.mult), reads=[pk(5), "DT"], writes=["ST"])
        R.op("pe", lambda e: e.matmul(PS(2, 0, 257), lhsT=STt, rhs=Vx[db][:, h, 0:257], start=True, stop=True), reads=["ST", "Vx%d" % db], writes=[pk(2)])
        R.op("pe", lambda e: e.matmul(PS(3, 0, 257), lhsT=qmT[db][:, h, :], rhs=stCb[j], start=True, stop=True), reads=["qmT%d" % db, "stCb%d" % j], writes=[pk(3)])
        R.op("act", lambda e: e.activation(out=tmpn, in_=PS(2, 0, 257), func=AF.Copy), reads=[pk(2)], writes=["tmpn"])
        R.op("dve", lambda e: e.scalar_tensor_tensor(out=tot, in0=PS(3, 0, 257), scalar=expb[db][:, j:j + 1], in1=tmpn, op0=ALU.mult, op1=ALU.add),
             reads=[pk(3), ck + "expb", "tmpn"], writes=["tot"])
        R.op("dve", lambda e: e.scalar_tensor_tensor(out=ddr[:, 0:1], in0=tot[:, 256:257], scalar=-1.0, in1=tot[:, 256:257], op0=ALU.mult, op1=ALU.max), reads=["tot"], writes=["dd"])
        R.op("dve", lambda e: e.tensor_scalar(out=ddr[:, 0:1], in0=ddr[:, 0:1], scalar1=1.0, scalar2=None, op0=ALU.max), reads=["dd"], writes=["dd"])
        R.op("dve", lambda e: e.reciprocal(out=ddr[:, 1:2], in_=ddr[:, 0:1]), reads=["dd"], writes=["rr"])
        hk_ = "hacc%d" % o
        if first:
            R.op("dve", lambda e: e.tensor_scalar(out=hacc[:, o, h, :], in0=tot[:, 0:256], scalar1=ddr[:, 1:2], scalar2=None, op0=ALU.mult), reads=["tot", "rr"], writes=[hk_])
        else:
            R.op("dve", lambda e: e.scalar_tensor_tensor(out=hacc[:, o, h, :], in0=tot[:, 0:256], scalar=ddr[:, 1:2], in1=hacc[:, o, h, :], op0=ALU.mult, op1=ALU.add),
                 reads=["tot", "rr", hk_], writes=[hk_])
        state_step(db, j, refresh_bf=True)


    n_own = NOWN if debug != "own" else 2
    visits = [("oth", s_, None) for s_ in range(n_oth)]
    visits += [("own", NOTH + o_, 0) for o_ in range(n_own)] + [("own", NOTH + o_, 1) for o_ in range(n_own - 1, -1, -1)]

    def v_front(vi):
        kind, s_, dirn = visits[vi]
        db = vi % 2
        if kind == "oth":
            slot_front(s_, db, kv=True)
            return
        h_, hk = slot_front(s_, db, kv=(dirn == 0))
        for hh in range(4):
            for c in range(NCH):
                R.op("pe", lambda e, c=c, hh=hh: e.matmul(PS(2, hh * 128, 128), lhsT=Wq[:, c, hh * 128:(hh + 1) * 128], rhs=h_[:, c, :], start=(c == 0), stop=(c == NCH - 1)),
                     reads=["Wq", hk], writes=[pk(2)])
        for hh in range(4):
            R.op("act", lambda e, hh=hh: e.activation(out=qmT[db][:, hh, :], in_=PS(2, hh * 128, 128), func=AF.Identity, bias=bfm[:, hh:hh + 1]), reads=[pk(2), "bfm"], writes=["qmT%d" % db])
        for hh in range(4):
            for c in range(NCH):
                R.op("pe", lambda e, c=c, hh=hh: e.matmul(PS(3, hh * 128, 128), lhsT=Wt[:, c, 512 + hh * 128:512 + (hh + 1) * 128], rhs=h_[:, c, :], start=(c == 0), stop=(c == NCH - 1)),
                     reads=["W", hk], writes=[pk(3)])
        for hh in range(4):
            R.op("act", lambda e, hh=hh: e.activation(out=kmT[db][:, hh, :], in_=PS(3, hh * 128, 128), func=AF.Identity, scale=SC, bias=bfm[:, 4 + hh:5 + hh]), reads=[pk(3), "bfm"], writes=["kmT%d" % db])

    def v_back(vi):
        kind, s_, dirn = visits[vi]
        db = vi % 2
        if kind == "oth":
            slot_back(s_, db, kv=True)
            if s_ == 0:
                for j in range(4):
                    state_step(0, j)
            elif s_ == 1:
                for j in range(8):
                    state_step(1, j)
                for j in range(4, 8):
                    state_step(0, j)
            else:
                for j in range(8):
                    state_step(db, j)
            return
        if vi == n_oth:
            for j in range(8):
                R.op("act", lambda e, j=j: e.activation(out=stCb[j], in_=stC[j], func=AF.Copy), reads=["stC%d" % j], writes=["stCb%d" % j])
        slot_back(s_, db, kv=(dirn == 0))
        for hh in range(4):
            full_step(s_, db, dirn * 4 + hh, s_ - NOTH, first=(dirn == 0))

    nv = len(visits)
    v_front(0)
    for vi in range(nv):
        if vi + 1 < nv and vi != 1:
            v_front(vi + 1)
        v_back(vi)
        if vi == 1 and vi + 1 < nv:
            v_front(vi + 1)
    if debug == "oth":
        s = n_oth - 1
        dump(hT[s % 2].rearrange("p c t -> p (c t)")[:, 0:0], "x", 0, 0) if False else None
        dump_bf(hT[s % 2].rearrange("p c t -> p (c t)"), "hT%d" % (s % 2), 0, 2048)
        dump_bf(kTst[:, :, s * 128:(s + 1) * 128], "kTst", 2048, 256) if False else None
        dump_bf(Vst[:, s, :], "Vst", 2304, 256)
        dump_bf(Kt[s % 2], "Kt%d" % (s % 2), 2560, 512)
        dump(Gt[s % 2], "G%d" % (s % 2), 3072, 16)
        dump(cq[s % 2], "cq%dwst" % (s % 2), 3088, 64)
        for j in range(8):
            dump(stC[j], "stC%d" % j, 3200 + 257 * j, 257)
        dump_bf(krot, "krot", 5300, 256)
        return finish()


    if debug == "own":
        dump_bf(hacc[:, 0, :, :].rearrange("p h v -> p (h v)"), "hacc0", 0, 1024)
        dump_bf(hacc[:, 1, :, :].rearrange("p h v -> p (h v)"), "hacc1", 1024, 1024)
        return finish()


    R.barrier()
    A.off = mark_own
    Wc = Wflat[:, 0:16 * 1024].rearrange("p (c n) -> p c n", n=1024)
    yT = Wflat[:, 16 * 1024:32 * 1024].rearrange("p (k t) -> p k t", t=1024)
    qf = A.f(1024)
    rtq_all = A.f(2048)
    rtq = [rtq_all[:, i * 512:(i + 1) * 512] for i in range(4)]
    qrot = A.b(1024)
    qTc = A.b(1024).rearrange("p (h t) -> p h t", h=8)
    PTt = [A.b(512) for _ in range(2)]
    dsb = A.f(512)
    qss = A.f(16)
    gmb = A.f(1024)

    def load_wc(lo, wd=win_v):
        R.op("pool", lambda e: e.dma_start(out=Wc, in_=wd[:, :, lo:lo + 1024]), writes=["W"], dma=True)

    load_wc(Q0)
    R.op("sp", lambda e: e.dma_start(out=bo[:, 0:1024], in_=pbc(bin_d[:, Q0:Q0 + 1024])), writes=["bo"], dma=True)
    R.op("sp", lambda e: e.dma_start(out=bo[:, 1024:2048], in_=pbc(bin_d[:, MO0:MO0 + 1024])), writes=["bo"], dma=True)
    R.op("sp", lambda e: e.dma_start(out=gmb, in_=pbc(gm_d)), writes=["gmb"], dma=True)
    qf3 = qf.rearrange("p (h d) -> p h d", h=8)
    for o in range(NOWN):
        s = NOTH + o
        b2 = s % 2
        h_, hk = make_hT(s)
        R.op("sp", lambda e, s=s, b2=b2: e.dma_start(out=rp[b2], in_=rope_d[s * 128:(s + 1) * 128, :]), writes=["rp%d" % b2], dma=True)
        proj_tok(h_, hk, 0, 512, 2, w=Wc)
        proj_tok(h_, hk, 512, 512, 3, w=Wc)
        for hf_ in range(2):
            R.op("dve", lambda e, hf_=hf_: e.tensor_tensor(out=qf[:, hf_ * 512:(hf_ + 1) * 512], in0=PS(2 + hf_), in1=bo[:, hf_ * 512:(hf_ + 1) * 512], op=ALU.add),
                 reads=[pk(2 + hf_), "bo"], writes=["qf"])
        R.op("pool", lambda e: e.memset(qss[:, 0:8], 0.0), writes=["qss"])
        for hh in range(8):
            R.op("act", lambda e, hh=hh: e.activation(out=rtq[0][:, 0:128], in_=qf[:, hh * 128:(hh + 1) * 128], func=AF.Square, accum_out=qss[:, hh:hh + 1]),
                 reads=["qf", "qss"], writes=["rt0", "qss"])
        rstd_op(qss[:, 8:16], qss[:, 0:8], 128.0, ["qss"], "qrs")
        R.op("dve", lambda e: e.tensor_tensor(out=qf3, in0=qf3, in1=qss[:, 8:16].unsqueeze(2).to_broadcast([128, 8, 128]), op=ALU.mult), reads=["qf", "qrs"], writes=["qf"])
        R.op("dve", lambda e: e.tensor_tensor(out=qf3, in0=qf3, in1=gqb.unsqueeze(1).to_broadcast([128, 8, 128]), op=ALU.mult), reads=["qf", "gqb"], writes=["qf"])
        rope(qf, "qf", qrot, "qrot", 8, rp[b2], "rp%d" % b2)
        for hh in range(8):
            R.op("pe", lambda e, hh=hh: e.transpose(out=pT[:, hh * 128:(hh + 1) * 128], in_=qrot[:, hh * 128:(hh + 1) * 128], identity=identb),
                 reads=["qrot", "identb"], writes=["pT0"])
        R.op("act", lambda e: e.activation(out=qTc, in_=pT[:, 0:1024].rearrange("p (h t) -> p h t", h=8), func=AF.Copy), reads=["pT0"], writes=["qTc"])
        for g in range(2):
            def s_mm(sk, g=g):
                sb = 2 + (sk % 2)
                R.op("pe", lambda e: e.matmul(PS(sb), lhsT=kTst[:, g, sk * 128:(sk + 1) * 128], rhs=qTc[:, 4 * g:4 * g + 4, :], start=True, stop=True),
                     reads=["kTst", "qTc"], writes=[pk(sb)])
            s_mm(0)
            for sk in range(NSLOT):
                sb = 2 + (sk % 2)
                pb = sk % 2
                if sk + 1 < NSLOT:
                    s_mm(sk + 1)
                R.op("act", lambda e, sb=sb, pb=pb: e.activation(out=PTt[pb], in_=PS(sb), func=AF.Exp), reads=[pk(sb)], writes=["PT%d" % pb])
                R.op("pe", lambda e, g=g, sk=sk, pb=pb: e.matmul(PS(4 + g), lhsT=Vst[:, sk, g * 128:(g + 1) * 128], rhs=PTt[pb], start=(sk == 0), stop=(sk == NSLOT - 1)),
                     reads=["Vst", "PT%d" % pb], writes=[pk(4 + g)])
                R.op("pe", lambda e, g=g, sk=sk, pb=pb: e.matmul(PS(6 + g), lhsT=onesb, rhs=PTt[pb], start=(sk == 0), stop=(sk == NSLOT - 1)),
                     reads=["onesb", "PT%d" % pb], writes=[pk(6 + g)])
            R.op("act", lambda e, g=g: e.activation(out=dsb, in_=PS(6 + g), func=AF.Copy), reads=[pk(6 + g)], writes=["dsb"])
            R.op("dve", lambda e: e.reciprocal(out=dsb, in_=dsb), reads=["dsb"], writes=["dsb"])
            R.op("dve", lambda e, g=g, o=o: e.tensor_tensor(out=yT[:, 4 * g:4 * g + 4, o * 128:(o + 1) * 128], in0=PS(4 + g).rearrange("p (h t) -> p h t", h=4),
                                                          in1=dsb.rearrange("p (h t) -> p h t", h=4), op=ALU.mult),
                 reads=[pk(4 + g), "dsb"], writes=["yT"])
    if debug == "att":
        for k in range(8):
            dump_bf(yT[:, k, 0:256], "yT", k * 256, 256)
        return finish()

    R.barrier()
    load_wc(MO0)
    hn = rtq_all[:, 0:1024]
    ym = qrot
    hn3 = hn.rearrange("p (h v) -> p h v", h=4)
    for o in range(NOWN):
        s = NOTH + o
        h_, hk = make_hT(s)
        proj_tok(h_, hk, 0, 512, 2, w=Wc)
        proj_tok(h_, hk, 512, 512, 3, w=Wc)
        for hf_ in range(2):
            R.op("dve", lambda e, hf_=hf_: e.tensor_tensor(out=qf[:, hf_ * 512:(hf_ + 1) * 512], in0=PS(2 + hf_), in1=bo[:, 1024 + hf_ * 512:1024 + (hf_ + 1) * 512], op=ALU.add),
                 reads=[pk(2 + hf_), "bo"], writes=["qf"])
        R.op("act", lambda e: e.activation(out=qf, in_=qf, func=AF.Sigmoid), reads=["qf"], writes=["qf"])
        R.op("pool", lambda e: e.memset(qss[:, 0:4], 0.0), writes=["qss"])
        for hh in range(4):
            R.op("act", lambda e, hh=hh, o=o: e.activation(out=hn[:, hh * 256:(hh + 1) * 256], in_=hacc[:, o, hh, :], func=AF.Square, accum_out=qss[:, hh:hh + 1]),
                 reads=["hacc%d" % o, "qss"], writes=["hn", "qss"])
        rstd_op(qss[:, 8:12], qss[:, 0:4], 256.0, ["qss"], "qrs")
        R.op("dve", lambda e, o=o: e.tensor_tensor(out=hn3, in0=hacc[:, o, :, :], in1=qss[:, 8:12].unsqueeze(2).to_broadcast([128, 4, 256]), op=ALU.mult),
             reads=["hacc%d" % o, "qrs"], writes=["hn"])
        R.op("pool", lambda e: e.tensor_tensor(out=hn, in0=hn, in1=gmb, op=ALU.mult), reads=["hn", "gmb"], writes=["hn"])
        R.op("dve", lambda e: e.tensor_tensor(out=ym, in0=hn, in1=qf, op=ALU.mult), reads=["hn", "qf"], writes=["qrot"])
        for k in range(8):
            R.op("pe", lambda e, k=k: e.transpose(out=pT[:, k * 128:(k + 1) * 128], in_=ym[:, k * 128:(k + 1) * 128], identity=identb),
                 reads=["qrot", "identb"], writes=["pT0"])
        R.op("act", lambda e, o=o: e.activation(out=yT[:, 8:16, o * 128:(o + 1) * 128], in_=pT[:, 0:1024].rearrange("p (k t) -> p k t", k=8), func=AF.Copy),
             reads=["pT0"], writes=["yT"])
    if debug == "ym":
        for k in range(8):
            dump_bf(yT[:, 8 + k, 0:256], "yT", k * 256, 256)
        return finish()

    R.barrier()
    A.off = mark_w_end
    x1 = A.f(NOWN * D).rearrange("p (o d) -> p o d", o=NOWN)
    bcA = A.f(1024)
    tmpd = [A.f(512) for _ in range(2)]
    lbm = A.f(128)
    wout_v = wout_d.rearrange("(c p) n -> p c n", p=128)

    def make_bc(dst, key, chunk_lo, nchunks, col, src=None, skey="modT"):
        for c in range(nchunks):
            if src is None:
                vec = modT[:, chunk_lo + c, col:col + 1]
            else:
                vec = src[:, chunk_lo + c:chunk_lo + c + 1]
            R.op("dve", lambda e, vec=vec: e.tensor_scalar(out=lbm, in0=ones, scalar1=vec, scalar2=None, op0=ALU.mult), reads=["cst", skey], writes=["lbm"])
            R.op("pe", lambda e, c=c: e.matmul(PS(2, (c % 4) * 128, 128), lhsT=lbm, rhs=ident, start=True, stop=True), reads=["lbm", "cst"], writes=[pk(2)])
            if c % 4 == 3:
                R.op("act", lambda e, c=c: e.activation(out=dst[:, (c - 3) * 128:(c + 1) * 128], in_=PS(2), func=AF.Copy), reads=[pk(2)], writes=[key])

    for o in range(NOWN):
        s = NOTH + o
        R.op("sp", lambda e, s=s, o=o: e.dma_start(out=x1[:, o, :], in_=xs_d[s * 128:(s + 1) * 128, :]), writes=["x1_%d" % o], dma=True)
    for half in range(2):
        load_wc(half * 1024, wd=wout_v)
        make_bc(bcA, "bcA", 32 + half * 8, 8, 0)
        for o in range(NOWN):
            for dh in range(2):
                for k in range(NCH):
                    R.op("pe", lambda e, o=o, dh=dh, k=k: e.matmul(PS(4 + dh), lhsT=yT[:, k, o * 128:(o + 1) * 128], rhs=Wc[:, k, dh * 512:(dh + 1) * 512],
                                                                  start=(k == 0), stop=(k == NCH - 1)), reads=["yT", "W"], writes=[pk(4 + dh)])
                cols = slice(half * 1024 + dh * 512, half * 1024 + (dh + 1) * 512)
                R.op("dve", lambda e, dh=dh: e.tensor_tensor(out=tmpd[dh], in0=PS(4 + dh), in1=bcA[:, dh * 512:(dh + 1) * 512], op=ALU.mult),
                     reads=[pk(4 + dh), "bcA"], writes=["tmpd%d" % dh])
                R.op("pool", lambda e, o=o, dh=dh, cols=cols: e.tensor_tensor(out=x1[:, o, cols], in0=x1[:, o, cols], in1=tmpd[dh], op=ALU.add),
                     reads=["x1_%d" % o, "tmpd%d" % dh], writes=["x1_%d" % o])
    if debug == "x1":
        for q in range(4):
            dump(x1[:, 0, q * 512:(q + 1) * 512], "x1_0", q * 512, 512)
        for q in range(4):
            dump(x1[:, 7, q * 512:(q + 1) * 512], "x1_7", 2048 + q * 512, 512)
        return finish()

    R.barrier()
    h2T = Wflat[:, 0:16 * 1024].rearrange("p (c t) -> p c t", t=1024)
    wfree = Wflat[:, 16 * 1024:16 * 1024 + 16384].bitcast(F32)
    gm2b = wfree[:, 0:2048]
    sh2b = wfree[:, 2048:4096]
    h2f = wfree[:, 4096:6144]
    h2Tr = wfree[:, 6144:8192].rearrange("p (c t) -> p c t", t=128)
    A.off = mark_w_end + NOWN * D
    sm2 = A.f(8)
    g2fm = A.f(16)
    gm2fm = A.f(16)
    wr = A.f(16 * NEXP).rearrange("p (c e) -> p c e", e=NEXP)
    brb = A.f(NEXP)
    wt = A.f(NOWN * NEXP).rearrange("p (o e) -> p o e", e=NEXP)
    rsm = A.f(64)
    ex = A.f(NEXP)
    msk = A.f(NEXP)
    R.op("sp", lambda e: e.dma_start(out=g2fm, in_=g2fm_d), writes=["g2fm"], dma=True)
    R.op("sp", lambda e: e.dma_start(out=wr, in_=wr_d.rearrange("(c p) e -> p c e", p=128)), writes=["wr"], dma=True)
    R.op("sp", lambda e: e.dma_start(out=brb, in_=pbc(br_d)), writes=["brb"], dma=True)
    R.op("dve", lambda e: e.scalar_tensor_tensor(out=gm2fm, in0=modT[:, 64:80, 0], scalar=1.0, in1=g2fm, op0=ALU.add, op1=ALU.mult), reads=["modT", "g2fm"], writes=["gm2fm"])
    make_bc(gm2b, "gm2b", 0, 16, 0, src=gm2fm, skey="gm2fm")
    make_bc(sh2b, "sh2b", 48, 16, 0)
    lg = rsm[:, 0:32]
    top8 = rsm[:, 32:40]
    for o in range(NOWN):
        xk = "x1_%d" % o
        R.op("pool", lambda e: e.memset(sm2[:, 0:1], 0.0), writes=["ss0"])
        R.op("act", lambda e, o=o: e.activation(out=h2f, in_=x1[:, o, :], func=AF.Square, accum_out=sm2[:, 0:1]), reads=[xk, "ss0"], writes=["h2f", "ss0"])
        rstd_op(sm2[:, 2:3], sm2[:, 0:1], float(D), ["ss0"], "rs0")
        R.op("dve", lambda e, o=o: e.scalar_tensor_tensor(out=h2f, in0=x1[:, o, :], scalar=sm2[:, 2:3], in1=gm2b, op0=ALU.mult, op1=ALU.mult),
             reads=[xk, "rs0", "gm2b"], writes=["h2f"])
        R.op("pool", lambda e: e.tensor_tensor(out=h2f, in0=h2f, in1=sh2b, op=ALU.add), reads=["h2f", "sh2b"], writes=["h2f"])
        for c in range(NCH):
            bk = 4 + (c // 4)
            R.op("pe", lambda e, c=c, bk=bk: e.transpose(out=PS(bk, (c % 4) * 128, 128), in_=h2f[:, c * 128:(c + 1) * 128], identity=ident),
                 reads=["h2f", "cst"], writes=[pk(bk)])
        for q in range(4):
            R.op("act", lambda e, q=q: e.activation(out=h2Tr[:, 4 * q:4 * q + 4, :], in_=PS(4 + q).rearrange("p (c t) -> p c t", t=128), func=AF.Copy),
                 reads=[pk(4 + q)], writes=["h2Tr"])
        R.op("dve", lambda e, o=o: e.tensor_copy(out=h2T[:, :, o * 128:(o + 1) * 128], in_=h2Tr), reads=["h2Tr"], writes=["h2T"])
        for c in range(NCH):
            R.op("pe", lambda e, c=c: e.matmul(PS(3, 0, NEXP), lhsT=h2Tr[:, c, :], rhs=wr[:, c, :], start=(c == 0), stop=(c == NCH - 1)), reads=["h2Tr", "wr"], writes=[pk(3)])
        R.op("dve", lambda e: e.tensor_tensor(out=lg, in0=PS(3, 0, NEXP), in1=brb, op=ALU.add), reads=[pk(3), "brb"], writes=["lg"])
        R.op("dve", lambda e: e.max(out=top8, in_=lg), reads=["lg"], writes=["top8"])
        R.op("dve", lambda e: e.tensor_scalar(out=msk, in0=lg, scalar1=top8[:, 3:4], scalar2=None, op0=ALU.is_ge), reads=["lg", "top8"], writes=["msk"])
        R.op("dve", lambda e: e.tensor_scalar(out=rsm[:, 40:41], in0=top8[:, 0:1], scalar1=-1.0, scalar2=None, op0=ALU.mult), reads=["top8"], writes=["nmx"])
        R.op("act", lambda e: e.activation(out=ex, in_=lg, func=AF.Exp, bias=rsm[:, 40:41]), reads=["lg", "nmx"], writes=["ex"])
        R.op("dve", lambda e: e.tensor_tensor(out=ex, in0=ex, in1=msk, op=ALU.mult), reads=["ex", "msk"], writes=["ex"])
        R.op("dve", lambda e: e.reduce_sum(out=rsm[:, 41:42], in_=ex, axis=AX.X), reads=["ex"], writes=["esum"])
        R.op("dve", lambda e: e.reciprocal(out=rsm[:, 42:43], in_=rsm[:, 41:42]), reads=["esum"], writes=["ersum"])
        R.op("dve", lambda e, o=o: e.tensor_scalar(out=wt[:, o, :], in0=ex, scalar1=rsm[:, 42:43], scalar2=None, op0=ALU.mult), reads=["ex", "ersum"], writes=["wt"])
    if debug == "rt":
        dump(wt.rearrange("p o e -> p (o e)"), "wt", 0, 256)
        dump_bf(h2T[:, 0, 0:256], "h2T", 256, 256)
        return finish()

    R.barrier()
    gt2b = wfree[:, 0:2048]
    ring = [wfree[:, 2048 * (1 + i):2048 * (2 + i)].bitcast(BF16).rearrange("p (c n) -> p c n", n=256) for i in range(3)]
    ring.append(A.b(16 * 256).rearrange("p (c n) -> p c n", n=256))
    actT = A.b(16 * 1024).rearrange("p (f t) -> p f t", t=1024)
    b1e = [A.f(32) for _ in range(2)]
    gtt = [A.f(512) for _ in range(2)]
    sgt = [A.f(512), wr.rearrange("p c e -> p (c e)")]
    utt = [A.f(512), A.f(512)]
    t10 = A.f(256)
    t1 = [t10, t10]
    make_bc(gt2b, "gt2b", 80, 16, 0)
    b2g = actT.rearrange("p f t -> p (f t)")[:, 0:4096].bitcast(F32)
    wtT = actT.rearrange("p f t -> p (f t)")[:, 4096:4096 + 2048].bitcast(F32)
    R.op("sp", lambda e: e.dma_start(out=b2g[0:NEXP, :], in_=b2_d), writes=["b2g"], dma=True)
    for o in range(NOWN):
        R.op("pe", lambda e, o=o: e.transpose(out=PS(2, o * 64, 128)[0:NEXP, :] if False else PS(2 + o // 4, (o % 4) * 128, 128)[0:NEXP, :], in_=wt[:, o, :], identity=ident),
             reads=["wt", "cst"], writes=[pk(2 + o // 4)])
    for q in range(2):
        R.op("act", lambda e, q=q: e.activation(out=wtT[0:NEXP, q * 512:(q + 1) * 512], in_=PS(2 + q)[0:NEXP, :], func=AF.Copy), reads=[pk(2 + q)], writes=["wtT"])
    for o in range(NOWN):
        for dq in range(4):
            bk = 4 + (dq % 2)
            R.op("pe", lambda e, o=o, dq=dq, bk=bk: e.matmul(PS(bk), lhsT=wtT[0:NEXP, o * 128:(o + 1) * 128], rhs=b2g[0:NEXP, dq * 512:(dq + 1) * 512], start=True, stop=True),
                 reads=["wtT", "b2g"], writes=[pk(bk)])
            R.op("dve", lambda e, dq=dq, bk=bk: e.tensor_tensor(out=gtt[dq % 2], in0=PS(bk), in1=gt2b[:, dq * 512:(dq + 1) * 512], op=ALU.mult),
                 reads=[pk(bk), "gt2b"], writes=["gtt%d" % (dq % 2)])
            R.op("pool", lambda e, o=o, dq=dq: e.tensor_tensor(out=x1[:, o, dq * 512:(dq + 1) * 512], in0=x1[:, o, dq * 512:(dq + 1) * 512], in1=gtt[dq % 2], op=ALU.add),
                 reads=["x1_%d" % o, "gtt%d" % (dq % 2)], writes=["x1_%d" % o])
    R.barrier()
    if big:
        w1_v = [w1_d[e_].rearrange("(c p) n -> p c n", p=128) for e_ in range(NEXP)]
        w2_v = [w2_d[e_].rearrange("(c p) n -> p c n", p=128) for e_ in range(NEXP)]
    nld = [0]

    def ring_load(src):
        i = nld[0] % 4
        nld[0] += 1
        R.op("pool", lambda e, i=i: e.dma_start(out=ring[i], in_=src), writes=["ring%d" % i], dma=True)
        return ring[i], "ring%d" % i

    for e_ in range(n_exp if big else 0):
        b1 = b1e[e_ % 2]
        b1k = "b1_%d" % (e_ % 2)
        R.op("sp", lambda e, e_=e_, b1=b1: e.dma_start(out=b1, in_=b1_d[:, e_ * 32:(e_ + 1) * 32]), writes=[b1k], dma=True)
        pend = []
        it = 0
        for u in range(8):
            rg, rgk = ring_load(w1_v[e_][:, :, u * 256:(u + 1) * 256])
            ru, ruk = ring_load(w1_v[e_][:, :, D + u * 256:D + (u + 1) * 256])
            for fq in range(2):
                fc = u * 2 + fq
                for th in range(2):
                    pb = it % 2
                    it += 1
                    for c in range(NCH):
                        R.op("pe", lambda e, c=c, fq=fq, th=th, rg=rg: e.matmul(PS(2 + th), lhsT=rg[:, c, fq * 128:(fq + 1) * 128], rhs=h2T[:, c, th * 512:(th + 1) * 512],
                                                                               start=(c == 0), stop=(c == NCH - 1)), reads=[rgk, "h2T"], writes=[pk(2 + th)])
                    for c in range(NCH):
                        R.op("pe", lambda e, c=c, fq=fq, th=th, ru=ru: e.matmul(PS(4 + th), lhsT=ru[:, c, fq * 128:(fq + 1) * 128], rhs=h2T[:, c, th * 512:(th + 1) * 512],
                                                                               start=(c == 0), stop=(c == NCH - 1)), reads=[ruk, "h2T"], writes=[pk(4 + th)])
                    bg = b1[:, fc:fc + 1]
                    bu = b1[:, 16 + fc:16 + fc + 1]
                    gk_, sk_, uk_ = "gtt%d" % pb, "sgt%d" % pb, "utt%d" % pb
                    R.op("dve", lambda e, th=th, bg=bg, pb=pb: e.tensor_scalar(out=gtt[pb], in0=PS(2 + th), scalar1=bg, scalar2=7.0, op0=ALU.add, op1=ALU.min),
                         reads=[pk(2 + th), b1k], writes=[gk_])
                    R.op("act", lambda e, pb=pb: e.activation(out=sgt[pb], in_=gtt[pb], func=AF.Sigmoid, scale=1.702), reads=[gk_], writes=[sk_])
                    R.op("dve", lambda e, th=th, bu=bu, pb=pb: e.tensor_scalar(out=utt[pb], in0=PS(4 + th), scalar1=bu, scalar2=7.0, op0=ALU.add, op1=ALU.min),
                         reads=[pk(4 + th), b1k], writes=[uk_])
                    R.op("dve", lambda e, pb=pb: e.tensor_scalar(out=utt[pb], in0=utt[pb], scalar1=-7.0, scalar2=1.0, op0=ALU.max, op1=ALU.add), reads=[uk_], writes=[uk_])
                    R.op("dve", lambda e, pb=pb: e.tensor_tensor(out=gtt[pb], in0=gtt[pb], in1=sgt[pb], op=ALU.mult), reads=[gk_, sk_], writes=[gk_])
                    for p_ in pend:
                        p_()
                    pend = [lambda th=th, fc=fc, pb=pb, gk_=gk_, uk_=uk_: R.op(
                        "dve", lambda e: e.tensor_tensor(out=actT[:, fc, th * 512:(th + 1) * 512], in0=utt[pb], in1=gtt[pb], op=ALU.mult),
                        reads=[uk_, gk_], writes=["actT"])]
        for p_ in pend:
            p_()
        for u in range(8):
            r2, r2k = ring_load(w2_v[e_][:, :, u * 256:(u + 1) * 256])
            for o in range(NOWN):
                bk = 6 + (o % 2)
                for fc in range(NCH):
                    R.op("pe", lambda e, o=o, fc=fc, bk=bk, r2=r2: e.matmul(PS(bk, 0, 256), lhsT=actT[:, fc, o * 128:(o + 1) * 128], rhs=r2[:, fc, :],
                                                                           start=(fc == 0), stop=(fc == NCH - 1)), reads=["actT", r2k], writes=[pk(bk)])
                cols = slice(u * 256, (u + 1) * 256)
                R.op("dve", lambda e, o=o, bk=bk, cols=cols: e.tensor_tensor(out=t1[o % 2], in0=PS(bk, 0, 256), in1=gt2b[:, cols], op=ALU.mult),
                     reads=[pk(bk), "gt2b"], writes=["t1"])
                R.op("dve", lambda e, o=o, cols=cols, e_=e_: e.scalar_tensor_tensor(out=x1[:, o, cols], in0=t1[o % 2], scalar=wt[:, o, e_:e_ + 1], in1=x1[:, o, cols],
                                                                                  op0=ALU.mult, op1=ALU.add),
                     reads=["t1", "wt", "x1_%d" % o], writes=["x1_%d" % o])
    R.barrier()
    gfb = actT.rearrange("p f t -> p (f t)")[:, 0:4096].bitcast(F32)
    R.op("sp", lambda e: e.dma_start(out=gfb, in_=pbc(gf_d)), writes=["gfb"], dma=True)
    for o in range(NOWN):
        xk = "x1_%d" % o
        R.op("pool", lambda e: e.memset(sm2[:, 0:1], 0.0), writes=["ss0"])
        R.op("act", lambda e, o=o: e.activation(out=h2f, in_=x1[:, o, :], func=AF.Square, accum_out=sm2[:, 0:1]), reads=[xk, "ss0"], writes=["h2f", "ss0"])
        rstd_op(sm2[:, 2:3], sm2[:, 0:1], float(D), ["ss0"], "rs0")
        R.op("dve", lambda e, o=o: e.scalar_tensor_tensor(out=x1[:, o, :], in0=x1[:, o, :], scalar=sm2[:, 2:3], in1=gfb, op0=ALU.mult, op1=ALU.mult),
             reads=[xk, "rs0", "gfb"], writes=[xk])
        oo = R.op("sp", lambda e, o=o: e.dma_start(out=out_d[o * 128:(o + 1) * 128, :], in_=x1[:, o, :]), reads=[xk], dma=True)
        R.final_ops.append(oo)
    if debug is not None and debug.startswith("moe"):
        for q in range(4):
            dump(x1[:, 0, q * 512:(q + 1) * 512], "x1_0", q * 512, 512)
    return finish()


def _consts():
    r = np.arange(128)
    ident = np.eye(128, dtype=np.float32)
    tri_f = (r[:, None] <= r[None, :]).astype(np.float32)
    tri_b = (r[:, None] >= r[None, :]).astype(np.float32)
    nm_f = np.where(r[:, None] <= r[None, :], 0.0, NEG).astype(np.float32)
    nm_b = np.where(r[:, None] >= r[None, :], 0.0, NEG).astype(np.float32)
    ones = np.ones((128, 128), np.float32)
    iota_c = np.tile(np.arange(512, dtype=np.float32)[None, :], (128, 1))
    iota_p = r.astype(np.float32)[:, None]
    return np.ascontiguousarray(np.concatenate([ident, tri_f, tri_b, nm_f, nm_b, ones, iota_c, iota_p], axis=1))


def _rope_tables():
    rows = 64
    row = np.repeat(np.arange(rows, dtype=np.float32), 64)
    col = np.tile(np.arange(64, dtype=np.float32), rows)
    inv = (np.float32(10000.0) ** (-np.arange(0, 64, 2, dtype=np.float32) / np.float32(64))).astype(np.float32)
    ang = np.concatenate([row[:, None] * inv, col[:, None] * inv], axis=-1).astype(np.float32)
    return np.cos(ang).astype(np.float32), np.sin(ang).astype(np.float32)


def slot_chunks(j):
    pre = list(range(0, 8 * j))
    post = list(range(31, 8 * j + 7, -1))
    own = list(range(8 * j, 8 * j + 8))
    return pre, post, own


def make_in_maps(inp):
    f = lambda a: np.ascontiguousarray(np.asarray(a, dtype=np.float32))
    x, c, ctx, c_ctx = f(inp["x"]), f(inp["c"]), f(inp["ctx"]), f(inp["c_ctx"])
    cos, sin = _rope_tables()
    consts = _consts()
    fm = lambda v, n: np.ascontiguousarray(v.reshape(n, 128).T)
    shared = {
        "bmod": fm(f(inp["b_mod"])[0], 96),
        "g1fm": fm(f(inp["g_norm1"])[0], 16),
        "g2fm": fm(f(inp["g_norm2"])[0], 16),
        "w_mod": f(inp["w_mod"])[0],
        "w_in": f(inp["w_in"])[0],
        "b_in": f(inp["b_in"]),
        "bfm": np.ascontiguousarray(np.concatenate([fm(f(inp["b_in"])[0, MQ0:MQ0 + 512], 4), fm(f(inp["b_in"])[0, MK0:MK0 + 512], 4)], axis=1)),
        "g_q": f(inp["g_q"]), "g_k": f(inp["g_k"]), "g_mlstm": f(inp["g_mlstm"]),
        "w_out": f(inp["w_out"])[0],
        "g_norm2": f(inp["g_norm2"]), "g_final": f(inp["g_final"])[None, :],
        "w_router": f(inp["w_router"])[0], "b_router": f(inp["b_router"]),
        "w1": inp["w1"],
        "b1fm": np.ascontiguousarray(f(inp["b1"])[0].reshape(NEXP, 32, 128).transpose(2, 0, 1).reshape(128, NEXP * 32)),
        "w2": inp["w2"], "b2": f(inp["b2"])[0],
        "consts": consts,
    }
    maps = []
    for core in range(8):
        b, j = core // 4, core % 4
        pre, post, own = slot_chunks(j)
        xs = np.empty((NSLOT * 128, D), np.float32)
        rope = np.empty((NSLOT * 128, 128), np.float32)
        gmask = np.zeros((NSLOT, 16), np.float32)
        xs[0:256] = ctx[b]
        rope[0:256, 0:64] = 1.0
        rope[0:256, 64:128] = 0.0
        gmask[0:2, 0:8] = 0.0
        gmask[0:2, 8:16] = -1.0
        s = 2
        for kind, lst in (("pre", pre), ("post", post), ("own", own)):
            for ch in lst:
                xs[s * 128:(s + 1) * 128] = x[b, ch * 128:(ch + 1) * 128]
                rope[s * 128:(s + 1) * 128, 0:64] = cos[ch * 128:(ch + 1) * 128]
                rope[s * 128:(s + 1) * 128, 64:128] = sin[ch * 128:(ch + 1) * 128]
                fa = kind in ("pre", "own")
                ba = kind in ("post", "own")
                gmask[s, 0:4] = 0.0 if fa else NEG
                gmask[s, 4:8] = 0.0 if ba else NEG
                gmask[s, 8:12] = -1.0 if fa else 0.0
                gmask[s, 12:16] = -1.0 if ba else 0.0
                s += 1
        assert s == NSLOT
        m = dict(shared)
        m["xs"] = xs
        m["rope"] = rope
        m["gmask"] = np.ascontiguousarray(np.tile(gmask.reshape(1, NSLOT * 16), (128, 1)))
        m["cfm"] = np.ascontiguousarray(np.concatenate([fm(c[b], 16), fm(c_ctx, 16)], axis=1))
        maps.append(m)
    return maps


_CACHE = {}


def kernel(**inputs):
    if "nc" not in _CACHE:
        _CACHE["nc"] = build()
    nc, es, declared = _CACHE["nc"]
    maps = make_in_maps(inputs)
    maps = [{k: v for k, v in m.items() if k in declared} for m in maps]
    res = run_bass_kernel_spmd(nc, maps, core_ids=list(range(8)))
    out = np.empty((2, 4096, D), np.float32)
    for core in range(8):
        b, j = core // 4, core % 4
        out[b, j * 1024:(j + 1) * 1024] = res.results[core]["out"]
    return out
```
